# Optimizing a Trainium2 kernel written in Bass

```python
import math
import jax, jax.numpy as jnp
from jax import lax
import numpy as np


D_MODEL = 1024
BATCH = 2
SEQ = 8192
DEPTH = 2

D_FF = 2816
DSA_HEADS = 4
DSA_HEAD_DIM = 128
IDX_HEADS = 8
IDX_DIM = 64
TOPK_MAX = 256
RWKV_HEADS = 8
RWKV_HEAD_DIM = 64
RWKV_WIDTH = RWKV_HEADS * RWKV_HEAD_DIM
DECAY_LORA = 64
AAA_LORA = 64
GATE_LORA = 128
RWKV_GN_EPS = 64e-5
DIFF_HEADS = 4
DIFF_QK_DIM = 64
DIFF_V_DIM = 2 * DIFF_QK_DIM
N_BRANCH = 3
BRANCH_WIDTH = 512
Q_BLOCK = 128
NORM_EPS = 1e-6

A_QKV = DSA_HEADS * DSA_HEAD_DIM
A_IQ = IDX_HEADS * IDX_DIM
DSA_SEGMENTS = (A_QKV, A_QKV, A_QKV, A_IQ, IDX_DIM, IDX_HEADS)
RWKV_SEGMENTS = (RWKV_WIDTH, RWKV_WIDTH, RWKV_WIDTH, DECAY_LORA, AAA_LORA, GATE_LORA)
DIFF_SEGMENTS = (DIFF_HEADS * 2 * DIFF_QK_DIM, DIFF_HEADS * 2 * DIFF_QK_DIM, DIFF_HEADS * DIFF_V_DIM)
GATE_COLS = N_BRANCH * D_MODEL
GROUP_SEGMENTS = (sum(DSA_SEGMENTS), sum(RWKV_SEGMENTS), sum(DIFF_SEGMENTS), GATE_COLS)
D_IN = sum(GROUP_SEGMENTS)
RWKV_STREAM = sum(RWKV_SEGMENTS)
GROUP_CUTS = tuple(sum(GROUP_SEGMENTS[:i]) for i in range(1, len(GROUP_SEGMENTS)))
DSA_CUTS = tuple(sum(DSA_SEGMENTS[:i]) for i in range(1, len(DSA_SEGMENTS)))
RWKV_CUTS = tuple(sum(RWKV_SEGMENTS[:i]) for i in range(1, len(RWKV_SEGMENTS)))
DIFF_CUTS = tuple(sum(DIFF_SEGMENTS[:i]) for i in range(1, len(DIFF_SEGMENTS)))

kernel_name = 'hybrid_dsa_rwkv7_diffattn_block'


def _rms_norm(x, g):
    xf = x.astype(jnp.float32)
    y = xf * lax.rsqrt(jnp.mean(xf * xf, axis=-1, keepdims=True) + NORM_EPS)
    return (y * g.astype(jnp.float32)).astype(x.dtype)


def _swiglu(h, w_gate, w_up, w_down):
    return (jax.nn.silu(h @ w_gate) * (h @ w_up)) @ w_down


def _dsa_branch(z):
    B, L, _ = z.shape
    topk = min(TOPK_MAX, L // 4)
    q, k, v, qi, ki, wi = jnp.split(z, DSA_CUTS, axis=-1)
    q = q.reshape(B, L, DSA_HEADS, DSA_HEAD_DIM)
    k = k.reshape(B, L, DSA_HEADS, DSA_HEAD_DIM)
    v = v.reshape(B, L, DSA_HEADS, DSA_HEAD_DIM)
    qi = qi.reshape(B, L, IDX_HEADS, IDX_DIM)
    ki32 = ki.astype(jnp.float32)
    wi32 = wi.astype(jnp.float32) * (IDX_HEADS ** -0.5)
    key_pos = jnp.arange(L)
    gather = jax.vmap(lambda a, ix: a[ix])

    def block(i):
        t0 = i * Q_BLOCK
        qb = lax.dynamic_slice_in_dim(q, t0, Q_BLOCK, axis=1)
        qib = lax.dynamic_slice_in_dim(qi, t0, Q_BLOCK, axis=1).astype(jnp.float32)
        wib = lax.dynamic_slice_in_dim(wi32, t0, Q_BLOCK, axis=1)
        qpos = t0 + jnp.arange(Q_BLOCK)
        causal = key_pos[None, :] <= qpos[:, None]
        dots = jnp.einsum('bqhd,bsd->bqhs', qib, ki32) * (IDX_DIM ** -0.5)
        score = jnp.einsum('bqhs,bqh->bqs', jax.nn.relu(dots), wib)
        score = jnp.where(causal[None], score, -jnp.inf)
        _, sel = lax.top_k(score, topk)
        valid = sel <= qpos[None, :, None]
        ks = gather(k, sel)
        vs = gather(v, sel)
        logits = jnp.einsum('bqhd,bqkhd->bhqk', qb, ks).astype(jnp.float32) * (DSA_HEAD_DIM ** -0.5)
        logits = jnp.where(valid[:, None], logits, -jnp.inf)
        p = jax.nn.softmax(logits, axis=-1).astype(v.dtype)
        return jnp.einsum('bhqk,bqkhd->bqhd', p, vs)

    out = lax.map(block, jnp.arange(L // Q_BLOCK))
    return jnp.moveaxis(out, 0, 1).reshape(B, L, A_QKV)


def _rwkv7_branch(z, mu, w0, w_up, a0, a_up, g_up, k_k, k_a, r_k, ln_w, ln_b):
    B, L, _ = z.shape
    f32 = jnp.float32
    z_prev = jnp.pad(z, ((0, 0), (1, 0), (0, 0)))[:, :L]
    z = z + (z_prev - z) * mu
    r, k, v, wc, ac, gc = jnp.split(z, RWKV_CUTS, axis=-1)
    w_log = -jax.nn.softplus(-(w0 + jnp.tanh(wc) @ w_up)) - 0.5
    decay = jnp.exp(-jnp.exp(w_log.astype(f32)))
    a = jax.nn.sigmoid(a0 + ac @ a_up)
    g = jax.nn.sigmoid(gc) @ g_up
    heads = lambda t: t.reshape(B, L, RWKV_HEADS, RWKV_HEAD_DIM).astype(f32)
    kk = heads(k * k_k)
    kk = kk / jnp.maximum(jnp.linalg.norm(kk, axis=-1, keepdims=True), 1e-12)
    k = k * (1 + (a - 1) * k_a)
    rh, kh, vh, ah, wh = heads(r), heads(k), heads(v), heads(a), heads(decay)

    def step(S, inp):
        r_t, w_t, k_t, v_t, kk_t, b_t = inp
        S = (S * w_t[:, :, None, :]
             + jnp.einsum('bhvk,bhk->bhv', S, -kk_t)[..., None] * b_t[:, :, None, :]
             + v_t[..., None] * k_t[:, :, None, :])
        return S, jnp.einsum('bhvk,bhk->bhv', S, r_t)

    xs = tuple(jnp.moveaxis(t, 1, 0) for t in (rh, wh, kh, vh, kk, kk * ah))
    S0 = jnp.zeros((B, RWKV_HEADS, RWKV_HEAD_DIM, RWKV_HEAD_DIM), f32)
    _, y = lax.scan(step, S0, xs)
    y = jnp.moveaxis(y, 0, 1)
    mean = jnp.mean(y, axis=-1, keepdims=True)
    var = jnp.mean(jnp.square(y - mean), axis=-1, keepdims=True)
    y = ((y - mean) * lax.rsqrt(var + RWKV_GN_EPS)).reshape(B, L, RWKV_WIDTH)
    y = y * ln_w.astype(f32) + ln_b.astype(f32)
    bonus = jnp.sum(rh * kh * r_k.reshape(RWKV_HEADS, RWKV_HEAD_DIM).astype(f32), axis=-1, keepdims=True) * vh
    out = (y + bonus.reshape(B, L, RWKV_WIDTH)) * g.astype(f32)
    return out.astype(z.dtype)


def _diff_branch(z, lq1, lk1, lq2, lk2, subln, lambda_init):
    B, L, _ = z.shape
    q, k, v = jnp.split(z, DIFF_CUTS, axis=-1)
    q = q.reshape(B, L, DIFF_HEADS, 2, DIFF_QK_DIM)
    k = k.reshape(B, L, DIFF_HEADS, 2, DIFF_QK_DIM)
    v = v.reshape(B, L, DIFF_HEADS, DIFF_V_DIM)
    lam = (jnp.exp(jnp.sum(lq1.astype(jnp.float32) * lk1.astype(jnp.float32)))
           - jnp.exp(jnp.sum(lq2.astype(jnp.float32) * lk2.astype(jnp.float32))) + lambda_init)
    key_pos = jnp.arange(L)

    def block(i):
        t0 = i * Q_BLOCK
        qb = lax.dynamic_slice_in_dim(q, t0, Q_BLOCK, axis=1)
        qpos = t0 + jnp.arange(Q_BLOCK)
        causal = key_pos[None, :] <= qpos[:, None]
        s = jnp.einsum('bqhmd,bshmd->bhmqs', qb, k).astype(jnp.float32) * (DIFF_QK_DIM ** -0.5)
        s = jnp.where(causal[None, None, None], s, -jnp.inf)
        p = jax.nn.softmax(s, axis=-1)
        attn = (p[:, :, 0] - lam * p[:, :, 1]).astype(v.dtype)
        return jnp.einsum('bhqs,bshd->bqhd', attn, v)

    out = jnp.moveaxis(lax.map(block, jnp.arange(L // Q_BLOCK)), 0, 1).reshape(B, L, DIFF_HEADS, DIFF_V_DIM)
    out = _rms_norm(out, subln) * (1.0 - lambda_init)
    return out.reshape(B, L, DIFF_HEADS * DIFF_V_DIM)


def _mixer(h, w_in, rwkv_params, diff_params, w_branch, w_out, lambda_init):
    B, L, _ = h.shape
    z = h @ w_in
    z_dsa, z_rwkv, z_diff, z_gate = jnp.split(z, GROUP_CUTS, axis=-1)
    y_a = _dsa_branch(z_dsa)
    y_b = _rwkv7_branch(z_rwkv, *rwkv_params)
    y_c = _diff_branch(z_diff, *diff_params, lambda_init)
    branches = jnp.stack([y_a, y_b, y_c], axis=2)
    proj = jnp.einsum('blnc,ncd->blnd', branches, w_branch)
    gates = jax.nn.sigmoid(z_gate.reshape(B, L, N_BRANCH, D_MODEL))
    merged = jnp.sum(gates * proj, axis=2)
    return merged @ w_out


def setup_inputs(seed: int = 0) -> dict:
    key = jax.random.key(seed)
    ks = jax.random.split(key, 32)
    f32 = jnp.float32
    nrm = lambda k, shape, s: jax.random.normal(k, shape, f32) * s
    gain = lambda k, shape: 1.0 + 0.05 * jax.random.normal(k, shape, f32)
    Ld = DEPTH
    return {
        'x': jax.random.normal(ks[0], (BATCH, SEQ, D_MODEL), f32),
        'ffn1_norm_pre': gain(ks[1], (Ld, D_MODEL)),
        'ffn1_norm_post': gain(ks[2], (Ld, D_MODEL)),
        'ffn1_w_gate': nrm(ks[3], (Ld, D_MODEL, D_FF), D_MODEL ** -0.5),
        'ffn1_w_up': nrm(ks[4], (Ld, D_MODEL, D_FF), D_MODEL ** -0.5),
        'ffn1_w_down': nrm(ks[5], (Ld, D_FF, D_MODEL), D_FF ** -0.5),
        'mix_norm_pre': gain(ks[6], (Ld, D_MODEL)),
        'mix_norm_post': gain(ks[7], (Ld, D_MODEL)),
        'w_in': nrm(ks[8], (Ld, D_MODEL, D_IN), D_MODEL ** -0.5),
        'rwkv_mu': jax.random.uniform(ks[9], (Ld, RWKV_STREAM), f32),
        'rwkv_w0': jax.random.uniform(ks[10], (Ld, RWKV_WIDTH), f32, -6.0, -1.0),
        'rwkv_w_up': nrm(ks[11], (Ld, DECAY_LORA, RWKV_WIDTH), 0.1),
        'rwkv_a0': nrm(ks[12], (Ld, RWKV_WIDTH), 0.1),
        'rwkv_a_up': nrm(ks[13], (Ld, AAA_LORA, RWKV_WIDTH), 0.5 * AAA_LORA ** -0.5),
        'rwkv_g_up': nrm(ks[14], (Ld, GATE_LORA, RWKV_WIDTH), GATE_LORA ** -0.5),
        'rwkv_k_k': 0.85 + 0.05 * jax.random.normal(ks[15], (Ld, RWKV_WIDTH), f32),
        'rwkv_k_a': gain(ks[16], (Ld, RWKV_WIDTH)),
        'rwkv_r_k': nrm(ks[17], (Ld, RWKV_WIDTH), 0.1),
        'rwkv_ln_w': gain(ks[18], (Ld, RWKV_WIDTH)),
        'rwkv_ln_b': nrm(ks[19], (Ld, RWKV_WIDTH), 0.01),
        'diff_lambda_q1': nrm(ks[20], (Ld, DIFF_QK_DIM), 0.1),
        'diff_lambda_k1': nrm(ks[21], (Ld, DIFF_QK_DIM), 0.1),
        'diff_lambda_q2': nrm(ks[22], (Ld, DIFF_QK_DIM), 0.1),
        'diff_lambda_k2': nrm(ks[23], (Ld, DIFF_QK_DIM), 0.1),
        'diff_subln': gain(ks[24], (Ld, DIFF_V_DIM)),
        'w_branch': nrm(ks[25], (Ld, N_BRANCH, BRANCH_WIDTH, D_MODEL), BRANCH_WIDTH ** -0.5),
        'w_out': nrm(ks[26], (Ld, D_MODEL, D_MODEL), D_MODEL ** -0.5),
        'ffn2_norm_pre': gain(ks[27], (Ld, D_MODEL)),
        'ffn2_norm_post': gain(ks[28], (Ld, D_MODEL)),
        'ffn2_w_gate': nrm(ks[29], (Ld, D_MODEL, D_FF), D_MODEL ** -0.5),
        'ffn2_w_up': nrm(ks[30], (Ld, D_MODEL, D_FF), D_MODEL ** -0.5),
        'ffn2_w_down': nrm(ks[31], (Ld, D_FF, D_MODEL), D_FF ** -0.5),
    }


def reference(x, ffn1_norm_pre, ffn1_norm_post, ffn1_w_gate, ffn1_w_up, ffn1_w_down,
              mix_norm_pre, mix_norm_post, w_in, rwkv_mu, rwkv_w0, rwkv_w_up, rwkv_a0, rwkv_a_up,
              rwkv_g_up, rwkv_k_k, rwkv_k_a, rwkv_r_k, rwkv_ln_w, rwkv_ln_b,
              diff_lambda_q1, diff_lambda_k1, diff_lambda_q2, diff_lambda_k2, diff_subln,
              w_branch, w_out, ffn2_norm_pre, ffn2_norm_post, ffn2_w_gate, ffn2_w_up, ffn2_w_down):
    for l in range(DEPTH):
        h = _rms_norm(x, ffn1_norm_pre[l])
        x = x + 0.5 * _rms_norm(_swiglu(h, ffn1_w_gate[l], ffn1_w_up[l], ffn1_w_down[l]), ffn1_norm_post[l])
        h = _rms_norm(x, mix_norm_pre[l])
        rwkv_params = (rwkv_mu[l], rwkv_w0[l], rwkv_w_up[l], rwkv_a0[l], rwkv_a_up[l], rwkv_g_up[l],
                       rwkv_k_k[l], rwkv_k_a[l], rwkv_r_k[l], rwkv_ln_w[l], rwkv_ln_b[l])
        diff_params = (diff_lambda_q1[l], diff_lambda_k1[l], diff_lambda_q2[l], diff_lambda_k2[l], diff_subln[l])
        lambda_init = 0.8 - 0.6 * math.exp(-0.3 * l)
        m = _mixer(h, w_in[l], rwkv_params, diff_params, w_branch[l], w_out[l], lambda_init)
        x = x + _rms_norm(m, mix_norm_post[l])
        h = _rms_norm(x, ffn2_norm_pre[l])
        x = x + 0.5 * _rms_norm(_swiglu(h, ffn2_w_gate[l], ffn2_w_up[l], ffn2_w_down[l]), ffn2_norm_post[l])
    return x
```

```python
import numpy as np
import concourse.bass as bass
import concourse.mybir as mybir
from concourse.bass_utils import run_bass_kernel_spmd
from contextlib import ExitStack

F32 = mybir.dt.float32
BF16 = mybir.dt.bfloat16
AF = mybir.ActivationFunctionType
ALU = mybir.AluOpType
AX = mybir.AxisListType


class Buf:
    __slots__ = ("name", "w", "r", "sem", "cnt")

    def __init__(self, name):
        self.name = name
        self.w = None
        self.r = {}
        self.sem = None
        self.cnt = 0


class Prog:
    ENG = ("pe", "act", "dve", "pool", "sp")

    def __init__(self, nc):
        self.nc = nc
        self.ops = {e: [] for e in self.ENG}
        self.cnt = {e: 0 for e in self.ENG}
        self.seen = {e: {} for e in self.ENG}
        self.dma_bufs = []
        self.stack = ExitStack()
        self.nbuf = 0

    def sb(self, name, shape, dt=F32):
        return self.stack.enter_context(self.nc.sbuf_tensor(name, list(shape), dt))

    def ps(self, name, shape, dt=F32):
        return self.stack.enter_context(self.nc.psum_tensor(name, list(shape), dt))

    def buf(self, name=None):
        self.nbuf += 1
        return Buf(name or f"b{self.nbuf}")

    def _waits(self, eng, reads, writes):
        need = {}

        def add(k, v):
            if need.get(k, 0) < v:
                need[k] = v
        for b in reads:
            if b.w is not None:
                add(*b.w)
        for b in writes:
            if b.w is not None:
                add(*b.w)
            for k, v in b.r.items():
                add(k, v)
        out = []
        seen = self.seen[eng]
        for k, v in need.items():
            if k == "pe" and eng == "pe":
                continue
            if seen.get(k, 0) >= v:
                continue
            seen[k] = v
            out.append((k, v))
        return out

    def _mark(self, tok, reads, writes):
        k, v = tok
        for b in reads:
            if b.r.get(k, 0) < v:
                b.r[k] = v
        for b in writes:
            b.w = tok
            b.r = {}

    def op(self, eng, fn, reads=(), writes=()):
        waits = self._waits(eng, reads, writes)
        self.cnt[eng] += 1
        tok = (eng, self.cnt[eng])
        self._mark(tok, reads, writes)
        self.ops[eng].append((waits, fn, (eng, 1)))

    def dma(self, q, fn, reads=(), writes=(), sembuf=None):
        waits = self._waits(q, reads, writes)
        sbf = sembuf if sembuf is not None else (writes[0] if writes else reads[0])
        if sbf.sem is None:
            sbf.sem = self.stack.enter_context(self.nc.semaphore(f"d_{sbf.name}_{len(self.dma_bufs)}"))
            self.dma_bufs.append(sbf)
        sbf.cnt += 16
        tok = (sbf, sbf.cnt)
        self._mark(tok, reads, writes)
        self.ops[q].append((waits, fn, (sbf, 16)))

    def emit(self):
        nc = self.nc
        sems = {e: self.stack.enter_context(nc.semaphore(f"s_{e}")) for e in self.ENG}

        def semof(k):
            return sems[k] if isinstance(k, str) else k.sem
        final_waits = [(b, b.cnt) for b in self.dma_bufs if self.seen["sp"].get(b, 0) < b.cnt]
        for e in ("pe", "act", "dve", "pool"):
            if self.cnt[e] > 0:
                final_waits.append((e, self.cnt[e]))
        self.ops["sp"].append((final_waits, None, None))
        block = self.stack.enter_context(nc.Block())

        def run(engobj, lst):
            for waits, fn, inc in lst:
                for k, v in waits:
                    engobj.wait_ge(semof(k), v)
                if fn is None:
                    continue
                ins = fn(engobj)
                if inc is not None:
                    ins.then_inc(semof(inc[0]), inc[1])

        @block.tensor
        def _(e):
            run(e, self.ops["pe"])

        @block.scalar
        def _(e):
            run(e, self.ops["act"])

        @block.vector
        def _(e):
            run(e, self.ops["dve"])

        @block.gpsimd
        def _(e):
            run(e, self.ops["pool"])

        @block.sync
        def _(e):
            run(e, self.ops["sp"])

    def close(self):
        self.stack.close()


D = 1024; DFF = 2816; NFF = 22; TOK = 2048; NT = 16; EPS = 1e-6
NWIN = 43

def build_tok(has_merge, has_win):
    nc = bass.Bass("TRN2", target_bir_lowering=False)
    P = Prog(nc)
    def din(name, shape): return nc.dram_tensor(name, list(shape), F32, kind="ExternalInput").ap()
    def dout(name, shape): return nc.dram_tensor(name, list(shape), F32, kind="ExternalOutput").ap()
    x_in = din("x", [TOK, D])
    g_pre = din("g_pre", [128, D]); g_post = din("g_post", [128, D])
    wg = din("wg", [NFF, 128, 8, 128]); wu = din("wu", [NFF, 128, 8, 128]); wd = din("wd", [NFF, 128, D])
    idn_d = din("idn", [128, 128])
    x_out = dout("xo", [TOK, D])
    if has_win:
        g_mix = din("g_mix", [128, D]); win = din("win", [NWIN, 128, 8, 128]); zT = dout("zT", [NWIN * 128, TOK])
    if has_merge:
        g_mpre = din("g_mpre", [128, D]); g_mpost = din("g_mpost", [128, D])
        wgate = din("wgate", [8, 128, 3072]); wbr = din("wbr", [12, 128, D]); wo = din("wo", [8, 128, D])
        yT = din("yT", [NT, 128, 12, 128])
        x2d = dout("x2", [TOK, D])

    ident_f = P.sb("ident_f", [128, 128]); ident = P.sb("ident", [128, 128], BF16)
    gpre_t = P.sb("gpre_t", [128, D]); gpost_t = P.sb("gpost_t", [128, D])
    hT = P.sb("hT", [128, 8, TOK], BF16)
    AT = P.sb("AT", [128, NFF, 1024], BF16)
    Wd = P.sb("Wd", [128, NFF, D], BF16)
    stg = [P.sb(f"stg{i}", [128, 3072]) for i in range(2)]
    wgb = [P.sb(f"wgb{i}", [128, 8, 128], BF16) for i in range(2)]
    wub = [P.sb(f"wub{i}", [128, 8, 128], BF16) for i in range(2)]
    xt = [P.sb(f"xt{i}", [128, D]) for i in range(2)]
    ot = [P.sb(f"ot{i}", [128, D]) for i in range(2)]
    junk = P.sb("junk", [128, D])
    hb = [P.sb(f"hb{i}", [128, D], BF16) for i in range(2)]
    sg = [P.sb(f"sg{i}", [128, 512]) for i in range(2)]
    st = P.sb("st", [128, 64])
    pA = [P.ps(f"pA{i}", [128, 512]) for i in range(2)]
    pB = [P.ps(f"pB{i}", [128, 512]) for i in range(2)]
    pC = [P.ps(f"pC{i}", [128, 512]) for i in range(2)]
    pT = [P.ps(f"pT{i}", [128, 1024], BF16) for i in range(2)]
    B = P.buf
    b_ident = B(); b_identf = B(); b_gpre = B(); b_gpost = B(); b_hT = [B() for _ in range(NT)]
    b_AT = [[B() for _ in range(2)] for _ in range(NFF)]
    b_Wd = [B() for _ in range(NFF)]
    b_stg = [B(), B()]; b_wgb = [B(), B()]; b_wub = [B(), B()]; b_xt = [B(), B()]; b_ot = [B(), B()]
    b_junk = B(); b_hb = [B(), B()]; b_sg = [B(), B()]
    b_pA = [B(), B()]; b_pB = [B(), B()]; b_pC = [B(), B()]; b_pT = [B(), B()]
    st_next = [0]
    b_st = {}
    def stcol():
        i = st_next[0] % 64; st_next[0] += 1
        if i not in b_st: b_st[i] = B()
        return st[:, i:i + 1], b_st[i]

    P.dma("sp", lambda e: e.dma_start(out=ident_f[:], in_=idn_d), writes=[b_identf])
    P.op("dve", lambda e: e.tensor_copy(out=ident[:], in_=ident_f[:]), reads=[b_identf], writes=[b_ident])
    P.dma("sp", lambda e: e.dma_start(out=gpre_t[:], in_=g_pre), writes=[b_gpre])
    P.dma("sp", lambda e: e.dma_start(out=gpost_t[:], in_=g_post), writes=[b_gpost])

    def rstd_of(src_ap, src_bufs):
        ss, bss = stcol(); rs, brs = stcol()
        P.op("act", lambda e: e.activation(out=junk[:], in_=src_ap, func=AF.Square, scale=float(D ** -0.5), accum_out=ss),
             reads=src_bufs, writes=[b_junk, bss])
        P.op("dve", lambda e: e.tensor_scalar(out=rs, in0=ss, scalar1=EPS, scalar2=None, op0=ALU.add),
             reads=[bss], writes=[brs])
        P.op("act", lambda e: e.activation(out=rs, in_=rs, func=AF.Sqrt), reads=[brs], writes=[brs])
        P.op("dve", lambda e: e.reciprocal(out=rs, in_=rs), reads=[brs], writes=[brs])
        return rs, brs

    def norm_to_hT(src_ap, src_bufs, g_t, b_g, ti, par):
        rs, brs = rstd_of(src_ap, src_bufs)
        P.op("dve", lambda e: e.scalar_tensor_tensor(out=hb[par][:], in0=src_ap, scalar=rs, in1=g_t[:], op0=ALU.mult, op1=ALU.mult),
             reads=src_bufs + [brs, b_g], writes=[b_hb[par]])
        for k in range(8):
            P.op("pe", lambda e, k=k: e.transpose(pT[par][:, k * 128:(k + 1) * 128], hb[par][:, k * 128:(k + 1) * 128], ident[:]),
                 reads=[b_hb[par], b_ident], writes=[b_pT[par]])
        P.op("act", lambda e: e.copy(out=hT[:, :, ti * 128:(ti + 1) * 128], in_=pT[par][:].rearrange("p (k t) -> p k t", k=8)),
             reads=[b_pT[par]], writes=[b_hT[ti]])

    def ffn_stage(xsrc, xsrc_bufs, xdst, post_tile=None):
        dst_bufs = [B() for _ in range(NT)]
        for ti in range(NT):
            par = ti % 2
            rb = [xsrc_bufs[ti]] if xsrc_bufs[ti] is not None else []
            P.dma("sp", lambda e, ti=ti, par=par: e.dma_start(out=xt[par][:], in_=xsrc[ti * 128:(ti + 1) * 128, :]),
                  reads=rb, writes=[b_xt[par]])
            norm_to_hT(xt[par][:], [b_xt[par]], gpre_t, b_gpre, ti, par)
        for c in range(NFF):
            s = c % 2
            P.dma("sp", lambda e, c=c, s=s: e.dma_start(out=stg[s][:, 0:D], in_=wd[c]), writes=[b_stg[s]])
            P.op("pool", lambda e, c=c, s=s: e.tensor_copy(out=Wd[:, c, :], in_=stg[s][:, 0:D]), reads=[b_stg[s]], writes=[b_Wd[c]])
        for half in range(2):
            for c in range(NFF):
                s = c % 2
                P.dma("sp", lambda e, c=c, s=s: e.dma_start(out=stg[s][:, 0:1024], in_=wg[c].rearrange("p k f -> p (k f)")), writes=[b_stg[s]])
                P.op("pool", lambda e, s=s: e.tensor_copy(out=wgb[s][:].rearrange("p k f -> p (k f)"), in_=stg[s][:, 0:1024]), reads=[b_stg[s]], writes=[b_wgb[s]])
                P.dma("sp", lambda e, c=c, s=s: e.dma_start(out=stg[s][:, 1024:2048], in_=wu[c].rearrange("p k f -> p (k f)")), writes=[b_stg[s]])
                P.op("pool", lambda e, s=s: e.tensor_copy(out=wub[s][:].rearrange("p k f -> p (k f)"), in_=stg[s][:, 1024:2048]), reads=[b_stg[s]], writes=[b_wub[s]])
                for tb in range(2):
                    t0 = half * 1024 + tb * 512
                    rd = [b_hT[(t0 // 128) + i] for i in range(4)]
                    q = tb
                    for k in range(8):
                        P.op("pe", lambda e, k=k, s=s, q=q, t0=t0: e.matmul(pA[q][:], lhsT=wgb[s][:, k, :], rhs=hT[:, k, t0:t0 + 512], start=(k == 0), stop=(k == 7)),
                             reads=[b_wgb[s]] + rd, writes=[b_pA[q]])
                    for k in range(8):
                        P.op("pe", lambda e, k=k, s=s, q=q, t0=t0: e.matmul(pB[q][:], lhsT=wub[s][:, k, :], rhs=hT[:, k, t0:t0 + 512], start=(k == 0), stop=(k == 7)),
                             reads=[b_wub[s]] + rd, writes=[b_pB[q]])
                    P.op("act", lambda e, q=q: e.activation(out=sg[q][:], in_=pA[q][:], func=AF.Silu), reads=[b_pA[q]], writes=[b_sg[q]])
                    P.op("dve", lambda e, q=q, c=c, tb=tb: e.tensor_tensor(out=AT[:, c, tb * 512:(tb + 1) * 512], in0=sg[q][:], in1=pB[q][:], op=ALU.mult),
                         reads=[b_sg[q], b_pB[q]], writes=[b_AT[c][tb]])
            for tl in range(8):
                ti = half * 8 + tl; par = ti % 2
                rb = [xsrc_bufs[ti]] if xsrc_bufs[ti] is not None else []
                P.dma("sp", lambda e, ti=ti, par=par: e.dma_start(out=xt[par][:], in_=xsrc[ti * 128:(ti + 1) * 128, :]),
                      reads=rb, writes=[b_xt[par]])
                for ch in range(2):
                    for c in range(NFF):
                        P.op("pe", lambda e, c=c, ch=ch, tl=tl: e.matmul(pC[ch][:], lhsT=AT[:, c, tl * 128:(tl + 1) * 128], rhs=Wd[:, c, ch * 512:(ch + 1) * 512], start=(c == 0), stop=(c == NFF - 1)),
                             reads=[b_AT[c][tl // 4], b_Wd[c]], writes=[b_pC[ch]])
                    P.op("act", lambda e, ch=ch, par=par: e.copy(out=ot[par][:, ch * 512:(ch + 1) * 512], in_=pC[ch][:]), reads=[b_pC[ch]], writes=[b_ot[par]])
                rs, brs = rstd_of(ot[par][:], [b_ot[par]])
                P.op("dve", lambda e, par=par, rs=rs: e.scalar_tensor_tensor(out=ot[par][:], in0=ot[par][:], scalar=rs, in1=gpost_t[:], op0=ALU.mult, op1=ALU.mult),
                     reads=[b_ot[par], brs, b_gpost], writes=[b_ot[par]])
                P.op("dve", lambda e, par=par: e.scalar_tensor_tensor(out=ot[par][:], in0=ot[par][:], scalar=0.5, in1=xt[par][:], op0=ALU.mult, op1=ALU.add),
                     reads=[b_ot[par], b_xt[par]], writes=[b_ot[par]])
                P.dma("pool", lambda e, ti=ti, par=par: e.dma_start(out=xdst[ti * 128:(ti + 1) * 128, :], in_=ot[par][:]),
                      reads=[b_ot[par]], writes=[dst_bufs[ti]], sembuf=b_ot[par])
                if post_tile is not None:
                    post_tile(ti, par)
        return dst_bufs

    src_bufs = [None] * NT
    xsrc = x_in
    if has_merge:
        raise NotImplementedError
    if has_win:
        gmix_t = P.sb("gmix_t", [128, D]); b_gmix = B()
        P.dma("sp", lambda e: e.dma_start(out=gmix_t[:], in_=g_mix), writes=[b_gmix])
        hT2 = hT; b_hT2 = b_hT
        def post(ti, par):
            rs, brs = rstd_of(ot[par][:], [b_ot[par]])
            P.op("dve", lambda e: e.scalar_tensor_tensor(out=hb[par][:], in0=ot[par][:], scalar=rs, in1=gmix_t[:], op0=ALU.mult, op1=ALU.mult),
                 reads=[b_ot[par], brs, b_gmix], writes=[b_hb[par]])
            for k in range(8):
                P.op("pe", lambda e, k=k: e.transpose(pT[par][:, k * 128:(k + 1) * 128], hb[par][:, k * 128:(k + 1) * 128], ident[:]),
                     reads=[b_hb[par], b_ident], writes=[b_pT[par]])
            P.op("act", lambda e: e.copy(out=hT2[:, :, ti * 128:(ti + 1) * 128], in_=pT[par][:].rearrange("p (k t) -> p k t", k=8)),
                 reads=[b_pT[par]], writes=[b_hT2[ti]])
        ffn_stage(xsrc, src_bufs, x_out, post)
        zs = [P.sb(f"zs{i}", [128, 1024]) for i in range(2)]; b_zs = [B(), B()]
        for c in range(NWIN):
            s = c % 2
            P.dma("sp", lambda e, c=c, s=s: e.dma_start(out=stg[s][:, 0:1024], in_=win[c].rearrange("p k f -> p (k f)")), writes=[b_stg[s]])
            P.op("pool", lambda e, s=s: e.tensor_copy(out=wgb[s][:].rearrange("p k f -> p (k f)"), in_=stg[s][:, 0:1024]), reads=[b_stg[s]], writes=[b_wgb[s]])
            for tb in range(4):
                q = tb % 2; t0 = tb * 512; zi = tb // 2
                rd = [b_hT2[(t0 // 128) + i] for i in range(4)]
                for k in range(8):
                    P.op("pe", lambda e, k=k, s=s, q=q, t0=t0: e.matmul(pA[q][:], lhsT=wgb[s][:, k, :], rhs=hT2[:, k, t0:t0 + 512], start=(k == 0), stop=(k == 7)),
                         reads=[b_wgb[s]] + rd, writes=[b_pA[q]])
                if tb % 2 == 0:
                    P.op("act", lambda e, q=q, zi=zi: e.copy(out=zs[zi][:, 0:512], in_=pA[q][:]), reads=[b_pA[q]], writes=[b_zs[zi]])
                else:
                    P.op("dve", lambda e, q=q, zi=zi: e.tensor_copy(out=zs[zi][:, 512:1024], in_=pA[q][:]), reads=[b_pA[q]], writes=[b_zs[zi]])
                    P.dma("pool", lambda e, c=c, zi=zi: e.dma_start(out=zT[c * 128:(c + 1) * 128, zi * 1024:(zi + 1) * 1024], in_=zs[zi][:]), reads=[b_zs[zi]], sembuf=b_zs[zi])
    else:
        ffn_stage(xsrc, src_bufs, x_out, None)
    P.emit(); P.close()
    return nc


def build_diff(L):
    NQ = L // 128
    nc = bass.Bass("TRN2", target_bir_lowering=False)
    P = Prog(nc); B = P.buf
    def din(name, shape): return nc.dram_tensor(name, list(shape), F32, kind="ExternalInput").ap()
    qk_d = din("qk", [4, 64, L])
    v_d = din("v", [128, NQ, 128])
    lam_d = din("lam", [128, 4, 64])
    cst_d = din("cst", [128, 2])
    gsub_d = din("gsub", [128, 128])
    tri_d = din("tri", [128, 128])
    y_d = nc.dram_tensor("y", [NQ, 128, 128], F32, kind="ExternalOutput").ap()

    qkb = [P.sb(f"qkb{i}", [64, L], BF16) for i in range(4)]; b_qkb = [B() for _ in range(4)]
    vb = P.sb("vb", [128, NQ, 130], BF16); b_vb = B()
    stg = [P.sb(f"stg{i}", [128, 2048]) for i in range(2)]; b_stg = [B(), B()]
    lam_t = P.sb("lam_t", [128, 4, 64]); b_lam = B()
    cst = P.sb("cst_t", [128, 2]); b_cst = B()
    gsub = P.sb("gsub_t", [128, 128]); b_gsub = B()
    tri_f = P.sb("tri_f", [128, 128]); b_trif = B()
    tri = P.sb("tri_b", [128, 128], BF16); b_tri = B()
    ones = P.sb("ones", [128, 2], BF16); b_ones = B()
    sm = P.sb("sm", [128, 16]); b_sm = [B() for _ in range(16)]
    junk = P.sb("junk", [128, 128]); b_junk = B()
    ET = [[P.sb(f"ET{m}{i}", [128, 4, 128], BF16) for i in range(2)] for m in range(2)]
    b_ET = [[B(), B()] for _ in range(2)]
    ob = [P.sb(f"ob{i}", [128, 128]) for i in range(2)]; b_ob = [B(), B()]
    t2 = P.sb("t2", [128, 128]); b_t2 = B()
    pS = [[P.ps(f"pS{m}{i}", [128, 512]) for i in range(2)] for m in range(2)]; b_pS = [[B(), B()] for _ in range(2)]
    pO = [P.ps(f"pO{m}", [128, 512]) for m in range(2)]; b_pO = [B(), B()]

    n = 0
    for i in range(4):
        CW = min(2048, L)
        for c0 in range(0, L, CW):
            s = n % 2; n += 1
            P.dma("sp", lambda e, i=i, c0=c0, s=s: e.dma_start(out=stg[s][0:64, 0:CW], in_=qk_d[i, :, c0:c0 + CW]), writes=[b_stg[s]])
            P.op("pool", lambda e, i=i, c0=c0, s=s: e.tensor_copy(out=qkb[i][:, c0:c0 + CW], in_=stg[s][0:64, 0:CW]), reads=[b_stg[s]], writes=[b_qkb[i]])
    TW = min(16, NQ)
    for t0 in range(0, NQ, TW):
        s = n % 2; n += 1
        P.dma("sp", lambda e, t0=t0, s=s: e.dma_start(out=stg[s][:, 0:TW * 128], in_=v_d[:, t0:t0 + TW, :].rearrange("p t d -> p (t d)")), writes=[b_stg[s]])
        P.op("pool", lambda e, t0=t0, s=s: e.tensor_copy(out=vb[:, t0:t0 + TW, 0:128], in_=stg[s][:, 0:TW * 128].rearrange("p (t d) -> p t d", d=128)), reads=[b_stg[s]], writes=[b_vb])
    P.dma("sp", lambda e: e.dma_start(out=lam_t[:], in_=lam_d), writes=[b_lam])
    P.dma("sp", lambda e: e.dma_start(out=cst[:], in_=cst_d), writes=[b_cst])
    P.dma("sp", lambda e: e.dma_start(out=gsub[:], in_=gsub_d), writes=[b_gsub])
    P.dma("sp", lambda e: e.dma_start(out=tri_f[:], in_=tri_d), writes=[b_trif])
    P.op("dve", lambda e: e.tensor_copy(out=tri[:], in_=tri_f[:]), reads=[b_trif], writes=[b_tri])
    P.op("dve", lambda e: e.memset(vb[:, :, 128:130], 1.0), writes=[b_vb])
    for j in range(2):
        P.op("dve", lambda e, j=j: e.tensor_tensor(out=junk[:, 0:64], in0=lam_t[:, 2 * j, :], in1=lam_t[:, 2 * j + 1, :], op=ALU.mult), reads=[b_lam], writes=[b_junk])
        P.op("dve", lambda e, j=j: e.reduce_sum(out=sm[:, j:j + 1], in_=junk[:, 0:64], axis=AX.X), reads=[b_junk], writes=[b_sm[j]])
        P.op("act", lambda e, j=j: e.activation(out=sm[:, j:j + 1], in_=sm[:, j:j + 1], func=AF.Exp), reads=[b_sm[j]], writes=[b_sm[j]])
    P.op("dve", lambda e: e.tensor_tensor(out=sm[:, 2:3], in0=sm[:, 1:2], in1=sm[:, 0:1], op=ALU.subtract), reads=[b_sm[0], b_sm[1]], writes=[b_sm[2]])
    P.op("dve", lambda e: e.tensor_tensor(out=sm[:, 2:3], in0=sm[:, 2:3], in1=cst[:, 0:1], op=ALU.subtract), reads=[b_sm[2], b_cst], writes=[b_sm[2]])
    NEGLAM = (sm[:, 2:3], b_sm[2])

    gi = 0
    for qi in range(NQ):
        nk = qi + 1
        groups = [(g0, min(4, nk - g0)) for g0 in range(0, nk, 4)]
        for gidx, (g0, gn) in enumerate(groups):
            par = gi % 2; gi += 1
            for m in range(2):
                for j in range(gn):
                    kt = g0 + j
                    P.op("pe", lambda e, m=m, j=j, kt=kt, par=par, qi=qi: e.matmul(pS[m][par][:, j * 128:(j + 1) * 128], lhsT=qkb[2 + m][:, kt * 128:(kt + 1) * 128], rhs=qkb[m][:, qi * 128:(qi + 1) * 128], start=True, stop=True),
                         reads=[b_qkb[2 + m], b_qkb[m]], writes=[b_pS[m][par]])
                P.op("act", lambda e, m=m, par=par, gn=gn: e.activation(out=ET[m][par][:, 0:gn, :].rearrange("p g q -> p (g q)"), in_=pS[m][par][:, 0:gn * 128], func=AF.Exp, scale=0.125),
                     reads=[b_pS[m][par]], writes=[b_ET[m][par]])
                if g0 + gn == nk:
                    j = gn - 1
                    P.op("dve", lambda e, m=m, par=par, j=j: e.tensor_tensor(out=ET[m][par][:, j, :], in0=ET[m][par][:, j, :], in1=tri[:], op=ALU.mult),
                         reads=[b_ET[m][par], b_tri], writes=[b_ET[m][par]])
                for j in range(gn):
                    kt = g0 + j
                    first = (kt == 0); last = (kt == nk - 1)
                    P.op("pe", lambda e, m=m, j=j, kt=kt, par=par, first=first, last=last: e.matmul(pO[m][:, 0:130], lhsT=ET[m][par][:, j, :], rhs=vb[:, kt, :], start=first, stop=last),
                         reads=[b_ET[m][par], b_vb], writes=[b_pO[m]])
        op_ = qi % 2
        P.op("dve", lambda e: e.reciprocal(out=sm[:, 4:5], in_=pO[0][:, 128:129]), reads=[b_pO[0]], writes=[b_sm[4]])
        P.op("dve", lambda e: e.reciprocal(out=sm[:, 5:6], in_=pO[1][:, 128:129]), reads=[b_pO[1]], writes=[b_sm[5]])
        P.op("dve", lambda e: e.tensor_tensor(out=sm[:, 5:6], in0=sm[:, 5:6], in1=NEGLAM[0], op=ALU.mult), reads=[b_sm[5], NEGLAM[1]], writes=[b_sm[5]])
        P.op("dve", lambda e: e.tensor_scalar(out=t2[:], in0=pO[1][:, 0:128], scalar1=sm[:, 5:6], scalar2=None, op0=ALU.mult), reads=[b_pO[1], b_sm[5]], writes=[b_t2])
        P.op("dve", lambda e, op_=op_: e.scalar_tensor_tensor(out=ob[op_][:], in0=pO[0][:, 0:128], scalar=sm[:, 4:5], in1=t2[:], op0=ALU.mult, op1=ALU.add), reads=[b_pO[0], b_sm[4], b_t2], writes=[b_ob[op_]])
        P.op("act", lambda e, op_=op_: e.activation(out=junk[:], in_=ob[op_][:], func=AF.Square, scale=float(128 ** -0.5), accum_out=sm[:, 6:7]), reads=[b_ob[op_]], writes=[b_junk, b_sm[6]])
        P.op("dve", lambda e: e.tensor_scalar(out=sm[:, 6:7], in0=sm[:, 6:7], scalar1=1e-6, scalar2=None, op0=ALU.add), reads=[b_sm[6]], writes=[b_sm[6]])
        P.op("act", lambda e: e.activation(out=sm[:, 6:7], in_=sm[:, 6:7], func=AF.Sqrt), reads=[b_sm[6]], writes=[b_sm[6]])
        P.op("dve", lambda e: e.reciprocal(out=sm[:, 6:7], in_=sm[:, 6:7]), reads=[b_sm[6]], writes=[b_sm[6]])
        P.op("dve", lambda e: e.tensor_tensor(out=sm[:, 6:7], in0=sm[:, 6:7], in1=cst[:, 1:2], op=ALU.mult), reads=[b_sm[6], b_cst], writes=[b_sm[6]])
        P.op("dve", lambda e, op_=op_: e.scalar_tensor_tensor(out=ob[op_][:], in0=ob[op_][:], scalar=sm[:, 6:7], in1=gsub[:], op0=ALU.mult, op1=ALU.mult), reads=[b_ob[op_], b_sm[6], b_gsub], writes=[b_ob[op_]])
        P.dma("pool", lambda e, qi=qi, op_=op_: e.dma_start(out=y_d[qi], in_=ob[op_][:]), reads=[b_ob[op_]], sembuf=b_ob[op_])
    P.emit(); P.close()
    return nc


def build_dsa(L, R=32.0, K=22):
    NQ = L // 128
    nc = bass.Bass("TRN2", target_bir_lowering=False)
    P = Prog(nc); B = P.buf
    def din(name, shape): return nc.dram_tensor(name, list(shape), F32, kind="ExternalInput").ap()
    qk_d = din("qk", [2, 128, L])
    v_d = din("v", [128, NQ, 128])
    qi_d = din("qi", [NQ, 64, 8, 128])
    ki_d = din("ki", [64, L])
    wi_d = din("wi", [128, NQ, 8])
    negm_d = din("negm", [128, 128])
    idn_d = din("idn", [128, 128])
    y_d = nc.dram_tensor("y", [NQ, 128, 128], F32, kind="ExternalOutput").ap()
    dbg_d = nc.dram_tensor("dbg", [NQ, 128, 8], F32, kind="ExternalOutput").ap()
    dbg = [P.sb(f"dbg{i}", [128, 8]) for i in range(2)]; b_dbg = [B(), B()]

    qkb = [P.sb(f"qkb{i}", [128, L], BF16) for i in range(2)]; b_qkb = [B(), B()]
    vb = P.sb("vb", [128, NQ, 130], BF16); b_vb = B()
    kiT = P.sb("kiT", [64, L]); b_ki = B()
    wi = P.sb("wi_t", [128, NQ, 8]); b_wi = B()
    negm = P.sb("negm_t", [128, 128]); b_negm = B()
    idf = P.sb("idf", [128, 128]); b_idf = B()
    idb = P.sb("idb", [128, 128], BF16); b_idb = B()
    stg = [P.sb(f"stg{i}", [128, 2048]) for i in range(2)]; b_stg = [B(), B()]
    qit = [P.sb(f"qit{i}", [64, 8, 128]) for i in range(2)]; b_qit = [B(), B()]
    score = [P.sb(f"score{i}", [128, L]) for i in range(2)]; b_score = [B(), B()]
    junkS = P.sb("junkS", [128, L], BF16); b_junkS = B()
    rl = [P.sb(f"rl{i}", [128, 512]) for i in range(2)]; b_rl = [B(), B()]
    Eb = [P.sb(f"Eb{i}", [128, 512], BF16) for i in range(2)]; b_Eb = [B(), B()]
    Pm = [P.sb(f"Pm{i}", [128, 512], BF16) for i in range(2)]; b_Pm = [B(), B()]
    PmT = [P.sb(f"PmT{i}", [128, 4, 128], BF16) for i in range(2)]; b_PmT = [B(), B()]
    ob = [P.sb(f"ob{i}", [128, 128]) for i in range(2)]; b_ob = [B(), B()]
    sm = P.sb("sm", [128, 8]); b_sm = [B() for _ in range(8)]
    pD = [P.ps(f"pD{i}", [128, 512]) for i in range(2)]; b_pD = [B(), B()]
    pS = [P.ps(f"pS{i}", [128, 512]) for i in range(2)]; b_pS = [B(), B()]
    pT = [P.ps(f"pT{i}", [128, 1024], BF16) for i in range(2)]; b_pT = [B(), B()]
    pO = P.ps("pO", [128, 512]); b_pO = B()

    n = 0
    CW = min(2048, L)
    for i in range(2):
        for c0 in range(0, L, CW):
            s = n % 2; n += 1
            P.dma("sp", lambda e, i=i, c0=c0, s=s: e.dma_start(out=stg[s][:, 0:CW], in_=qk_d[i, :, c0:c0 + CW]), writes=[b_stg[s]])
            P.op("pool", lambda e, i=i, c0=c0, s=s: e.tensor_copy(out=qkb[i][:, c0:c0 + CW], in_=stg[s][:, 0:CW]), reads=[b_stg[s]], writes=[b_qkb[i]])
    TW = min(16, NQ)
    for t0 in range(0, NQ, TW):
        s = n % 2; n += 1
        P.dma("sp", lambda e, t0=t0, s=s: e.dma_start(out=stg[s][:, 0:TW * 128], in_=v_d[:, t0:t0 + TW, :].rearrange("p t d -> p (t d)")), writes=[b_stg[s]])
        P.op("pool", lambda e, t0=t0, s=s: e.tensor_copy(out=vb[:, t0:t0 + TW, 0:128], in_=stg[s][:, 0:TW * 128].rearrange("p (t d) -> p t d", d=128)), reads=[b_stg[s]], writes=[b_vb])
    P.op("dve", lambda e: e.memset(vb[:, :, 128:130], 1.0), writes=[b_vb])
    P.dma("sp", lambda e: e.dma_start(out=kiT[:], in_=ki_d), writes=[b_ki])
    P.dma("sp", lambda e: e.dma_start(out=wi[:], in_=wi_d), writes=[b_wi])
    P.dma("sp", lambda e: e.dma_start(out=negm[:], in_=negm_d), writes=[b_negm])
    P.dma("sp", lambda e: e.dma_start(out=idf[:], in_=idn_d), writes=[b_idf])
    P.op("dve", lambda e: e.tensor_copy(out=idb[:], in_=idf[:]), reads=[b_idf], writes=[b_idb])
    SC = float((64 ** -0.5) * (8 ** -0.5))
    ci = 0; ai = 0
    for qi in range(NQ):
        nk = qi + 1; nkeys = nk * 128
        sp_ = qi % 2
        sc = score[sp_]; bsc = b_score[sp_]
        P.dma("sp", lambda e, qi=qi, sp_=sp_: e.dma_start(out=qit[sp_][:], in_=qi_d[qi]), writes=[b_qit[sp_]])
        chunks = [(c0, min(4, nk - c0)) for c0 in range(0, nk, 4)]
        for (c0, cn) in chunks:
            w = cn * 128; k0 = c0 * 128
            for h in range(8):
                p = ci % 2; ci += 1
                P.op("pe", lambda e, h=h, p=p, k0=k0, w=w, sp_=sp_: e.matmul(pD[p][:, 0:w], lhsT=qit[sp_][:, h, :], rhs=kiT[:, k0:k0 + w], start=True, stop=True),
                     reads=[b_qit[sp_], b_ki], writes=[b_pD[p]])
                P.op("act", lambda e, p=p, w=w: e.activation(out=rl[p][:, 0:w], in_=pD[p][:, 0:w], func=AF.Relu, scale=SC), reads=[b_pD[p]], writes=[b_rl[p]])
                if h == 0:
                    P.op("dve", lambda e, p=p, w=w, k0=k0, sc=sc, qi=qi, h=h: e.tensor_scalar(out=sc[:, k0:k0 + w], in0=rl[p][:, 0:w], scalar1=wi[:, qi, h:h + 1], scalar2=None, op0=ALU.mult),
                         reads=[b_rl[p], b_wi], writes=[bsc])
                else:
                    P.op("dve", lambda e, p=p, w=w, k0=k0, sc=sc, qi=qi, h=h: e.scalar_tensor_tensor(out=sc[:, k0:k0 + w], in0=rl[p][:, 0:w], scalar=wi[:, qi, h:h + 1], in1=sc[:, k0:k0 + w], op0=ALU.mult, op1=ALU.add),
                         reads=[b_rl[p], b_wi, bsc], writes=[bsc])
        d0 = (nk - 1) * 128
        P.op("dve", lambda e, sc=sc, d0=d0: e.tensor_tensor(out=sc[:, d0:d0 + 128], in0=sc[:, d0:d0 + 128], in1=negm[:], op=ALU.add), reads=[bsc, b_negm], writes=[bsc])
        tau = sm[:, 0:1]; mid = sm[:, 1:2]; cnt = sm[:, 2:3]; s_ = sm[:, 3:4]
        if nkeys <= 256:
            P.op("dve", lambda e: e.memset(tau, -R), writes=[b_sm[0]])
        else:
            P.op("dve", lambda e: e.memset(mid, 0.0), writes=[b_sm[1]])
            for it in range(K):
                P.op("dve", lambda e, sc=sc, nkeys=nkeys: e.tensor_scalar(out=junkS[:, 0:nkeys], in0=sc[:, 0:nkeys], scalar1=mid, scalar2=0.0, op0=ALU.is_ge, op1=ALU.add, accum_out=cnt),
                     reads=[bsc, b_sm[1]], writes=[b_junkS, b_sm[2]])
                if it < K - 1:
                    wn = R / 2 ** (it + 1)
                    P.op("dve", lambda e, wn=wn: e.tensor_scalar(out=s_, in0=cnt, scalar1=255.5, scalar2=2 * wn, op0=ALU.is_ge, op1=ALU.mult), reads=[b_sm[2]], writes=[b_sm[3]])
                    P.op("dve", lambda e, wn=wn: e.scalar_tensor_tensor(out=mid, in0=s_, scalar=-wn, in1=mid, op0=ALU.add, op1=ALU.add), reads=[b_sm[3], b_sm[1]], writes=[b_sm[1]])
                else:
                    wl = R / 2 ** (K - 1)
                    P.op("dve", lambda e, wl=wl: e.tensor_scalar(out=s_, in0=cnt, scalar1=255.5, scalar2=wl, op0=ALU.is_ge, op1=ALU.mult), reads=[b_sm[2]], writes=[b_sm[3]])
                    P.op("dve", lambda e, wl=wl: e.scalar_tensor_tensor(out=tau, in0=s_, scalar=-wl, in1=mid, op0=ALU.add, op1=ALU.add), reads=[b_sm[3], b_sm[1]], writes=[b_sm[0]])
        P.op("dve", lambda e, sp_=sp_: e.tensor_copy(out=dbg[sp_][:], in_=sm[:]), reads=b_sm, writes=[b_dbg[sp_]])
        P.dma("pool", lambda e, qi=qi, sp_=sp_: e.dma_start(out=dbg_d[qi], in_=dbg[sp_][:]), reads=[b_dbg[sp_]], sembuf=b_dbg[sp_])
        for (c0, cn) in chunks:
            w = cn * 128; k0 = c0 * 128
            p = ai % 2; ai += 1
            P.op("pe", lambda e, p=p, k0=k0, w=w, qi=qi: e.matmul(pS[p][:, 0:w], lhsT=qkb[0][:, qi * 128:(qi + 1) * 128], rhs=qkb[1][:, k0:k0 + w], start=True, stop=True),
                 reads=[b_qkb[0], b_qkb[1]], writes=[b_pS[p]])
            P.op("act", lambda e, p=p, w=w: e.activation(out=Eb[p][:, 0:w], in_=pS[p][:, 0:w], func=AF.Exp, scale=float(128 ** -0.5)), reads=[b_pS[p]], writes=[b_Eb[p]])
            P.op("dve", lambda e, p=p, w=w, k0=k0, sc=sc: e.scalar_tensor_tensor(out=Pm[p][:, 0:w], in0=sc[:, k0:k0 + w], scalar=tau, in1=Eb[p][:, 0:w], op0=ALU.is_ge, op1=ALU.mult),
                 reads=[bsc, b_sm[0], b_Eb[p]], writes=[b_Pm[p]])
            for j in range(cn):
                P.op("pe", lambda e, p=p, j=j: e.transpose(pT[p][:, j * 128:(j + 1) * 128], Pm[p][:, j * 128:(j + 1) * 128], idb[:]), reads=[b_Pm[p], b_idb], writes=[b_pT[p]])
            P.op("act", lambda e, p=p, w=w, cn=cn: e.copy(out=PmT[p][:, 0:cn, :].rearrange("p g q -> p (g q)"), in_=pT[p][:, 0:w]), reads=[b_pT[p]], writes=[b_PmT[p]])
            for j in range(cn):
                kt = c0 + j
                P.op("pe", lambda e, p=p, j=j, kt=kt, nk=nk: e.matmul(pO[:, 0:130], lhsT=PmT[p][:, j, :], rhs=vb[:, kt, :], start=(kt == 0), stop=(kt == nk - 1)),
                     reads=[b_PmT[p], b_vb], writes=[b_pO])
        op_ = qi % 2
        P.op("dve", lambda e: e.reciprocal(out=sm[:, 4:5], in_=pO[:, 128:129]), reads=[b_pO], writes=[b_sm[4]])
        P.op("dve", lambda e, op_=op_: e.tensor_scalar(out=ob[op_][:], in0=pO[:, 0:128], scalar1=sm[:, 4:5], scalar2=None, op0=ALU.mult), reads=[b_pO, b_sm[4]], writes=[b_ob[op_]])
        P.dma("pool", lambda e, qi=qi, op_=op_: e.dma_start(out=y_d[qi], in_=ob[op_][:]), reads=[b_ob[op_]], sembuf=b_ob[op_])
    P.emit(); P.close()
    return nc

D = 1024; TOK = 2048; NT = 16; EPS = 1e-6

def build_merge():
    nc = bass.Bass("TRN2", target_bir_lowering=False)
    P = Prog(nc); B = P.buf
    def din(name, shape): return nc.dram_tensor(name, list(shape), F32, kind="ExternalInput").ap()
    x_in = din("x", [TOK, D]); g_mpre = din("g_mpre", [128, D]); g_mpost = din("g_mpost", [128, D])
    wgate = din("wgate", [8, 128, 3072]); wbr = din("wbr", [12, 128, D]); wo = din("wo", [8, 128, D])
    yT = din("yT", [NT, 128, 12 * 128]); idn_d = din("idn", [128, 128])
    x_out = nc.dram_tensor("xo", [TOK, D], F32, kind="ExternalOutput").ap()
    ident_f = P.sb("ident_f", [128, 128]); ident = P.sb("ident", [128, 128], BF16)
    gpre_t = P.sb("gpre_t", [128, D]); gpost_t = P.sb("gpost_t", [128, D])
    Wg = P.sb("Wg", [128, 8, 3072], BF16); Wb = P.sb("Wb", [128, 12, D], BF16); Wo = P.sb("Wo", [128, 8, D], BF16)
    stg = [P.sb(f"stg{i}", [128, 3072]) for i in range(2)]
    xt = [P.sb(f"xt{i}", [128, D]) for i in range(2)]; ot = [P.sb(f"ot{i}", [128, D]) for i in range(2)]
    mg = P.sb("mg", [128, D]); tmp = P.sb("tmp", [128, 512]); junk = P.sb("junk", [128, D])
    hb = P.sb("hb", [128, D], BF16); mb = P.sb("mb", [128, D], BF16)
    hTt = P.sb("hTt", [128, 8, 128], BF16); mT = P.sb("mT", [128, 8, 128], BF16)
    ystg = [P.sb(f"ystg{i}", [128, 1536]) for i in range(2)]; ytb = [P.sb(f"ytb{i}", [128, 12, 128], BF16) for i in range(2)]
    sgt = [P.sb(f"sgt{i}", [128, 512]) for i in range(2)]
    st = P.sb("st", [128, 8])
    pA = [P.ps(f"pA{i}", [128, 512]) for i in range(2)]; pB = [P.ps(f"pB{i}", [128, 512]) for i in range(2)]
    pC = [P.ps(f"pC{i}", [128, 512]) for i in range(2)]; pT = P.ps("pT", [128, 1024], BF16)
    b_idf = B(); b_id = B(); b_gpre = B(); b_gpost = B(); b_Wg = B(); b_Wb = B(); b_Wo = B(); b_stg = [B(), B()]
    b_xt = [B(), B()]; b_ot = [B(), B()]; b_mg = B(); b_tmp = B(); b_junk = B(); b_hb = B(); b_mb = B(); b_hTt = B(); b_mT = B()
    b_ystg = [B(), B()]; b_ytb = [B(), B()]; b_sgt = [B(), B()]; b_st = [B() for _ in range(8)]
    b_pA = [B(), B()]; b_pB = [B(), B()]; b_pC = [B(), B()]; b_pT = B()
    P.dma("sp", lambda e: e.dma_start(out=ident_f[:], in_=idn_d), writes=[b_idf])
    P.op("dve", lambda e: e.tensor_copy(out=ident[:], in_=ident_f[:]), reads=[b_idf], writes=[b_id])
    P.dma("sp", lambda e: e.dma_start(out=gpre_t[:], in_=g_mpre), writes=[b_gpre])
    P.dma("sp", lambda e: e.dma_start(out=gpost_t[:], in_=g_mpost), writes=[b_gpost])
    n = 0
    for k in range(8):
        s = n % 2; n += 1
        P.dma("sp", lambda e, k=k, s=s: e.dma_start(out=stg[s][:, :], in_=wgate[k]), writes=[b_stg[s]])
        P.op("pool", lambda e, k=k, s=s: e.tensor_copy(out=Wg[:, k, :], in_=stg[s][:, :]), reads=[b_stg[s]], writes=[b_Wg])
    for k in range(12):
        s = n % 2; n += 1
        P.dma("sp", lambda e, k=k, s=s: e.dma_start(out=stg[s][:, 0:D], in_=wbr[k]), writes=[b_stg[s]])
        P.op("pool", lambda e, k=k, s=s: e.tensor_copy(out=Wb[:, k, :], in_=stg[s][:, 0:D]), reads=[b_stg[s]], writes=[b_Wb])
    for k in range(8):
        s = n % 2; n += 1
        P.dma("sp", lambda e, k=k, s=s: e.dma_start(out=stg[s][:, 0:D], in_=wo[k]), writes=[b_stg[s]])
        P.op("pool", lambda e, k=k, s=s: e.tensor_copy(out=Wo[:, k, :], in_=stg[s][:, 0:D]), reads=[b_stg[s]], writes=[b_Wo])

    def rstd_of(src_ap, src_bufs, col):
        ss = st[:, col:col + 1]; bss = b_st[col]
        P.op("act", lambda e: e.activation(out=junk[:], in_=src_ap, func=AF.Square, scale=float(D ** -0.5), accum_out=ss), reads=src_bufs, writes=[b_junk, bss])
        P.op("dve", lambda e: e.tensor_scalar(out=ss, in0=ss, scalar1=EPS, scalar2=None, op0=ALU.add), reads=[bss], writes=[bss])
        P.op("act", lambda e: e.activation(out=ss, in_=ss, func=AF.Sqrt), reads=[bss], writes=[bss])
        P.op("dve", lambda e: e.reciprocal(out=ss, in_=ss), reads=[bss], writes=[bss])
        return ss, bss

    qn = 0
    for ti in range(NT):
        par = ti % 2
        P.dma("sp", lambda e, ti=ti, par=par: e.dma_start(out=xt[par][:], in_=x_in[ti * 128:(ti + 1) * 128, :]), writes=[b_xt[par]])
        P.dma("sp", lambda e, ti=ti, par=par: e.dma_start(out=ystg[par][:], in_=yT[ti]), writes=[b_ystg[par]])
        P.op("pool", lambda e, par=par: e.tensor_copy(out=ytb[par][:].rearrange("p a b -> p (a b)"), in_=ystg[par][:]), reads=[b_ystg[par]], writes=[b_ytb[par]])
        rs, brs = rstd_of(xt[par][:], [b_xt[par]], 0)
        P.op("dve", lambda e, par=par, rs=rs: e.scalar_tensor_tensor(out=hb[:], in0=xt[par][:], scalar=rs, in1=gpre_t[:], op0=ALU.mult, op1=ALU.mult), reads=[b_xt[par], brs, b_gpre], writes=[b_hb])
        for k in range(8):
            P.op("pe", lambda e, k=k: e.transpose(pT[:, k * 128:(k + 1) * 128], hb[:, k * 128:(k + 1) * 128], ident[:]), reads=[b_hb, b_id], writes=[b_pT])
        P.op("act", lambda e: e.copy(out=hTt[:].rearrange("p k t -> p (k t)"), in_=pT[:]), reads=[b_pT], writes=[b_hTt])
        for half in range(2):
            for nb in range(3):
                q = qn % 2; qn += 1
                c0 = nb * 1024 + half * 512
                for k in range(8):
                    P.op("pe", lambda e, k=k, q=q, c0=c0: e.matmul(pA[q][:], lhsT=hTt[:, k, :], rhs=Wg[:, k, c0:c0 + 512], start=(k == 0), stop=(k == 7)), reads=[b_hTt, b_Wg], writes=[b_pA[q]])
                P.op("act", lambda e, q=q: e.activation(out=sgt[q][:], in_=pA[q][:], func=AF.Sigmoid), reads=[b_pA[q]], writes=[b_sgt[q]])
                for kc in range(4):
                    P.op("pe", lambda e, kc=kc, q=q, nb=nb, half=half, par=par: e.matmul(pB[q][:], lhsT=ytb[par][:, nb * 4 + kc, :], rhs=Wb[:, nb * 4 + kc, half * 512:(half + 1) * 512], start=(kc == 0), stop=(kc == 3)), reads=[b_ytb[par], b_Wb], writes=[b_pB[q]])
                if nb == 0:
                    P.op("dve", lambda e, q=q, half=half: e.tensor_tensor(out=mg[:, half * 512:(half + 1) * 512], in0=sgt[q][:], in1=pB[q][:], op=ALU.mult), reads=[b_sgt[q], b_pB[q]], writes=[b_mg])
                else:
                    P.op("dve", lambda e, q=q: e.tensor_tensor(out=tmp[:], in0=sgt[q][:], in1=pB[q][:], op=ALU.mult), reads=[b_sgt[q], b_pB[q]], writes=[b_tmp])
                    P.op("dve", lambda e, half=half: e.tensor_tensor(out=mg[:, half * 512:(half + 1) * 512], in0=mg[:, half * 512:(half + 1) * 512], in1=tmp[:], op=ALU.add), reads=[b_mg, b_tmp], writes=[b_mg])
        P.op("act", lambda e: e.copy(out=mb[:], in_=mg[:]), reads=[b_mg], writes=[b_mb])
        for k in range(8):
            P.op("pe", lambda e, k=k: e.transpose(pT[:, k * 128:(k + 1) * 128], mb[:, k * 128:(k + 1) * 128], ident[:]), reads=[b_mb, b_id], writes=[b_pT])
        P.op("act", lambda e: e.copy(out=mT[:].rearrange("p k t -> p (k t)"), in_=pT[:]), reads=[b_pT], writes=[b_mT])
        for half in range(2):
            for k in range(8):
                P.op("pe", lambda e, k=k, half=half: e.matmul(pC[half][:], lhsT=mT[:, k, :], rhs=Wo[:, k, half * 512:(half + 1) * 512], start=(k == 0), stop=(k == 7)), reads=[b_mT, b_Wo], writes=[b_pC[half]])
            P.op("act", lambda e, half=half, par=par: e.copy(out=ot[par][:, half * 512:(half + 1) * 512], in_=pC[half][:]), reads=[b_pC[half]], writes=[b_ot[par]])
        rs, brs = rstd_of(ot[par][:], [b_ot[par]], 1)
        P.op("dve", lambda e, par=par, rs=rs: e.scalar_tensor_tensor(out=ot[par][:], in0=ot[par][:], scalar=rs, in1=gpost_t[:], op0=ALU.mult, op1=ALU.mult), reads=[b_ot[par], brs, b_gpost], writes=[b_ot[par]])
        P.op("dve", lambda e, par=par: e.tensor_tensor(out=ot[par][:], in0=ot[par][:], in1=xt[par][:], op=ALU.add), reads=[b_ot[par], b_xt[par]], writes=[b_ot[par]])
        P.dma("pool", lambda e, ti=ti, par=par: e.dma_start(out=x_out[ti * 128:(ti + 1) * 128, :], in_=ot[par][:]), reads=[b_ot[par]], sembuf=b_ot[par])
    P.emit(); P.close()
    return nc


def build_rwkv(L):
    SEG = min(L, 512); NSEG = L // SEG; NCH = SEG // 64
    nc = bass.Bass("TRN2", target_bir_lowering=False)
    P = Prog(nc); B = P.buf
    def din(name, shape): return nc.dram_tensor(name, list(shape), F32, kind="ExternalInput").ap()
    zr_d = din("zr", [3, 64, 2, L + 1]); zl_d = din("zl", [64, 2, L + 1]); zg_d = din("zg", [128, L + 1])
    mu3_d = din("mu3", [64, 3, 2]); mul_d = din("mul", [64, 2]); mug_d = din("mug", [128, 1])
    pp_d = din("pp", [64, 5, 2]); wup_d = din("wup", [64, 2, 64]); aup_d = din("aup", [64, 2, 64]); gup_d = din("gup", [128, 128])
    lnwb_d = din("lnwb", [64, 2, 128]); cmask_d = din("cmask", [64, 2 * SEG]); mask5_d = din("mask5", [64, 320])
    idn_d = din("idn", [64, 64])
    y_d = nc.dram_tensor("y", [L // 64, 64, 128], F32, kind="ExternalOutput").ap()
    def T(name, shape, dt=F32):
        return P.sb(name, shape, dt), B(name)
    raw3, b_raw3 = T("raw3", [64, 3, 2, SEG + 1]); rawl, b_rawl = T("rawl", [64, 2, SEG + 1]); rawg, b_rawg = T("rawg", [128, SEG + 1])
    mu3, b_mu3 = T("mu3t", [64, 3, 2]); mul, b_mul = T("mult", [64, 2]); mug, b_mug = T("mugt", [128, 1])
    pp, b_pp = T("ppt", [64, 5, 2]); wup, b_wup = T("wupt", [64, 2, 64]); aup, b_aup = T("aupt", [64, 2, 64]); gup, b_gup = T("gupt", [128, 128])
    lnwb, b_lnwb = T("lnwbt", [64, 2, 128]); cmask, b_cmask = T("cmaskt", [64, 2 * SEG]); mask5, b_mask5 = T("mask5t", [64, 320])
    idn, b_idn = T("idnt", [64, 64]); ones, b_ones = T("onest", [64, 64])
    d3, b_d3 = T("d3", [64, 3, 2, SEG]); dl, b_dl = T("dl", [64, 2, SEG]); dg, b_dg = T("dg", [128, SEG])
    x3, b_x3 = T("x3", [64, 3, 2, SEG]); xl, b_xl = T("xl", [64, 2, SEG]); xg, b_xg = T("xg", [128, SEG])
    tw, b_tw = T("tw", [64, SEG]); sgc, b_sgc = T("sgc", [128, SEG]); sgw, b_sgw = T("sgw", [64, 2, SEG]); aa, b_aa = T("aa", [64, 2, SEG])
    t1, b_t1 = T("t1", [64, 2, SEG]); sq, b_sq = T("sq", [64, 2, SEG]); rn, b_rn = T("rn", [64, 2, SEG]); kk, b_kk = T("kk", [64, 2, SEG])
    kp, b_kp = T("kp", [64, 2, SEG]); bb, b_bb = T("bb", [64, 2, SEG]); cs, b_cs = T("cs", [64, 2, SEG])
    epos, b_epos = T("epos", [64, 2, SEG]); eneg, b_eneg = T("eneg", [64, 2, SEG]); eprev, b_eprev = T("eprev", [64, 2, SEG])
    AR, b_AR = T("AR", [64, 2, NCH, 2, 64]); Bt, b_Bt = T("Bt", [64, 2, SEG]); Kt, b_Kt = T("Kt", [64, 2, SEG]); rkr, b_rkr = T("rkr", [64, 2, SEG])
    Hs = [T(f"H{i}", [64, 2, 64]) for i in range(2)]
    Msb = [T(f"Msb{h}", [64, 320]) for h in range(2)]
    TK, b_TK = T("TK", [64, 6, 64])
    PPs = [T(f"PP{i}", [64, 2, 2, 64]) for i in range(2)]
    Xs = [T(f"X{i}", [64, 2, 64]) for i in range(2)]
    Wsb, b_Wsb = T("Wsb", [64, 128]); Usb, b_Usb = T("Usb", [64, 128]); Ysb, b_Ysb = T("Ysb", [64, 128]); yc, b_yc = T("yc", [64, 128])
    outs = [T(f"out{i}", [64, 128]) for i in range(2)]
    sm, _ = T("sm", [64, 16]); b_sm = [B() for _ in range(16)]
    junk, b_junk = T("junk", [64, 64])
    def PS(name, shape): return P.ps(name, shape), B(name)
    pM = [PS(f"pM{h}", [64, 512]) for h in range(2)]
    pK, b_pK = PS("pK", [64, 512]); pI, b_pI = PS("pI", [64, 512]); pX, b_pX = PS("pX", [64, 512])
    pW, b_pW = PS("pW", [64, 512]); pY, b_pY = PS("pY", [64, 512]); pH, b_pH = PS("pH", [64, 512])

    for (t, b, d) in [(mu3, b_mu3, mu3_d), (mul, b_mul, mul_d), (mug, b_mug, mug_d), (pp, b_pp, pp_d), (wup, b_wup, wup_d), (aup, b_aup, aup_d),
                      (gup, b_gup, gup_d), (lnwb, b_lnwb, lnwb_d), (cmask, b_cmask, cmask_d), (mask5, b_mask5, mask5_d), (idn, b_idn, idn_d)]:
        P.dma("sp", lambda e, t=t, d=d: e.dma_start(out=t[:], in_=d), writes=[b])
    P.op("dve", lambda e: e.memset(ones[:], 1.0), writes=[b_ones])
    P.op("dve", lambda e: e.memset(Hs[0][0][:], 0.0), writes=[Hs[0][1]])
    hcur = 0
    NEG = -0.6065306597126334
    oi = 0
    for sg_ in range(NSEG):
        s0 = sg_ * SEG
        for a in range(3):
            P.dma("sp", lambda e, s0=s0, a=a: e.dma_start(out=raw3[:, a, :, :], in_=zr_d[a, :, :, s0:s0 + SEG + 1]), writes=[b_raw3])
        P.dma("sp", lambda e, s0=s0: e.dma_start(out=rawl[:], in_=zl_d[:, :, s0:s0 + SEG + 1]), writes=[b_rawl])
        P.dma("sp", lambda e, s0=s0: e.dma_start(out=rawg[:], in_=zg_d[:, s0:s0 + SEG + 1]), writes=[b_rawg])
        P.op("dve", lambda e: e.tensor_tensor(out=d3[:], in0=raw3[:, :, :, 0:SEG], in1=raw3[:, :, :, 1:SEG + 1], op=ALU.subtract), reads=[b_raw3], writes=[b_d3])
        P.op("dve", lambda e: e.tensor_tensor(out=dl[:], in0=rawl[:, :, 0:SEG], in1=rawl[:, :, 1:SEG + 1], op=ALU.subtract), reads=[b_rawl], writes=[b_dl])
        P.op("dve", lambda e: e.tensor_tensor(out=dg[:], in0=rawg[:, 0:SEG], in1=rawg[:, 1:SEG + 1], op=ALU.subtract), reads=[b_rawg], writes=[b_dg])
        for a in range(3):
            for h in range(2):
                P.op("dve", lambda e, a=a, h=h: e.scalar_tensor_tensor(out=x3[:, a, h, :], in0=d3[:, a, h, :], scalar=mu3[:, a, h:h + 1], in1=raw3[:, a, h, 1:SEG + 1], op0=ALU.mult, op1=ALU.add),
                     reads=[b_d3, b_mu3, b_raw3], writes=[b_x3])
        for a in range(2):
            P.op("dve", lambda e, a=a: e.scalar_tensor_tensor(out=xl[:, a, :], in0=dl[:, a, :], scalar=mul[:, a:a + 1], in1=rawl[:, a, 1:SEG + 1], op0=ALU.mult, op1=ALU.add),
                 reads=[b_dl, b_mul, b_rawl], writes=[b_xl])
        P.op("dve", lambda e: e.scalar_tensor_tensor(out=xg[:], in0=dg[:], scalar=mug[:, 0:1], in1=rawg[:, 1:SEG + 1], op0=ALU.mult, op1=ALU.add), reads=[b_dg, b_mug, b_rawg], writes=[b_xg])
        XR = lambda h: x3[:, 0, h, :]
        XK = lambda h: x3[:, 1, h, :]
        XV = lambda h: x3[:, 2, h, :]
        P.op("act", lambda e: e.activation(out=tw[:], in_=xl[:, 0, :], func=AF.Tanh), reads=[b_xl], writes=[b_tw])
        P.op("act", lambda e: e.activation(out=sgc[:], in_=xg[:], func=AF.Sigmoid), reads=[b_xg], writes=[b_sgc])
        for h in range(2):
            P.op("pe", lambda e, h=h: e.matmul(pK[:, 0:SEG], lhsT=wup[:, h, :], rhs=tw[:], start=True, stop=True), reads=[b_wup, b_tw], writes=[b_pK])
            P.op("act", lambda e, h=h: e.activation(out=sgw[:, h, :], in_=pK[:, 0:SEG], func=AF.Sigmoid, bias=pp[:, 0, h:h + 1]), reads=[b_pK, b_pp], writes=[b_sgw])
            P.op("pe", lambda e, h=h: e.matmul(pK[:, 0:SEG], lhsT=aup[:, h, :], rhs=xl[:, 1, :], start=True, stop=True), reads=[b_aup, b_xl], writes=[b_pK])
            P.op("act", lambda e, h=h: e.activation(out=aa[:, h, :], in_=pK[:, 0:SEG], func=AF.Sigmoid, bias=pp[:, 1, h:h + 1]), reads=[b_pK, b_pp], writes=[b_aa])
        for h in range(2):
            P.op("dve", lambda e, h=h: e.tensor_scalar(out=t1[:, h, :], in0=XK(h), scalar1=pp[:, 2, h:h + 1], scalar2=None, op0=ALU.mult), reads=[b_x3, b_pp], writes=[b_t1])
        P.op("dve", lambda e: e.tensor_tensor(out=sq[:], in0=t1[:], in1=t1[:], op=ALU.mult), reads=[b_t1], writes=[b_sq])
        for h in range(2):
            P.op("pe", lambda e, h=h: e.matmul(pK[:, 0:SEG], lhsT=ones[:], rhs=sq[:, h, :], start=True, stop=True), reads=[b_ones, b_sq], writes=[b_pK])
            P.op("dve", lambda e, h=h: e.tensor_scalar(out=rn[:, h, :], in0=pK[:, 0:SEG], scalar1=1e-24, scalar2=None, op0=ALU.max), reads=[b_pK], writes=[b_rn])
        P.op("act", lambda e: e.activation(out=rn[:], in_=rn[:], func=AF.Sqrt), reads=[b_rn], writes=[b_rn])
        P.op("dve", lambda e: e.reciprocal(out=rn[:], in_=rn[:]), reads=[b_rn], writes=[b_rn])
        P.op("dve", lambda e: e.tensor_tensor(out=kk[:], in0=t1[:], in1=rn[:], op=ALU.mult), reads=[b_t1, b_rn], writes=[b_kk])
        for h in range(2):
            P.op("dve", lambda e, h=h: e.tensor_scalar(out=kp[:, h, :], in0=aa[:, h, :], scalar1=pp[:, 3, h:h + 1], scalar2=pp[:, 3, h:h + 1], op0=ALU.mult, op1=ALU.subtract), reads=[b_aa, b_pp], writes=[b_kp])
            P.op("dve", lambda e, h=h: e.scalar_tensor_tensor(out=kp[:, h, :], in0=kp[:, h, :], scalar=1.0, in1=XK(h), op0=ALU.add, op1=ALU.mult), reads=[b_kp, b_x3], writes=[b_kp])
        P.op("dve", lambda e: e.tensor_tensor(out=bb[:], in0=kk[:], in1=aa[:], op=ALU.mult), reads=[b_kk, b_aa], writes=[b_bb])
        FL = lambda t: t[:].rearrange("p h s -> p (h s)")
        P.op("dve", lambda e: e.tensor_tensor_scan(out=FL(cs), data0=cmask[:], data1=FL(sgw), initial=0.0, op0=ALU.mult, op1=ALU.add), reads=[b_cmask, b_sgw], writes=[b_cs])
        P.op("act", lambda e: e.activation(out=epos[:], in_=cs[:], func=AF.Exp, scale=NEG), reads=[b_cs], writes=[b_epos])
        P.op("act", lambda e: e.activation(out=eneg[:], in_=cs[:], func=AF.Exp, scale=-NEG), reads=[b_cs], writes=[b_eneg])
        P.op("dve", lambda e: e.tensor_tensor(out=eprev[:], in0=cs[:], in1=sgw[:], op=ALU.subtract), reads=[b_cs, b_sgw], writes=[b_eprev])
        P.op("act", lambda e: e.activation(out=eprev[:], in_=eprev[:], func=AF.Exp, scale=NEG), reads=[b_eprev], writes=[b_eprev])
        for h in range(2):
            P.op("dve", lambda e, h=h: e.scalar_tensor_tensor(out=AR[:, h, :, 0, :], in0=kk[:, h, :].rearrange("p (c t) -> p c t", t=64), scalar=-1.0, in1=eprev[:, h, :].rearrange("p (c t) -> p c t", t=64), op0=ALU.mult, op1=ALU.mult),
                 reads=[b_kk, b_eprev], writes=[b_AR])
            P.op("dve", lambda e, h=h: e.tensor_tensor(out=AR[:, h, :, 1, :], in0=XR(h).rearrange("p (c t) -> p c t", t=64), in1=epos[:, h, :].rearrange("p (c t) -> p c t", t=64), op=ALU.mult),
                 reads=[b_x3, b_epos], writes=[b_AR])
            P.op("dve", lambda e, h=h: e.scalar_tensor_tensor(out=rkr[:, h, :], in0=XR(h), scalar=pp[:, 4, h:h + 1], in1=kp[:, h, :], op0=ALU.mult, op1=ALU.mult), reads=[b_x3, b_pp, b_kp], writes=[b_rkr])
        P.op("dve", lambda e: e.tensor_tensor(out=Bt[:], in0=bb[:], in1=eneg[:], op=ALU.mult), reads=[b_bb, b_eneg], writes=[b_Bt])
        P.op("dve", lambda e: e.tensor_tensor(out=Kt[:], in0=kp[:], in1=eneg[:], op=ALU.mult), reads=[b_kp, b_eneg], writes=[b_Kt])
        for c in range(NCH):
            cs_ = slice(c * 64, (c + 1) * 64)
            for h in range(2):
                pm, bpm = pM[h]
                P.op("pe", lambda e, h=h, c=c, pm=pm, cs_=cs_: e.matmul(pm[:, 0:64], lhsT=AR[:, h, c, 0, :], rhs=Bt[:, h, cs_], start=True, stop=True), reads=[b_AR, b_Bt], writes=[bpm])
                P.op("pe", lambda e, h=h, c=c, pm=pm, cs_=cs_: e.matmul(pm[:, 64:192], lhsT=Bt[:, h, cs_], rhs=AR[:, h, c, :, :].rearrange("p a t -> p (a t)"), start=True, stop=True), reads=[b_AR, b_Bt], writes=[bpm])
                P.op("pe", lambda e, h=h, c=c, pm=pm, cs_=cs_: e.matmul(pm[:, 192:320], lhsT=Kt[:, h, cs_], rhs=AR[:, h, c, :, :].rearrange("p a t -> p (a t)"), start=True, stop=True), reads=[b_AR, b_Kt], writes=[bpm])
                P.op("dve", lambda e, h=h, pm=pm: e.tensor_tensor(out=Msb[h][0][:], in0=pm[:, 0:320], in1=mask5[:], op=ALU.mult), reads=[bpm, b_mask5], writes=[Msb[h][1]])
            for h in range(2):
                for a, (src, bsrc) in enumerate([(Bt[:, h, cs_], b_Bt), (Kt[:, h, cs_], b_Kt), (x3[:, 2, h, cs_], b_x3)]):
                    P.op("pe", lambda e, h=h, a=a, src=src: e.transpose(pK[:, (h * 3 + a) * 64:(h * 3 + a + 1) * 64], src, idn[:]), reads=[bsrc, b_idn], writes=[b_pK])
            P.op("act", lambda e: e.copy(out=TK[:].rearrange("p a t -> p (a t)"), in_=pK[:, 0:384]), reads=[b_pK], writes=[b_TK])
            pcur = 0; xcur = 0
            for h in range(2):
                P.op("act", lambda e, h=h: e.copy(out=PPs[0][0][:, h, :, :].rearrange("p a t -> p (a t)"), in_=Msb[h][0][:, 0:128]), reads=[Msb[h][1]], writes=[PPs[0][1]])
                P.op("dve", lambda e, h=h: e.tensor_tensor(out=Xs[0][0][:, h, :], in0=Msb[h][0][:, 64:128], in1=idn[:], op=ALU.add), reads=[Msb[h][1], b_idn], writes=[Xs[0][1]])
            for stp in range(5):
                pp_t, pp_b = PPs[pcur]; pn_t, pn_b = PPs[1 - pcur]
                x_t, x_b = Xs[xcur]; xn_t, xn_b = Xs[1 - xcur]
                for h in range(2):
                    P.op("pe", lambda e, h=h, pp_t=pp_t: e.matmul(pI[:, (h * 2) * 64:(h * 2 + 1) * 64], lhsT=pp_t[:, h, 1, :], rhs=pp_t[:, h, 0, :], start=True, stop=True), reads=[pp_b], writes=[b_pI])
                    P.op("pe", lambda e, h=h, pp_t=pp_t: e.matmul(pI[:, (h * 2 + 1) * 64:(h * 2 + 2) * 64], lhsT=pp_t[:, h, 0, :], rhs=pp_t[:, h, 1, :], start=True, stop=True), reads=[pp_b], writes=[b_pI])
                P.op("act", lambda e, pn_t=pn_t: e.copy(out=pn_t[:].rearrange("p h a t -> p (h a t)"), in_=pI[:, 0:256]), reads=[b_pI], writes=[pn_b])
                for h in range(2):
                    P.op("pe", lambda e, h=h, x_t=x_t: e.matmul(pX[:, h * 64:(h + 1) * 64], lhsT=idn[:], rhs=x_t[:, h, :], start=True, stop=False), reads=[b_idn, x_b], writes=[b_pX])
                    P.op("pe", lambda e, h=h, x_t=x_t, pn_t=pn_t: e.matmul(pX[:, h * 64:(h + 1) * 64], lhsT=pn_t[:, h, 0, :], rhs=x_t[:, h, :], start=False, stop=True), reads=[pn_b, x_b], writes=[b_pX])
                P.op("dve", lambda e, xn_t=xn_t: e.tensor_copy(out=xn_t[:].rearrange("p h t -> p (h t)"), in_=pX[:, 0:128]), reads=[b_pX], writes=[xn_b])
                pcur = 1 - pcur; xcur = 1 - xcur
            X_t, X_b = Xs[xcur]
            H_t, H_b = Hs[hcur]; Hn_t, Hn_b = Hs[1 - hcur]
            for h in range(2):
                P.op("pe", lambda e, h=h, c=c, H_t=H_t: e.matmul(pW[:, h * 64:(h + 1) * 64], lhsT=AR[:, h, c, 0, :], rhs=H_t[:, h, :], start=True, stop=False), reads=[b_AR, H_b], writes=[b_pW])
                P.op("pe", lambda e, h=h: e.matmul(pW[:, h * 64:(h + 1) * 64], lhsT=Msb[h][0][:, 192:256], rhs=TK[:, h * 3 + 2, :], start=False, stop=True), reads=[Msb[h][1], b_TK], writes=[b_pW])
            P.op("act", lambda e: e.copy(out=Wsb[:], in_=pW[:, 0:128]), reads=[b_pW], writes=[b_Wsb])
            for h in range(2):
                P.op("pe", lambda e, h=h, X_t=X_t: e.matmul(pW[:, 128 + h * 64:128 + (h + 1) * 64], lhsT=X_t[:, h, :], rhs=Wsb[:, h * 64:(h + 1) * 64], start=True, stop=True), reads=[X_b, b_Wsb], writes=[b_pW])
            P.op("dve", lambda e: e.tensor_copy(out=Usb[:], in_=pW[:, 128:256]), reads=[b_pW], writes=[b_Usb])
            for h in range(2):
                hs = slice(h * 64, (h + 1) * 64)
                P.op("pe", lambda e, h=h, c=c, hs=hs, H_t=H_t: e.matmul(pY[:, hs], lhsT=AR[:, h, c, 1, :], rhs=H_t[:, h, :], start=True, stop=False), reads=[b_AR, H_b], writes=[b_pY])
                P.op("pe", lambda e, h=h, hs=hs: e.matmul(pY[:, hs], lhsT=Msb[h][0][:, 128:192], rhs=Usb[:, hs], start=False, stop=False), reads=[Msb[h][1], b_Usb], writes=[b_pY])
                P.op("pe", lambda e, h=h, hs=hs: e.matmul(pY[:, hs], lhsT=Msb[h][0][:, 256:320], rhs=TK[:, h * 3 + 2, :], start=False, stop=True), reads=[Msb[h][1], b_TK], writes=[b_pY])
            P.op("pe", lambda e, cs_=cs_: e.matmul(pY[:, 128:256], lhsT=sgc[:, cs_], rhs=gup[:], start=True, stop=True), reads=[b_sgc, b_gup], writes=[b_pY])
            for h in range(2):
                P.op("pe", lambda e, h=h, cs_=cs_: e.matmul(pY[:, 256 + 2 * h:258 + 2 * h], lhsT=rkr[:, h, cs_], rhs=ones[:, 0:2], start=True, stop=True), reads=[b_rkr, b_ones], writes=[b_pY])
            for h in range(2):
                hs = slice(h * 64, (h + 1) * 64)
                P.op("pe", lambda e, h=h, hs=hs, H_t=H_t: e.matmul(pH[:, hs], lhsT=idn[:], rhs=H_t[:, h, :], start=True, stop=False), reads=[b_idn, H_b], writes=[b_pH])
                P.op("pe", lambda e, h=h, hs=hs: e.matmul(pH[:, hs], lhsT=TK[:, h * 3 + 0, :], rhs=Usb[:, hs], start=False, stop=False), reads=[b_TK, b_Usb], writes=[b_pH])
                P.op("pe", lambda e, h=h, hs=hs: e.matmul(pH[:, hs], lhsT=TK[:, h * 3 + 1, :], rhs=TK[:, h * 3 + 2, :], start=False, stop=True), reads=[b_TK], writes=[b_pH])
            for h in range(2):
                ce = c * 64 + 63
                P.op("dve", lambda e, h=h, ce=ce, Hn_t=Hn_t: e.tensor_scalar(out=Hn_t[:, h, :], in0=pH[:, h * 64:(h + 1) * 64], scalar1=epos[:, h, ce:ce + 1], scalar2=None, op0=ALU.mult), reads=[b_pH, b_epos], writes=[Hn_b])
            hcur = 1 - hcur
            P.op("act", lambda e: e.copy(out=Ysb[:], in_=pY[:, 0:128]), reads=[b_pY], writes=[b_Ysb])
            P.op("dve", lambda e: e.tensor_copy(out=sm[:, 8:12], in_=pY[:, 256:260]), reads=[b_pY], writes=[b_sm[8]])
            o_t, o_b = outs[oi % 2]; oi += 1
            for h in range(2):
                hs = slice(h * 64, (h + 1) * 64)
                mcol = sm[:, h:h + 1]; vcol = sm[:, 2 + h:3 + h]
                P.op("dve", lambda e, hs=hs, mcol=mcol: e.reduce_sum(out=mcol, in_=Ysb[:, hs], axis=AX.X), reads=[b_Ysb], writes=[b_sm[h]])
                P.op("dve", lambda e, mcol=mcol: e.tensor_scalar(out=mcol, in0=mcol, scalar1=-1.0 / 64, scalar2=None, op0=ALU.mult), reads=[b_sm[h]], writes=[b_sm[h]])
                P.op("dve", lambda e, hs=hs, mcol=mcol: e.tensor_scalar(out=yc[:, hs], in0=Ysb[:, hs], scalar1=mcol, scalar2=None, op0=ALU.add), reads=[b_Ysb, b_sm[h]], writes=[b_yc])
                P.op("act", lambda e, hs=hs, vcol=vcol: e.activation(out=junk[:], in_=yc[:, hs], func=AF.Square, scale=0.125, accum_out=vcol), reads=[b_yc], writes=[b_junk, b_sm[2 + h]])
                P.op("dve", lambda e, vcol=vcol: e.tensor_scalar(out=vcol, in0=vcol, scalar1=64e-5, scalar2=None, op0=ALU.add), reads=[b_sm[2 + h]], writes=[b_sm[2 + h]])
                P.op("act", lambda e, vcol=vcol: e.activation(out=vcol, in_=vcol, func=AF.Sqrt), reads=[b_sm[2 + h]], writes=[b_sm[2 + h]])
                P.op("dve", lambda e, vcol=vcol: e.reciprocal(out=vcol, in_=vcol), reads=[b_sm[2 + h]], writes=[b_sm[2 + h]])
                P.op("dve", lambda e, hs=hs, vcol=vcol: e.scalar_tensor_tensor(out=yc[:, hs], in0=yc[:, hs], scalar=vcol, in1=lnwb[:, 0, hs], op0=ALU.mult, op1=ALU.mult), reads=[b_yc, b_sm[2 + h], b_lnwb], writes=[b_yc])
                P.op("dve", lambda e, hs=hs: e.tensor_tensor(out=yc[:, hs], in0=yc[:, hs], in1=lnwb[:, 1, hs], op=ALU.add), reads=[b_yc, b_lnwb], writes=[b_yc])
                P.op("dve", lambda e, hs=hs, h=h: e.scalar_tensor_tensor(out=yc[:, hs], in0=TK[:, h * 3 + 2, :], scalar=sm[:, 8 + 2 * h:9 + 2 * h], in1=yc[:, hs], op0=ALU.mult, op1=ALU.add), reads=[b_TK, b_sm[8], b_yc], writes=[b_yc])
            P.op("dve", lambda e, o_t=o_t: e.tensor_tensor(out=o_t[:], in0=yc[:], in1=pY[:, 128:256], op=ALU.mult), reads=[b_yc, b_pY], writes=[o_b])
            gci = sg_ * NCH + c
            P.dma("pool", lambda e, gci=gci, o_t=o_t: e.dma_start(out=y_d[gci], in_=o_t[:]), reads=[o_b], sembuf=o_b)
    P.emit(); P.close()
    return nc

import math as _math
_PROGS = {}
def _prog(key, fn):
    if key not in _PROGS:
        _PROGS[key] = fn()
    return _PROGS[key]

def _c(a):
    return np.ascontiguousarray(a, dtype=np.float32)

def _bc(v, p=128):
    return _c(np.broadcast_to(v[None, :], (p, v.shape[0])))

def _wl(w, nchunk):
    return _c(w.reshape(8, 128, nchunk, 128).transpose(2, 1, 0, 3))

def _pad1(a):
    return np.concatenate([np.zeros(a.shape[:-1] + (1,), np.float32), a], -1)

def _run(nc, maps):
    res = run_bass_kernel_spmd(nc, maps, core_ids=list(range(8)))
    return res.results

def kernel(**inp):
    inp = {k: np.asarray(v) for k, v in inp.items()}
    Bsz, L, Dm = 2, 8192, 1024
    NQ = L // 128
    x = _c(inp["x"].reshape(Bsz * L, Dm))
    idn = np.eye(128, dtype=np.float32)
    tri = np.triu(np.ones((128, 128), np.float32))
    negm = np.where(np.arange(128)[None, :] <= np.arange(128)[:, None], 0.0, -1e30).astype(np.float32)
    SEG = 512
    cmask = np.ones((64, 2 * SEG), np.float32); cmask[:, ::64] = 0
    sl = np.tril(np.ones((64, 64), np.float32), -1); su = sl.T; iu = np.triu(np.ones((64, 64), np.float32))
    mask5 = _c(np.concatenate([sl, su, iu, su, iu], 1))
    for l in range(2):
        w_in = inp["w_in"][l]
        w_in_p = np.zeros((1024, 43 * 128), np.float32); w_in_p[:, :5448] = w_in[:, :5448]
        common = {"g_pre": _bc(inp["ffn1_norm_pre"][l]), "g_post": _bc(inp["ffn1_norm_post"][l]),
                  "wg": _wl(inp["ffn1_w_gate"][l], 22), "wu": _wl(inp["ffn1_w_up"][l], 22), "wd": _c(inp["ffn1_w_down"][l].reshape(22, 128, 1024)),
                  "idn": idn, "g_mix": _bc(inp["mix_norm_pre"][l]), "win": _wl(w_in_p, 43)}
        ncA = _prog("A", lambda: build_tok(False, True))
        res = _run(ncA, [dict(common, x=x[c * 2048:(c + 1) * 2048]) for c in range(8)])
        x1 = np.concatenate([r["xo"] for r in res], 0)
        zT = np.concatenate([r["zT"] for r in res], 1)
        del res
        li = 0.8 - 0.6 * _math.exp(-0.3 * l)
        lam = np.stack([inp["diff_lambda_q1"][l], inp["diff_lambda_k1"][l], inp["diff_lambda_q2"][l], inp["diff_lambda_k2"][l]])
        mu = inp["rwkv_mu"][l]
        m_diff, m_dsa, m_rwkv = [], [], []
        for c in range(8):
            b, j = c // 4, c % 4
            zb = zT[:, b * L:(b + 1) * L]
            DO = 2120 + 1792
            zq = zb[DO:DO + 512]; zk = zb[DO + 512:DO + 1024]; zv = zb[DO + 1024:DO + 1536]
            qk = np.stack([zq[j * 128:j * 128 + 64], zq[j * 128 + 64:j * 128 + 128], zk[j * 128:j * 128 + 64], zk[j * 128 + 64:j * 128 + 128]])
            m_diff.append({"qk": _c(qk), "v": _c(zv[j * 128:(j + 1) * 128].T.reshape(NQ, 128, 128).transpose(1, 0, 2)),
                           "lam": _c(np.broadcast_to(lam[None], (128, 4, 64))),
                           "cst": _c(np.broadcast_to(np.array([li, 1 - li], np.float32)[None], (128, 2))),
                           "gsub": _bc(inp["diff_subln"][l]), "tri": tri})
            q = zb[0:512][j * 128:(j + 1) * 128]; k = zb[512:1024][j * 128:(j + 1) * 128]; vv = zb[1024:1536][j * 128:(j + 1) * 128]
            qi = zb[1536:2048]; ki = zb[2048:2112]; wi = zb[2112:2120]
            m_dsa.append({"qk": _c(np.stack([q, k])), "v": _c(vv.T.reshape(NQ, 128, 128).transpose(1, 0, 2)),
                          "qi": _c(qi.reshape(8, 64, NQ, 128).transpose(2, 1, 0, 3)), "ki": _c(ki),
                          "wi": _c(wi.T.reshape(NQ, 128, 8).transpose(1, 0, 2)), "negm": negm, "idn": idn})
            zrw = zb[2120:2120 + 1792]
            r_ = zrw[0:512].reshape(8, 64, L)[2 * j:2 * j + 2]; k_ = zrw[512:1024].reshape(8, 64, L)[2 * j:2 * j + 2]; v_ = zrw[1024:1536].reshape(8, 64, L)[2 * j:2 * j + 2]
            hp = lambda vec: np.ascontiguousarray(vec.reshape(8, 64)[2 * j:2 * j + 2].T)
            up = lambda w: w.reshape(64, 8, 64)[:, 2 * j:2 * j + 2, :]
            lnwb = np.stack([inp["rwkv_ln_w"][l][2 * j * 64:(2 * j + 2) * 64], inp["rwkv_ln_b"][l][2 * j * 64:(2 * j + 2) * 64]])
            m_rwkv.append({"zr": _c(_pad1(np.stack([r_, k_, v_]).transpose(0, 2, 1, 3))),
                           "zl": _c(_pad1(np.stack([zrw[1536:1600], zrw[1600:1664]], 1))), "zg": _c(_pad1(zrw[1664:1792])),
                           "mu3": _c(np.stack([hp(mu[0:512]), hp(mu[512:1024]), hp(mu[1024:1536])], 1)),
                           "mul": _c(np.stack([mu[1536:1600], mu[1600:1664]], 1)), "mug": _c(mu[1664:1792][:, None]),
                           "pp": _c(np.stack([hp(inp["rwkv_w0"][l]), hp(inp["rwkv_a0"][l]), hp(inp["rwkv_k_k"][l]), hp(inp["rwkv_k_a"][l]), hp(inp["rwkv_r_k"][l])], 1)),
                           "wup": _c(up(inp["rwkv_w_up"][l])), "aup": _c(up(inp["rwkv_a_up"][l])),
                           "gup": _c(inp["rwkv_g_up"][l][:, 2 * j * 64:(2 * j + 2) * 64]),
                           "lnwb": _c(np.broadcast_to(lnwb[None], (64, 2, 128))), "cmask": cmask, "mask5": mask5, "idn": np.eye(64, dtype=np.float32)})
        del zT
        Y = np.zeros((3, Bsz * L, 512), np.float32)
        res = _run(_prog("DSA", lambda: build_dsa(L)), m_dsa); del m_dsa
        for c in range(8):
            b, j = c // 4, c % 4
            Y[0, b * L:(b + 1) * L, j * 128:(j + 1) * 128] = res[c]["y"].reshape(L, 128)
        res = _run(_prog("RWKV", lambda: build_rwkv(L)), m_rwkv); del m_rwkv
        for c in range(8):
            b, j = c // 4, c % 4
            Y[1, b * L:(b + 1) * L, j * 128:(j + 1) * 128] = res[c]["y"].reshape(L, 128)
        res = _run(_prog("DIFF", lambda: build_diff(L)), m_diff); del m_diff
        for c in range(8):
            b, j = c // 4, c % 4
            Y[2, b * L:(b + 1) * L, j * 128:(j + 1) * 128] = res[c]["y"].reshape(L, 128)
        cm = {"g_mpre": _bc(inp["mix_norm_pre"][l]), "g_mpost": _bc(inp["mix_norm_post"][l]),
              "wgate": _c(w_in[:, 5448:8520].reshape(8, 128, 3072)), "wbr": _c(inp["w_branch"][l].reshape(12, 128, 1024)),
              "wo": _c(inp["w_out"][l].reshape(8, 128, 1024)), "idn": idn}
        maps = []
        for c in range(8):
            yc_ = Y[:, c * 2048:(c + 1) * 2048, :].reshape(3, 16, 128, 4, 128)
            maps.append(dict(cm, x=x1[c * 2048:(c + 1) * 2048], yT=_c(yc_.transpose(1, 4, 0, 3, 2).reshape(16, 128, 12 * 128))))
        res = _run(_prog("MERGE", build_merge), maps)
        x2 = np.concatenate([r["xo"] for r in res], 0)
        cf = {"g_pre": _bc(inp["ffn2_norm_pre"][l]), "g_post": _bc(inp["ffn2_norm_post"][l]),
              "wg": _wl(inp["ffn2_w_gate"][l], 22), "wu": _wl(inp["ffn2_w_up"][l], 22), "wd": _c(inp["ffn2_w_down"][l].reshape(22, 128, 1024)), "idn": idn}
        res = _run(_prog("F", lambda: build_tok(False, False)), [dict(cf, x=x2[c * 2048:(c + 1) * 2048]) for c in range(8)])
        x = np.concatenate([r["xo"] for r in res], 0)
    return x.reshape(Bsz, L, Dm).astype(np.float32)
```

```python
import numpy as np
import concourse.bass as bass
import concourse.mybir as mybir
from concourse.bass_utils import run_bass_kernel_spmd
from contextlib import ExitStack

F32 = mybir.dt.float32
BF16 = mybir.dt.bfloat16
AF = mybir.ActivationFunctionType
ALU = mybir.AluOpType
AX = mybir.AxisListType


class BufOld:
    __slots__ = ("name", "w", "r", "sem", "cnt")

    def __init__(self, name):
        self.name = name
        self.w = None
        self.r = {}
        self.sem = None
        self.cnt = 0


class ProgOld:
    ENG = ("pe", "act", "dve", "pool", "sp")

    def __init__(self, nc):
        self.nc = nc
        self.ops = {e: [] for e in self.ENG}
        self.cnt = {e: 0 for e in self.ENG}
        self.seen = {e: {} for e in self.ENG}
        self.dma_bufs = []
        self.stack = ExitStack()
        self.nbuf = 0

    def sb(self, name, shape, dt=F32):
        return self.stack.enter_context(self.nc.sbuf_tensor(name, list(shape), dt))

    def ps(self, name, shape, dt=F32):
        return self.stack.enter_context(self.nc.psum_tensor(name, list(shape), dt))

    def buf(self, name=None):
        self.nbuf += 1
        return BufOld(name or f"b{self.nbuf}")

    def _waits(self, eng, reads, writes):
        need = {}

        def add(k, v):
            if need.get(k, 0) < v:
                need[k] = v
        for b in reads:
            if b.w is not None:
                add(*b.w)
        for b in writes:
            if b.w is not None:
                add(*b.w)
            for k, v in b.r.items():
                add(k, v)
        out = []
        seen = self.seen[eng]
        for k, v in need.items():
            if k == "pe" and eng == "pe":
                continue
            if seen.get(k, 0) >= v:
                continue
            seen[k] = v
            out.append((k, v))
        return out

    def _mark(self, tok, reads, writes):
        k, v = tok
        for b in reads:
            if b.r.get(k, 0) < v:
                b.r[k] = v
        for b in writes:
            b.w = tok
            b.r = {}

    def op(self, eng, fn, reads=(), writes=()):
        waits = self._waits(eng, reads, writes)
        self.cnt[eng] += 1
        tok = (eng, self.cnt[eng])
        self._mark(tok, reads, writes)
        self.ops[eng].append((waits, fn, (eng, 1)))

    def dma(self, q, fn, reads=(), writes=(), sembuf=None):
        waits = self._waits(q, reads, writes)
        sbf = sembuf if sembuf is not None else (writes[0] if writes else reads[0])
        if sbf.sem is None:
            sbf.sem = self.stack.enter_context(self.nc.semaphore(f"d_{sbf.name}_{len(self.dma_bufs)}"))
            self.dma_bufs.append(sbf)
        sbf.cnt += 16
        tok = (sbf, sbf.cnt)
        self._mark(tok, reads, writes)
        self.ops[q].append((waits, fn, (sbf, 16)))

    def emit(self):
        nc = self.nc
        sems = {e: self.stack.enter_context(nc.semaphore(f"s_{e}")) for e in self.ENG}

        def semof(k):
            return sems[k] if isinstance(k, str) else k.sem
        final_waits = [(b, b.cnt) for b in self.dma_bufs if self.seen["sp"].get(b, 0) < b.cnt]
        for e in ("pe", "act", "dve", "pool"):
            if self.cnt[e] > 0:
                final_waits.append((e, self.cnt[e]))
        self.ops["sp"].append((final_waits, None, None))
        block = self.stack.enter_context(nc.Block())

        def run(engobj, lst):
            for waits, fn, inc in lst:
                for k, v in waits:
                    engobj.wait_ge(semof(k), v)
                if fn is None:
                    continue
                ins = fn(engobj)
                if inc is not None:
                    ins.then_inc(semof(inc[0]), inc[1])

        @block.tensor
        def _(e):
            run(e, self.ops["pe"])

        @block.scalar
        def _(e):
            run(e, self.ops["act"])

        @block.vector
        def _(e):
            run(e, self.ops["dve"])

        @block.gpsimd
        def _(e):
            run(e, self.ops["pool"])

        @block.sync
        def _(e):
            run(e, self.ops["sp"])

    def close(self):
        self.stack.close()


D = 1024; DFF = 2816; NFF = 22; TOK = 2048; NT = 16; EPS = 1e-6
NWIN = 43

def build_tok(has_merge, has_win):
    nc = bass.Bass("TRN2", target_bir_lowering=False)
    P = ProgOld(nc)
    def din(name, shape): return nc.dram_tensor(name, list(shape), F32, kind="ExternalInput").ap()
    def dout(name, shape): return nc.dram_tensor(name, list(shape), F32, kind="ExternalOutput").ap()
    x_in = din("x", [TOK, D])
    g_pre = din("g_pre", [128, D]); g_post = din("g_post", [128, D])
    wg = din("wg", [NFF, 128, 8, 128]); wu = din("wu", [NFF, 128, 8, 128]); wd = din("wd", [NFF, 128, D])
    idn_d = din("idn", [128, 128])
    x_out = dout("xo", [TOK, D])
    if has_win:
        g_mix = din("g_mix", [128, D]); win = din("win", [NWIN, 128, 8, 128]); zT = dout("zT", [NWIN * 128, TOK])
    if has_merge:
        g_mpre = din("g_mpre", [128, D]); g_mpost = din("g_mpost", [128, D])
        wgate = din("wgate", [8, 128, 3072]); wbr = din("wbr", [12, 128, D]); wo = din("wo", [8, 128, D])
        yT = din("yT", [NT, 128, 12, 128])
        x2d = dout("x2", [TOK, D])

    ident_f = P.sb("ident_f", [128, 128]); ident = P.sb("ident", [128, 128], BF16)
    gpre_t = P.sb("gpre_t", [128, D]); gpost_t = P.sb("gpost_t", [128, D])
    hT = P.sb("hT", [128, 8, TOK], BF16)
    AT = P.sb("AT", [128, NFF, 1024], BF16)
    Wd = P.sb("Wd", [128, NFF, D], BF16)
    stg = [P.sb(f"stg{i}", [128, 3072]) for i in range(2)]
    wgb = [P.sb(f"wgb{i}", [128, 8, 128], BF16) for i in range(2)]
    wub = [P.sb(f"wub{i}", [128, 8, 128], BF16) for i in range(2)]
    xt = [P.sb(f"xt{i}", [128, D]) for i in range(2)]
    ot = [P.sb(f"ot{i}", [128, D]) for i in range(2)]
    junk = P.sb("junk", [128, D])
    hb = [P.sb(f"hb{i}", [128, D], BF16) for i in range(2)]
    sg = [P.sb(f"sg{i}", [128, 512]) for i in range(2)]
    st = P.sb("st", [128, 64])
    pA = [P.ps(f"pA{i}", [128, 512]) for i in range(2)]
    pB = [P.ps(f"pB{i}", [128, 512]) for i in range(2)]
    pC = [P.ps(f"pC{i}", [128, 512]) for i in range(2)]
    pT = [P.ps(f"pT{i}", [128, 1024], BF16) for i in range(2)]
    B = P.buf
    b_ident = B(); b_identf = B(); b_gpre = B(); b_gpost = B(); b_hT = [B() for _ in range(NT)]
    b_AT = [[B() for _ in range(2)] for _ in range(NFF)]
    b_Wd = [B() for _ in range(NFF)]
    b_stg = [B(), B()]; b_wgb = [B(), B()]; b_wub = [B(), B()]; b_xt = [B(), B()]; b_ot = [B(), B()]
    b_junk = B(); b_hb = [B(), B()]; b_sg = [B(), B()]
    b_pA = [B(), B()]; b_pB = [B(), B()]; b_pC = [B(), B()]; b_pT = [B(), B()]
    st_next = [0]
    b_st = {}
    def stcol():
        i = st_next[0] % 64; st_next[0] += 1
        if i not in b_st: b_st[i] = B()
        return st[:, i:i + 1], b_st[i]

    P.dma("sp", lambda e: e.dma_start(out=ident_f[:], in_=idn_d), writes=[b_identf])
    P.op("dve", lambda e: e.tensor_copy(out=ident[:], in_=ident_f[:]), reads=[b_identf], writes=[b_ident])
    P.dma("sp", lambda e: e.dma_start(out=gpre_t[:], in_=g_pre), writes=[b_gpre])
    P.dma("sp", lambda e: e.dma_start(out=gpost_t[:], in_=g_post), writes=[b_gpost])

    def rstd_of(src_ap, src_bufs):
        ss, bss = stcol(); rs, brs = stcol()
        P.op("act", lambda e: e.activation(out=junk[:], in_=src_ap, func=AF.Square, scale=float(D ** -0.5), accum_out=ss),
             reads=src_bufs, writes=[b_junk, bss])
        P.op("dve", lambda e: e.tensor_scalar(out=rs, in0=ss, scalar1=EPS, scalar2=None, op0=ALU.add),
             reads=[bss], writes=[brs])
        P.op("act", lambda e: e.activation(out=rs, in_=rs, func=AF.Sqrt), reads=[brs], writes=[brs])
        P.op("dve", lambda e: e.reciprocal(out=rs, in_=rs), reads=[brs], writes=[brs])
        return rs, brs

    def norm_to_hT(src_ap, src_bufs, g_t, b_g, ti, par):
        rs, brs = rstd_of(src_ap, src_bufs)
        P.op("dve", lambda e: e.scalar_tensor_tensor(out=hb[par][:], in0=src_ap, scalar=rs, in1=g_t[:], op0=ALU.mult, op1=ALU.mult),
             reads=src_bufs + [brs, b_g], writes=[b_hb[par]])
        for k in range(8):
            P.op("pe", lambda e, k=k: e.transpose(pT[par][:, k * 128:(k + 1) * 128], hb[par][:, k * 128:(k + 1) * 128], ident[:]),
                 reads=[b_hb[par], b_ident], writes=[b_pT[par]])
        P.op("act", lambda e: e.copy(out=hT[:, :, ti * 128:(ti + 1) * 128], in_=pT[par][:].rearrange("p (k t) -> p k t", k=8)),
             reads=[b_pT[par]], writes=[b_hT[ti]])

    def ffn_stage(xsrc, xsrc_bufs, xdst, post_tile=None):
        dst_bufs = [B() for _ in range(NT)]
        for ti in range(NT):
            par = ti % 2
            rb = [xsrc_bufs[ti]] if xsrc_bufs[ti] is not None else []
            P.dma("sp", lambda e, ti=ti, par=par: e.dma_start(out=xt[par][:], in_=xsrc[ti * 128:(ti + 1) * 128, :]),
                  reads=rb, writes=[b_xt[par]])
            norm_to_hT(xt[par][:], [b_xt[par]], gpre_t, b_gpre, ti, par)
        for c in range(NFF):
            s = c % 2
            P.dma("sp", lambda e, c=c, s=s: e.dma_start(out=stg[s][:, 0:D], in_=wd[c]), writes=[b_stg[s]])
            P.op("pool", lambda e, c=c, s=s: e.tensor_copy(out=Wd[:, c, :], in_=stg[s][:, 0:D]), reads=[b_stg[s]], writes=[b_Wd[c]])
        for half in range(2):
            for c in range(NFF):
                s = c % 2
                P.dma("sp", lambda e, c=c, s=s: e.dma_start(out=stg[s][:, 0:1024], in_=wg[c].rearrange("p k f -> p (k f)")), writes=[b_stg[s]])
                P.op("pool", lambda e, s=s: e.tensor_copy(out=wgb[s][:].rearrange("p k f -> p (k f)"), in_=stg[s][:, 0:1024]), reads=[b_stg[s]], writes=[b_wgb[s]])
                P.dma("sp", lambda e, c=c, s=s: e.dma_start(out=stg[s][:, 1024:2048], in_=wu[c].rearrange("p k f -> p (k f)")), writes=[b_stg[s]])
                P.op("pool", lambda e, s=s: e.tensor_copy(out=wub[s][:].rearrange("p k f -> p (k f)"), in_=stg[s][:, 1024:2048]), reads=[b_stg[s]], writes=[b_wub[s]])
                for tb in range(2):
                    t0 = half * 1024 + tb * 512
                    rd = [b_hT[(t0 // 128) + i] for i in range(4)]
                    q = tb
                    for k in range(8):
                        P.op("pe", lambda e, k=k, s=s, q=q, t0=t0: e.matmul(pA[q][:], lhsT=wgb[s][:, k, :], rhs=hT[:, k, t0:t0 + 512], start=(k == 0), stop=(k == 7)),
                             reads=[b_wgb[s]] + rd, writes=[b_pA[q]])
                    for k in range(8):
                        P.op("pe", lambda e, k=k, s=s, q=q, t0=t0: e.matmul(pB[q][:], lhsT=wub[s][:, k, :], rhs=hT[:, k, t0:t0 + 512], start=(k == 0), stop=(k == 7)),
                             reads=[b_wub[s]] + rd, writes=[b_pB[q]])
                    P.op("act", lambda e, q=q: e.activation(out=sg[q][:], in_=pA[q][:], func=AF.Silu), reads=[b_pA[q]], writes=[b_sg[q]])
                    P.op("dve", lambda e, q=q, c=c, tb=tb: e.tensor_tensor(out=AT[:, c, tb * 512:(tb + 1) * 512], in0=sg[q][:], in1=pB[q][:], op=ALU.mult),
                         reads=[b_sg[q], b_pB[q]], writes=[b_AT[c][tb]])
            for tl in range(8):
                ti = half * 8 + tl; par = ti % 2
                rb = [xsrc_bufs[ti]] if xsrc_bufs[ti] is not None else []
                P.dma("sp", lambda e, ti=ti, par=par: e.dma_start(out=xt[par][:], in_=xsrc[ti * 128:(ti + 1) * 128, :]),
                      reads=rb, writes=[b_xt[par]])
                for ch in range(2):
                    for c in range(NFF):
                        P.op("pe", lambda e, c=c, ch=ch, tl=tl: e.matmul(pC[ch][:], lhsT=AT[:, c, tl * 128:(tl + 1) * 128], rhs=Wd[:, c, ch * 512:(ch + 1) * 512], start=(c == 0), stop=(c == NFF - 1)),
                             reads=[b_AT[c][tl // 4], b_Wd[c]], writes=[b_pC[ch]])
                    P.op("act", lambda e, ch=ch, par=par: e.copy(out=ot[par][:, ch * 512:(ch + 1) * 512], in_=pC[ch][:]), reads=[b_pC[ch]], writes=[b_ot[par]])
                rs, brs = rstd_of(ot[par][:], [b_ot[par]])
                P.op("dve", lambda e, par=par, rs=rs: e.scalar_tensor_tensor(out=ot[par][:], in0=ot[par][:], scalar=rs, in1=gpost_t[:], op0=ALU.mult, op1=ALU.mult),
                     reads=[b_ot[par], brs, b_gpost], writes=[b_ot[par]])
                P.op("dve", lambda e, par=par: e.scalar_tensor_tensor(out=ot[par][:], in0=ot[par][:], scalar=0.5, in1=xt[par][:], op0=ALU.mult, op1=ALU.add),
                     reads=[b_ot[par], b_xt[par]], writes=[b_ot[par]])
                P.dma("pool", lambda e, ti=ti, par=par: e.dma_start(out=xdst[ti * 128:(ti + 1) * 128, :], in_=ot[par][:]),
                      reads=[b_ot[par]], writes=[dst_bufs[ti]], sembuf=b_ot[par])
                if post_tile is not None:
                    post_tile(ti, par)
        return dst_bufs

    src_bufs = [None] * NT
    xsrc = x_in
    if has_merge:
        raise NotImplementedError
    if has_win:
        gmix_t = P.sb("gmix_t", [128, D]); b_gmix = B()
        P.dma("sp", lambda e: e.dma_start(out=gmix_t[:], in_=g_mix), writes=[b_gmix])
        hT2 = hT; b_hT2 = b_hT
        def post(ti, par):
            rs, brs = rstd_of(ot[par][:], [b_ot[par]])
            P.op("dve", lambda e: e.scalar_tensor_tensor(out=hb[par][:], in0=ot[par][:], scalar=rs, in1=gmix_t[:], op0=ALU.mult, op1=ALU.mult),
                 reads=[b_ot[par], brs, b_gmix], writes=[b_hb[par]])
            for k in range(8):
                P.op("pe", lambda e, k=k: e.transpose(pT[par][:, k * 128:(k + 1) * 128], hb[par][:, k * 128:(k + 1) * 128], ident[:]),
                     reads=[b_hb[par], b_ident], writes=[b_pT[par]])
            P.op("act", lambda e: e.copy(out=hT2[:, :, ti * 128:(ti + 1) * 128], in_=pT[par][:].rearrange("p (k t) -> p k t", k=8)),
                 reads=[b_pT[par]], writes=[b_hT2[ti]])
        ffn_stage(xsrc, src_bufs, x_out, post)
        zs = [P.sb(f"zs{i}", [128, 1024]) for i in range(2)]; b_zs = [B(), B()]
        for c in range(NWIN):
            s = c % 2
            P.dma("sp", lambda e, c=c, s=s: e.dma_start(out=stg[s][:, 0:1024], in_=win[c].rearrange("p k f -> p (k f)")), writes=[b_stg[s]])
            P.op("pool", lambda e, s=s: e.tensor_copy(out=wgb[s][:].rearrange("p k f -> p (k f)"), in_=stg[s][:, 0:1024]), reads=[b_stg[s]], writes=[b_wgb[s]])
            for tb in range(4):
                q = tb % 2; t0 = tb * 512; zi = tb // 2
                rd = [b_hT2[(t0 // 128) + i] for i in range(4)]
                for k in range(8):
                    P.op("pe", lambda e, k=k, s=s, q=q, t0=t0: e.matmul(pA[q][:], lhsT=wgb[s][:, k, :], rhs=hT2[:, k, t0:t0 + 512], start=(k == 0), stop=(k == 7)),
                         reads=[b_wgb[s]] + rd, writes=[b_pA[q]])
                if tb % 2 == 0:
                    P.op("act", lambda e, q=q, zi=zi: e.copy(out=zs[zi][:, 0:512], in_=pA[q][:]), reads=[b_pA[q]], writes=[b_zs[zi]])
                else:
                    P.op("dve", lambda e, q=q, zi=zi: e.tensor_copy(out=zs[zi][:, 512:1024], in_=pA[q][:]), reads=[b_pA[q]], writes=[b_zs[zi]])
                    P.dma("pool", lambda e, c=c, zi=zi: e.dma_start(out=zT[c * 128:(c + 1) * 128, zi * 1024:(zi + 1) * 1024], in_=zs[zi][:]), reads=[b_zs[zi]], sembuf=b_zs[zi])
    else:
        ffn_stage(xsrc, src_bufs, x_out, None)
    P.emit(); P.close()
    return nc


def build_diff(L):
    NQ = L // 128
    nc = bass.Bass("TRN2", target_bir_lowering=False)
    P = ProgOld(nc); B = P.buf
    def din(name, shape): return nc.dram_tensor(name, list(shape), F32, kind="ExternalInput").ap()
    qk_d = din("qk", [4, 64, L])
    v_d = din("v", [128, NQ, 128])
    lam_d = din("lam", [128, 4, 64])
    cst_d = din("cst", [128, 2])
    gsub_d = din("gsub", [128, 128])
    tri_d = din("tri", [128, 128])
    y_d = nc.dram_tensor("y", [NQ, 128, 128], F32, kind="ExternalOutput").ap()

    qkb = [P.sb(f"qkb{i}", [64, L], BF16) for i in range(4)]; b_qkb = [B() for _ in range(4)]
    vb = P.sb("vb", [128, NQ, 130], BF16); b_vb = B()
    stg = [P.sb(f"stg{i}", [128, 2048]) for i in range(2)]; b_stg = [B(), B()]
    lam_t = P.sb("lam_t", [128, 4, 64]); b_lam = B()
    cst = P.sb("cst_t", [128, 2]); b_cst = B()
    gsub = P.sb("gsub_t", [128, 128]); b_gsub = B()
    tri_f = P.sb("tri_f", [128, 128]); b_trif = B()
    tri = P.sb("tri_b", [128, 128], BF16); b_tri = B()
    ones = P.sb("ones", [128, 2], BF16); b_ones = B()
    sm = P.sb("sm", [128, 16]); b_sm = [B() for _ in range(16)]
    junk = P.sb("junk", [128, 128]); b_junk = B()
    ET = [[P.sb(f"ET{m}{i}", [128, 4, 128], BF16) for i in range(2)] for m in range(2)]
    b_ET = [[B(), B()] for _ in range(2)]
    ob = [P.sb(f"ob{i}", [128, 128]) for i in range(2)]; b_ob = [B(), B()]
    t2 = P.sb("t2", [128, 128]); b_t2 = B()
    pS = [[P.ps(f"pS{m}{i}", [128, 512]) for i in range(2)] for m in range(2)]; b_pS = [[B(), B()] for _ in range(2)]
    pO = [P.ps(f"pO{m}", [128, 512]) for m in range(2)]; b_pO = [B(), B()]

    n = 0
    for i in range(4):
        CW = min(2048, L)
        for c0 in range(0, L, CW):
            s = n % 2; n += 1
            P.dma("sp", lambda e, i=i, c0=c0, s=s: e.dma_start(out=stg[s][0:64, 0:CW], in_=qk_d[i, :, c0:c0 + CW]), writes=[b_stg[s]])
            P.op("pool", lambda e, i=i, c0=c0, s=s: e.tensor_copy(out=qkb[i][:, c0:c0 + CW], in_=stg[s][0:64, 0:CW]), reads=[b_stg[s]], writes=[b_qkb[i]])
    TW = min(16, NQ)
    for t0 in range(0, NQ, TW):
        s = n % 2; n += 1
        P.dma("sp", lambda e, t0=t0, s=s: e.dma_start(out=stg[s][:, 0:TW * 128], in_=v_d[:, t0:t0 + TW, :].rearrange("p t d -> p (t d)")), writes=[b_stg[s]])
        P.op("pool", lambda e, t0=t0, s=s: e.tensor_copy(out=vb[:, t0:t0 + TW, 0:128], in_=stg[s][:, 0:TW * 128].rearrange("p (t d) -> p t d", d=128)), reads=[b_stg[s]], writes=[b_vb])
    P.dma("sp", lambda e: e.dma_start(out=lam_t[:], in_=lam_d), writes=[b_lam])
    P.dma("sp", lambda e: e.dma_start(out=cst[:], in_=cst_d), writes=[b_cst])
    P.dma("sp", lambda e: e.dma_start(out=gsub[:], in_=gsub_d), writes=[b_gsub])
    P.dma("sp", lambda e: e.dma_start(out=tri_f[:], in_=tri_d), writes=[b_trif])
    P.op("dve", lambda e: e.tensor_copy(out=tri[:], in_=tri_f[:]), reads=[b_trif], writes=[b_tri])
    P.op("dve", lambda e: e.memset(vb[:, :, 128:130], 1.0), writes=[b_vb])
    for j in range(2):
        P.op("dve", lambda e, j=j: e.tensor_tensor(out=junk[:, 0:64], in0=lam_t[:, 2 * j, :], in1=lam_t[:, 2 * j + 1, :], op=ALU.mult), reads=[b_lam], writes=[b_junk])
        P.op("dve", lambda e, j=j: e.reduce_sum(out=sm[:, j:j + 1], in_=junk[:, 0:64], axis=AX.X), reads=[b_junk], writes=[b_sm[j]])
        P.op("act", lambda e, j=j: e.activation(out=sm[:, j:j + 1], in_=sm[:, j:j + 1], func=AF.Exp), reads=[b_sm[j]], writes=[b_sm[j]])
    P.op("dve", lambda e: e.tensor_tensor(out=sm[:, 2:3], in0=sm[:, 1:2], in1=sm[:, 0:1], op=ALU.subtract), reads=[b_sm[0], b_sm[1]], writes=[b_sm[2]])
    P.op("dve", lambda e: e.tensor_tensor(out=sm[:, 2:3], in0=sm[:, 2:3], in1=cst[:, 0:1], op=ALU.subtract), reads=[b_sm[2], b_cst], writes=[b_sm[2]])
    NEGLAM = (sm[:, 2:3], b_sm[2])

    gi = 0
    for qi in range(NQ):
        nk = qi + 1
        groups = [(g0, min(4, nk - g0)) for g0 in range(0, nk, 4)]
        for gidx, (g0, gn) in enumerate(groups):
            par = gi % 2; gi += 1
            for m in range(2):
                for j in range(gn):
                    kt = g0 + j
                    P.op("pe", lambda e, m=m, j=j, kt=kt, par=par, qi=qi: e.matmul(pS[m][par][:, j * 128:(j + 1) * 128], lhsT=qkb[2 + m][:, kt * 128:(kt + 1) * 128], rhs=qkb[m][:, qi * 128:(qi + 1) * 128], start=True, stop=True),
                         reads=[b_qkb[2 + m], b_qkb[m]], writes=[b_pS[m][par]])
                P.op("act", lambda e, m=m, par=par, gn=gn: e.activation(out=ET[m][par][:, 0:gn, :].rearrange("p g q -> p (g q)"), in_=pS[m][par][:, 0:gn * 128], func=AF.Exp, scale=0.125),
                     reads=[b_pS[m][par]], writes=[b_ET[m][par]])
                if g0 + gn == nk:
                    j = gn - 1
                    P.op("dve", lambda e, m=m, par=par, j=j: e.tensor_tensor(out=ET[m][par][:, j, :], in0=ET[m][par][:, j, :], in1=tri[:], op=ALU.mult),
                         reads=[b_ET[m][par], b_tri], writes=[b_ET[m][par]])
                for j in range(gn):
                    kt = g0 + j
                    first = (kt == 0); last = (kt == nk - 1)
                    P.op("pe", lambda e, m=m, j=j, kt=kt, par=par, first=first, last=last: e.matmul(pO[m][:, 0:130], lhsT=ET[m][par][:, j, :], rhs=vb[:, kt, :], start=first, stop=last),
                         reads=[b_ET[m][par], b_vb], writes=[b_pO[m]])
        op_ = qi % 2
        P.op("dve", lambda e: e.reciprocal(out=sm[:, 4:5], in_=pO[0][:, 128:129]), reads=[b_pO[0]], writes=[b_sm[4]])
        P.op("dve", lambda e: e.reciprocal(out=sm[:, 5:6], in_=pO[1][:, 128:129]), reads=[b_pO[1]], writes=[b_sm[5]])
        P.op("dve", lambda e: e.tensor_tensor(out=sm[:, 5:6], in0=sm[:, 5:6], in1=NEGLAM[0], op=ALU.mult), reads=[b_sm[5], NEGLAM[1]], writes=[b_sm[5]])
        P.op("dve", lambda e: e.tensor_scalar(out=t2[:], in0=pO[1][:, 0:128], scalar1=sm[:, 5:6], scalar2=None, op0=ALU.mult), reads=[b_pO[1], b_sm[5]], writes=[b_t2])
        P.op("dve", lambda e, op_=op_: e.scalar_tensor_tensor(out=ob[op_][:], in0=pO[0][:, 0:128], scalar=sm[:, 4:5], in1=t2[:], op0=ALU.mult, op1=ALU.add), reads=[b_pO[0], b_sm[4], b_t2], writes=[b_ob[op_]])
        P.op("act", lambda e, op_=op_: e.activation(out=junk[:], in_=ob[op_][:], func=AF.Square, scale=float(128 ** -0.5), accum_out=sm[:, 6:7]), reads=[b_ob[op_]], writes=[b_junk, b_sm[6]])
        P.op("dve", lambda e: e.tensor_scalar(out=sm[:, 6:7], in0=sm[:, 6:7], scalar1=1e-6, scalar2=None, op0=ALU.add), reads=[b_sm[6]], writes=[b_sm[6]])
        P.op("act", lambda e: e.activation(out=sm[:, 6:7], in_=sm[:, 6:7], func=AF.Sqrt), reads=[b_sm[6]], writes=[b_sm[6]])
        P.op("dve", lambda e: e.reciprocal(out=sm[:, 6:7], in_=sm[:, 6:7]), reads=[b_sm[6]], writes=[b_sm[6]])
        P.op("dve", lambda e: e.tensor_tensor(out=sm[:, 6:7], in0=sm[:, 6:7], in1=cst[:, 1:2], op=ALU.mult), reads=[b_sm[6], b_cst], writes=[b_sm[6]])
        P.op("dve", lambda e, op_=op_: e.scalar_tensor_tensor(out=ob[op_][:], in0=ob[op_][:], scalar=sm[:, 6:7], in1=gsub[:], op0=ALU.mult, op1=ALU.mult), reads=[b_ob[op_], b_sm[6], b_gsub], writes=[b_ob[op_]])
        P.dma("pool", lambda e, qi=qi, op_=op_: e.dma_start(out=y_d[qi], in_=ob[op_][:]), reads=[b_ob[op_]], sembuf=b_ob[op_])
    P.emit(); P.close()
    return nc


def build_dsa(L, R=32.0, K=22):
    NQ = L // 128
    nc = bass.Bass("TRN2", target_bir_lowering=False)
    P = ProgOld(nc); B = P.buf
    def din(name, shape): return nc.dram_tensor(name, list(shape), F32, kind="ExternalInput").ap()
    qk_d = din("qk", [2, 128, L])
    v_d = din("v", [128, NQ, 128])
    qi_d = din("qi", [NQ, 64, 8, 128])
    ki_d = din("ki", [64, L])
    wi_d = din("wi", [128, NQ, 8])
    negm_d = din("negm", [128, 128])
    idn_d = din("idn", [128, 128])
    y_d = nc.dram_tensor("y", [NQ, 128, 128], F32, kind="ExternalOutput").ap()
    dbg_d = nc.dram_tensor("dbg", [NQ, 128, 8], F32, kind="ExternalOutput").ap()
    dbg = [P.sb(f"dbg{i}", [128, 8]) for i in range(2)]; b_dbg = [B(), B()]

    qkb = [P.sb(f"qkb{i}", [128, L], BF16) for i in range(2)]; b_qkb = [B(), B()]
    vb = P.sb("vb", [128, NQ, 130], BF16); b_vb = B()
    kiT = P.sb("kiT", [64, L]); b_ki = B()
    wi = P.sb("wi_t", [128, NQ, 8]); b_wi = B()
    negm = P.sb("negm_t", [128, 128]); b_negm = B()
    idf = P.sb("idf", [128, 128]); b_idf = B()
    idb = P.sb("idb", [128, 128], BF16); b_idb = B()
    stg = [P.sb(f"stg{i}", [128, 2048]) for i in range(2)]; b_stg = [B(), B()]
    qit = [P.sb(f"qit{i}", [64, 8, 128]) for i in range(2)]; b_qit = [B(), B()]
    score = [P.sb(f"score{i}", [128, L]) for i in range(2)]; b_score = [B(), B()]
    junkS = P.sb("junkS", [128, L], BF16); b_junkS = B()
    rl = [P.sb(f"rl{i}", [128, 512]) for i in range(2)]; b_rl = [B(), B()]
    Eb = [P.sb(f"Eb{i}", [128, 512], BF16) for i in range(2)]; b_Eb = [B(), B()]
    Pm = [P.sb(f"Pm{i}", [128, 512], BF16) for i in range(2)]; b_Pm = [B(), B()]
    PmT = [P.sb(f"PmT{i}", [128, 4, 128], BF16) for i in range(2)]; b_PmT = [B(), B()]
    ob = [P.sb(f"ob{i}", [128, 128]) for i in range(2)]; b_ob = [B(), B()]
    sm = P.sb("sm", [128, 8]); b_sm = [B() for _ in range(8)]
    pD = [P.ps(f"pD{i}", [128, 512]) for i in range(2)]; b_pD = [B(), B()]
    pS = [P.ps(f"pS{i}", [128, 512]) for i in range(2)]; b_pS = [B(), B()]
    pT = [P.ps(f"pT{i}", [128, 1024], BF16) for i in range(2)]; b_pT = [B(), B()]
    pO = P.ps("pO", [128, 512]); b_pO = B()

    n = 0
    CW = min(2048, L)
    for i in range(2):
        for c0 in range(0, L, CW):
            s = n % 2; n += 1
            P.dma("sp", lambda e, i=i, c0=c0, s=s: e.dma_start(out=stg[s][:, 0:CW], in_=qk_d[i, :, c0:c0 + CW]), writes=[b_stg[s]])
            P.op("pool", lambda e, i=i, c0=c0, s=s: e.tensor_copy(out=qkb[i][:, c0:c0 + CW], in_=stg[s][:, 0:CW]), reads=[b_stg[s]], writes=[b_qkb[i]])
    TW = min(16, NQ)
    for t0 in range(0, NQ, TW):
        s = n % 2; n += 1
        P.dma("sp", lambda e, t0=t0, s=s: e.dma_start(out=stg[s][:, 0:TW * 128], in_=v_d[:, t0:t0 + TW, :].rearrange("p t d -> p (t d)")), writes=[b_stg[s]])
        P.op("pool", lambda e, t0=t0, s=s: e.tensor_copy(out=vb[:, t0:t0 + TW, 0:128], in_=stg[s][:, 0:TW * 128].rearrange("p (t d) -> p t d", d=128)), reads=[b_stg[s]], writes=[b_vb])
    P.op("dve", lambda e: e.memset(vb[:, :, 128:130], 1.0), writes=[b_vb])
    P.dma("sp", lambda e: e.dma_start(out=kiT[:], in_=ki_d), writes=[b_ki])
    P.dma("sp", lambda e: e.dma_start(out=wi[:], in_=wi_d), writes=[b_wi])
    P.dma("sp", lambda e: e.dma_start(out=negm[:], in_=negm_d), writes=[b_negm])
    P.dma("sp", lambda e: e.dma_start(out=idf[:], in_=idn_d), writes=[b_idf])
    P.op("dve", lambda e: e.tensor_copy(out=idb[:], in_=idf[:]), reads=[b_idf], writes=[b_idb])
    SC = float((64 ** -0.5) * (8 ** -0.5))
    ci = 0; ai = 0
    for qi in range(NQ):
        nk = qi + 1; nkeys = nk * 128
        sp_ = qi % 2
        sc = score[sp_]; bsc = b_score[sp_]
        P.dma("sp", lambda e, qi=qi, sp_=sp_: e.dma_start(out=qit[sp_][:], in_=qi_d[qi]), writes=[b_qit[sp_]])
        chunks = [(c0, min(4, nk - c0)) for c0 in range(0, nk, 4)]
        for (c0, cn) in chunks:
            w = cn * 128; k0 = c0 * 128
            for h in range(8):
                p = ci % 2; ci += 1
                P.op("pe", lambda e, h=h, p=p, k0=k0, w=w, sp_=sp_: e.matmul(pD[p][:, 0:w], lhsT=qit[sp_][:, h, :], rhs=kiT[:, k0:k0 + w], start=True, stop=True),
                     reads=[b_qit[sp_], b_ki], writes=[b_pD[p]])
                P.op("act", lambda e, p=p, w=w: e.activation(out=rl[p][:, 0:w], in_=pD[p][:, 0:w], func=AF.Relu, scale=SC), reads=[b_pD[p]], writes=[b_rl[p]])
                if h == 0:
                    P.op("dve", lambda e, p=p, w=w, k0=k0, sc=sc, qi=qi, h=h: e.tensor_scalar(out=sc[:, k0:k0 + w], in0=rl[p][:, 0:w], scalar1=wi[:, qi, h:h + 1], scalar2=None, op0=ALU.mult),
                         reads=[b_rl[p], b_wi], writes=[bsc])
                else:
                    P.op("dve", lambda e, p=p, w=w, k0=k0, sc=sc, qi=qi, h=h: e.scalar_tensor_tensor(out=sc[:, k0:k0 + w], in0=rl[p][:, 0:w], scalar=wi[:, qi, h:h + 1], in1=sc[:, k0:k0 + w], op0=ALU.mult, op1=ALU.add),
                         reads=[b_rl[p], b_wi, bsc], writes=[bsc])
        d0 = (nk - 1) * 128
        P.op("dve", lambda e, sc=sc, d0=d0: e.tensor_tensor(out=sc[:, d0:d0 + 128], in0=sc[:, d0:d0 + 128], in1=negm[:], op=ALU.add), reads=[bsc, b_negm], writes=[bsc])
        tau = sm[:, 0:1]; mid = sm[:, 1:2]; cnt = sm[:, 2:3]; s_ = sm[:, 3:4]
        if nkeys <= 256:
            P.op("dve", lambda e: e.memset(tau, -R), writes=[b_sm[0]])
        else:
            P.op("dve", lambda e: e.memset(mid, 0.0), writes=[b_sm[1]])
            for it in range(K):
                P.op("dve", lambda e, sc=sc, nkeys=nkeys: e.tensor_scalar(out=junkS[:, 0:nkeys], in0=sc[:, 0:nkeys], scalar1=mid, scalar2=0.0, op0=ALU.is_ge, op1=ALU.add, accum_out=cnt),
                     reads=[bsc, b_sm[1]], writes=[b_junkS, b_sm[2]])
                if it < K - 1:
                    wn = R / 2 ** (it + 1)
                    P.op("dve", lambda e, wn=wn: e.tensor_scalar(out=s_, in0=cnt, scalar1=255.5, scalar2=2 * wn, op0=ALU.is_ge, op1=ALU.mult), reads=[b_sm[2]], writes=[b_sm[3]])
                    P.op("dve", lambda e, wn=wn: e.scalar_tensor_tensor(out=mid, in0=s_, scalar=-wn, in1=mid, op0=ALU.add, op1=ALU.add), reads=[b_sm[3], b_sm[1]], writes=[b_sm[1]])
                else:
                    wl = R / 2 ** (K - 1)
                    P.op("dve", lambda e, wl=wl: e.tensor_scalar(out=s_, in0=cnt, scalar1=255.5, scalar2=wl, op0=ALU.is_ge, op1=ALU.mult), reads=[b_sm[2]], writes=[b_sm[3]])
                    P.op("dve", lambda e, wl=wl: e.scalar_tensor_tensor(out=tau, in0=s_, scalar=-wl, in1=mid, op0=ALU.add, op1=ALU.add), reads=[b_sm[3], b_sm[1]], writes=[b_sm[0]])
        P.op("dve", lambda e, sp_=sp_: e.tensor_copy(out=dbg[sp_][:], in_=sm[:]), reads=b_sm, writes=[b_dbg[sp_]])
        P.dma("pool", lambda e, qi=qi, sp_=sp_: e.dma_start(out=dbg_d[qi], in_=dbg[sp_][:]), reads=[b_dbg[sp_]], sembuf=b_dbg[sp_])
        for (c0, cn) in chunks:
            w = cn * 128; k0 = c0 * 128
            p = ai % 2; ai += 1
            P.op("pe", lambda e, p=p, k0=k0, w=w, qi=qi: e.matmul(pS[p][:, 0:w], lhsT=qkb[0][:, qi * 128:(qi + 1) * 128], rhs=qkb[1][:, k0:k0 + w], start=True, stop=True),
                 reads=[b_qkb[0], b_qkb[1]], writes=[b_pS[p]])
            P.op("act", lambda e, p=p, w=w: e.activation(out=Eb[p][:, 0:w], in_=pS[p][:, 0:w], func=AF.Exp, scale=float(128 ** -0.5)), reads=[b_pS[p]], writes=[b_Eb[p]])
            P.op("dve", lambda e, p=p, w=w, k0=k0, sc=sc: e.scalar_tensor_tensor(out=Pm[p][:, 0:w], in0=sc[:, k0:k0 + w], scalar=tau, in1=Eb[p][:, 0:w], op0=ALU.is_ge, op1=ALU.mult),
                 reads=[bsc, b_sm[0], b_Eb[p]], writes=[b_Pm[p]])
            for j in range(cn):
                P.op("pe", lambda e, p=p, j=j: e.transpose(pT[p][:, j * 128:(j + 1) * 128], Pm[p][:, j * 128:(j + 1) * 128], idb[:]), reads=[b_Pm[p], b_idb], writes=[b_pT[p]])
            P.op("act", lambda e, p=p, w=w, cn=cn: e.copy(out=PmT[p][:, 0:cn, :].rearrange("p g q -> p (g q)"), in_=pT[p][:, 0:w]), reads=[b_pT[p]], writes=[b_PmT[p]])
            for j in range(cn):
                kt = c0 + j
                P.op("pe", lambda e, p=p, j=j, kt=kt, nk=nk: e.matmul(pO[:, 0:130], lhsT=PmT[p][:, j, :], rhs=vb[:, kt, :], start=(kt == 0), stop=(kt == nk - 1)),
                     reads=[b_PmT[p], b_vb], writes=[b_pO])
        op_ = qi % 2
        P.op("dve", lambda e: e.reciprocal(out=sm[:, 4:5], in_=pO[:, 128:129]), reads=[b_pO], writes=[b_sm[4]])
        P.op("dve", lambda e, op_=op_: e.tensor_scalar(out=ob[op_][:], in0=pO[:, 0:128], scalar1=sm[:, 4:5], scalar2=None, op0=ALU.mult), reads=[b_pO, b_sm[4]], writes=[b_ob[op_]])
        P.dma("pool", lambda e, qi=qi, op_=op_: e.dma_start(out=y_d[qi], in_=ob[op_][:]), reads=[b_ob[op_]], sembuf=b_ob[op_])
    P.emit(); P.close()
    return nc

D = 1024; TOK = 2048; NT = 16; EPS = 1e-6

def build_merge():
    nc = bass.Bass("TRN2", target_bir_lowering=False)
    P = ProgOld(nc); B = P.buf
    def din(name, shape): return nc.dram_tensor(name, list(shape), F32, kind="ExternalInput").ap()
    x_in = din("x", [TOK, D]); g_mpre = din("g_mpre", [128, D]); g_mpost = din("g_mpost", [128, D])
    wgate = din("wgate", [8, 128, 3072]); wbr = din("wbr", [12, 128, D]); wo = din("wo", [8, 128, D])
    yT = din("yT", [NT, 128, 12 * 128]); idn_d = din("idn", [128, 128])
    x_out = nc.dram_tensor("xo", [TOK, D], F32, kind="ExternalOutput").ap()
    ident_f = P.sb("ident_f", [128, 128]); ident = P.sb("ident", [128, 128], BF16)
    gpre_t = P.sb("gpre_t", [128, D]); gpost_t = P.sb("gpost_t", [128, D])
    Wg = P.sb("Wg", [128, 8, 3072], BF16); Wb = P.sb("Wb", [128, 12, D], BF16); Wo = P.sb("Wo", [128, 8, D], BF16)
    stg = [P.sb(f"stg{i}", [128, 3072]) for i in range(2)]
    xt = [P.sb(f"xt{i}", [128, D]) for i in range(2)]; ot = [P.sb(f"ot{i}", [128, D]) for i in range(2)]
    mg = P.sb("mg", [128, D]); tmp = P.sb("tmp", [128, 512]); junk = P.sb("junk", [128, D])
    hb = P.sb("hb", [128, D], BF16); mb = P.sb("mb", [128, D], BF16)
    hTt = P.sb("hTt", [128, 8, 128], BF16); mT = P.sb("mT", [128, 8, 128], BF16)
    ystg = [P.sb(f"ystg{i}", [128, 1536]) for i in range(2)]; ytb = [P.sb(f"ytb{i}", [128, 12, 128], BF16) for i in range(2)]
    sgt = [P.sb(f"sgt{i}", [128, 512]) for i in range(2)]
    st = P.sb("st", [128, 8])
    pA = [P.ps(f"pA{i}", [128, 512]) for i in range(2)]; pB = [P.ps(f"pB{i}", [128, 512]) for i in range(2)]
    pC = [P.ps(f"pC{i}", [128, 512]) for i in range(2)]; pT = P.ps("pT", [128, 1024], BF16)
    b_idf = B(); b_id = B(); b_gpre = B(); b_gpost = B(); b_Wg = B(); b_Wb = B(); b_Wo = B(); b_stg = [B(), B()]
    b_xt = [B(), B()]; b_ot = [B(), B()]; b_mg = B(); b_tmp = B(); b_junk = B(); b_hb = B(); b_mb = B(); b_hTt = B(); b_mT = B()
    b_ystg = [B(), B()]; b_ytb = [B(), B()]; b_sgt = [B(), B()]; b_st = [B() for _ in range(8)]
    b_pA = [B(), B()]; b_pB = [B(), B()]; b_pC = [B(), B()]; b_pT = B()
    P.dma("sp", lambda e: e.dma_start(out=ident_f[:], in_=idn_d), writes=[b_idf])
    P.op("dve", lambda e: e.tensor_copy(out=ident[:], in_=ident_f[:]), reads=[b_idf], writes=[b_id])
    P.dma("sp", lambda e: e.dma_start(out=gpre_t[:], in_=g_mpre), writes=[b_gpre])
    P.dma("sp", lambda e: e.dma_start(out=gpost_t[:], in_=g_mpost), writes=[b_gpost])
    n = 0
    for k in range(8):
        s = n % 2; n += 1
        P.dma("sp", lambda e, k=k, s=s: e.dma_start(out=stg[s][:, :], in_=wgate[k]), writes=[b_stg[s]])
        P.op("pool", lambda e, k=k, s=s: e.tensor_copy(out=Wg[:, k, :], in_=stg[s][:, :]), reads=[b_stg[s]], writes=[b_Wg])
    for k in range(12):
        s = n % 2; n += 1
        P.dma("sp", lambda e, k=k, s=s: e.dma_start(out=stg[s][:, 0:D], in_=wbr[k]), writes=[b_stg[s]])
        P.op("pool", lambda e, k=k, s=s: e.tensor_copy(out=Wb[:, k, :], in_=stg[s][:, 0:D]), reads=[b_stg[s]], writes=[b_Wb])
    for k in range(8):
        s = n % 2; n += 1
        P.dma("sp", lambda e, k=k, s=s: e.dma_start(out=stg[s][:, 0:D], in_=wo[k]), writes=[b_stg[s]])
        P.op("pool", lambda e, k=k, s=s: e.tensor_copy(out=Wo[:, k, :], in_=stg[s][:, 0:D]), reads=[b_stg[s]], writes=[b_Wo])

    def rstd_of(src_ap, src_bufs, col):
        ss = st[:, col:col + 1]; bss = b_st[col]
        P.op("act", lambda e: e.activation(out=junk[:], in_=src_ap, func=AF.Square, scale=float(D ** -0.5), accum_out=ss), reads=src_bufs, writes=[b_junk, bss])
        P.op("dve", lambda e: e.tensor_scalar(out=ss, in0=ss, scalar1=EPS, scalar2=None, op0=ALU.add), reads=[bss], writes=[bss])
        P.op("act", lambda e: e.activation(out=ss, in_=ss, func=AF.Sqrt), reads=[bss], writes=[bss])
        P.op("dve", lambda e: e.reciprocal(out=ss, in_=ss), reads=[bss], writes=[bss])
        return ss, bss

    qn = 0
    for ti in range(NT):
        par = ti % 2
        P.dma("sp", lambda e, ti=ti, par=par: e.dma_start(out=xt[par][:], in_=x_in[ti * 128:(ti + 1) * 128, :]), writes=[b_xt[par]])
        P.dma("sp", lambda e, ti=ti, par=par: e.dma_start(out=ystg[par][:], in_=yT[ti]), writes=[b_ystg[par]])
        P.op("pool", lambda e, par=par: e.tensor_copy(out=ytb[par][:].rearrange("p a b -> p (a b)"), in_=ystg[par][:]), reads=[b_ystg[par]], writes=[b_ytb[par]])
        rs, brs = rstd_of(xt[par][:], [b_xt[par]], 0)
        P.op("dve", lambda e, par=par, rs=rs: e.scalar_tensor_tensor(out=hb[:], in0=xt[par][:], scalar=rs, in1=gpre_t[:], op0=ALU.mult, op1=ALU.mult), reads=[b_xt[par], brs, b_gpre], writes=[b_hb])
        for k in range(8):
            P.op("pe", lambda e, k=k: e.transpose(pT[:, k * 128:(k + 1) * 128], hb[:, k * 128:(k + 1) * 128], ident[:]), reads=[b_hb, b_id], writes=[b_pT])
        P.op("act", lambda e: e.copy(out=hTt[:].rearrange("p k t -> p (k t)"), in_=pT[:]), reads=[b_pT], writes=[b_hTt])
        for half in range(2):
            for nb in range(3):
                q = qn % 2; qn += 1
                c0 = nb * 1024 + half * 512
                for k in range(8):
                    P.op("pe", lambda e, k=k, q=q, c0=c0: e.matmul(pA[q][:], lhsT=hTt[:, k, :], rhs=Wg[:, k, c0:c0 + 512], start=(k == 0), stop=(k == 7)), reads=[b_hTt, b_Wg], writes=[b_pA[q]])
                P.op("act", lambda e, q=q: e.activation(out=sgt[q][:], in_=pA[q][:], func=AF.Sigmoid), reads=[b_pA[q]], writes=[b_sgt[q]])
                for kc in range(4):
                    P.op("pe", lambda e, kc=kc, q=q, nb=nb, half=half, par=par: e.matmul(pB[q][:], lhsT=ytb[par][:, nb * 4 + kc, :], rhs=Wb[:, nb * 4 + kc, half * 512:(half + 1) * 512], start=(kc == 0), stop=(kc == 3)), reads=[b_ytb[par], b_Wb], writes=[b_pB[q]])
                if nb == 0:
                    P.op("dve", lambda e, q=q, half=half: e.tensor_tensor(out=mg[:, half * 512:(half + 1) * 512], in0=sgt[q][:], in1=pB[q][:], op=ALU.mult), reads=[b_sgt[q], b_pB[q]], writes=[b_mg])
                else:
                    P.op("dve", lambda e, q=q: e.tensor_tensor(out=tmp[:], in0=sgt[q][:], in1=pB[q][:], op=ALU.mult), reads=[b_sgt[q], b_pB[q]], writes=[b_tmp])
                    P.op("dve", lambda e, half=half: e.tensor_tensor(out=mg[:, half * 512:(half + 1) * 512], in0=mg[:, half * 512:(half + 1) * 512], in1=tmp[:], op=ALU.add), reads=[b_mg, b_tmp], writes=[b_mg])
        P.op("act", lambda e: e.copy(out=mb[:], in_=mg[:]), reads=[b_mg], writes=[b_mb])
        for k in range(8):
            P.op("pe", lambda e, k=k: e.transpose(pT[:, k * 128:(k + 1) * 128], mb[:, k * 128:(k + 1) * 128], ident[:]), reads=[b_mb, b_id], writes=[b_pT])
        P.op("act", lambda e: e.copy(out=mT[:].rearrange("p k t -> p (k t)"), in_=pT[:]), reads=[b_pT], writes=[b_mT])
        for half in range(2):
            for k in range(8):
                P.op("pe", lambda e, k=k, half=half: e.matmul(pC[half][:], lhsT=mT[:, k, :], rhs=Wo[:, k, half * 512:(half + 1) * 512], start=(k == 0), stop=(k == 7)), reads=[b_mT, b_Wo], writes=[b_pC[half]])
            P.op("act", lambda e, half=half, par=par: e.copy(out=ot[par][:, half * 512:(half + 1) * 512], in_=pC[half][:]), reads=[b_pC[half]], writes=[b_ot[par]])
        rs, brs = rstd_of(ot[par][:], [b_ot[par]], 1)
        P.op("dve", lambda e, par=par, rs=rs: e.scalar_tensor_tensor(out=ot[par][:], in0=ot[par][:], scalar=rs, in1=gpost_t[:], op0=ALU.mult, op1=ALU.mult), reads=[b_ot[par], brs, b_gpost], writes=[b_ot[par]])
        P.op("dve", lambda e, par=par: e.tensor_tensor(out=ot[par][:], in0=ot[par][:], in1=xt[par][:], op=ALU.add), reads=[b_ot[par], b_xt[par]], writes=[b_ot[par]])
        P.dma("pool", lambda e, ti=ti, par=par: e.dma_start(out=x_out[ti * 128:(ti + 1) * 128, :], in_=ot[par][:]), reads=[b_ot[par]], sembuf=b_ot[par])
    P.emit(); P.close()
    return nc


def build_rwkv(L):
    SEG = min(L, 512); NSEG = L // SEG; NCH = SEG // 64
    nc = bass.Bass("TRN2", target_bir_lowering=False)
    P = ProgOld(nc); B = P.buf
    def din(name, shape): return nc.dram_tensor(name, list(shape), F32, kind="ExternalInput").ap()
    zr_d = din("zr", [3, 64, 2, L + 1]); zl_d = din("zl", [64, 2, L + 1]); zg_d = din("zg", [128, L + 1])
    mu3_d = din("mu3", [64, 3, 2]); mul_d = din("mul", [64, 2]); mug_d = din("mug", [128, 1])
    pp_d = din("pp", [64, 5, 2]); wup_d = din("wup", [64, 2, 64]); aup_d = din("aup", [64, 2, 64]); gup_d = din("gup", [128, 128])
    lnwb_d = din("lnwb", [64, 2, 128]); cmask_d = din("cmask", [64, 2 * SEG]); mask5_d = din("mask5", [64, 320])
    idn_d = din("idn", [64, 64])
    y_d = nc.dram_tensor("y", [L // 64, 64, 128], F32, kind="ExternalOutput").ap()
    def T(name, shape, dt=F32):
        return P.sb(name, shape, dt), B(name)
    raw3, b_raw3 = T("raw3", [64, 3, 2, SEG + 1]); rawl, b_rawl = T("rawl", [64, 2, SEG + 1]); rawg, b_rawg = T("rawg", [128, SEG + 1])
    mu3, b_mu3 = T("mu3t", [64, 3, 2]); mul, b_mul = T("mult", [64, 2]); mug, b_mug = T("mugt", [128, 1])
    pp, b_pp = T("ppt", [64, 5, 2]); wup, b_wup = T("wupt", [64, 2, 64]); aup, b_aup = T("aupt", [64, 2, 64]); gup, b_gup = T("gupt", [128, 128])
    lnwb, b_lnwb = T("lnwbt", [64, 2, 128]); cmask, b_cmask = T("cmaskt", [64, 2 * SEG]); mask5, b_mask5 = T("mask5t", [64, 320])
    idn, b_idn = T("idnt", [64, 64]); ones, b_ones = T("onest", [64, 64])
    d3, b_d3 = T("d3", [64, 3, 2, SEG]); dl, b_dl = T("dl", [64, 2, SEG]); dg, b_dg = T("dg", [128, SEG])
    x3, b_x3 = T("x3", [64, 3, 2, SEG]); xl, b_xl = T("xl", [64, 2, SEG]); xg, b_xg = T("xg", [128, SEG])
    tw, b_tw = T("tw", [64, SEG]); sgc, b_sgc = T("sgc", [128, SEG]); sgw, b_sgw = T("sgw", [64, 2, SEG]); aa, b_aa = T("aa", [64, 2, SEG])
    t1, b_t1 = T("t1", [64, 2, SEG]); sq, b_sq = T("sq", [64, 2, SEG]); rn, b_rn = T("rn", [64, 2, SEG]); kk, b_kk = T("kk", [64, 2, SEG])
    kp, b_kp = T("kp", [64, 2, SEG]); bb, b_bb = T("bb", [64, 2, SEG]); cs, b_cs = T("cs", [64, 2, SEG])
    epos, b_epos = T("epos", [64, 2, SEG]); eneg, b_eneg = T("eneg", [64, 2, SEG]); eprev, b_eprev = T("eprev", [64, 2, SEG])
    AR, b_AR = T("AR", [64, 2, NCH, 2, 64]); Bt, b_Bt = T("Bt", [64, 2, SEG]); Kt, b_Kt = T("Kt", [64, 2, SEG]); rkr, b_rkr = T("rkr", [64, 2, SEG])
    Hs = [T(f"H{i}", [64, 2, 64]) for i in range(2)]
    Msb = [T(f"Msb{h}", [64, 320]) for h in range(2)]
    TK, b_TK = T("TK", [64, 6, 64])
    PPs = [T(f"PP{i}", [64, 2, 2, 64]) for i in range(2)]
    Xs = [T(f"X{i}", [64, 2, 64]) for i in range(2)]
    Wsb, b_Wsb = T("Wsb", [64, 128]); Usb, b_Usb = T("Usb", [64, 128]); Ysb, b_Ysb = T("Ysb", [64, 128]); yc, b_yc = T("yc", [64, 128])
    outs = [T(f"out{i}", [64, 128]) for i in range(2)]
    sm, _ = T("sm", [64, 16]); b_sm = [B() for _ in range(16)]
    junk, b_junk = T("junk", [64, 64])
    def PS(name, shape): return P.ps(name, shape), B(name)
    pM = [PS(f"pM{h}", [64, 512]) for h in range(2)]
    pK, b_pK = PS("pK", [64, 512]); pI, b_pI = PS("pI", [64, 512]); pX, b_pX = PS("pX", [64, 512])
    pW, b_pW = PS("pW", [64, 512]); pY, b_pY = PS("pY", [64, 512]); pH, b_pH = PS("pH", [64, 512])

    for (t, b, d) in [(mu3, b_mu3, mu3_d), (mul, b_mul, mul_d), (mug, b_mug, mug_d), (pp, b_pp, pp_d), (wup, b_wup, wup_d), (aup, b_aup, aup_d),
                      (gup, b_gup, gup_d), (lnwb, b_lnwb, lnwb_d), (cmask, b_cmask, cmask_d), (mask5, b_mask5, mask5_d), (idn, b_idn, idn_d)]:
        P.dma("sp", lambda e, t=t, d=d: e.dma_start(out=t[:], in_=d), writes=[b])
    P.op("dve", lambda e: e.memset(ones[:], 1.0), writes=[b_ones])
    P.op("dve", lambda e: e.memset(Hs[0][0][:], 0.0), writes=[Hs[0][1]])
    hcur = 0
    NEG = -0.6065306597126334
    oi = 0
    for sg_ in range(NSEG):
        s0 = sg_ * SEG
        for a in range(3):
            P.dma("sp", lambda e, s0=s0, a=a: e.dma_start(out=raw3[:, a, :, :], in_=zr_d[a, :, :, s0:s0 + SEG + 1]), writes=[b_raw3])
        P.dma("sp", lambda e, s0=s0: e.dma_start(out=rawl[:], in_=zl_d[:, :, s0:s0 + SEG + 1]), writes=[b_rawl])
        P.dma("sp", lambda e, s0=s0: e.dma_start(out=rawg[:], in_=zg_d[:, s0:s0 + SEG + 1]), writes=[b_rawg])
        P.op("dve", lambda e: e.tensor_tensor(out=d3[:], in0=raw3[:, :, :, 0:SEG], in1=raw3[:, :, :, 1:SEG + 1], op=ALU.subtract), reads=[b_raw3], writes=[b_d3])
        P.op("dve", lambda e: e.tensor_tensor(out=dl[:], in0=rawl[:, :, 0:SEG], in1=rawl[:, :, 1:SEG + 1], op=ALU.subtract), reads=[b_rawl], writes=[b_dl])
        P.op("dve", lambda e: e.tensor_tensor(out=dg[:], in0=rawg[:, 0:SEG], in1=rawg[:, 1:SEG + 1], op=ALU.subtract), reads=[b_rawg], writes=[b_dg])
        for a in range(3):
            for h in range(2):
                P.op("dve", lambda e, a=a, h=h: e.scalar_tensor_tensor(out=x3[:, a, h, :], in0=d3[:, a, h, :], scalar=mu3[:, a, h:h + 1], in1=raw3[:, a, h, 1:SEG + 1], op0=ALU.mult, op1=ALU.add),
                     reads=[b_d3, b_mu3, b_raw3], writes=[b_x3])
        for a in range(2):
            P.op("dve", lambda e, a=a: e.scalar_tensor_tensor(out=xl[:, a, :], in0=dl[:, a, :], scalar=mul[:, a:a + 1], in1=rawl[:, a, 1:SEG + 1], op0=ALU.mult, op1=ALU.add),
                 reads=[b_dl, b_mul, b_rawl], writes=[b_xl])
        P.op("dve", lambda e: e.scalar_tensor_tensor(out=xg[:], in0=dg[:], scalar=mug[:, 0:1], in1=rawg[:, 1:SEG + 1], op0=ALU.mult, op1=ALU.add), reads=[b_dg, b_mug, b_rawg], writes=[b_xg])
        XR = lambda h: x3[:, 0, h, :]
        XK = lambda h: x3[:, 1, h, :]
        XV = lambda h: x3[:, 2, h, :]
        P.op("act", lambda e: e.activation(out=tw[:], in_=xl[:, 0, :], func=AF.Tanh), reads=[b_xl], writes=[b_tw])
        P.op("act", lambda e: e.activation(out=sgc[:], in_=xg[:], func=AF.Sigmoid), reads=[b_xg], writes=[b_sgc])
        for h in range(2):
            P.op("pe", lambda e, h=h: e.matmul(pK[:, 0:SEG], lhsT=wup[:, h, :], rhs=tw[:], start=True, stop=True), reads=[b_wup, b_tw], writes=[b_pK])
            P.op("act", lambda e, h=h: e.activation(out=sgw[:, h, :], in_=pK[:, 0:SEG], func=AF.Sigmoid, bias=pp[:, 0, h:h + 1]), reads=[b_pK, b_pp], writes=[b_sgw])
            P.op("pe", lambda e, h=h: e.matmul(pK[:, 0:SEG], lhsT=aup[:, h, :], rhs=xl[:, 1, :], start=True, stop=True), reads=[b_aup, b_xl], writes=[b_pK])
            P.op("act", lambda e, h=h: e.activation(out=aa[:, h, :], in_=pK[:, 0:SEG], func=AF.Sigmoid, bias=pp[:, 1, h:h + 1]), reads=[b_pK, b_pp], writes=[b_aa])
        for h in range(2):
            P.op("dve", lambda e, h=h: e.tensor_scalar(out=t1[:, h, :], in0=XK(h), scalar1=pp[:, 2, h:h + 1], scalar2=None, op0=ALU.mult), reads=[b_x3, b_pp], writes=[b_t1])
        P.op("dve", lambda e: e.tensor_tensor(out=sq[:], in0=t1[:], in1=t1[:], op=ALU.mult), reads=[b_t1], writes=[b_sq])
        for h in range(2):
            P.op("pe", lambda e, h=h: e.matmul(pK[:, 0:SEG], lhsT=ones[:], rhs=sq[:, h, :], start=True, stop=True), reads=[b_ones, b_sq], writes=[b_pK])
            P.op("dve", lambda e, h=h: e.tensor_scalar(out=rn[:, h, :], in0=pK[:, 0:SEG], scalar1=1e-24, scalar2=None, op0=ALU.max), reads=[b_pK], writes=[b_rn])
        P.op("act", lambda e: e.activation(out=rn[:], in_=rn[:], func=AF.Sqrt), reads=[b_rn], writes=[b_rn])
        P.op("dve", lambda e: e.reciprocal(out=rn[:], in_=rn[:]), reads=[b_rn], writes=[b_rn])
        P.op("dve", lambda e: e.tensor_tensor(out=kk[:], in0=t1[:], in1=rn[:], op=ALU.mult), reads=[b_t1, b_rn], writes=[b_kk])
        for h in range(2):
            P.op("dve", lambda e, h=h: e.tensor_scalar(out=kp[:, h, :], in0=aa[:, h, :], scalar1=pp[:, 3, h:h + 1], scalar2=pp[:, 3, h:h + 1], op0=ALU.mult, op1=ALU.subtract), reads=[b_aa, b_pp], writes=[b_kp])
            P.op("dve", lambda e, h=h: e.scalar_tensor_tensor(out=kp[:, h, :], in0=kp[:, h, :], scalar=1.0, in1=XK(h), op0=ALU.add, op1=ALU.mult), reads=[b_kp, b_x3], writes=[b_kp])
        P.op("dve", lambda e: e.tensor_tensor(out=bb[:], in0=kk[:], in1=aa[:], op=ALU.mult), reads=[b_kk, b_aa], writes=[b_bb])
        FL = lambda t: t[:].rearrange("p h s -> p (h s)")
        P.op("dve", lambda e: e.tensor_tensor_scan(out=FL(cs), data0=cmask[:], data1=FL(sgw), initial=0.0, op0=ALU.mult, op1=ALU.add), reads=[b_cmask, b_sgw], writes=[b_cs])
        P.op("act", lambda e: e.activation(out=epos[:], in_=cs[:], func=AF.Exp, scale=NEG), reads=[b_cs], writes=[b_epos])
        P.op("act", lambda e: e.activation(out=eneg[:], in_=cs[:], func=AF.Exp, scale=-NEG), reads=[b_cs], writes=[b_eneg])
        P.op("dve", lambda e: e.tensor_tensor(out=eprev[:], in0=cs[:], in1=sgw[:], op=ALU.subtract), reads=[b_cs, b_sgw], writes=[b_eprev])
        P.op("act", lambda e: e.activation(out=eprev[:], in_=eprev[:], func=AF.Exp, scale=NEG), reads=[b_eprev], writes=[b_eprev])
        for h in range(2):
            P.op("dve", lambda e, h=h: e.scalar_tensor_tensor(out=AR[:, h, :, 0, :], in0=kk[:, h, :].rearrange("p (c t) -> p c t", t=64), scalar=-1.0, in1=eprev[:, h, :].rearrange("p (c t) -> p c t", t=64), op0=ALU.mult, op1=ALU.mult),
                 reads=[b_kk, b_eprev], writes=[b_AR])
            P.op("dve", lambda e, h=h: e.tensor_tensor(out=AR[:, h, :, 1, :], in0=XR(h).rearrange("p (c t) -> p c t", t=64), in1=epos[:, h, :].rearrange("p (c t) -> p c t", t=64), op=ALU.mult),
                 reads=[b_x3, b_epos], writes=[b_AR])
            P.op("dve", lambda e, h=h: e.scalar_tensor_tensor(out=rkr[:, h, :], in0=XR(h), scalar=pp[:, 4, h:h + 1], in1=kp[:, h, :], op0=ALU.mult, op1=ALU.mult), reads=[b_x3, b_pp, b_kp], writes=[b_rkr])
        P.op("dve", lambda e: e.tensor_tensor(out=Bt[:], in0=bb[:], in1=eneg[:], op=ALU.mult), reads=[b_bb, b_eneg], writes=[b_Bt])
        P.op("dve", lambda e: e.tensor_tensor(out=Kt[:], in0=kp[:], in1=eneg[:], op=ALU.mult), reads=[b_kp, b_eneg], writes=[b_Kt])
        for c in range(NCH):
            cs_ = slice(c * 64, (c + 1) * 64)
            for h in range(2):
                pm, bpm = pM[h]
                P.op("pe", lambda e, h=h, c=c, pm=pm, cs_=cs_: e.matmul(pm[:, 0:64], lhsT=AR[:, h, c, 0, :], rhs=Bt[:, h, cs_], start=True, stop=True), reads=[b_AR, b_Bt], writes=[bpm])
                P.op("pe", lambda e, h=h, c=c, pm=pm, cs_=cs_: e.matmul(pm[:, 64:192], lhsT=Bt[:, h, cs_], rhs=AR[:, h, c, :, :].rearrange("p a t -> p (a t)"), start=True, stop=True), reads=[b_AR, b_Bt], writes=[bpm])
                P.op("pe", lambda e, h=h, c=c, pm=pm, cs_=cs_: e.matmul(pm[:, 192:320], lhsT=Kt[:, h, cs_], rhs=AR[:, h, c, :, :].rearrange("p a t -> p (a t)"), start=True, stop=True), reads=[b_AR, b_Kt], writes=[bpm])
                P.op("dve", lambda e, h=h, pm=pm: e.tensor_tensor(out=Msb[h][0][:], in0=pm[:, 0:320], in1=mask5[:], op=ALU.mult), reads=[bpm, b_mask5], writes=[Msb[h][1]])
            for h in range(2):
                for a, (src, bsrc) in enumerate([(Bt[:, h, cs_], b_Bt), (Kt[:, h, cs_], b_Kt), (x3[:, 2, h, cs_], b_x3)]):
                    P.op("pe", lambda e, h=h, a=a, src=src: e.transpose(pK[:, (h * 3 + a) * 64:(h * 3 + a + 1) * 64], src, idn[:]), reads=[bsrc, b_idn], writes=[b_pK])
            P.op("act", lambda e: e.copy(out=TK[:].rearrange("p a t -> p (a t)"), in_=pK[:, 0:384]), reads=[b_pK], writes=[b_TK])
            pcur = 0; xcur = 0
            for h in range(2):
                P.op("act", lambda e, h=h: e.copy(out=PPs[0][0][:, h, :, :].rearrange("p a t -> p (a t)"), in_=Msb[h][0][:, 0:128]), reads=[Msb[h][1]], writes=[PPs[0][1]])
                P.op("dve", lambda e, h=h: e.tensor_tensor(out=Xs[0][0][:, h, :], in0=Msb[h][0][:, 64:128], in1=idn[:], op=ALU.add), reads=[Msb[h][1], b_idn], writes=[Xs[0][1]])
            for stp in range(5):
                pp_t, pp_b = PPs[pcur]; pn_t, pn_b = PPs[1 - pcur]
                x_t, x_b = Xs[xcur]; xn_t, xn_b = Xs[1 - xcur]
                for h in range(2):
                    P.op("pe", lambda e, h=h, pp_t=pp_t: e.matmul(pI[:, (h * 2) * 64:(h * 2 + 1) * 64], lhsT=pp_t[:, h, 1, :], rhs=pp_t[:, h, 0, :], start=True, stop=True), reads=[pp_b], writes=[b_pI])
                    P.op("pe", lambda e, h=h, pp_t=pp_t: e.matmul(pI[:, (h * 2 + 1) * 64:(h * 2 + 2) * 64], lhsT=pp_t[:, h, 0, :], rhs=pp_t[:, h, 1, :], start=True, stop=True), reads=[pp_b], writes=[b_pI])
                P.op("act", lambda e, pn_t=pn_t: e.copy(out=pn_t[:].rearrange("p h a t -> p (h a t)"), in_=pI[:, 0:256]), reads=[b_pI], writes=[pn_b])
                for h in range(2):
                    P.op("pe", lambda e, h=h, x_t=x_t: e.matmul(pX[:, h * 64:(h + 1) * 64], lhsT=idn[:], rhs=x_t[:, h, :], start=True, stop=False), reads=[b_idn, x_b], writes=[b_pX])
                    P.op("pe", lambda e, h=h, x_t=x_t, pn_t=pn_t: e.matmul(pX[:, h * 64:(h + 1) * 64], lhsT=pn_t[:, h, 0, :], rhs=x_t[:, h, :], start=False, stop=True), reads=[pn_b, x_b], writes=[b_pX])
                P.op("dve", lambda e, xn_t=xn_t: e.tensor_copy(out=xn_t[:].rearrange("p h t -> p (h t)"), in_=pX[:, 0:128]), reads=[b_pX], writes=[xn_b])
                pcur = 1 - pcur; xcur = 1 - xcur
            X_t, X_b = Xs[xcur]
            H_t, H_b = Hs[hcur]; Hn_t, Hn_b = Hs[1 - hcur]
            for h in range(2):
                P.op("pe", lambda e, h=h, c=c, H_t=H_t: e.matmul(pW[:, h * 64:(h + 1) * 64], lhsT=AR[:, h, c, 0, :], rhs=H_t[:, h, :], start=True, stop=False), reads=[b_AR, H_b], writes=[b_pW])
                P.op("pe", lambda e, h=h: e.matmul(pW[:, h * 64:(h + 1) * 64], lhsT=Msb[h][0][:, 192:256], rhs=TK[:, h * 3 + 2, :], start=False, stop=True), reads=[Msb[h][1], b_TK], writes=[b_pW])
            P.op("act", lambda e: e.copy(out=Wsb[:], in_=pW[:, 0:128]), reads=[b_pW], writes=[b_Wsb])
            for h in range(2):
                P.op("pe", lambda e, h=h, X_t=X_t: e.matmul(pW[:, 128 + h * 64:128 + (h + 1) * 64], lhsT=X_t[:, h, :], rhs=Wsb[:, h * 64:(h + 1) * 64], start=True, stop=True), reads=[X_b, b_Wsb], writes=[b_pW])
            P.op("dve", lambda e: e.tensor_copy(out=Usb[:], in_=pW[:, 128:256]), reads=[b_pW], writes=[b_Usb])
            for h in range(2):
                hs = slice(h * 64, (h + 1) * 64)
                P.op("pe", lambda e, h=h, c=c, hs=hs, H_t=H_t: e.matmul(pY[:, hs], lhsT=AR[:, h, c, 1, :], rhs=H_t[:, h, :], start=True, stop=False), reads=[b_AR, H_b], writes=[b_pY])
                P.op("pe", lambda e, h=h, hs=hs: e.matmul(pY[:, hs], lhsT=Msb[h][0][:, 128:192], rhs=Usb[:, hs], start=False, stop=False), reads=[Msb[h][1], b_Usb], writes=[b_pY])
                P.op("pe", lambda e, h=h, hs=hs: e.matmul(pY[:, hs], lhsT=Msb[h][0][:, 256:320], rhs=TK[:, h * 3 + 2, :], start=False, stop=True), reads=[Msb[h][1], b_TK], writes=[b_pY])
            P.op("pe", lambda e, cs_=cs_: e.matmul(pY[:, 128:256], lhsT=sgc[:, cs_], rhs=gup[:], start=True, stop=True), reads=[b_sgc, b_gup], writes=[b_pY])
            for h in range(2):
                P.op("pe", lambda e, h=h, cs_=cs_: e.matmul(pY[:, 256 + 2 * h:258 + 2 * h], lhsT=rkr[:, h, cs_], rhs=ones[:, 0:2], start=True, stop=True), reads=[b_rkr, b_ones], writes=[b_pY])
            for h in range(2):
                hs = slice(h * 64, (h + 1) * 64)
                P.op("pe", lambda e, h=h, hs=hs, H_t=H_t: e.matmul(pH[:, hs], lhsT=idn[:], rhs=H_t[:, h, :], start=True, stop=False), reads=[b_idn, H_b], writes=[b_pH])
                P.op("pe", lambda e, h=h, hs=hs: e.matmul(pH[:, hs], lhsT=TK[:, h * 3 + 0, :], rhs=Usb[:, hs], start=False, stop=False), reads=[b_TK, b_Usb], writes=[b_pH])
                P.op("pe", lambda e, h=h, hs=hs: e.matmul(pH[:, hs], lhsT=TK[:, h * 3 + 1, :], rhs=TK[:, h * 3 + 2, :], start=False, stop=True), reads=[b_TK], writes=[b_pH])
            for h in range(2):
                ce = c * 64 + 63
                P.op("dve", lambda e, h=h, ce=ce, Hn_t=Hn_t: e.tensor_scalar(out=Hn_t[:, h, :], in0=pH[:, h * 64:(h + 1) * 64], scalar1=epos[:, h, ce:ce + 1], scalar2=None, op0=ALU.mult), reads=[b_pH, b_epos], writes=[Hn_b])
            hcur = 1 - hcur
            P.op("act", lambda e: e.copy(out=Ysb[:], in_=pY[:, 0:128]), reads=[b_pY], writes=[b_Ysb])
            P.op("dve", lambda e: e.tensor_copy(out=sm[:, 8:12], in_=pY[:, 256:260]), reads=[b_pY], writes=[b_sm[8]])
            o_t, o_b = outs[oi % 2]; oi += 1
            for h in range(2):
                hs = slice(h * 64, (h + 1) * 64)
                mcol = sm[:, h:h + 1]; vcol = sm[:, 2 + h:3 + h]
                P.op("dve", lambda e, hs=hs, mcol=mcol: e.reduce_sum(out=mcol, in_=Ysb[:, hs], axis=AX.X), reads=[b_Ysb], writes=[b_sm[h]])
                P.op("dve", lambda e, mcol=mcol: e.tensor_scalar(out=mcol, in0=mcol, scalar1=-1.0 / 64, scalar2=None, op0=ALU.mult), reads=[b_sm[h]], writes=[b_sm[h]])
                P.op("dve", lambda e, hs=hs, mcol=mcol: e.tensor_scalar(out=yc[:, hs], in0=Ysb[:, hs], scalar1=mcol, scalar2=None, op0=ALU.add), reads=[b_Ysb, b_sm[h]], writes=[b_yc])
                P.op("act", lambda e, hs=hs, vcol=vcol: e.activation(out=junk[:], in_=yc[:, hs], func=AF.Square, scale=0.125, accum_out=vcol), reads=[b_yc], writes=[b_junk, b_sm[2 + h]])
                P.op("dve", lambda e, vcol=vcol: e.tensor_scalar(out=vcol, in0=vcol, scalar1=64e-5, scalar2=None, op0=ALU.add), reads=[b_sm[2 + h]], writes=[b_sm[2 + h]])
                P.op("act", lambda e, vcol=vcol: e.activation(out=vcol, in_=vcol, func=AF.Sqrt), reads=[b_sm[2 + h]], writes=[b_sm[2 + h]])
                P.op("dve", lambda e, vcol=vcol: e.reciprocal(out=vcol, in_=vcol), reads=[b_sm[2 + h]], writes=[b_sm[2 + h]])
                P.op("dve", lambda e, hs=hs, vcol=vcol: e.scalar_tensor_tensor(out=yc[:, hs], in0=yc[:, hs], scalar=vcol, in1=lnwb[:, 0, hs], op0=ALU.mult, op1=ALU.mult), reads=[b_yc, b_sm[2 + h], b_lnwb], writes=[b_yc])
                P.op("dve", lambda e, hs=hs: e.tensor_tensor(out=yc[:, hs], in0=yc[:, hs], in1=lnwb[:, 1, hs], op=ALU.add), reads=[b_yc, b_lnwb], writes=[b_yc])
                P.op("dve", lambda e, hs=hs, h=h: e.scalar_tensor_tensor(out=yc[:, hs], in0=TK[:, h * 3 + 2, :], scalar=sm[:, 8 + 2 * h:9 + 2 * h], in1=yc[:, hs], op0=ALU.mult, op1=ALU.add), reads=[b_TK, b_sm[8], b_yc], writes=[b_yc])
            P.op("dve", lambda e, o_t=o_t: e.tensor_tensor(out=o_t[:], in0=yc[:], in1=pY[:, 128:256], op=ALU.mult), reads=[b_yc, b_pY], writes=[o_b])
            gci = sg_ * NCH + c
            P.dma("pool", lambda e, gci=gci, o_t=o_t: e.dma_start(out=y_d[gci], in_=o_t[:]), reads=[o_b], sembuf=o_b)
    P.emit(); P.close()
    return nc


F32 = mybir.dt.float32
BF16 = mybir.dt.bfloat16
I32 = mybir.dt.int32
AF = mybir.ActivationFunctionType
ALU = mybir.AluOpType
AX = mybir.AxisListType


class Buf:
    __slots__ = ("name", "w", "r", "semidx", "cnt")

    def __init__(self, name):
        self.name = name
        self.w = None
        self.r = {}
        self.semidx = None
        self.cnt = 0


class Prog:
    ENG = ("pe", "act", "dve", "pool", "sp")

    def __init__(self, nc):
        self.nc = nc
        self.g = ExitStack()
        self.esem = {e: self.g.enter_context(nc.semaphore(f"s_{e}")) for e in self.ENG}
        self.cnt = {e: 0 for e in self.ENG}
        self.seen = {e: {} for e in self.ENG}
        self.dsem = []
        self.dfree = []
        self.ops = None
        self.ph = None
        self.live = []
        self.nbuf = 0
        self.nph = 0
        self.jreg = None

    def sb(self, name, shape, dt=F32):
        return self.ph.enter_context(self.nc.sbuf_tensor(f"{name}_p{self.nph}", list(shape), dt))

    def ps(self, name, shape, dt=F32):
        return self.ph.enter_context(self.nc.psum_tensor(f"{name}_p{self.nph}", list(shape), dt))

    def buf(self, name=None):
        self.nbuf += 1
        return Buf(name or f"b{self.nbuf}")

    def _barrier_waits(self, eng):
        need = [(e2, self.cnt[e2]) for e2 in self.ENG if self.cnt[e2] > 0]
        need += [(("d", i), c) for i, (h, c) in enumerate(self.dsem) if c > 0]
        out = []
        seen = self.seen[eng]
        for k, v in need:
            if seen.get(k, 0) >= v:
                continue
            seen[k] = v
            out.append((k, v))
        return out

    def begin(self):
        self.nph += 1
        self.ph = ExitStack()
        self.ops = {e: [] for e in self.ENG}
        self.live = []
        for e in self.ENG:
            self.ops[e].append((self._barrier_waits(e), None, None))

    def _waits(self, eng, reads, writes):
        need = {}

        def add(k, v):
            if need.get(k, 0) < v:
                need[k] = v
        for b in reads:
            if b.w is not None:
                add(*b.w)
        for b in writes:
            if b.w is not None:
                add(*b.w)
            for k, v in b.r.items():
                add(k, v)
        out = []
        seen = self.seen[eng]
        for k, v in need.items():
            if k == "pe" and eng == "pe":
                continue
            if seen.get(k, 0) >= v:
                continue
            seen[k] = v
            out.append((k, v))
        return out

    def _mark(self, tok, reads, writes):
        k, v = tok
        for b in reads:
            if b.r.get(k, 0) < v:
                b.r[k] = v
        for b in writes:
            b.w = tok
            b.r = {}

    def op(self, eng, fn, reads=(), writes=()):
        waits = self._waits(eng, reads, writes)
        self.cnt[eng] += 1
        tok = (eng, self.cnt[eng])
        self._mark(tok, reads, writes)
        self.ops[eng].append((waits, fn, (eng, 1)))

    def dma(self, q, fn, reads=(), writes=(), sembuf=None, inc=16):
        waits = self._waits(q, reads, writes)
        sbf = sembuf if sembuf is not None else (writes[0] if writes else reads[0])
        if sbf.semidx is None:
            if self.dfree:
                sbf.semidx = self.dfree.pop()
            else:
                h = self.g.enter_context(self.nc.semaphore(f"d{len(self.dsem)}"))
                self.dsem.append([h, 0])
                sbf.semidx = len(self.dsem) - 1
            sbf.cnt = self.dsem[sbf.semidx][1]
            self.live.append(sbf)
        sbf.cnt += inc
        self.dsem[sbf.semidx][1] = sbf.cnt
        key = ("d", sbf.semidx)
        tok = (key, sbf.cnt)
        self._mark(tok, reads, writes)
        self.ops[q].append((waits, fn, (key, inc)))

    def _semof(self, k):
        return self.esem[k] if isinstance(k, str) else self.dsem[k[1]][0]

    def end(self):
        nc = self.nc
        ops = self.ops
        with nc.Block() as block:
            def run(engobj, lst):
                for waits, fn, inc in lst:
                    for k, v in waits:
                        engobj.wait_ge(self._semof(k), v)
                    if fn is None:
                        continue
                    ins = fn(engobj)
                    if inc is not None:
                        ins.then_inc(self._semof(inc[0]), inc[1])

            @block.tensor
            def _(e):
                run(e, ops["pe"])

            @block.scalar
            def _(e):
                run(e, ops["act"])

            @block.vector
            def _(e):
                run(e, ops["dve"])

            @block.gpsimd
            def _(e):
                run(e, ops["pool"])

            @block.sync
            def _(e):
                run(e, ops["sp"])
        for b in self.live:
            self.dfree.append(b.semidx)
            b.semidx = None
        self.live = []
        self.ph.close()
        self.ph = None

    def finish(self):
        self.begin()
        self.end()
        self.g.close()


D = 1024; DFF = 2816; NFF = 22; EPS = 1e-6
NWIN = 43
ZR = NWIN * 128
JB = 1152
CB = 4 * JB


def emit_tok(P, TOK, xsrc, xdst, W, zdst=None):
    NT = TOK // 128
    HALF = min(1024, TOK); NH = TOK // HALF; BLK = min(512, HALF); NB = HALF // BLK; TPH = HALF // 128
    has_win = zdst is not None
    P.begin()
    B = P.buf
    g_pre, g_post, wg, wu, wd, idn_d = W["g_pre"], W["g_post"], W["wg"], W["wu"], W["wd"], W["idn"]
    ident_f = P.sb("ident_f", [128, 128]); ident = P.sb("ident", [128, 128], BF16)
    gpre_t = P.sb("gpre_t", [128, D]); gpost_t = P.sb("gpost_t", [128, D])
    hT = P.sb("hT", [128, 8, TOK], BF16)
    AT = P.sb("AT", [128, NFF, HALF], BF16)
    Wd = P.sb("Wd", [128, NFF, D], BF16)
    stg = [P.sb(f"stg{i}", [128, 2048]) for i in range(2)]
    wgb = [P.sb(f"wgb{i}", [128, 8, 128], BF16) for i in range(2)]
    wub = [P.sb(f"wub{i}", [128, 8, 128], BF16) for i in range(2)]
    xt = [P.sb(f"xt{i}", [128, D]) for i in range(2)]
    ot = [P.sb(f"ot{i}", [128, D]) for i in range(2)]
    junk = P.sb("junk", [128, D])
    hb = [P.sb(f"hb{i}", [128, D], BF16) for i in range(2)]
    sg = [P.sb(f"sg{i}", [128, 512]) for i in range(2)]
    st = P.sb("st", [128, 64])
    pA = [P.ps(f"pA{i}", [128, 512]) for i in range(2)]
    pB = [P.ps(f"pB{i}", [128, 512]) for i in range(2)]
    pC = [P.ps(f"pC{i}", [128, 512]) for i in range(2)]
    pT = [P.ps(f"pT{i}", [128, 1024], BF16) for i in range(2)]
    b_ident = B(); b_identf = B(); b_gpre = B(); b_gpost = B(); b_hT = [B() for _ in range(NT)]
    b_AT = [[B() for _ in range(NB)] for _ in range(NFF)]
    b_Wd = [B() for _ in range(NFF)]
    b_stg = [B(), B()]; b_wgb = [B(), B()]; b_wub = [B(), B()]; b_xt = [B(), B()]; b_ot = [B(), B()]
    b_junk = B(); b_hb = [B(), B()]; b_sg = [B(), B()]
    b_pA = [B(), B()]; b_pB = [B(), B()]; b_pC = [B(), B()]; b_pT = [B(), B()]
    st_next = [0]; b_st = {}

    def stcol():
        i = st_next[0] % 64; st_next[0] += 1
        if i not in b_st: b_st[i] = B()
        return st[:, i:i + 1], b_st[i]

    P.dma("sp", lambda e: e.dma_start(out=ident_f[:], in_=idn_d), writes=[b_identf])
    P.op("dve", lambda e: e.tensor_copy(out=ident[:], in_=ident_f[:]), reads=[b_identf], writes=[b_ident])
    P.dma("sp", lambda e: e.dma_start(out=gpre_t[:], in_=g_pre), writes=[b_gpre])
    P.dma("sp", lambda e: e.dma_start(out=gpost_t[:], in_=g_post), writes=[b_gpost])

    def rstd_of(src_ap, src_bufs):
        ss, bss = stcol(); rs, brs = stcol()
        P.op("act", lambda e: e.activation(out=junk[:], in_=src_ap, func=AF.Square, scale=float(D ** -0.5), accum_out=ss), reads=src_bufs, writes=[b_junk, bss])
        P.op("dve", lambda e: e.tensor_scalar(out=rs, in0=ss, scalar1=EPS, scalar2=None, op0=ALU.add), reads=[bss], writes=[brs])
        P.op("act", lambda e: e.activation(out=rs, in_=rs, func=AF.Sqrt), reads=[brs], writes=[brs])
        P.op("dve", lambda e: e.reciprocal(out=rs, in_=rs), reads=[brs], writes=[brs])
        return rs, brs

    def norm_to_hT(src_ap, src_bufs, g_t, b_g, ti, par):
        rs, brs = rstd_of(src_ap, src_bufs)
        P.op("dve", lambda e: e.scalar_tensor_tensor(out=hb[par][:], in0=src_ap, scalar=rs, in1=g_t[:], op0=ALU.mult, op1=ALU.mult), reads=src_bufs + [brs, b_g], writes=[b_hb[par]])
        for k in range(8):
            P.op("pe", lambda e, k=k: e.transpose(pT[par][:, k * 128:(k + 1) * 128], hb[par][:, k * 128:(k + 1) * 128], ident[:]), reads=[b_hb[par], b_ident], writes=[b_pT[par]])
        P.op("act", lambda e: e.copy(out=hT[:, :, ti * 128:(ti + 1) * 128], in_=pT[par][:].rearrange("p (k t) -> p k t", k=8)), reads=[b_pT[par]], writes=[b_hT[ti]])

    for ti in range(NT):
        par = ti % 2
        P.dma("sp", lambda e, ti=ti, par=par: e.dma_start(out=xt[par][:], in_=xsrc[ti * 128:(ti + 1) * 128, :]), writes=[b_xt[par]])
        norm_to_hT(xt[par][:], [b_xt[par]], gpre_t, b_gpre, ti, par)
    for c in range(NFF):
        s = c % 2
        P.dma("sp", lambda e, c=c, s=s: e.dma_start(out=stg[s][:, 0:D], in_=wd[c]), writes=[b_stg[s]])
        P.op("pool", lambda e, c=c, s=s: e.tensor_copy(out=Wd[:, c, :], in_=stg[s][:, 0:D]), reads=[b_stg[s]], writes=[b_Wd[c]])
    if has_win:
        gmix_t = P.sb("gmix_t", [128, D]); b_gmix = B()
        P.dma("sp", lambda e: e.dma_start(out=gmix_t[:], in_=W["g_mix"]), writes=[b_gmix])
    for half in range(NH):
        for c in range(NFF):
            s = c % 2
            P.dma("sp", lambda e, c=c, s=s: e.dma_start(out=stg[s][:, 0:1024], in_=wg[c].rearrange("p k f -> p (k f)")), writes=[b_stg[s]])
            P.op("pool", lambda e, s=s: e.tensor_copy(out=wgb[s][:].rearrange("p k f -> p (k f)"), in_=stg[s][:, 0:1024]), reads=[b_stg[s]], writes=[b_wgb[s]])
            P.dma("sp", lambda e, c=c, s=s: e.dma_start(out=stg[s][:, 1024:2048], in_=wu[c].rearrange("p k f -> p (k f)")), writes=[b_stg[s]])
            P.op("pool", lambda e, s=s: e.tensor_copy(out=wub[s][:].rearrange("p k f -> p (k f)"), in_=stg[s][:, 1024:2048]), reads=[b_stg[s]], writes=[b_wub[s]])
            for tb in range(NB):
                t0 = half * HALF + tb * BLK
                rd = [b_hT[(t0 // 128) + i] for i in range(BLK // 128)]
                q = tb % 2
                for k in range(8):
                    P.op("pe", lambda e, k=k, s=s, q=q, t0=t0: e.matmul(pA[q][:, 0:BLK], lhsT=wgb[s][:, k, :], rhs=hT[:, k, t0:t0 + BLK], start=(k == 0), stop=(k == 7)), reads=[b_wgb[s]] + rd, writes=[b_pA[q]])
                for k in range(8):
                    P.op("pe", lambda e, k=k, s=s, q=q, t0=t0: e.matmul(pB[q][:, 0:BLK], lhsT=wub[s][:, k, :], rhs=hT[:, k, t0:t0 + BLK], start=(k == 0), stop=(k == 7)), reads=[b_wub[s]] + rd, writes=[b_pB[q]])
                P.op("act", lambda e, q=q: e.activation(out=sg[q][:, 0:BLK], in_=pA[q][:, 0:BLK], func=AF.Silu), reads=[b_pA[q]], writes=[b_sg[q]])
                P.op("dve", lambda e, q=q, c=c, tb=tb: e.tensor_tensor(out=AT[:, c, tb * BLK:(tb + 1) * BLK], in0=sg[q][:, 0:BLK], in1=pB[q][:, 0:BLK], op=ALU.mult), reads=[b_sg[q], b_pB[q]], writes=[b_AT[c][tb]])
        for tl in range(TPH):
            ti = half * TPH + tl; par = ti % 2
            P.dma("sp", lambda e, ti=ti, par=par: e.dma_start(out=xt[par][:], in_=xsrc[ti * 128:(ti + 1) * 128, :]), writes=[b_xt[par]])
            for ch in range(2):
                for c in range(NFF):
                    P.op("pe", lambda e, c=c, ch=ch, tl=tl: e.matmul(pC[ch][:], lhsT=AT[:, c, tl * 128:(tl + 1) * 128], rhs=Wd[:, c, ch * 512:(ch + 1) * 512], start=(c == 0), stop=(c == NFF - 1)), reads=[b_AT[c][(tl * 128) // BLK], b_Wd[c]], writes=[b_pC[ch]])
                P.op("act", lambda e, ch=ch, par=par: e.copy(out=ot[par][:, ch * 512:(ch + 1) * 512], in_=pC[ch][:]), reads=[b_pC[ch]], writes=[b_ot[par]])
            rs, brs = rstd_of(ot[par][:], [b_ot[par]])
            P.op("dve", lambda e, par=par, rs=rs: e.scalar_tensor_tensor(out=ot[par][:], in0=ot[par][:], scalar=rs, in1=gpost_t[:], op0=ALU.mult, op1=ALU.mult), reads=[b_ot[par], brs, b_gpost], writes=[b_ot[par]])
            P.op("dve", lambda e, par=par: e.scalar_tensor_tensor(out=ot[par][:], in0=ot[par][:], scalar=0.5, in1=xt[par][:], op0=ALU.mult, op1=ALU.add), reads=[b_ot[par], b_xt[par]], writes=[b_ot[par]])
            P.dma("pool", lambda e, ti=ti, par=par: e.dma_start(out=xdst[ti * 128:(ti + 1) * 128, :], in_=ot[par][:]), reads=[b_ot[par]], sembuf=b_ot[par])
            if has_win:
                norm_to_hT(ot[par][:], [b_ot[par]], gmix_t, b_gmix, ti, par)
    if has_win:
        win = W["win"]
        ZW = min(1024, TOK)
        zs = [P.sb(f"zs{i}", [128, ZW]) for i in range(2)]; b_zs = [B(), B()]
        zi_n = 0
        for c in range(NWIN):
            s = c % 2
            P.dma("sp", lambda e, c=c, s=s: e.dma_start(out=stg[s][:, 0:1024], in_=win[c].rearrange("p k f -> p (k f)")), writes=[b_stg[s]])
            P.op("pool", lambda e, s=s: e.tensor_copy(out=wgb[s][:].rearrange("p k f -> p (k f)"), in_=stg[s][:, 0:1024]), reads=[b_stg[s]], writes=[b_wgb[s]])
            for z0 in range(0, TOK, ZW):
                zi = zi_n % 2; zi_n += 1
                for bi, t0 in enumerate(range(z0, z0 + ZW, BLK)):
                    q = bi % 2
                    rd = [b_hT[(t0 // 128) + i] for i in range(BLK // 128)]
                    for k in range(8):
                        P.op("pe", lambda e, k=k, s=s, q=q, t0=t0: e.matmul(pA[q][:, 0:BLK], lhsT=wgb[s][:, k, :], rhs=hT[:, k, t0:t0 + BLK], start=(k == 0), stop=(k == 7)), reads=[b_wgb[s]] + rd, writes=[b_pA[q]])
                    if bi % 2 == 0:
                        P.op("act", lambda e, q=q, zi=zi, t0=t0, z0=z0: e.copy(out=zs[zi][:, t0 - z0:t0 - z0 + BLK], in_=pA[q][:, 0:BLK]), reads=[b_pA[q]], writes=[b_zs[zi]])
                    else:
                        P.op("dve", lambda e, q=q, zi=zi, t0=t0, z0=z0: e.tensor_copy(out=zs[zi][:, t0 - z0:t0 - z0 + BLK], in_=pA[q][:, 0:BLK]), reads=[b_pA[q]], writes=[b_zs[zi]])
                P.dma("pool", lambda e, c=c, zi=zi, z0=z0: e.dma_start(out=zdst[c * 128:(c + 1) * 128, z0:z0 + ZW], in_=zs[zi][:]), reads=[b_zs[zi]], sembuf=b_zs[zi])
    P.end()


def emit_diff_h(nc, P, PFX, L):
    NQ = L // 128
    P.begin(); B = P.buf
    def din(name, shape): return nc.dram_tensor(PFX + name, list(shape), F32, kind="ExternalInput").ap()
    qk_d = din("qk", [4, 64, L])
    v_d = din("v", [128, NQ, 128])
    lam_d = din("lam", [128, 4, 64])
    cst_d = din("cst", [128, 2])
    gsub_d = din("gsub", [128, 128])
    tri_d = din("tri", [128, 128])
    y_d = nc.dram_tensor(PFX + "y", [NQ, 128, 128], F32, kind="ExternalOutput").ap()

    qkb = [P.sb(f"qkb{i}", [64, L], BF16) for i in range(4)]; b_qkb = [B() for _ in range(4)]
    vb = P.sb("vb", [128, NQ, 130], BF16); b_vb = B()
    stg = [P.sb(f"stg{i}", [128, 2048]) for i in range(2)]; b_stg = [B(), B()]
    lam_t = P.sb("lam_t", [128, 4, 64]); b_lam = B()
    cst = P.sb("cst_t", [128, 2]); b_cst = B()
    gsub = P.sb("gsub_t", [128, 128]); b_gsub = B()
    tri_f = P.sb("tri_f", [128, 128]); b_trif = B()
    tri = P.sb("tri_b", [128, 128], BF16); b_tri = B()
    ones = P.sb("ones", [128, 2], BF16); b_ones = B()
    sm = P.sb("sm", [128, 16]); b_sm = [B() for _ in range(16)]
    junk = P.sb("junk", [128, 128]); b_junk = B()
    ET = [[P.sb(f"ET{m}{i}", [128, 4, 128], BF16) for i in range(2)] for m in range(2)]
    b_ET = [[B(), B()] for _ in range(2)]
    ob = [P.sb(f"ob{i}", [128, 128]) for i in range(2)]; b_ob = [B(), B()]
    t2 = P.sb("t2", [128, 128]); b_t2 = B()
    pS = [[P.ps(f"pS{m}{i}", [128, 512]) for i in range(2)] for m in range(2)]; b_pS = [[B(), B()] for _ in range(2)]
    pO = [P.ps(f"pO{m}", [128, 512]) for m in range(2)]; b_pO = [B(), B()]

    n = 0
    for i in range(4):
        CW = min(2048, L)
        for c0 in range(0, L, CW):
            s = n % 2; n += 1
            P.dma("sp", lambda e, i=i, c0=c0, s=s: e.dma_start(out=stg[s][0:64, 0:CW], in_=qk_d[i, :, c0:c0 + CW]), writes=[b_stg[s]])
            P.op("pool", lambda e, i=i, c0=c0, s=s: e.tensor_copy(out=qkb[i][:, c0:c0 + CW], in_=stg[s][0:64, 0:CW]), reads=[b_stg[s]], writes=[b_qkb[i]])
    TW = min(16, NQ)
    for t0 in range(0, NQ, TW):
        s = n % 2; n += 1
        P.dma("sp", lambda e, t0=t0, s=s: e.dma_start(out=stg[s][:, 0:TW * 128], in_=v_d[:, t0:t0 + TW, :].rearrange("p t d -> p (t d)")), writes=[b_stg[s]])
        P.op("pool", lambda e, t0=t0, s=s: e.tensor_copy(out=vb[:, t0:t0 + TW, 0:128], in_=stg[s][:, 0:TW * 128].rearrange("p (t d) -> p t d", d=128)), reads=[b_stg[s]], writes=[b_vb])
    P.dma("sp", lambda e: e.dma_start(out=lam_t[:], in_=lam_d), writes=[b_lam])
    P.dma("sp", lambda e: e.dma_start(out=cst[:], in_=cst_d), writes=[b_cst])
    P.dma("sp", lambda e: e.dma_start(out=gsub[:], in_=gsub_d), writes=[b_gsub])
    P.dma("sp", lambda e: e.dma_start(out=tri_f[:], in_=tri_d), writes=[b_trif])
    P.op("dve", lambda e: e.tensor_copy(out=tri[:], in_=tri_f[:]), reads=[b_trif], writes=[b_tri])
    P.op("dve", lambda e: e.memset(vb[:, :, 128:130], 1.0), writes=[b_vb])
    for j in range(2):
        P.op("dve", lambda e, j=j: e.tensor_tensor(out=junk[:, 0:64], in0=lam_t[:, 2 * j, :], in1=lam_t[:, 2 * j + 1, :], op=ALU.mult), reads=[b_lam], writes=[b_junk])
        P.op("dve", lambda e, j=j: e.reduce_sum(out=sm[:, j:j + 1], in_=junk[:, 0:64], axis=AX.X), reads=[b_junk], writes=[b_sm[j]])
        P.op("act", lambda e, j=j: e.activation(out=sm[:, j:j + 1], in_=sm[:, j:j + 1], func=AF.Exp), reads=[b_sm[j]], writes=[b_sm[j]])
    P.op("dve", lambda e: e.tensor_tensor(out=sm[:, 2:3], in0=sm[:, 1:2], in1=sm[:, 0:1], op=ALU.subtract), reads=[b_sm[0], b_sm[1]], writes=[b_sm[2]])
    P.op("dve", lambda e: e.tensor_tensor(out=sm[:, 2:3], in0=sm[:, 2:3], in1=cst[:, 0:1], op=ALU.subtract), reads=[b_sm[2], b_cst], writes=[b_sm[2]])
    NEGLAM = (sm[:, 2:3], b_sm[2])

    gi = 0
    for qi in range(NQ):
        nk = qi + 1
        groups = [(g0, min(4, nk - g0)) for g0 in range(0, nk, 4)]
        for gidx, (g0, gn) in enumerate(groups):
            par = gi % 2; gi += 1
            for m in range(2):
                for j in range(gn):
                    kt = g0 + j
                    P.op("pe", lambda e, m=m, j=j, kt=kt, par=par, qi=qi: e.matmul(pS[m][par][:, j * 128:(j + 1) * 128], lhsT=qkb[2 + m][:, kt * 128:(kt + 1) * 128], rhs=qkb[m][:, qi * 128:(qi + 1) * 128], start=True, stop=True),
                         reads=[b_qkb[2 + m], b_qkb[m]], writes=[b_pS[m][par]])
                P.op("act", lambda e, m=m, par=par, gn=gn: e.activation(out=ET[m][par][:, 0:gn, :].rearrange("p g q -> p (g q)"), in_=pS[m][par][:, 0:gn * 128], func=AF.Exp, scale=0.125),
                     reads=[b_pS[m][par]], writes=[b_ET[m][par]])
                if g0 + gn == nk:
                    j = gn - 1
                    P.op("dve", lambda e, m=m, par=par, j=j: e.tensor_tensor(out=ET[m][par][:, j, :], in0=ET[m][par][:, j, :], in1=tri[:], op=ALU.mult),
                         reads=[b_ET[m][par], b_tri], writes=[b_ET[m][par]])
                for j in range(gn):
                    kt = g0 + j
                    first = (kt == 0); last = (kt == nk - 1)
                    P.op("pe", lambda e, m=m, j=j, kt=kt, par=par, first=first, last=last: e.matmul(pO[m][:, 0:130], lhsT=ET[m][par][:, j, :], rhs=vb[:, kt, :], start=first, stop=last),
                         reads=[b_ET[m][par], b_vb], writes=[b_pO[m]])
        op_ = qi % 2
        P.op("dve", lambda e: e.reciprocal(out=sm[:, 4:5], in_=pO[0][:, 128:129]), reads=[b_pO[0]], writes=[b_sm[4]])
        P.op("dve", lambda e: e.reciprocal(out=sm[:, 5:6], in_=pO[1][:, 128:129]), reads=[b_pO[1]], writes=[b_sm[5]])
        P.op("dve", lambda e: e.tensor_tensor(out=sm[:, 5:6], in0=sm[:, 5:6], in1=NEGLAM[0], op=ALU.mult), reads=[b_sm[5], NEGLAM[1]], writes=[b_sm[5]])
        P.op("dve", lambda e: e.tensor_scalar(out=t2[:], in0=pO[1][:, 0:128], scalar1=sm[:, 5:6], scalar2=None, op0=ALU.mult), reads=[b_pO[1], b_sm[5]], writes=[b_t2])
        P.op("dve", lambda e, op_=op_: e.scalar_tensor_tensor(out=ob[op_][:], in0=pO[0][:, 0:128], scalar=sm[:, 4:5], in1=t2[:], op0=ALU.mult, op1=ALU.add), reads=[b_pO[0], b_sm[4], b_t2], writes=[b_ob[op_]])
        P.op("act", lambda e, op_=op_: e.activation(out=junk[:], in_=ob[op_][:], func=AF.Square, scale=float(128 ** -0.5), accum_out=sm[:, 6:7]), reads=[b_ob[op_]], writes=[b_junk, b_sm[6]])
        P.op("dve", lambda e: e.tensor_scalar(out=sm[:, 6:7], in0=sm[:, 6:7], scalar1=1e-6, scalar2=None, op0=ALU.add), reads=[b_sm[6]], writes=[b_sm[6]])
        P.op("act", lambda e: e.activation(out=sm[:, 6:7], in_=sm[:, 6:7], func=AF.Sqrt), reads=[b_sm[6]], writes=[b_sm[6]])
        P.op("dve", lambda e: e.reciprocal(out=sm[:, 6:7], in_=sm[:, 6:7]), reads=[b_sm[6]], writes=[b_sm[6]])
        P.op("dve", lambda e: e.tensor_tensor(out=sm[:, 6:7], in0=sm[:, 6:7], in1=cst[:, 1:2], op=ALU.mult), reads=[b_sm[6], b_cst], writes=[b_sm[6]])
        P.op("dve", lambda e, op_=op_: e.scalar_tensor_tensor(out=ob[op_][:], in0=ob[op_][:], scalar=sm[:, 6:7], in1=gsub[:], op0=ALU.mult, op1=ALU.mult), reads=[b_ob[op_], b_sm[6], b_gsub], writes=[b_ob[op_]])
        P.dma("pool", lambda e, qi=qi, op_=op_: e.dma_start(out=y_d[qi], in_=ob[op_][:]), reads=[b_ob[op_]], sembuf=b_ob[op_])
    P.end()


def emit_dsa_h(nc, P, PFX, L, R=32.0, K=22):
    NQ = L // 128
    P.begin(); B = P.buf
    def din(name, shape): return nc.dram_tensor(PFX + name, list(shape), F32, kind="ExternalInput").ap()
    qk_d = din("qk", [2, 128, L])
    v_d = din("v", [128, NQ, 128])
    qi_d = din("qi", [NQ, 64, 8, 128])
    ki_d = din("ki", [64, L])
    wi_d = din("wi", [128, NQ, 8])
    negm_d = din("negm", [128, 128])
    idn_d = din("idn", [128, 128])
    y_d = nc.dram_tensor(PFX + "y", [NQ, 128, 128], F32, kind="ExternalOutput").ap()
    dbg_d = nc.dram_tensor(PFX + "dbg", [NQ, 128, 8], F32, kind="ExternalOutput").ap()
    dbg = [P.sb(f"dbg{i}", [128, 8]) for i in range(2)]; b_dbg = [B(), B()]

    qkb = [P.sb(f"qkb{i}", [128, L], BF16) for i in range(2)]; b_qkb = [B(), B()]
    vb = P.sb("vb", [128, NQ, 130], BF16); b_vb = B()
    kiT = P.sb("kiT", [64, L]); b_ki = B()
    wi = P.sb("wi_t", [128, NQ, 8]); b_wi = B()
    negm = P.sb("negm_t", [128, 128]); b_negm = B()
    idf = P.sb("idf", [128, 128]); b_idf = B()
    idb = P.sb("idb", [128, 128], BF16); b_idb = B()
    stg = [P.sb(f"stg{i}", [128, 2048]) for i in range(2)]; b_stg = [B(), B()]
    qit = [P.sb(f"qit{i}", [64, 8, 128]) for i in range(2)]; b_qit = [B(), B()]
    score = [P.sb(f"score{i}", [128, L]) for i in range(2)]; b_score = [B(), B()]
    junkS = P.sb("junkS", [128, L], BF16); b_junkS = B()
    rl = [P.sb(f"rl{i}", [128, 512]) for i in range(2)]; b_rl = [B(), B()]
    Eb = [P.sb(f"Eb{i}", [128, 512], BF16) for i in range(2)]; b_Eb = [B(), B()]
    Pm = [P.sb(f"Pm{i}", [128, 512], BF16) for i in range(2)]; b_Pm = [B(), B()]
    PmT = [P.sb(f"PmT{i}", [128, 4, 128], BF16) for i in range(2)]; b_PmT = [B(), B()]
    ob = [P.sb(f"ob{i}", [128, 128]) for i in range(2)]; b_ob = [B(), B()]
    sm = P.sb("sm", [128, 8]); b_sm = [B() for _ in range(8)]
    pD = [P.ps(f"pD{i}", [128, 512]) for i in range(2)]; b_pD = [B(), B()]
    pS = [P.ps(f"pS{i}", [128, 512]) for i in range(2)]; b_pS = [B(), B()]
    pT = [P.ps(f"pT{i}", [128, 1024], BF16) for i in range(2)]; b_pT = [B(), B()]
    pO = P.ps("pO", [128, 512]); b_pO = B()

    n = 0
    CW = min(2048, L)
    for i in range(2):
        for c0 in range(0, L, CW):
            s = n % 2; n += 1
            P.dma("sp", lambda e, i=i, c0=c0, s=s: e.dma_start(out=stg[s][:, 0:CW], in_=qk_d[i, :, c0:c0 + CW]), writes=[b_stg[s]])
            P.op("pool", lambda e, i=i, c0=c0, s=s: e.tensor_copy(out=qkb[i][:, c0:c0 + CW], in_=stg[s][:, 0:CW]), reads=[b_stg[s]], writes=[b_qkb[i]])
    TW = min(16, NQ)
    for t0 in range(0, NQ, TW):
        s = n % 2; n += 1
        P.dma("sp", lambda e, t0=t0, s=s: e.dma_start(out=stg[s][:, 0:TW * 128], in_=v_d[:, t0:t0 + TW, :].rearrange("p t d -> p (t d)")), writes=[b_stg[s]])
        P.op("pool", lambda e, t0=t0, s=s: e.tensor_copy(out=vb[:, t0:t0 + TW, 0:128], in_=stg[s][:, 0:TW * 128].rearrange("p (t d) -> p t d", d=128)), reads=[b_stg[s]], writes=[b_vb])
    P.op("dve", lambda e: e.memset(vb[:, :, 128:130], 1.0), writes=[b_vb])
    P.dma("sp", lambda e: e.dma_start(out=kiT[:], in_=ki_d), writes=[b_ki])
    P.dma("sp", lambda e: e.dma_start(out=wi[:], in_=wi_d), writes=[b_wi])
    P.dma("sp", lambda e: e.dma_start(out=negm[:], in_=negm_d), writes=[b_negm])
    P.dma("sp", lambda e: e.dma_start(out=idf[:], in_=idn_d), writes=[b_idf])
    P.op("dve", lambda e: e.tensor_copy(out=idb[:], in_=idf[:]), reads=[b_idf], writes=[b_idb])
    SC = float((64 ** -0.5) * (8 ** -0.5))
    ci = 0; ai = 0
    for qi in range(NQ):
        nk = qi + 1; nkeys = nk * 128
        sp_ = qi % 2
        sc = score[sp_]; bsc = b_score[sp_]
        P.dma("sp", lambda e, qi=qi, sp_=sp_: e.dma_start(out=qit[sp_][:], in_=qi_d[qi]), writes=[b_qit[sp_]])
        chunks = [(c0, min(4, nk - c0)) for c0 in range(0, nk, 4)]
        for (c0, cn) in chunks:
            w = cn * 128; k0 = c0 * 128
            for h in range(8):
                p = ci % 2; ci += 1
                P.op("pe", lambda e, h=h, p=p, k0=k0, w=w, sp_=sp_: e.matmul(pD[p][:, 0:w], lhsT=qit[sp_][:, h, :], rhs=kiT[:, k0:k0 + w], start=True, stop=True),
                     reads=[b_qit[sp_], b_ki], writes=[b_pD[p]])
                P.op("act", lambda e, p=p, w=w: e.activation(out=rl[p][:, 0:w], in_=pD[p][:, 0:w], func=AF.Relu, scale=SC), reads=[b_pD[p]], writes=[b_rl[p]])
                if h == 0:
                    P.op("dve", lambda e, p=p, w=w, k0=k0, sc=sc, qi=qi, h=h: e.tensor_scalar(out=sc[:, k0:k0 + w], in0=rl[p][:, 0:w], scalar1=wi[:, qi, h:h + 1], scalar2=None, op0=ALU.mult),
                         reads=[b_rl[p], b_wi], writes=[bsc])
                else:
                    P.op("dve", lambda e, p=p, w=w, k0=k0, sc=sc, qi=qi, h=h: e.scalar_tensor_tensor(out=sc[:, k0:k0 + w], in0=rl[p][:, 0:w], scalar=wi[:, qi, h:h + 1], in1=sc[:, k0:k0 + w], op0=ALU.mult, op1=ALU.add),
                         reads=[b_rl[p], b_wi, bsc], writes=[bsc])
        d0 = (nk - 1) * 128
        P.op("dve", lambda e, sc=sc, d0=d0: e.tensor_tensor(out=sc[:, d0:d0 + 128], in0=sc[:, d0:d0 + 128], in1=negm[:], op=ALU.add), reads=[bsc, b_negm], writes=[bsc])
        tau = sm[:, 0:1]; mid = sm[:, 1:2]; cnt = sm[:, 2:3]; s_ = sm[:, 3:4]
        if nkeys <= 256:
            P.op("dve", lambda e: e.memset(tau, -R), writes=[b_sm[0]])
        else:
            P.op("dve", lambda e: e.memset(mid, 0.0), writes=[b_sm[1]])
            for it in range(K):
                P.op("dve", lambda e, sc=sc, nkeys=nkeys: e.tensor_scalar(out=junkS[:, 0:nkeys], in0=sc[:, 0:nkeys], scalar1=mid, scalar2=0.0, op0=ALU.is_ge, op1=ALU.add, accum_out=cnt),
                     reads=[bsc, b_sm[1]], writes=[b_junkS, b_sm[2]])
                if it < K - 1:
                    wn = R / 2 ** (it + 1)
                    P.op("dve", lambda e, wn=wn: e.tensor_scalar(out=s_, in0=cnt, scalar1=255.5, scalar2=2 * wn, op0=ALU.is_ge, op1=ALU.mult), reads=[b_sm[2]], writes=[b_sm[3]])
                    P.op("dve", lambda e, wn=wn: e.scalar_tensor_tensor(out=mid, in0=s_, scalar=-wn, in1=mid, op0=ALU.add, op1=ALU.add), reads=[b_sm[3], b_sm[1]], writes=[b_sm[1]])
                else:
                    wl = R / 2 ** (K - 1)
                    P.op("dve", lambda e, wl=wl: e.tensor_scalar(out=s_, in0=cnt, scalar1=255.5, scalar2=wl, op0=ALU.is_ge, op1=ALU.mult), reads=[b_sm[2]], writes=[b_sm[3]])
                    P.op("dve", lambda e, wl=wl: e.scalar_tensor_tensor(out=tau, in0=s_, scalar=-wl, in1=mid, op0=ALU.add, op1=ALU.add), reads=[b_sm[3], b_sm[1]], writes=[b_sm[0]])
        P.op("dve", lambda e, sp_=sp_: e.tensor_copy(out=dbg[sp_][:], in_=sm[:]), reads=b_sm, writes=[b_dbg[sp_]])
        P.dma("pool", lambda e, qi=qi, sp_=sp_: e.dma_start(out=dbg_d[qi], in_=dbg[sp_][:]), reads=[b_dbg[sp_]], sembuf=b_dbg[sp_])
        for (c0, cn) in chunks:
            w = cn * 128; k0 = c0 * 128
            p = ai % 2; ai += 1
            P.op("pe", lambda e, p=p, k0=k0, w=w, qi=qi: e.matmul(pS[p][:, 0:w], lhsT=qkb[0][:, qi * 128:(qi + 1) * 128], rhs=qkb[1][:, k0:k0 + w], start=True, stop=True),
                 reads=[b_qkb[0], b_qkb[1]], writes=[b_pS[p]])
            P.op("act", lambda e, p=p, w=w: e.activation(out=Eb[p][:, 0:w], in_=pS[p][:, 0:w], func=AF.Exp, scale=float(128 ** -0.5)), reads=[b_pS[p]], writes=[b_Eb[p]])
            P.op("dve", lambda e, p=p, w=w, k0=k0, sc=sc: e.scalar_tensor_tensor(out=Pm[p][:, 0:w], in0=sc[:, k0:k0 + w], scalar=tau, in1=Eb[p][:, 0:w], op0=ALU.is_ge, op1=ALU.mult),
                 reads=[bsc, b_sm[0], b_Eb[p]], writes=[b_Pm[p]])
            for j in range(cn):
                P.op("pe", lambda e, p=p, j=j: e.transpose(pT[p][:, j * 128:(j + 1) * 128], Pm[p][:, j * 128:(j + 1) * 128], idb[:]), reads=[b_Pm[p], b_idb], writes=[b_pT[p]])
            P.op("act", lambda e, p=p, w=w, cn=cn: e.copy(out=PmT[p][:, 0:cn, :].rearrange("p g q -> p (g q)"), in_=pT[p][:, 0:w]), reads=[b_pT[p]], writes=[b_PmT[p]])
            for j in range(cn):
                kt = c0 + j
                P.op("pe", lambda e, p=p, j=j, kt=kt, nk=nk: e.matmul(pO[:, 0:130], lhsT=PmT[p][:, j, :], rhs=vb[:, kt, :], start=(kt == 0), stop=(kt == nk - 1)),
                     reads=[b_PmT[p], b_vb], writes=[b_pO])
        op_ = qi % 2
        P.op("dve", lambda e: e.reciprocal(out=sm[:, 4:5], in_=pO[:, 128:129]), reads=[b_pO], writes=[b_sm[4]])
        P.op("dve", lambda e, op_=op_: e.tensor_scalar(out=ob[op_][:], in0=pO[:, 0:128], scalar1=sm[:, 4:5], scalar2=None, op0=ALU.mult), reads=[b_pO, b_sm[4]], writes=[b_ob[op_]])
        P.dma("pool", lambda e, qi=qi, op_=op_: e.dma_start(out=y_d[qi], in_=ob[op_][:]), reads=[b_ob[op_]], sembuf=b_ob[op_])
    P.end()


def emit_rwkv_h(nc, P, PFX, L):
    SEG = min(L, 512); NSEG = L // SEG; NCH = SEG // 64
    P.begin(); B = P.buf
    def din(name, shape): return nc.dram_tensor(PFX + name, list(shape), F32, kind="ExternalInput").ap()
    zr_d = din("zr", [3, 64, 2, L + 1]); zl_d = din("zl", [64, 2, L + 1]); zg_d = din("zg", [128, L + 1])
    mu3_d = din("mu3", [64, 3, 2]); mul_d = din("mul", [64, 2]); mug_d = din("mug", [128, 1])
    pp_d = din("pp", [64, 5, 2]); wup_d = din("wup", [64, 2, 64]); aup_d = din("aup", [64, 2, 64]); gup_d = din("gup", [128, 128])
    lnwb_d = din("lnwb", [64, 2, 128]); cmask_d = din("cmask", [64, 2 * SEG]); mask5_d = din("mask5", [64, 320])
    idn_d = din("idn", [64, 64])
    y_d = nc.dram_tensor(PFX + "y", [L // 64, 64, 128], F32, kind="ExternalOutput").ap()
    def T(name, shape, dt=F32):
        return P.sb(name, shape, dt), B(name)
    raw3, b_raw3 = T("raw3", [64, 3, 2, SEG + 1]); rawl, b_rawl = T("rawl", [64, 2, SEG + 1]); rawg, b_rawg = T("rawg", [128, SEG + 1])
    mu3, b_mu3 = T("mu3t", [64, 3, 2]); mul, b_mul = T("mult", [64, 2]); mug, b_mug = T("mugt", [128, 1])
    pp, b_pp = T("ppt", [64, 5, 2]); wup, b_wup = T("wupt", [64, 2, 64]); aup, b_aup = T("aupt", [64, 2, 64]); gup, b_gup = T("gupt", [128, 128])
    lnwb, b_lnwb = T("lnwbt", [64, 2, 128]); cmask, b_cmask = T("cmaskt", [64, 2 * SEG]); mask5, b_mask5 = T("mask5t", [64, 320])
    idn, b_idn = T("idnt", [64, 64]); ones, b_ones = T("onest", [64, 64])
    d3, b_d3 = T("d3", [64, 3, 2, SEG]); dl, b_dl = T("dl", [64, 2, SEG]); dg, b_dg = T("dg", [128, SEG])
    x3, b_x3 = T("x3", [64, 3, 2, SEG]); xl, b_xl = T("xl", [64, 2, SEG]); xg, b_xg = T("xg", [128, SEG])
    tw, b_tw = T("tw", [64, SEG]); sgc, b_sgc = T("sgc", [128, SEG]); sgw, b_sgw = T("sgw", [64, 2, SEG]); aa, b_aa = T("aa", [64, 2, SEG])
    t1, b_t1 = T("t1", [64, 2, SEG]); sq, b_sq = T("sq", [64, 2, SEG]); rn, b_rn = T("rn", [64, 2, SEG]); kk, b_kk = T("kk", [64, 2, SEG])
    kp, b_kp = T("kp", [64, 2, SEG]); bb, b_bb = T("bb", [64, 2, SEG]); cs, b_cs = T("cs", [64, 2, SEG])
    epos, b_epos = T("epos", [64, 2, SEG]); eneg, b_eneg = T("eneg", [64, 2, SEG]); eprev, b_eprev = T("eprev", [64, 2, SEG])
    AR, b_AR = T("AR", [64, 2, NCH, 2, 64]); Bt, b_Bt = T("Bt", [64, 2, SEG]); Kt, b_Kt = T("Kt", [64, 2, SEG]); rkr, b_rkr = T("rkr", [64, 2, SEG])
    Hs = [T(f"H{i}", [64, 2, 64]) for i in range(2)]
    Msb = [T(f"Msb{h}", [64, 320]) for h in range(2)]
    TK, b_TK = T("TK", [64, 6, 64])
    PPs = [T(f"PP{i}", [64, 2, 2, 64]) for i in range(2)]
    Xs = [T(f"X{i}", [64, 2, 64]) for i in range(2)]
    Wsb, b_Wsb = T("Wsb", [64, 128]); Usb, b_Usb = T("Usb", [64, 128]); Ysb, b_Ysb = T("Ysb", [64, 128]); yc, b_yc = T("yc", [64, 128])
    outs = [T(f"out{i}", [64, 128]) for i in range(2)]
    sm, _ = T("sm", [64, 16]); b_sm = [B() for _ in range(16)]
    junk, b_junk = T("junk", [64, 64])
    def PS(name, shape): return P.ps(name, shape), B(name)
    pM = [PS(f"pM{h}", [64, 512]) for h in range(2)]
    pK, b_pK = PS("pK", [64, 512]); pI, b_pI = PS("pI", [64, 512]); pX, b_pX = PS("pX", [64, 512])
    pW, b_pW = PS("pW", [64, 512]); pY, b_pY = PS("pY", [64, 512]); pH, b_pH = PS("pH", [64, 512])

    for (t, b, d) in [(mu3, b_mu3, mu3_d), (mul, b_mul, mul_d), (mug, b_mug, mug_d), (pp, b_pp, pp_d), (wup, b_wup, wup_d), (aup, b_aup, aup_d),
                      (gup, b_gup, gup_d), (lnwb, b_lnwb, lnwb_d), (cmask, b_cmask, cmask_d), (mask5, b_mask5, mask5_d), (idn, b_idn, idn_d)]:
        P.dma("sp", lambda e, t=t, d=d: e.dma_start(out=t[:], in_=d), writes=[b])
    P.op("dve", lambda e: e.memset(ones[:], 1.0), writes=[b_ones])
    P.op("dve", lambda e: e.memset(Hs[0][0][:], 0.0), writes=[Hs[0][1]])
    hcur = 0
    NEG = -0.6065306597126334
    oi = 0
    for sg_ in range(NSEG):
        s0 = sg_ * SEG
        for a in range(3):
            P.dma("sp", lambda e, s0=s0, a=a: e.dma_start(out=raw3[:, a, :, :], in_=zr_d[a, :, :, s0:s0 + SEG + 1]), writes=[b_raw3])
        P.dma("sp", lambda e, s0=s0: e.dma_start(out=rawl[:], in_=zl_d[:, :, s0:s0 + SEG + 1]), writes=[b_rawl])
        P.dma("sp", lambda e, s0=s0: e.dma_start(out=rawg[:], in_=zg_d[:, s0:s0 + SEG + 1]), writes=[b_rawg])
        P.op("dve", lambda e: e.tensor_tensor(out=d3[:], in0=raw3[:, :, :, 0:SEG], in1=raw3[:, :, :, 1:SEG + 1], op=ALU.subtract), reads=[b_raw3], writes=[b_d3])
        P.op("dve", lambda e: e.tensor_tensor(out=dl[:], in0=rawl[:, :, 0:SEG], in1=rawl[:, :, 1:SEG + 1], op=ALU.subtract), reads=[b_rawl], writes=[b_dl])
        P.op("dve", lambda e: e.tensor_tensor(out=dg[:], in0=rawg[:, 0:SEG], in1=rawg[:, 1:SEG + 1], op=ALU.subtract), reads=[b_rawg], writes=[b_dg])
        for a in range(3):
            for h in range(2):
                P.op("dve", lambda e, a=a, h=h: e.scalar_tensor_tensor(out=x3[:, a, h, :], in0=d3[:, a, h, :], scalar=mu3[:, a, h:h + 1], in1=raw3[:, a, h, 1:SEG + 1], op0=ALU.mult, op1=ALU.add),
                     reads=[b_d3, b_mu3, b_raw3], writes=[b_x3])
        for a in range(2):
            P.op("dve", lambda e, a=a: e.scalar_tensor_tensor(out=xl[:, a, :], in0=dl[:, a, :], scalar=mul[:, a:a + 1], in1=rawl[:, a, 1:SEG + 1], op0=ALU.mult, op1=ALU.add),
                 reads=[b_dl, b_mul, b_rawl], writes=[b_xl])
        P.op("dve", lambda e: e.scalar_tensor_tensor(out=xg[:], in0=dg[:], scalar=mug[:, 0:1], in1=rawg[:, 1:SEG + 1], op0=ALU.mult, op1=ALU.add), reads=[b_dg, b_mug, b_rawg], writes=[b_xg])
        XR = lambda h: x3[:, 0, h, :]
        XK = lambda h: x3[:, 1, h, :]
        XV = lambda h: x3[:, 2, h, :]
        P.op("act", lambda e: e.activation(out=tw[:], in_=xl[:, 0, :], func=AF.Tanh), reads=[b_xl], writes=[b_tw])
        P.op("act", lambda e: e.activation(out=sgc[:], in_=xg[:], func=AF.Sigmoid), reads=[b_xg], writes=[b_sgc])
        for h in range(2):
            P.op("pe", lambda e, h=h: e.matmul(pK[:, 0:SEG], lhsT=wup[:, h, :], rhs=tw[:], start=True, stop=True), reads=[b_wup, b_tw], writes=[b_pK])
            P.op("act", lambda e, h=h: e.activation(out=sgw[:, h, :], in_=pK[:, 0:SEG], func=AF.Sigmoid, bias=pp[:, 0, h:h + 1]), reads=[b_pK, b_pp], writes=[b_sgw])
            P.op("pe", lambda e, h=h: e.matmul(pK[:, 0:SEG], lhsT=aup[:, h, :], rhs=xl[:, 1, :], start=True, stop=True), reads=[b_aup, b_xl], writes=[b_pK])
            P.op("act", lambda e, h=h: e.activation(out=aa[:, h, :], in_=pK[:, 0:SEG], func=AF.Sigmoid, bias=pp[:, 1, h:h + 1]), reads=[b_pK, b_pp], writes=[b_aa])
        for h in range(2):
            P.op("dve", lambda e, h=h: e.tensor_scalar(out=t1[:, h, :], in0=XK(h), scalar1=pp[:, 2, h:h + 1], scalar2=None, op0=ALU.mult), reads=[b_x3, b_pp], writes=[b_t1])
        P.op("dve", lambda e: e.tensor_tensor(out=sq[:], in0=t1[:], in1=t1[:], op=ALU.mult), reads=[b_t1], writes=[b_sq])
        for h in range(2):
            P.op("pe", lambda e, h=h: e.matmul(pK[:, 0:SEG], lhsT=ones[:], rhs=sq[:, h, :], start=True, stop=True), reads=[b_ones, b_sq], writes=[b_pK])
            P.op("dve", lambda e, h=h: e.tensor_scalar(out=rn[:, h, :], in0=pK[:, 0:SEG], scalar1=1e-24, scalar2=None, op0=ALU.max), reads=[b_pK], writes=[b_rn])
        P.op("act", lambda e: e.activation(out=rn[:], in_=rn[:], func=AF.Sqrt), reads=[b_rn], writes=[b_rn])
        P.op("dve", lambda e: e.reciprocal(out=rn[:], in_=rn[:]), reads=[b_rn], writes=[b_rn])
        P.op("dve", lambda e: e.tensor_tensor(out=kk[:], in0=t1[:], in1=rn[:], op=ALU.mult), reads=[b_t1, b_rn], writes=[b_kk])
        for h in range(2):
            P.op("dve", lambda e, h=h: e.tensor_scalar(out=kp[:, h, :], in0=aa[:, h, :], scalar1=pp[:, 3, h:h + 1], scalar2=pp[:, 3, h:h + 1], op0=ALU.mult, op1=ALU.subtract), reads=[b_aa, b_pp], writes=[b_kp])
            P.op("dve", lambda e, h=h: e.scalar_tensor_tensor(out=kp[:, h, :], in0=kp[:, h, :], scalar=1.0, in1=XK(h), op0=ALU.add, op1=ALU.mult), reads=[b_kp, b_x3], writes=[b_kp])
        P.op("dve", lambda e: e.tensor_tensor(out=bb[:], in0=kk[:], in1=aa[:], op=ALU.mult), reads=[b_kk, b_aa], writes=[b_bb])
        FL = lambda t: t[:].rearrange("p h s -> p (h s)")
        P.op("dve", lambda e: e.tensor_tensor_scan(out=FL(cs), data0=cmask[:], data1=FL(sgw), initial=0.0, op0=ALU.mult, op1=ALU.add), reads=[b_cmask, b_sgw], writes=[b_cs])
        P.op("act", lambda e: e.activation(out=epos[:], in_=cs[:], func=AF.Exp, scale=NEG), reads=[b_cs], writes=[b_epos])
        P.op("act", lambda e: e.activation(out=eneg[:], in_=cs[:], func=AF.Exp, scale=-NEG), reads=[b_cs], writes=[b_eneg])
        P.op("dve", lambda e: e.tensor_tensor(out=eprev[:], in0=cs[:], in1=sgw[:], op=ALU.subtract), reads=[b_cs, b_sgw], writes=[b_eprev])
        P.op("act", lambda e: e.activation(out=eprev[:], in_=eprev[:], func=AF.Exp, scale=NEG), reads=[b_eprev], writes=[b_eprev])
        for h in range(2):
            P.op("dve", lambda e, h=h: e.scalar_tensor_tensor(out=AR[:, h, :, 0, :], in0=kk[:, h, :].rearrange("p (c t) -> p c t", t=64), scalar=-1.0, in1=eprev[:, h, :].rearrange("p (c t) -> p c t", t=64), op0=ALU.mult, op1=ALU.mult),
                 reads=[b_kk, b_eprev], writes=[b_AR])
            P.op("dve", lambda e, h=h: e.tensor_tensor(out=AR[:, h, :, 1, :], in0=XR(h).rearrange("p (c t) -> p c t", t=64), in1=epos[:, h, :].rearrange("p (c t) -> p c t", t=64), op=ALU.mult),
                 reads=[b_x3, b_epos], writes=[b_AR])
            P.op("dve", lambda e, h=h: e.scalar_tensor_tensor(out=rkr[:, h, :], in0=XR(h), scalar=pp[:, 4, h:h + 1], in1=kp[:, h, :], op0=ALU.mult, op1=ALU.mult), reads=[b_x3, b_pp, b_kp], writes=[b_rkr])
        P.op("dve", lambda e: e.tensor_tensor(out=Bt[:], in0=bb[:], in1=eneg[:], op=ALU.mult), reads=[b_bb, b_eneg], writes=[b_Bt])
        P.op("dve", lambda e: e.tensor_tensor(out=Kt[:], in0=kp[:], in1=eneg[:], op=ALU.mult), reads=[b_kp, b_eneg], writes=[b_Kt])
        for c in range(NCH):
            cs_ = slice(c * 64, (c + 1) * 64)
            for h in range(2):
                pm, bpm = pM[h]
                P.op("pe", lambda e, h=h, c=c, pm=pm, cs_=cs_: e.matmul(pm[:, 0:64], lhsT=AR[:, h, c, 0, :], rhs=Bt[:, h, cs_], start=True, stop=True), reads=[b_AR, b_Bt], writes=[bpm])
                P.op("pe", lambda e, h=h, c=c, pm=pm, cs_=cs_: e.matmul(pm[:, 64:192], lhsT=Bt[:, h, cs_], rhs=AR[:, h, c, :, :].rearrange("p a t -> p (a t)"), start=True, stop=True), reads=[b_AR, b_Bt], writes=[bpm])
                P.op("pe", lambda e, h=h, c=c, pm=pm, cs_=cs_: e.matmul(pm[:, 192:320], lhsT=Kt[:, h, cs_], rhs=AR[:, h, c, :, :].rearrange("p a t -> p (a t)"), start=True, stop=True), reads=[b_AR, b_Kt], writes=[bpm])
                P.op("dve", lambda e, h=h, pm=pm: e.tensor_tensor(out=Msb[h][0][:], in0=pm[:, 0:320], in1=mask5[:], op=ALU.mult), reads=[bpm, b_mask5], writes=[Msb[h][1]])
            for h in range(2):
                for a, (src, bsrc) in enumerate([(Bt[:, h, cs_], b_Bt), (Kt[:, h, cs_], b_Kt), (x3[:, 2, h, cs_], b_x3)]):
                    P.op("pe", lambda e, h=h, a=a, src=src: e.transpose(pK[:, (h * 3 + a) * 64:(h * 3 + a + 1) * 64], src, idn[:]), reads=[bsrc, b_idn], writes=[b_pK])
            P.op("act", lambda e: e.copy(out=TK[:].rearrange("p a t -> p (a t)"), in_=pK[:, 0:384]), reads=[b_pK], writes=[b_TK])
            pcur = 0; xcur = 0
            for h in range(2):
                P.op("act", lambda e, h=h: e.copy(out=PPs[0][0][:, h, :, :].rearrange("p a t -> p (a t)"), in_=Msb[h][0][:, 0:128]), reads=[Msb[h][1]], writes=[PPs[0][1]])
                P.op("dve", lambda e, h=h: e.tensor_tensor(out=Xs[0][0][:, h, :], in0=Msb[h][0][:, 64:128], in1=idn[:], op=ALU.add), reads=[Msb[h][1], b_idn], writes=[Xs[0][1]])
            for stp in range(5):
                pp_t, pp_b = PPs[pcur]; pn_t, pn_b = PPs[1 - pcur]
                x_t, x_b = Xs[xcur]; xn_t, xn_b = Xs[1 - xcur]
                for h in range(2):
                    P.op("pe", lambda e, h=h, pp_t=pp_t: e.matmul(pI[:, (h * 2) * 64:(h * 2 + 1) * 64], lhsT=pp_t[:, h, 1, :], rhs=pp_t[:, h, 0, :], start=True, stop=True), reads=[pp_b], writes=[b_pI])
                    P.op("pe", lambda e, h=h, pp_t=pp_t: e.matmul(pI[:, (h * 2 + 1) * 64:(h * 2 + 2) * 64], lhsT=pp_t[:, h, 0, :], rhs=pp_t[:, h, 1, :], start=True, stop=True), reads=[pp_b], writes=[b_pI])
                P.op("act", lambda e, pn_t=pn_t: e.copy(out=pn_t[:].rearrange("p h a t -> p (h a t)"), in_=pI[:, 0:256]), reads=[b_pI], writes=[pn_b])
                for h in range(2):
                    P.op("pe", lambda e, h=h, x_t=x_t: e.matmul(pX[:, h * 64:(h + 1) * 64], lhsT=idn[:], rhs=x_t[:, h, :], start=True, stop=False), reads=[b_idn, x_b], writes=[b_pX])
                    P.op("pe", lambda e, h=h, x_t=x_t, pn_t=pn_t: e.matmul(pX[:, h * 64:(h + 1) * 64], lhsT=pn_t[:, h, 0, :], rhs=x_t[:, h, :], start=False, stop=True), reads=[pn_b, x_b], writes=[b_pX])
                P.op("dve", lambda e, xn_t=xn_t: e.tensor_copy(out=xn_t[:].rearrange("p h t -> p (h t)"), in_=pX[:, 0:128]), reads=[b_pX], writes=[xn_b])
                pcur = 1 - pcur; xcur = 1 - xcur
            X_t, X_b = Xs[xcur]
            H_t, H_b = Hs[hcur]; Hn_t, Hn_b = Hs[1 - hcur]
            for h in range(2):
                P.op("pe", lambda e, h=h, c=c, H_t=H_t: e.matmul(pW[:, h * 64:(h + 1) * 64], lhsT=AR[:, h, c, 0, :], rhs=H_t[:, h, :], start=True, stop=False), reads=[b_AR, H_b], writes=[b_pW])
                P.op("pe", lambda e, h=h: e.matmul(pW[:, h * 64:(h + 1) * 64], lhsT=Msb[h][0][:, 192:256], rhs=TK[:, h * 3 + 2, :], start=False, stop=True), reads=[Msb[h][1], b_TK], writes=[b_pW])
            P.op("act", lambda e: e.copy(out=Wsb[:], in_=pW[:, 0:128]), reads=[b_pW], writes=[b_Wsb])
            for h in range(2):
                P.op("pe", lambda e, h=h, X_t=X_t: e.matmul(pW[:, 128 + h * 64:128 + (h + 1) * 64], lhsT=X_t[:, h, :], rhs=Wsb[:, h * 64:(h + 1) * 64], start=True, stop=True), reads=[X_b, b_Wsb], writes=[b_pW])
            P.op("dve", lambda e: e.tensor_copy(out=Usb[:], in_=pW[:, 128:256]), reads=[b_pW], writes=[b_Usb])
            for h in range(2):
                hs = slice(h * 64, (h + 1) * 64)
                P.op("pe", lambda e, h=h, c=c, hs=hs, H_t=H_t: e.matmul(pY[:, hs], lhsT=AR[:, h, c, 1, :], rhs=H_t[:, h, :], start=True, stop=False), reads=[b_AR, H_b], writes=[b_pY])
                P.op("pe", lambda e, h=h, hs=hs: e.matmul(pY[:, hs], lhsT=Msb[h][0][:, 128:192], rhs=Usb[:, hs], start=False, stop=False), reads=[Msb[h][1], b_Usb], writes=[b_pY])
                P.op("pe", lambda e, h=h, hs=hs: e.matmul(pY[:, hs], lhsT=Msb[h][0][:, 256:320], rhs=TK[:, h * 3 + 2, :], start=False, stop=True), reads=[Msb[h][1], b_TK], writes=[b_pY])
            P.op("pe", lambda e, cs_=cs_: e.matmul(pY[:, 128:256], lhsT=sgc[:, cs_], rhs=gup[:], start=True, stop=True), reads=[b_sgc, b_gup], writes=[b_pY])
            for h in range(2):
                P.op("pe", lambda e, h=h, cs_=cs_: e.matmul(pY[:, 256 + 2 * h:258 + 2 * h], lhsT=rkr[:, h, cs_], rhs=ones[:, 0:2], start=True, stop=True), reads=[b_rkr, b_ones], writes=[b_pY])
            for h in range(2):
                hs = slice(h * 64, (h + 1) * 64)
                P.op("pe", lambda e, h=h, hs=hs, H_t=H_t: e.matmul(pH[:, hs], lhsT=idn[:], rhs=H_t[:, h, :], start=True, stop=False), reads=[b_idn, H_b], writes=[b_pH])
                P.op("pe", lambda e, h=h, hs=hs: e.matmul(pH[:, hs], lhsT=TK[:, h * 3 + 0, :], rhs=Usb[:, hs], start=False, stop=False), reads=[b_TK, b_Usb], writes=[b_pH])
                P.op("pe", lambda e, h=h, hs=hs: e.matmul(pH[:, hs], lhsT=TK[:, h * 3 + 1, :], rhs=TK[:, h * 3 + 2, :], start=False, stop=True), reads=[b_TK], writes=[b_pH])
            for h in range(2):
                ce = c * 64 + 63
                P.op("dve", lambda e, h=h, ce=ce, Hn_t=Hn_t: e.tensor_scalar(out=Hn_t[:, h, :], in0=pH[:, h * 64:(h + 1) * 64], scalar1=epos[:, h, ce:ce + 1], scalar2=None, op0=ALU.mult), reads=[b_pH, b_epos], writes=[Hn_b])
            hcur = 1 - hcur
            P.op("act", lambda e: e.copy(out=Ysb[:], in_=pY[:, 0:128]), reads=[b_pY], writes=[b_Ysb])
            P.op("dve", lambda e: e.tensor_copy(out=sm[:, 8:12], in_=pY[:, 256:260]), reads=[b_pY], writes=[b_sm[8]])
            o_t, o_b = outs[oi % 2]; oi += 1
            for h in range(2):
                hs = slice(h * 64, (h + 1) * 64)
                mcol = sm[:, h:h + 1]; vcol = sm[:, 2 + h:3 + h]
                P.op("dve", lambda e, hs=hs, mcol=mcol: e.reduce_sum(out=mcol, in_=Ysb[:, hs], axis=AX.X), reads=[b_Ysb], writes=[b_sm[h]])
                P.op("dve", lambda e, mcol=mcol: e.tensor_scalar(out=mcol, in0=mcol, scalar1=-1.0 / 64, scalar2=None, op0=ALU.mult), reads=[b_sm[h]], writes=[b_sm[h]])
                P.op("dve", lambda e, hs=hs, mcol=mcol: e.tensor_scalar(out=yc[:, hs], in0=Ysb[:, hs], scalar1=mcol, scalar2=None, op0=ALU.add), reads=[b_Ysb, b_sm[h]], writes=[b_yc])
                P.op("act", lambda e, hs=hs, vcol=vcol: e.activation(out=junk[:], in_=yc[:, hs], func=AF.Square, scale=0.125, accum_out=vcol), reads=[b_yc], writes=[b_junk, b_sm[2 + h]])
                P.op("dve", lambda e, vcol=vcol: e.tensor_scalar(out=vcol, in0=vcol, scalar1=64e-5, scalar2=None, op0=ALU.add), reads=[b_sm[2 + h]], writes=[b_sm[2 + h]])
                P.op("act", lambda e, vcol=vcol: e.activation(out=vcol, in_=vcol, func=AF.Sqrt), reads=[b_sm[2 + h]], writes=[b_sm[2 + h]])
                P.op("dve", lambda e, vcol=vcol: e.reciprocal(out=vcol, in_=vcol), reads=[b_sm[2 + h]], writes=[b_sm[2 + h]])
                P.op("dve", lambda e, hs=hs, vcol=vcol: e.scalar_tensor_tensor(out=yc[:, hs], in0=yc[:, hs], scalar=vcol, in1=lnwb[:, 0, hs], op0=ALU.mult, op1=ALU.mult), reads=[b_yc, b_sm[2 + h], b_lnwb], writes=[b_yc])
                P.op("dve", lambda e, hs=hs: e.tensor_tensor(out=yc[:, hs], in0=yc[:, hs], in1=lnwb[:, 1, hs], op=ALU.add), reads=[b_yc, b_lnwb], writes=[b_yc])
                P.op("dve", lambda e, hs=hs, h=h: e.scalar_tensor_tensor(out=yc[:, hs], in0=TK[:, h * 3 + 2, :], scalar=sm[:, 8 + 2 * h:9 + 2 * h], in1=yc[:, hs], op0=ALU.mult, op1=ALU.add), reads=[b_TK, b_sm[8], b_yc], writes=[b_yc])
            P.op("dve", lambda e, o_t=o_t: e.tensor_tensor(out=o_t[:], in0=yc[:], in1=pY[:, 128:256], op=ALU.mult), reads=[b_yc, b_pY], writes=[o_b])
            gci = sg_ * NCH + c
            P.dma("pool", lambda e, gci=gci, o_t=o_t: e.dma_start(out=y_d[gci], in_=o_t[:]), reads=[o_b], sembuf=o_b)
    P.end()


TOK = 2048; NT = 16
def emit_merge_h(nc, P, PFX, x_out=None):
    P.begin(); B = P.buf
    def din(name, shape): return nc.dram_tensor(PFX + name, list(shape), F32, kind="ExternalInput").ap()
    x_in = din("x", [TOK, D]); g_mpre = din("g_mpre", [128, D]); g_mpost = din("g_mpost", [128, D])
    wgate = din("wgate", [8, 128, 3072]); wbr = din("wbr", [12, 128, D]); wo = din("wo", [8, 128, D])
    yT = din("yT", [NT, 128, 12 * 128]); idn_d = din("idn", [128, 128])
    ident_f = P.sb("ident_f", [128, 128]); ident = P.sb("ident", [128, 128], BF16)
    gpre_t = P.sb("gpre_t", [128, D]); gpost_t = P.sb("gpost_t", [128, D])
    Wg = P.sb("Wg", [128, 8, 3072], BF16); Wb = P.sb("Wb", [128, 12, D], BF16); Wo = P.sb("Wo", [128, 8, D], BF16)
    stg = [P.sb(f"stg{i}", [128, 3072]) for i in range(2)]
    xt = [P.sb(f"xt{i}", [128, D]) for i in range(2)]; ot = [P.sb(f"ot{i}", [128, D]) for i in range(2)]
    mg = P.sb("mg", [128, D]); tmp = P.sb("tmp", [128, 512]); junk = P.sb("junk", [128, D])
    hb = P.sb("hb", [128, D], BF16); mb = P.sb("mb", [128, D], BF16)
    hTt = P.sb("hTt", [128, 8, 128], BF16); mT = P.sb("mT", [128, 8, 128], BF16)
    ystg = [P.sb(f"ystg{i}", [128, 1536]) for i in range(2)]; ytb = [P.sb(f"ytb{i}", [128, 12, 128], BF16) for i in range(2)]
    sgt = [P.sb(f"sgt{i}", [128, 512]) for i in range(2)]
    st = P.sb("st", [128, 8])
    pA = [P.ps(f"pA{i}", [128, 512]) for i in range(2)]; pB = [P.ps(f"pB{i}", [128, 512]) for i in range(2)]
    pC = [P.ps(f"pC{i}", [128, 512]) for i in range(2)]; pT = P.ps("pT", [128, 1024], BF16)
    b_idf = B(); b_id = B(); b_gpre = B(); b_gpost = B(); b_Wg = B(); b_Wb = B(); b_Wo = B(); b_stg = [B(), B()]
    b_xt = [B(), B()]; b_ot = [B(), B()]; b_mg = B(); b_tmp = B(); b_junk = B(); b_hb = B(); b_mb = B(); b_hTt = B(); b_mT = B()
    b_ystg = [B(), B()]; b_ytb = [B(), B()]; b_sgt = [B(), B()]; b_st = [B() for _ in range(8)]
    b_pA = [B(), B()]; b_pB = [B(), B()]; b_pC = [B(), B()]; b_pT = B()
    P.dma("sp", lambda e: e.dma_start(out=ident_f[:], in_=idn_d), writes=[b_idf])
    P.op("dve", lambda e: e.tensor_copy(out=ident[:], in_=ident_f[:]), reads=[b_idf], writes=[b_id])
    P.dma("sp", lambda e: e.dma_start(out=gpre_t[:], in_=g_mpre), writes=[b_gpre])
    P.dma("sp", lambda e: e.dma_start(out=gpost_t[:], in_=g_mpost), writes=[b_gpost])
    n = 0
    for k in range(8):
        s = n % 2; n += 1
        P.dma("sp", lambda e, k=k, s=s: e.dma_start(out=stg[s][:, :], in_=wgate[k]), writes=[b_stg[s]])
        P.op("pool", lambda e, k=k, s=s: e.tensor_copy(out=Wg[:, k, :], in_=stg[s][:, :]), reads=[b_stg[s]], writes=[b_Wg])
    for k in range(12):
        s = n % 2; n += 1
        P.dma("sp", lambda e, k=k, s=s: e.dma_start(out=stg[s][:, 0:D], in_=wbr[k]), writes=[b_stg[s]])
        P.op("pool", lambda e, k=k, s=s: e.tensor_copy(out=Wb[:, k, :], in_=stg[s][:, 0:D]), reads=[b_stg[s]], writes=[b_Wb])
    for k in range(8):
        s = n % 2; n += 1
        P.dma("sp", lambda e, k=k, s=s: e.dma_start(out=stg[s][:, 0:D], in_=wo[k]), writes=[b_stg[s]])
        P.op("pool", lambda e, k=k, s=s: e.tensor_copy(out=Wo[:, k, :], in_=stg[s][:, 0:D]), reads=[b_stg[s]], writes=[b_Wo])

    def rstd_of(src_ap, src_bufs, col):
        ss = st[:, col:col + 1]; bss = b_st[col]
        P.op("act", lambda e: e.activation(out=junk[:], in_=src_ap, func=AF.Square, scale=float(D ** -0.5), accum_out=ss), reads=src_bufs, writes=[b_junk, bss])
        P.op("dve", lambda e: e.tensor_scalar(out=ss, in0=ss, scalar1=EPS, scalar2=None, op0=ALU.add), reads=[bss], writes=[bss])
        P.op("act", lambda e: e.activation(out=ss, in_=ss, func=AF.Sqrt), reads=[bss], writes=[bss])
        P.op("dve", lambda e: e.reciprocal(out=ss, in_=ss), reads=[bss], writes=[bss])
        return ss, bss

    qn = 0
    for ti in range(NT):
        par = ti % 2
        P.dma("sp", lambda e, ti=ti, par=par: e.dma_start(out=xt[par][:], in_=x_in[ti * 128:(ti + 1) * 128, :]), writes=[b_xt[par]])
        P.dma("sp", lambda e, ti=ti, par=par: e.dma_start(out=ystg[par][:], in_=yT[ti]), writes=[b_ystg[par]])
        P.op("pool", lambda e, par=par: e.tensor_copy(out=ytb[par][:].rearrange("p a b -> p (a b)"), in_=ystg[par][:]), reads=[b_ystg[par]], writes=[b_ytb[par]])
        rs, brs = rstd_of(xt[par][:], [b_xt[par]], 0)
        P.op("dve", lambda e, par=par, rs=rs: e.scalar_tensor_tensor(out=hb[:], in0=xt[par][:], scalar=rs, in1=gpre_t[:], op0=ALU.mult, op1=ALU.mult), reads=[b_xt[par], brs, b_gpre], writes=[b_hb])
        for k in range(8):
            P.op("pe", lambda e, k=k: e.transpose(pT[:, k * 128:(k + 1) * 128], hb[:, k * 128:(k + 1) * 128], ident[:]), reads=[b_hb, b_id], writes=[b_pT])
        P.op("act", lambda e: e.copy(out=hTt[:].rearrange("p k t -> p (k t)"), in_=pT[:]), reads=[b_pT], writes=[b_hTt])
        for half in range(2):
            for nb in range(3):
                q = qn % 2; qn += 1
                c0 = nb * 1024 + half * 512
                for k in range(8):
                    P.op("pe", lambda e, k=k, q=q, c0=c0: e.matmul(pA[q][:], lhsT=hTt[:, k, :], rhs=Wg[:, k, c0:c0 + 512], start=(k == 0), stop=(k == 7)), reads=[b_hTt, b_Wg], writes=[b_pA[q]])
                P.op("act", lambda e, q=q: e.activation(out=sgt[q][:], in_=pA[q][:], func=AF.Sigmoid), reads=[b_pA[q]], writes=[b_sgt[q]])
                for kc in range(4):
                    P.op("pe", lambda e, kc=kc, q=q, nb=nb, half=half, par=par: e.matmul(pB[q][:], lhsT=ytb[par][:, nb * 4 + kc, :], rhs=Wb[:, nb * 4 + kc, half * 512:(half + 1) * 512], start=(kc == 0), stop=(kc == 3)), reads=[b_ytb[par], b_Wb], writes=[b_pB[q]])
                if nb == 0:
                    P.op("dve", lambda e, q=q, half=half: e.tensor_tensor(out=mg[:, half * 512:(half + 1) * 512], in0=sgt[q][:], in1=pB[q][:], op=ALU.mult), reads=[b_sgt[q], b_pB[q]], writes=[b_mg])
                else:
                    P.op("dve", lambda e, q=q: e.tensor_tensor(out=tmp[:], in0=sgt[q][:], in1=pB[q][:], op=ALU.mult), reads=[b_sgt[q], b_pB[q]], writes=[b_tmp])
                    P.op("dve", lambda e, half=half: e.tensor_tensor(out=mg[:, half * 512:(half + 1) * 512], in0=mg[:, half * 512:(half + 1) * 512], in1=tmp[:], op=ALU.add), reads=[b_mg, b_tmp], writes=[b_mg])
        P.op("act", lambda e: e.copy(out=mb[:], in_=mg[:]), reads=[b_mg], writes=[b_mb])
        for k in range(8):
            P.op("pe", lambda e, k=k: e.transpose(pT[:, k * 128:(k + 1) * 128], mb[:, k * 128:(k + 1) * 128], ident[:]), reads=[b_mb, b_id], writes=[b_pT])
        P.op("act", lambda e: e.copy(out=mT[:].rearrange("p k t -> p (k t)"), in_=pT[:]), reads=[b_pT], writes=[b_mT])
        for half in range(2):
            for k in range(8):
                P.op("pe", lambda e, k=k, half=half: e.matmul(pC[half][:], lhsT=mT[:, k, :], rhs=Wo[:, k, half * 512:(half + 1) * 512], start=(k == 0), stop=(k == 7)), reads=[b_mT, b_Wo], writes=[b_pC[half]])
            P.op("act", lambda e, half=half, par=par: e.copy(out=ot[par][:, half * 512:(half + 1) * 512], in_=pC[half][:]), reads=[b_pC[half]], writes=[b_ot[par]])
        rs, brs = rstd_of(ot[par][:], [b_ot[par]], 1)
        P.op("dve", lambda e, par=par, rs=rs: e.scalar_tensor_tensor(out=ot[par][:], in0=ot[par][:], scalar=rs, in1=gpost_t[:], op0=ALU.mult, op1=ALU.mult), reads=[b_ot[par], brs, b_gpost], writes=[b_ot[par]])
        P.op("dve", lambda e, par=par: e.tensor_tensor(out=ot[par][:], in0=ot[par][:], in1=xt[par][:], op=ALU.add), reads=[b_ot[par], b_xt[par]], writes=[b_ot[par]])
        P.dma("pool", lambda e, ti=ti, par=par: e.dma_start(out=x_out[ti * 128:(ti + 1) * 128, :], in_=ot[par][:]), reads=[b_ot[par]], sembuf=b_ot[par])
    P.end()


import math as _math
_PROGS = {}
def _prog(key, fn):
    if key not in _PROGS:
        _PROGS[key] = fn()
    return _PROGS[key]

def _c(a):
    return np.ascontiguousarray(a, dtype=np.float32)

def _bc(v, p=128):
    return _c(np.broadcast_to(v[None, :], (p, v.shape[0])))

def _wl(w, nchunk):
    return _c(w.reshape(8, 128, nchunk, 128).transpose(2, 1, 0, 3))

def _pad1(a):
    return np.concatenate([np.zeros(a.shape[:-1] + (1,), np.float32), a], -1)

def _run(nc, maps):
    res = run_bass_kernel_spmd(nc, maps, core_ids=list(range(8)))
    return res.results

def _pfx(p, d):
    return {p + k: v for k, v in d.items()}

def build_mix(L):
    nc = bass.Bass("TRN2", target_bir_lowering=False)
    P = Prog(nc)
    emit_dsa_h(nc, P, "a_", L)
    emit_rwkv_h(nc, P, "b_", L)
    emit_diff_h(nc, P, "c_", L)
    P.finish()
    return nc

def build_mfa(with_a):
    nc = bass.Bass("TRN2", target_bir_lowering=False)
    P = Prog(nc)
    def din(name, shape): return nc.dram_tensor(name, list(shape), F32, kind="ExternalInput").ap()
    xb = nc.dram_tensor("xb_scr", [2048, D], F32)
    xc = nc.dram_tensor("xc_scr", [2048, D], F32)
    xo = nc.dram_tensor("xo", [2048, D], F32, kind="ExternalOutput").ap()
    emit_merge_h(nc, P, "m_", x_out=xb.ap())
    idn = din("idn", [128, 128])
    WF = {"g_pre": din("f2_gpre", [128, D]), "g_post": din("f2_gpost", [128, D]), "wg": din("f2_wg", [NFF, 128, 8, 128]),
          "wu": din("f2_wu", [NFF, 128, 8, 128]), "wd": din("f2_wd", [NFF, 128, D]), "idn": idn}
    if with_a:
        emit_tok(P, 2048, xb.ap(), xc.ap(), WF, zdst=None)
        zT = nc.dram_tensor("zT", [NWIN * 128, 2048], F32, kind="ExternalOutput").ap()
        WA = {"g_pre": din("f1_gpre", [128, D]), "g_post": din("f1_gpost", [128, D]), "wg": din("f1_wg", [NFF, 128, 8, 128]),
              "wu": din("f1_wu", [NFF, 128, 8, 128]), "wd": din("f1_wd", [NFF, 128, D]), "idn": idn,
              "g_mix": din("g_mix", [128, D]), "win": din("win", [NWIN, 128, 8, 128])}
        emit_tok(P, 2048, xc.ap(), xo, WA, zdst=zT)
    else:
        emit_tok(P, 2048, xb.ap(), xo, WF, zdst=None)
    P.finish()
    return nc

def kernel(**inp):
    inp = {k: np.asarray(v) for k, v in inp.items()}
    Bsz, L, Dm = 2, 8192, 1024
    NQ = L // 128
    x = _c(inp["x"].reshape(Bsz * L, Dm))
    idn = np.eye(128, dtype=np.float32)
    tri = np.triu(np.ones((128, 128), np.float32))
    negm = np.where(np.arange(128)[None, :] <= np.arange(128)[:, None], 0.0, -1e30).astype(np.float32)
    SEG = 512
    cmask = np.ones((64, 2 * SEG), np.float32); cmask[:, ::64] = 0
    sl = np.tril(np.ones((64, 64), np.float32), -1); su = sl.T; iu = np.triu(np.ones((64, 64), np.float32))
    mask5 = _c(np.concatenate([sl, su, iu, su, iu], 1))

    def a_weights(l):
        w_in = inp["w_in"][l]
        w_in_p = np.zeros((1024, 43 * 128), np.float32); w_in_p[:, :5448] = w_in[:, :5448]
        return {"g_pre": _bc(inp["ffn1_norm_pre"][l]), "g_post": _bc(inp["ffn1_norm_post"][l]),
                "wg": _wl(inp["ffn1_w_gate"][l], 22), "wu": _wl(inp["ffn1_w_up"][l], 22), "wd": _c(inp["ffn1_w_down"][l].reshape(22, 128, 1024)),
                "g_mix": _bc(inp["mix_norm_pre"][l]), "win": _wl(w_in_p, 43)}

    aw = a_weights(0)
    ncA = _prog("A", lambda: build_tok(False, True))
    res = _run(ncA, [dict(aw, idn=idn, x=x[c * 2048:(c + 1) * 2048]) for c in range(8)])
    x1 = np.concatenate([r["xo"] for r in res], 0)
    zT = np.concatenate([r["zT"] for r in res], 1)
    del res
    for l in range(2):
        w_in = inp["w_in"][l]
        li = 0.8 - 0.6 * _math.exp(-0.3 * l)
        lam = np.stack([inp["diff_lambda_q1"][l], inp["diff_lambda_k1"][l], inp["diff_lambda_q2"][l], inp["diff_lambda_k2"][l]])
        mu = inp["rwkv_mu"][l]
        maps = []
        for c in range(8):
            b, j = c // 4, c % 4
            zb = zT[:, b * L:(b + 1) * L]
            DO = 2120 + 1792
            zq = zb[DO:DO + 512]; zk = zb[DO + 512:DO + 1024]; zv = zb[DO + 1024:DO + 1536]
            qk = np.stack([zq[j * 128:j * 128 + 64], zq[j * 128 + 64:j * 128 + 128], zk[j * 128:j * 128 + 64], zk[j * 128 + 64:j * 128 + 128]])
            m_diff = {"qk": _c(qk), "v": _c(zv[j * 128:(j + 1) * 128].T.reshape(NQ, 128, 128).transpose(1, 0, 2)),
                      "lam": _c(np.broadcast_to(lam[None], (128, 4, 64))),
                      "cst": _c(np.broadcast_to(np.array([li, 1 - li], np.float32)[None], (128, 2))),
                      "gsub": _bc(inp["diff_subln"][l]), "tri": tri}
            q = zb[0:512][j * 128:(j + 1) * 128]; k = zb[512:1024][j * 128:(j + 1) * 128]; vv = zb[1024:1536][j * 128:(j + 1) * 128]
            qi = zb[1536:2048]; ki = zb[2048:2112]; wi = zb[2112:2120]
            m_dsa = {"qk": _c(np.stack([q, k])), "v": _c(vv.T.reshape(NQ, 128, 128).transpose(1, 0, 2)),
                     "qi": _c(qi.reshape(8, 64, NQ, 128).transpose(2, 1, 0, 3)), "ki": _c(ki),
                     "wi": _c(wi.T.reshape(NQ, 128, 8).transpose(1, 0, 2)), "negm": negm, "idn": idn}
            zrw = zb[2120:2120 + 1792]
            r_ = zrw[0:512].reshape(8, 64, L)[2 * j:2 * j + 2]; k_ = zrw[512:1024].reshape(8, 64, L)[2 * j:2 * j + 2]; v_ = zrw[1024:1536].reshape(8, 64, L)[2 * j:2 * j + 2]
            hp = lambda vec: np.ascontiguousarray(vec.reshape(8, 64)[2 * j:2 * j + 2].T)
            up = lambda w: w.reshape(64, 8, 64)[:, 2 * j:2 * j + 2, :]
            lnwb = np.stack([inp["rwkv_ln_w"][l][2 * j * 64:(2 * j + 2) * 64], inp["rwkv_ln_b"][l][2 * j * 64:(2 * j + 2) * 64]])
            m_rwkv = {"zr": _c(_pad1(np.stack([r_, k_, v_]).transpose(0, 2, 1, 3))),
                      "zl": _c(_pad1(np.stack([zrw[1536:1600], zrw[1600:1664]], 1))), "zg": _c(_pad1(zrw[1664:1792])),
                      "mu3": _c(np.stack([hp(mu[0:512]), hp(mu[512:1024]), hp(mu[1024:1536])], 1)),
                      "mul": _c(np.stack([mu[1536:1600], mu[1600:1664]], 1)), "mug": _c(mu[1664:1792][:, None]),
                      "pp": _c(np.stack([hp(inp["rwkv_w0"][l]), hp(inp["rwkv_a0"][l]), hp(inp["rwkv_k_k"][l]), hp(inp["rwkv_k_a"][l]), hp(inp["rwkv_r_k"][l])], 1)),
                      "wup": _c(up(inp["rwkv_w_up"][l])), "aup": _c(up(inp["rwkv_a_up"][l])),
                      "gup": _c(inp["rwkv_g_up"][l][:, 2 * j * 64:(2 * j + 2) * 64]),
                      "lnwb": _c(np.broadcast_to(lnwb[None], (64, 2, 128))), "cmask": cmask, "mask5": mask5, "idn": np.eye(64, dtype=np.float32)}
            m = {}
            m.update(_pfx("a_", m_dsa)); m.update(_pfx("b_", m_rwkv)); m.update(_pfx("c_", m_diff))
            maps.append(m)
        del zT
        res = _run(_prog("MIX", lambda: build_mix(L)), maps); del maps
        Y = np.zeros((3, Bsz * L, 512), np.float32)
        for c in range(8):
            b, j = c // 4, c % 4
            Y[0, b * L:(b + 1) * L, j * 128:(j + 1) * 128] = res[c]["a_y"].reshape(L, 128)
            Y[1, b * L:(b + 1) * L, j * 128:(j + 1) * 128] = res[c]["b_y"].reshape(L, 128)
            Y[2, b * L:(b + 1) * L, j * 128:(j + 1) * 128] = res[c]["c_y"].reshape(L, 128)
        del res
        cm = {"m_g_mpre": _bc(inp["mix_norm_pre"][l]), "m_g_mpost": _bc(inp["mix_norm_post"][l]),
              "m_wgate": _c(w_in[:, 5448:8520].reshape(8, 128, 3072)), "m_wbr": _c(inp["w_branch"][l].reshape(12, 128, 1024)),
              "m_wo": _c(inp["w_out"][l].reshape(8, 128, 1024)), "m_idn": idn, "idn": idn,
              "f2_gpre": _bc(inp["ffn2_norm_pre"][l]), "f2_gpost": _bc(inp["ffn2_norm_post"][l]),
              "f2_wg": _wl(inp["ffn2_w_gate"][l], 22), "f2_wu": _wl(inp["ffn2_w_up"][l], 22), "f2_wd": _c(inp["ffn2_w_down"][l].reshape(22, 128, 1024))}
        last = (l == 1)
        if not last:
            aw = a_weights(l + 1)
            cm.update({"f1_gpre": aw["g_pre"], "f1_gpost": aw["g_post"], "f1_wg": aw["wg"], "f1_wu": aw["wu"], "f1_wd": aw["wd"], "g_mix": aw["g_mix"], "win": aw["win"]})
        maps = []
        for c in range(8):
            yc_ = Y[:, c * 2048:(c + 1) * 2048, :].reshape(3, 16, 128, 4, 128)
            maps.append(dict(cm, m_x=x1[c * 2048:(c + 1) * 2048], m_yT=_c(yc_.transpose(1, 4, 0, 3, 2).reshape(16, 128, 12 * 128))))
        del Y
        res = _run(_prog("MF" if last else "MFA", (lambda: build_mfa(False)) if last else (lambda: build_mfa(True))), maps); del maps
        x1 = np.concatenate([r["xo"] for r in res], 0)
        if not last:
            zT = np.concatenate([r["zT"] for r in res], 1)
        del res
    return x1.reshape(Bsz, L, Dm).astype(np.float32)
```

```python
import numpy as np
import concourse.bass as bass
import concourse.mybir as mybir
from concourse.bass_utils import run_bass_kernel_spmd
from contextlib import ExitStack

F32 = mybir.dt.float32
BF16 = mybir.dt.bfloat16
AF = mybir.ActivationFunctionType
ALU = mybir.AluOpType
AX = mybir.AxisListType


class BufOld:
    __slots__ = ("name", "w", "r", "sem", "cnt")

    def __init__(self, name):
        self.name = name
        self.w = None
        self.r = {}
        self.sem = None
        self.cnt = 0


class ProgOld:
    ENG = ("pe", "act", "dve", "pool", "sp")

    def __init__(self, nc):
        self.nc = nc
        self.ops = {e: [] for e in self.ENG}
        self.cnt = {e: 0 for e in self.ENG}
        self.seen = {e: {} for e in self.ENG}
        self.dma_bufs = []
        self.stack = ExitStack()
        self.nbuf = 0

    def sb(self, name, shape, dt=F32):
        return self.stack.enter_context(self.nc.sbuf_tensor(name, list(shape), dt))

    def ps(self, name, shape, dt=F32):
        return self.stack.enter_context(self.nc.psum_tensor(name, list(shape), dt))

    def buf(self, name=None):
        self.nbuf += 1
        return BufOld(name or f"b{self.nbuf}")

    def _waits(self, eng, reads, writes):
        need = {}

        def add(k, v):
            if need.get(k, 0) < v:
                need[k] = v
        for b in reads:
            if b.w is not None:
                add(*b.w)
        for b in writes:
            if b.w is not None:
                add(*b.w)
            for k, v in b.r.items():
                add(k, v)
        out = []
        seen = self.seen[eng]
        for k, v in need.items():
            if k == "pe" and eng == "pe":
                continue
            if seen.get(k, 0) >= v:
                continue
            seen[k] = v
            out.append((k, v))
        return out

    def _mark(self, tok, reads, writes):
        k, v = tok
        for b in reads:
            if b.r.get(k, 0) < v:
                b.r[k] = v
        for b in writes:
            b.w = tok
            b.r = {}

    def op(self, eng, fn, reads=(), writes=()):
        waits = self._waits(eng, reads, writes)
        self.cnt[eng] += 1
        tok = (eng, self.cnt[eng])
        self._mark(tok, reads, writes)
        self.ops[eng].append((waits, fn, (eng, 1)))

    def dma(self, q, fn, reads=(), writes=(), sembuf=None):
        waits = self._waits(q, reads, writes)
        sbf = sembuf if sembuf is not None else (writes[0] if writes else reads[0])
        if sbf.sem is None:
            sbf.sem = self.stack.enter_context(self.nc.semaphore(f"d_{sbf.name}_{len(self.dma_bufs)}"))
            self.dma_bufs.append(sbf)
        sbf.cnt += 16
        tok = (sbf, sbf.cnt)
        self._mark(tok, reads, writes)
        self.ops[q].append((waits, fn, (sbf, 16)))

    def emit(self):
        nc = self.nc
        sems = {e: self.stack.enter_context(nc.semaphore(f"s_{e}")) for e in self.ENG}

        def semof(k):
            return sems[k] if isinstance(k, str) else k.sem
        final_waits = [(b, b.cnt) for b in self.dma_bufs if self.seen["sp"].get(b, 0) < b.cnt]
        for e in ("pe", "act", "dve", "pool"):
            if self.cnt[e] > 0:
                final_waits.append((e, self.cnt[e]))
        self.ops["sp"].append((final_waits, None, None))
        block = self.stack.enter_context(nc.Block())

        def run(engobj, lst):
            for waits, fn, inc in lst:
                for k, v in waits:
                    engobj.wait_ge(semof(k), v)
                if fn is None:
                    continue
                ins = fn(engobj)
                if inc is not None:
                    ins.then_inc(semof(inc[0]), inc[1])

        @block.tensor
        def _(e):
            run(e, self.ops["pe"])

        @block.scalar
        def _(e):
            run(e, self.ops["act"])

        @block.vector
        def _(e):
            run(e, self.ops["dve"])

        @block.gpsimd
        def _(e):
            run(e, self.ops["pool"])

        @block.sync
        def _(e):
            run(e, self.ops["sp"])

    def close(self):
        self.stack.close()


D = 1024; DFF = 2816; NFF = 22; TOK = 2048; NT = 16; EPS = 1e-6
NWIN = 43

def build_tok(has_merge, has_win):
    nc = bass.Bass("TRN2", target_bir_lowering=False)
    P = ProgOld(nc)
    def din(name, shape): return nc.dram_tensor(name, list(shape), F32, kind="ExternalInput").ap()
    def dout(name, shape): return nc.dram_tensor(name, list(shape), F32, kind="ExternalOutput").ap()
    x_in = din("x", [TOK, D])
    g_pre = din("g_pre", [128, D]); g_post = din("g_post", [128, D])
    wg = din("wg", [NFF, 128, 8, 128]); wu = din("wu", [NFF, 128, 8, 128]); wd = din("wd", [NFF, 128, D])
    idn_d = din("idn", [128, 128])
    x_out = dout("xo", [TOK, D])
    if has_win:
        g_mix = din("g_mix", [128, D]); win = din("win", [NWIN, 128, 8, 128]); zT = dout("zT", [NWIN * 128, TOK])
    if has_merge:
        g_mpre = din("g_mpre", [128, D]); g_mpost = din("g_mpost", [128, D])
        wgate = din("wgate", [8, 128, 3072]); wbr = din("wbr", [12, 128, D]); wo = din("wo", [8, 128, D])
        yT = din("yT", [NT, 128, 12, 128])
        x2d = dout("x2", [TOK, D])

    ident_f = P.sb("ident_f", [128, 128]); ident = P.sb("ident", [128, 128], BF16)
    gpre_t = P.sb("gpre_t", [128, D]); gpost_t = P.sb("gpost_t", [128, D])
    hT = P.sb("hT", [128, 8, TOK], BF16)
    AT = P.sb("AT", [128, NFF, 1024], BF16)
    Wd = P.sb("Wd", [128, NFF, D], BF16)
    stg = [P.sb(f"stg{i}", [128, 3072]) for i in range(2)]
    wgb = [P.sb(f"wgb{i}", [128, 8, 128], BF16) for i in range(2)]
    wub = [P.sb(f"wub{i}", [128, 8, 128], BF16) for i in range(2)]
    xt = [P.sb(f"xt{i}", [128, D]) for i in range(2)]
    ot = [P.sb(f"ot{i}", [128, D]) for i in range(2)]
    junk = P.sb("junk", [128, D])
    hb = [P.sb(f"hb{i}", [128, D], BF16) for i in range(2)]
    sg = [P.sb(f"sg{i}", [128, 512]) for i in range(2)]
    st = P.sb("st", [128, 64])
    pA = [P.ps(f"pA{i}", [128, 512]) for i in range(2)]
    pB = [P.ps(f"pB{i}", [128, 512]) for i in range(2)]
    pC = [P.ps(f"pC{i}", [128, 512]) for i in range(2)]
    pT = [P.ps(f"pT{i}", [128, 1024], BF16) for i in range(2)]
    B = P.buf
    b_ident = B(); b_identf = B(); b_gpre = B(); b_gpost = B(); b_hT = [B() for _ in range(NT)]
    b_AT = [[B() for _ in range(2)] for _ in range(NFF)]
    b_Wd = [B() for _ in range(NFF)]
    b_stg = [B(), B()]; b_wgb = [B(), B()]; b_wub = [B(), B()]; b_xt = [B(), B()]; b_ot = [B(), B()]
    b_junk = B(); b_hb = [B(), B()]; b_sg = [B(), B()]
    b_pA = [B(), B()]; b_pB = [B(), B()]; b_pC = [B(), B()]; b_pT = [B(), B()]
    st_next = [0]
    b_st = {}
    def stcol():
        i = st_next[0] % 64; st_next[0] += 1
        if i not in b_st: b_st[i] = B()
        return st[:, i:i + 1], b_st[i]

    P.dma("sp", lambda e: e.dma_start(out=ident_f[:], in_=idn_d), writes=[b_identf])
    P.op("dve", lambda e: e.tensor_copy(out=ident[:], in_=ident_f[:]), reads=[b_identf], writes=[b_ident])
    P.dma("sp", lambda e: e.dma_start(out=gpre_t[:], in_=g_pre), writes=[b_gpre])
    P.dma("sp", lambda e: e.dma_start(out=gpost_t[:], in_=g_post), writes=[b_gpost])

    def rstd_of(src_ap, src_bufs):
        ss, bss = stcol(); rs, brs = stcol()
        P.op("act", lambda e: e.activation(out=junk[:], in_=src_ap, func=AF.Square, scale=float(D ** -0.5), accum_out=ss),
             reads=src_bufs, writes=[b_junk, bss])
        P.op("dve", lambda e: e.tensor_scalar(out=rs, in0=ss, scalar1=EPS, scalar2=None, op0=ALU.add),
             reads=[bss], writes=[brs])
        P.op("act", lambda e: e.activation(out=rs, in_=rs, func=AF.Sqrt), reads=[brs], writes=[brs])
        P.op("dve", lambda e: e.reciprocal(out=rs, in_=rs), reads=[brs], writes=[brs])
        return rs, brs

    def norm_to_hT(src_ap, src_bufs, g_t, b_g, ti, par):
        rs, brs = rstd_of(src_ap, src_bufs)
        P.op("dve", lambda e: e.scalar_tensor_tensor(out=hb[par][:], in0=src_ap, scalar=rs, in1=g_t[:], op0=ALU.mult, op1=ALU.mult),
             reads=src_bufs + [brs, b_g], writes=[b_hb[par]])
        for k in range(8):
            P.op("pe", lambda e, k=k: e.transpose(pT[par][:, k * 128:(k + 1) * 128], hb[par][:, k * 128:(k + 1) * 128], ident[:]),
                 reads=[b_hb[par], b_ident], writes=[b_pT[par]])
        P.op("act", lambda e: e.copy(out=hT[:, :, ti * 128:(ti + 1) * 128], in_=pT[par][:].rearrange("p (k t) -> p k t", k=8)),
             reads=[b_pT[par]], writes=[b_hT[ti]])

    def ffn_stage(xsrc, xsrc_bufs, xdst, post_tile=None):
        dst_bufs = [B() for _ in range(NT)]
        for ti in range(NT):
            par = ti % 2
            rb = [xsrc_bufs[ti]] if xsrc_bufs[ti] is not None else []
            P.dma("sp", lambda e, ti=ti, par=par: e.dma_start(out=xt[par][:], in_=xsrc[ti * 128:(ti + 1) * 128, :]),
                  reads=rb, writes=[b_xt[par]])
            norm_to_hT(xt[par][:], [b_xt[par]], gpre_t, b_gpre, ti, par)
        for c in range(NFF):
            s = c % 2
            P.dma("sp", lambda e, c=c, s=s: e.dma_start(out=stg[s][:, 0:D], in_=wd[c]), writes=[b_stg[s]])
            P.op("pool", lambda e, c=c, s=s: e.tensor_copy(out=Wd[:, c, :], in_=stg[s][:, 0:D]), reads=[b_stg[s]], writes=[b_Wd[c]])
        for half in range(2):
            for c in range(NFF):
                s = c % 2
                P.dma("sp", lambda e, c=c, s=s: e.dma_start(out=stg[s][:, 0:1024], in_=wg[c].rearrange("p k f -> p (k f)")), writes=[b_stg[s]])
                P.op("pool", lambda e, s=s: e.tensor_copy(out=wgb[s][:].rearrange("p k f -> p (k f)"), in_=stg[s][:, 0:1024]), reads=[b_stg[s]], writes=[b_wgb[s]])
                P.dma("sp", lambda e, c=c, s=s: e.dma_start(out=stg[s][:, 1024:2048], in_=wu[c].rearrange("p k f -> p (k f)")), writes=[b_stg[s]])
                P.op("pool", lambda e, s=s: e.tensor_copy(out=wub[s][:].rearrange("p k f -> p (k f)"), in_=stg[s][:, 1024:2048]), reads=[b_stg[s]], writes=[b_wub[s]])
                for tb in range(2):
                    t0 = half * 1024 + tb * 512
                    rd = [b_hT[(t0 // 128) + i] for i in range(4)]
                    q = tb
                    for k in range(8):
                        P.op("pe", lambda e, k=k, s=s, q=q, t0=t0: e.matmul(pA[q][:], lhsT=wgb[s][:, k, :], rhs=hT[:, k, t0:t0 + 512], start=(k == 0), stop=(k == 7)),
                             reads=[b_wgb[s]] + rd, writes=[b_pA[q]])
                    for k in range(8):
                        P.op("pe", lambda e, k=k, s=s, q=q, t0=t0: e.matmul(pB[q][:], lhsT=wub[s][:, k, :], rhs=hT[:, k, t0:t0 + 512], start=(k == 0), stop=(k == 7)),
                             reads=[b_wub[s]] + rd, writes=[b_pB[q]])
                    P.op("act", lambda e, q=q: e.activation(out=sg[q][:], in_=pA[q][:], func=AF.Silu), reads=[b_pA[q]], writes=[b_sg[q]])
                    P.op("dve", lambda e, q=q, c=c, tb=tb: e.tensor_tensor(out=AT[:, c, tb * 512:(tb + 1) * 512], in0=sg[q][:], in1=pB[q][:], op=ALU.mult),
                         reads=[b_sg[q], b_pB[q]], writes=[b_AT[c][tb]])
            for tl in range(8):
                ti = half * 8 + tl; par = ti % 2
                rb = [xsrc_bufs[ti]] if xsrc_bufs[ti] is not None else []
                P.dma("sp", lambda e, ti=ti, par=par: e.dma_start(out=xt[par][:], in_=xsrc[ti * 128:(ti + 1) * 128, :]),
                      reads=rb, writes=[b_xt[par]])
                for ch in range(2):
                    for c in range(NFF):
                        P.op("pe", lambda e, c=c, ch=ch, tl=tl: e.matmul(pC[ch][:], lhsT=AT[:, c, tl * 128:(tl + 1) * 128], rhs=Wd[:, c, ch * 512:(ch + 1) * 512], start=(c == 0), stop=(c == NFF - 1)),
                             reads=[b_AT[c][tl // 4], b_Wd[c]], writes=[b_pC[ch]])
                    P.op("act", lambda e, ch=ch, par=par: e.copy(out=ot[par][:, ch * 512:(ch + 1) * 512], in_=pC[ch][:]), reads=[b_pC[ch]], writes=[b_ot[par]])
                rs, brs = rstd_of(ot[par][:], [b_ot[par]])
                P.op("dve", lambda e, par=par, rs=rs: e.scalar_tensor_tensor(out=ot[par][:], in0=ot[par][:], scalar=rs, in1=gpost_t[:], op0=ALU.mult, op1=ALU.mult),
                     reads=[b_ot[par], brs, b_gpost], writes=[b_ot[par]])
                P.op("dve", lambda e, par=par: e.scalar_tensor_tensor(out=ot[par][:], in0=ot[par][:], scalar=0.5, in1=xt[par][:], op0=ALU.mult, op1=ALU.add),
                     reads=[b_ot[par], b_xt[par]], writes=[b_ot[par]])
                P.dma("pool", lambda e, ti=ti, par=par: e.dma_start(out=xdst[ti * 128:(ti + 1) * 128, :], in_=ot[par][:]),
                      reads=[b_ot[par]], writes=[dst_bufs[ti]], sembuf=b_ot[par])
                if post_tile is not None:
                    post_tile(ti, par)
        return dst_bufs

    src_bufs = [None] * NT
    xsrc = x_in
    if has_merge:
        raise NotImplementedError
    if has_win:
        gmix_t = P.sb("gmix_t", [128, D]); b_gmix = B()
        P.dma("sp", lambda e: e.dma_start(out=gmix_t[:], in_=g_mix), writes=[b_gmix])
        hT2 = hT; b_hT2 = b_hT
        def post(ti, par):
            rs, brs = rstd_of(ot[par][:], [b_ot[par]])
            P.op("dve", lambda e: e.scalar_tensor_tensor(out=hb[par][:], in0=ot[par][:], scalar=rs, in1=gmix_t[:], op0=ALU.mult, op1=ALU.mult),
                 reads=[b_ot[par], brs, b_gmix], writes=[b_hb[par]])
            for k in range(8):
                P.op("pe", lambda e, k=k: e.transpose(pT[par][:, k * 128:(k + 1) * 128], hb[par][:, k * 128:(k + 1) * 128], ident[:]),
                     reads=[b_hb[par], b_ident], writes=[b_pT[par]])
            P.op("act", lambda e: e.copy(out=hT2[:, :, ti * 128:(ti + 1) * 128], in_=pT[par][:].rearrange("p (k t) -> p k t", k=8)),
                 reads=[b_pT[par]], writes=[b_hT2[ti]])
        ffn_stage(xsrc, src_bufs, x_out, post)
        zs = [P.sb(f"zs{i}", [128, 1024]) for i in range(2)]; b_zs = [B(), B()]
        for c in range(NWIN):
            s = c % 2
            P.dma("sp", lambda e, c=c, s=s: e.dma_start(out=stg[s][:, 0:1024], in_=win[c].rearrange("p k f -> p (k f)")), writes=[b_stg[s]])
            P.op("pool", lambda e, s=s: e.tensor_copy(out=wgb[s][:].rearrange("p k f -> p (k f)"), in_=stg[s][:, 0:1024]), reads=[b_stg[s]], writes=[b_wgb[s]])
            for tb in range(4):
                q = tb % 2; t0 = tb * 512; zi = tb // 2
                rd = [b_hT2[(t0 // 128) + i] for i in range(4)]
                for k in range(8):
                    P.op("pe", lambda e, k=k, s=s, q=q, t0=t0: e.matmul(pA[q][:], lhsT=wgb[s][:, k, :], rhs=hT2[:, k, t0:t0 + 512], start=(k == 0), stop=(k == 7)),
                         reads=[b_wgb[s]] + rd, writes=[b_pA[q]])
                if tb % 2 == 0:
                    P.op("act", lambda e, q=q, zi=zi: e.copy(out=zs[zi][:, 0:512], in_=pA[q][:]), reads=[b_pA[q]], writes=[b_zs[zi]])
                else:
                    P.op("dve", lambda e, q=q, zi=zi: e.tensor_copy(out=zs[zi][:, 512:1024], in_=pA[q][:]), reads=[b_pA[q]], writes=[b_zs[zi]])
                    P.dma("pool", lambda e, c=c, zi=zi: e.dma_start(out=zT[c * 128:(c + 1) * 128, zi * 1024:(zi + 1) * 1024], in_=zs[zi][:]), reads=[b_zs[zi]], sembuf=b_zs[zi])
    else:
        ffn_stage(xsrc, src_bufs, x_out, None)
    P.emit(); P.close()
    return nc


def build_diff(L):
    NQ = L // 128
    nc = bass.Bass("TRN2", target_bir_lowering=False)
    P = ProgOld(nc); B = P.buf
    def din(name, shape): return nc.dram_tensor(name, list(shape), F32, kind="ExternalInput").ap()
    qk_d = din("qk", [4, 64, L])
    v_d = din("v", [128, NQ, 128])
    lam_d = din("lam", [128, 4, 64])
    cst_d = din("cst", [128, 2])
    gsub_d = din("gsub", [128, 128])
    tri_d = din("tri", [128, 128])
    y_d = nc.dram_tensor("y", [NQ, 128, 128], F32, kind="ExternalOutput").ap()

    qkb = [P.sb(f"qkb{i}", [64, L], BF16) for i in range(4)]; b_qkb = [B() for _ in range(4)]
    vb = P.sb("vb", [128, NQ, 130], BF16); b_vb = B()
    stg = [P.sb(f"stg{i}", [128, 2048]) for i in range(2)]; b_stg = [B(), B()]
    lam_t = P.sb("lam_t", [128, 4, 64]); b_lam = B()
    cst = P.sb("cst_t", [128, 2]); b_cst = B()
    gsub = P.sb("gsub_t", [128, 128]); b_gsub = B()
    tri_f = P.sb("tri_f", [128, 128]); b_trif = B()
    tri = P.sb("tri_b", [128, 128], BF16); b_tri = B()
    ones = P.sb("ones", [128, 2], BF16); b_ones = B()
    sm = P.sb("sm", [128, 16]); b_sm = [B() for _ in range(16)]
    junk = P.sb("junk", [128, 128]); b_junk = B()
    ET = [[P.sb(f"ET{m}{i}", [128, 4, 128], BF16) for i in range(2)] for m in range(2)]
    b_ET = [[B(), B()] for _ in range(2)]
    ob = [P.sb(f"ob{i}", [128, 128]) for i in range(2)]; b_ob = [B(), B()]
    t2 = P.sb("t2", [128, 128]); b_t2 = B()
    pS = [[P.ps(f"pS{m}{i}", [128, 512]) for i in range(2)] for m in range(2)]; b_pS = [[B(), B()] for _ in range(2)]
    pO = [P.ps(f"pO{m}", [128, 512]) for m in range(2)]; b_pO = [B(), B()]

    n = 0
    for i in range(4):
        CW = min(2048, L)
        for c0 in range(0, L, CW):
            s = n % 2; n += 1
            P.dma("sp", lambda e, i=i, c0=c0, s=s: e.dma_start(out=stg[s][0:64, 0:CW], in_=qk_d[i, :, c0:c0 + CW]), writes=[b_stg[s]])
            P.op("pool", lambda e, i=i, c0=c0, s=s: e.tensor_copy(out=qkb[i][:, c0:c0 + CW], in_=stg[s][0:64, 0:CW]), reads=[b_stg[s]], writes=[b_qkb[i]])
    TW = min(16, NQ)
    for t0 in range(0, NQ, TW):
        s = n % 2; n += 1
        P.dma("sp", lambda e, t0=t0, s=s: e.dma_start(out=stg[s][:, 0:TW * 128], in_=v_d[:, t0:t0 + TW, :].rearrange("p t d -> p (t d)")), writes=[b_stg[s]])
        P.op("pool", lambda e, t0=t0, s=s: e.tensor_copy(out=vb[:, t0:t0 + TW, 0:128], in_=stg[s][:, 0:TW * 128].rearrange("p (t d) -> p t d", d=128)), reads=[b_stg[s]], writes=[b_vb])
    P.dma("sp", lambda e: e.dma_start(out=lam_t[:], in_=lam_d), writes=[b_lam])
    P.dma("sp", lambda e: e.dma_start(out=cst[:], in_=cst_d), writes=[b_cst])
    P.dma("sp", lambda e: e.dma_start(out=gsub[:], in_=gsub_d), writes=[b_gsub])
    P.dma("sp", lambda e: e.dma_start(out=tri_f[:], in_=tri_d), writes=[b_trif])
    P.op("dve", lambda e: e.tensor_copy(out=tri[:], in_=tri_f[:]), reads=[b_trif], writes=[b_tri])
    P.op("dve", lambda e: e.memset(vb[:, :, 128:130], 1.0), writes=[b_vb])
    for j in range(2):
        P.op("dve", lambda e, j=j: e.tensor_tensor(out=junk[:, 0:64], in0=lam_t[:, 2 * j, :], in1=lam_t[:, 2 * j + 1, :], op=ALU.mult), reads=[b_lam], writes=[b_junk])
        P.op("dve", lambda e, j=j: e.reduce_sum(out=sm[:, j:j + 1], in_=junk[:, 0:64], axis=AX.X), reads=[b_junk], writes=[b_sm[j]])
        P.op("act", lambda e, j=j: e.activation(out=sm[:, j:j + 1], in_=sm[:, j:j + 1], func=AF.Exp), reads=[b_sm[j]], writes=[b_sm[j]])
    P.op("dve", lambda e: e.tensor_tensor(out=sm[:, 2:3], in0=sm[:, 1:2], in1=sm[:, 0:1], op=ALU.subtract), reads=[b_sm[0], b_sm[1]], writes=[b_sm[2]])
    P.op("dve", lambda e: e.tensor_tensor(out=sm[:, 2:3], in0=sm[:, 2:3], in1=cst[:, 0:1], op=ALU.subtract), reads=[b_sm[2], b_cst], writes=[b_sm[2]])
    NEGLAM = (sm[:, 2:3], b_sm[2])

    gi = 0
    for qi in range(NQ):
        nk = qi + 1
        groups = [(g0, min(4, nk - g0)) for g0 in range(0, nk, 4)]
        for gidx, (g0, gn) in enumerate(groups):
            par = gi % 2; gi += 1
            for m in range(2):
                for j in range(gn):
                    kt = g0 + j
                    P.op("pe", lambda e, m=m, j=j, kt=kt, par=par, qi=qi: e.matmul(pS[m][par][:, j * 128:(j + 1) * 128], lhsT=qkb[2 + m][:, kt * 128:(kt + 1) * 128], rhs=qkb[m][:, qi * 128:(qi + 1) * 128], start=True, stop=True),
                         reads=[b_qkb[2 + m], b_qkb[m]], writes=[b_pS[m][par]])
                P.op("act", lambda e, m=m, par=par, gn=gn: e.activation(out=ET[m][par][:, 0:gn, :].rearrange("p g q -> p (g q)"), in_=pS[m][par][:, 0:gn * 128], func=AF.Exp, scale=0.125),
                     reads=[b_pS[m][par]], writes=[b_ET[m][par]])
                if g0 + gn == nk:
                    j = gn - 1
                    P.op("dve", lambda e, m=m, par=par, j=j: e.tensor_tensor(out=ET[m][par][:, j, :], in0=ET[m][par][:, j, :], in1=tri[:], op=ALU.mult),
                         reads=[b_ET[m][par], b_tri], writes=[b_ET[m][par]])
                for j in range(gn):
                    kt = g0 + j
                    first = (kt == 0); last = (kt == nk - 1)
                    P.op("pe", lambda e, m=m, j=j, kt=kt, par=par, first=first, last=last: e.matmul(pO[m][:, 0:130], lhsT=ET[m][par][:, j, :], rhs=vb[:, kt, :], start=first, stop=last),
                         reads=[b_ET[m][par], b_vb], writes=[b_pO[m]])
        op_ = qi % 2
        P.op("dve", lambda e: e.reciprocal(out=sm[:, 4:5], in_=pO[0][:, 128:129]), reads=[b_pO[0]], writes=[b_sm[4]])
        P.op("dve", lambda e: e.reciprocal(out=sm[:, 5:6], in_=pO[1][:, 128:129]), reads=[b_pO[1]], writes=[b_sm[5]])
        P.op("dve", lambda e: e.tensor_tensor(out=sm[:, 5:6], in0=sm[:, 5:6], in1=NEGLAM[0], op=ALU.mult), reads=[b_sm[5], NEGLAM[1]], writes=[b_sm[5]])
        P.op("dve", lambda e: e.tensor_scalar(out=t2[:], in0=pO[1][:, 0:128], scalar1=sm[:, 5:6], scalar2=None, op0=ALU.mult), reads=[b_pO[1], b_sm[5]], writes=[b_t2])
        P.op("dve", lambda e, op_=op_: e.scalar_tensor_tensor(out=ob[op_][:], in0=pO[0][:, 0:128], scalar=sm[:, 4:5], in1=t2[:], op0=ALU.mult, op1=ALU.add), reads=[b_pO[0], b_sm[4], b_t2], writes=[b_ob[op_]])
        P.op("act", lambda e, op_=op_: e.activation(out=junk[:], in_=ob[op_][:], func=AF.Square, scale=float(128 ** -0.5), accum_out=sm[:, 6:7]), reads=[b_ob[op_]], writes=[b_junk, b_sm[6]])
        P.op("dve", lambda e: e.tensor_scalar(out=sm[:, 6:7], in0=sm[:, 6:7], scalar1=1e-6, scalar2=None, op0=ALU.add), reads=[b_sm[6]], writes=[b_sm[6]])
        P.op("act", lambda e: e.activation(out=sm[:, 6:7], in_=sm[:, 6:7], func=AF.Sqrt), reads=[b_sm[6]], writes=[b_sm[6]])
        P.op("dve", lambda e: e.reciprocal(out=sm[:, 6:7], in_=sm[:, 6:7]), reads=[b_sm[6]], writes=[b_sm[6]])
        P.op("dve", lambda e: e.tensor_tensor(out=sm[:, 6:7], in0=sm[:, 6:7], in1=cst[:, 1:2], op=ALU.mult), reads=[b_sm[6], b_cst], writes=[b_sm[6]])
        P.op("dve", lambda e, op_=op_: e.scalar_tensor_tensor(out=ob[op_][:], in0=ob[op_][:], scalar=sm[:, 6:7], in1=gsub[:], op0=ALU.mult, op1=ALU.mult), reads=[b_ob[op_], b_sm[6], b_gsub], writes=[b_ob[op_]])
        P.dma("pool", lambda e, qi=qi, op_=op_: e.dma_start(out=y_d[qi], in_=ob[op_][:]), reads=[b_ob[op_]], sembuf=b_ob[op_])
    P.emit(); P.close()
    return nc


def build_dsa(L, R=32.0, K=22):
    NQ = L // 128
    nc = bass.Bass("TRN2", target_bir_lowering=False)
    P = ProgOld(nc); B = P.buf
    def din(name, shape): return nc.dram_tensor(name, list(shape), F32, kind="ExternalInput").ap()
    qk_d = din("qk", [2, 128, L])
    v_d = din("v", [128, NQ, 128])
    qi_d = din("qi", [NQ, 64, 8, 128])
    ki_d = din("ki", [64, L])
    wi_d = din("wi", [128, NQ, 8])
    negm_d = din("negm", [128, 128])
    idn_d = din("idn", [128, 128])
    y_d = nc.dram_tensor("y", [NQ, 128, 128], F32, kind="ExternalOutput").ap()
    dbg_d = nc.dram_tensor("dbg", [NQ, 128, 8], F32, kind="ExternalOutput").ap()
    dbg = [P.sb(f"dbg{i}", [128, 8]) for i in range(2)]; b_dbg = [B(), B()]

    qkb = [P.sb(f"qkb{i}", [128, L], BF16) for i in range(2)]; b_qkb = [B(), B()]
    vb = P.sb("vb", [128, NQ, 130], BF16); b_vb = B()
    kiT = P.sb("kiT", [64, L]); b_ki = B()
    wi = P.sb("wi_t", [128, NQ, 8]); b_wi = B()
    negm = P.sb("negm_t", [128, 128]); b_negm = B()
    idf = P.sb("idf", [128, 128]); b_idf = B()
    idb = P.sb("idb", [128, 128], BF16); b_idb = B()
    stg = [P.sb(f"stg{i}", [128, 2048]) for i in range(2)]; b_stg = [B(), B()]
    qit = [P.sb(f"qit{i}", [64, 8, 128]) for i in range(2)]; b_qit = [B(), B()]
    score = [P.sb(f"score{i}", [128, L]) for i in range(2)]; b_score = [B(), B()]
    junkS = P.sb("junkS", [128, L], BF16); b_junkS = B()
    rl = [P.sb(f"rl{i}", [128, 512]) for i in range(2)]; b_rl = [B(), B()]
    Eb = [P.sb(f"Eb{i}", [128, 512], BF16) for i in range(2)]; b_Eb = [B(), B()]
    Pm = [P.sb(f"Pm{i}", [128, 512], BF16) for i in range(2)]; b_Pm = [B(), B()]
    PmT = [P.sb(f"PmT{i}", [128, 4, 128], BF16) for i in range(2)]; b_PmT = [B(), B()]
    ob = [P.sb(f"ob{i}", [128, 128]) for i in range(2)]; b_ob = [B(), B()]
    sm = P.sb("sm", [128, 8]); b_sm = [B() for _ in range(8)]
    pD = [P.ps(f"pD{i}", [128, 512]) for i in range(2)]; b_pD = [B(), B()]
    pS = [P.ps(f"pS{i}", [128, 512]) for i in range(2)]; b_pS = [B(), B()]
    pT = [P.ps(f"pT{i}", [128, 1024], BF16) for i in range(2)]; b_pT = [B(), B()]
    pO = P.ps("pO", [128, 512]); b_pO = B()

    n = 0
    CW = min(2048, L)
    for i in range(2):
        for c0 in range(0, L, CW):
            s = n % 2; n += 1
            P.dma("sp", lambda e, i=i, c0=c0, s=s: e.dma_start(out=stg[s][:, 0:CW], in_=qk_d[i, :, c0:c0 + CW]), writes=[b_stg[s]])
            P.op("pool", lambda e, i=i, c0=c0, s=s: e.tensor_copy(out=qkb[i][:, c0:c0 + CW], in_=stg[s][:, 0:CW]), reads=[b_stg[s]], writes=[b_qkb[i]])
    TW = min(16, NQ)
    for t0 in range(0, NQ, TW):
        s = n % 2; n += 1
        P.dma("sp", lambda e, t0=t0, s=s: e.dma_start(out=stg[s][:, 0:TW * 128], in_=v_d[:, t0:t0 + TW, :].rearrange("p t d -> p (t d)")), writes=[b_stg[s]])
        P.op("pool", lambda e, t0=t0, s=s: e.tensor_copy(out=vb[:, t0:t0 + TW, 0:128], in_=stg[s][:, 0:TW * 128].rearrange("p (t d) -> p t d", d=128)), reads=[b_stg[s]], writes=[b_vb])
    P.op("dve", lambda e: e.memset(vb[:, :, 128:130], 1.0), writes=[b_vb])
    P.dma("sp", lambda e: e.dma_start(out=kiT[:], in_=ki_d), writes=[b_ki])
    P.dma("sp", lambda e: e.dma_start(out=wi[:], in_=wi_d), writes=[b_wi])
    P.dma("sp", lambda e: e.dma_start(out=negm[:], in_=negm_d), writes=[b_negm])
    P.dma("sp", lambda e: e.dma_start(out=idf[:], in_=idn_d), writes=[b_idf])
    P.op("dve", lambda e: e.tensor_copy(out=idb[:], in_=idf[:]), reads=[b_idf], writes=[b_idb])
    SC = float((64 ** -0.5) * (8 ** -0.5))
    ci = 0; ai = 0
    for qi in range(NQ):
        nk = qi + 1; nkeys = nk * 128
        sp_ = qi % 2
        sc = score[sp_]; bsc = b_score[sp_]
        P.dma("sp", lambda e, qi=qi, sp_=sp_: e.dma_start(out=qit[sp_][:], in_=qi_d[qi]), writes=[b_qit[sp_]])
        chunks = [(c0, min(4, nk - c0)) for c0 in range(0, nk, 4)]
        for (c0, cn) in chunks:
            w = cn * 128; k0 = c0 * 128
            for h in range(8):
                p = ci % 2; ci += 1
                P.op("pe", lambda e, h=h, p=p, k0=k0, w=w, sp_=sp_: e.matmul(pD[p][:, 0:w], lhsT=qit[sp_][:, h, :], rhs=kiT[:, k0:k0 + w], start=True, stop=True),
                     reads=[b_qit[sp_], b_ki], writes=[b_pD[p]])
                P.op("act", lambda e, p=p, w=w: e.activation(out=rl[p][:, 0:w], in_=pD[p][:, 0:w], func=AF.Relu, scale=SC), reads=[b_pD[p]], writes=[b_rl[p]])
                if h == 0:
                    P.op("dve", lambda e, p=p, w=w, k0=k0, sc=sc, qi=qi, h=h: e.tensor_scalar(out=sc[:, k0:k0 + w], in0=rl[p][:, 0:w], scalar1=wi[:, qi, h:h + 1], scalar2=None, op0=ALU.mult),
                         reads=[b_rl[p], b_wi], writes=[bsc])
                else:
                    P.op("dve", lambda e, p=p, w=w, k0=k0, sc=sc, qi=qi, h=h: e.scalar_tensor_tensor(out=sc[:, k0:k0 + w], in0=rl[p][:, 0:w], scalar=wi[:, qi, h:h + 1], in1=sc[:, k0:k0 + w], op0=ALU.mult, op1=ALU.add),
                         reads=[b_rl[p], b_wi, bsc], writes=[bsc])
        d0 = (nk - 1) * 128
        P.op("dve", lambda e, sc=sc, d0=d0: e.tensor_tensor(out=sc[:, d0:d0 + 128], in0=sc[:, d0:d0 + 128], in1=negm[:], op=ALU.add), reads=[bsc, b_negm], writes=[bsc])
        tau = sm[:, 0:1]; mid = sm[:, 1:2]; cnt = sm[:, 2:3]; s_ = sm[:, 3:4]
        if nkeys <= 256:
            P.op("dve", lambda e: e.memset(tau, -R), writes=[b_sm[0]])
        else:
            P.op("dve", lambda e: e.memset(mid, 0.0), writes=[b_sm[1]])
            for it in range(K):
                P.op("dve", lambda e, sc=sc, nkeys=nkeys: e.tensor_scalar(out=junkS[:, 0:nkeys], in0=sc[:, 0:nkeys], scalar1=mid, scalar2=0.0, op0=ALU.is_ge, op1=ALU.add, accum_out=cnt),
                     reads=[bsc, b_sm[1]], writes=[b_junkS, b_sm[2]])
                if it < K - 1:
                    wn = R / 2 ** (it + 1)
                    P.op("dve", lambda e, wn=wn: e.tensor_scalar(out=s_, in0=cnt, scalar1=255.5, scalar2=2 * wn, op0=ALU.is_ge, op1=ALU.mult), reads=[b_sm[2]], writes=[b_sm[3]])
                    P.op("dve", lambda e, wn=wn: e.scalar_tensor_tensor(out=mid, in0=s_, scalar=-wn, in1=mid, op0=ALU.add, op1=ALU.add), reads=[b_sm[3], b_sm[1]], writes=[b_sm[1]])
                else:
                    wl = R / 2 ** (K - 1)
                    P.op("dve", lambda e, wl=wl: e.tensor_scalar(out=s_, in0=cnt, scalar1=255.5, scalar2=wl, op0=ALU.is_ge, op1=ALU.mult), reads=[b_sm[2]], writes=[b_sm[3]])
                    P.op("dve", lambda e, wl=wl: e.scalar_tensor_tensor(out=tau, in0=s_, scalar=-wl, in1=mid, op0=ALU.add, op1=ALU.add), reads=[b_sm[3], b_sm[1]], writes=[b_sm[0]])
        P.op("dve", lambda e, sp_=sp_: e.tensor_copy(out=dbg[sp_][:], in_=sm[:]), reads=b_sm, writes=[b_dbg[sp_]])
        P.dma("pool", lambda e, qi=qi, sp_=sp_: e.dma_start(out=dbg_d[qi], in_=dbg[sp_][:]), reads=[b_dbg[sp_]], sembuf=b_dbg[sp_])
        for (c0, cn) in chunks:
            w = cn * 128; k0 = c0 * 128
            p = ai % 2; ai += 1
            P.op("pe", lambda e, p=p, k0=k0, w=w, qi=qi: e.matmul(pS[p][:, 0:w], lhsT=qkb[0][:, qi * 128:(qi + 1) * 128], rhs=qkb[1][:, k0:k0 + w], start=True, stop=True),
                 reads=[b_qkb[0], b_qkb[1]], writes=[b_pS[p]])
            P.op("act", lambda e, p=p, w=w: e.activation(out=Eb[p][:, 0:w], in_=pS[p][:, 0:w], func=AF.Exp, scale=float(128 ** -0.5)), reads=[b_pS[p]], writes=[b_Eb[p]])
            P.op("dve", lambda e, p=p, w=w, k0=k0, sc=sc: e.scalar_tensor_tensor(out=Pm[p][:, 0:w], in0=sc[:, k0:k0 + w], scalar=tau, in1=Eb[p][:, 0:w], op0=ALU.is_ge, op1=ALU.mult),
                 reads=[bsc, b_sm[0], b_Eb[p]], writes=[b_Pm[p]])
            for j in range(cn):
                P.op("pe", lambda e, p=p, j=j: e.transpose(pT[p][:, j * 128:(j + 1) * 128], Pm[p][:, j * 128:(j + 1) * 128], idb[:]), reads=[b_Pm[p], b_idb], writes=[b_pT[p]])
            P.op("act", lambda e, p=p, w=w, cn=cn: e.copy(out=PmT[p][:, 0:cn, :].rearrange("p g q -> p (g q)"), in_=pT[p][:, 0:w]), reads=[b_pT[p]], writes=[b_PmT[p]])
            for j in range(cn):
                kt = c0 + j
                P.op("pe", lambda e, p=p, j=j, kt=kt, nk=nk: e.matmul(pO[:, 0:130], lhsT=PmT[p][:, j, :], rhs=vb[:, kt, :], start=(kt == 0), stop=(kt == nk - 1)),
                     reads=[b_PmT[p], b_vb], writes=[b_pO])
        op_ = qi % 2
        P.op("dve", lambda e: e.reciprocal(out=sm[:, 4:5], in_=pO[:, 128:129]), reads=[b_pO], writes=[b_sm[4]])
        P.op("dve", lambda e, op_=op_: e.tensor_scalar(out=ob[op_][:], in0=pO[:, 0:128], scalar1=sm[:, 4:5], scalar2=None, op0=ALU.mult), reads=[b_pO, b_sm[4]], writes=[b_ob[op_]])
        P.dma("pool", lambda e, qi=qi, op_=op_: e.dma_start(out=y_d[qi], in_=ob[op_][:]), reads=[b_ob[op_]], sembuf=b_ob[op_])
    P.emit(); P.close()
    return nc

D = 1024; TOK = 2048; NT = 16; EPS = 1e-6

def build_merge():
    nc = bass.Bass("TRN2", target_bir_lowering=False)
    P = ProgOld(nc); B = P.buf
    def din(name, shape): return nc.dram_tensor(name, list(shape), F32, kind="ExternalInput").ap()
    x_in = din("x", [TOK, D]); g_mpre = din("g_mpre", [128, D]); g_mpost = din("g_mpost", [128, D])
    wgate = din("wgate", [8, 128, 3072]); wbr = din("wbr", [12, 128, D]); wo = din("wo", [8, 128, D])
    yT = din("yT", [NT, 128, 12 * 128]); idn_d = din("idn", [128, 128])
    x_out = nc.dram_tensor("xo", [TOK, D], F32, kind="ExternalOutput").ap()
    ident_f = P.sb("ident_f", [128, 128]); ident = P.sb("ident", [128, 128], BF16)
    gpre_t = P.sb("gpre_t", [128, D]); gpost_t = P.sb("gpost_t", [128, D])
    Wg = P.sb("Wg", [128, 8, 3072], BF16); Wb = P.sb("Wb", [128, 12, D], BF16); Wo = P.sb("Wo", [128, 8, D], BF16)
    stg = [P.sb(f"stg{i}", [128, 3072]) for i in range(2)]
    xt = [P.sb(f"xt{i}", [128, D]) for i in range(2)]; ot = [P.sb(f"ot{i}", [128, D]) for i in range(2)]
    mg = P.sb("mg", [128, D]); tmp = P.sb("tmp", [128, 512]); junk = P.sb("junk", [128, D])
    hb = P.sb("hb", [128, D], BF16); mb = P.sb("mb", [128, D], BF16)
    hTt = P.sb("hTt", [128, 8, 128], BF16); mT = P.sb("mT", [128, 8, 128], BF16)
    ystg = [P.sb(f"ystg{i}", [128, 1536]) for i in range(2)]; ytb = [P.sb(f"ytb{i}", [128, 12, 128], BF16) for i in range(2)]
    sgt = [P.sb(f"sgt{i}", [128, 512]) for i in range(2)]
    st = P.sb("st", [128, 8])
    pA = [P.ps(f"pA{i}", [128, 512]) for i in range(2)]; pB = [P.ps(f"pB{i}", [128, 512]) for i in range(2)]
    pC = [P.ps(f"pC{i}", [128, 512]) for i in range(2)]; pT = P.ps("pT", [128, 1024], BF16)
    b_idf = B(); b_id = B(); b_gpre = B(); b_gpost = B(); b_Wg = B(); b_Wb = B(); b_Wo = B(); b_stg = [B(), B()]
    b_xt = [B(), B()]; b_ot = [B(), B()]; b_mg = B(); b_tmp = B(); b_junk = B(); b_hb = B(); b_mb = B(); b_hTt = B(); b_mT = B()
    b_ystg = [B(), B()]; b_ytb = [B(), B()]; b_sgt = [B(), B()]; b_st = [B() for _ in range(8)]
    b_pA = [B(), B()]; b_pB = [B(), B()]; b_pC = [B(), B()]; b_pT = B()
    P.dma("sp", lambda e: e.dma_start(out=ident_f[:], in_=idn_d), writes=[b_idf])
    P.op("dve", lambda e: e.tensor_copy(out=ident[:], in_=ident_f[:]), reads=[b_idf], writes=[b_id])
    P.dma("sp", lambda e: e.dma_start(out=gpre_t[:], in_=g_mpre), writes=[b_gpre])
    P.dma("sp", lambda e: e.dma_start(out=gpost_t[:], in_=g_mpost), writes=[b_gpost])
    n = 0
    for k in range(8):
        s = n % 2; n += 1
        P.dma("sp", lambda e, k=k, s=s: e.dma_start(out=stg[s][:, :], in_=wgate[k]), writes=[b_stg[s]])
        P.op("pool", lambda e, k=k, s=s: e.tensor_copy(out=Wg[:, k, :], in_=stg[s][:, :]), reads=[b_stg[s]], writes=[b_Wg])
    for k in range(12):
        s = n % 2; n += 1
        P.dma("sp", lambda e, k=k, s=s: e.dma_start(out=stg[s][:, 0:D], in_=wbr[k]), writes=[b_stg[s]])
        P.op("pool", lambda e, k=k, s=s: e.tensor_copy(out=Wb[:, k, :], in_=stg[s][:, 0:D]), reads=[b_stg[s]], writes=[b_Wb])
    for k in range(8):
        s = n % 2; n += 1
        P.dma("sp", lambda e, k=k, s=s: e.dma_start(out=stg[s][:, 0:D], in_=wo[k]), writes=[b_stg[s]])
        P.op("pool", lambda e, k=k, s=s: e.tensor_copy(out=Wo[:, k, :], in_=stg[s][:, 0:D]), reads=[b_stg[s]], writes=[b_Wo])

    def rstd_of(src_ap, src_bufs, col):
        ss = st[:, col:col + 1]; bss = b_st[col]
        P.op("act", lambda e: e.activation(out=junk[:], in_=src_ap, func=AF.Square, scale=float(D ** -0.5), accum_out=ss), reads=src_bufs, writes=[b_junk, bss])
        P.op("dve", lambda e: e.tensor_scalar(out=ss, in0=ss, scalar1=EPS, scalar2=None, op0=ALU.add), reads=[bss], writes=[bss])
        P.op("act", lambda e: e.activation(out=ss, in_=ss, func=AF.Sqrt), reads=[bss], writes=[bss])
        P.op("dve", lambda e: e.reciprocal(out=ss, in_=ss), reads=[bss], writes=[bss])
        return ss, bss

    qn = 0
    for ti in range(NT):
        par = ti % 2
        P.dma("sp", lambda e, ti=ti, par=par: e.dma_start(out=xt[par][:], in_=x_in[ti * 128:(ti + 1) * 128, :]), writes=[b_xt[par]])
        P.dma("sp", lambda e, ti=ti, par=par: e.dma_start(out=ystg[par][:], in_=yT[ti]), writes=[b_ystg[par]])
        P.op("pool", lambda e, par=par: e.tensor_copy(out=ytb[par][:].rearrange("p a b -> p (a b)"), in_=ystg[par][:]), reads=[b_ystg[par]], writes=[b_ytb[par]])
        rs, brs = rstd_of(xt[par][:], [b_xt[par]], 0)
        P.op("dve", lambda e, par=par, rs=rs: e.scalar_tensor_tensor(out=hb[:], in0=xt[par][:], scalar=rs, in1=gpre_t[:], op0=ALU.mult, op1=ALU.mult), reads=[b_xt[par], brs, b_gpre], writes=[b_hb])
        for k in range(8):
            P.op("pe", lambda e, k=k: e.transpose(pT[:, k * 128:(k + 1) * 128], hb[:, k * 128:(k + 1) * 128], ident[:]), reads=[b_hb, b_id], writes=[b_pT])
        P.op("act", lambda e: e.copy(out=hTt[:].rearrange("p k t -> p (k t)"), in_=pT[:]), reads=[b_pT], writes=[b_hTt])
        for half in range(2):
            for nb in range(3):
                q = qn % 2; qn += 1
                c0 = nb * 1024 + half * 512
                for k in range(8):
                    P.op("pe", lambda e, k=k, q=q, c0=c0: e.matmul(pA[q][:], lhsT=hTt[:, k, :], rhs=Wg[:, k, c0:c0 + 512], start=(k == 0), stop=(k == 7)), reads=[b_hTt, b_Wg], writes=[b_pA[q]])
                P.op("act", lambda e, q=q: e.activation(out=sgt[q][:], in_=pA[q][:], func=AF.Sigmoid), reads=[b_pA[q]], writes=[b_sgt[q]])
                for kc in range(4):
                    P.op("pe", lambda e, kc=kc, q=q, nb=nb, half=half, par=par: e.matmul(pB[q][:], lhsT=ytb[par][:, nb * 4 + kc, :], rhs=Wb[:, nb * 4 + kc, half * 512:(half + 1) * 512], start=(kc == 0), stop=(kc == 3)), reads=[b_ytb[par], b_Wb], writes=[b_pB[q]])
                if nb == 0:
                    P.op("dve", lambda e, q=q, half=half: e.tensor_tensor(out=mg[:, half * 512:(half + 1) * 512], in0=sgt[q][:], in1=pB[q][:], op=ALU.mult), reads=[b_sgt[q], b_pB[q]], writes=[b_mg])
                else:
                    P.op("dve", lambda e, q=q: e.tensor_tensor(out=tmp[:], in0=sgt[q][:], in1=pB[q][:], op=ALU.mult), reads=[b_sgt[q], b_pB[q]], writes=[b_tmp])
                    P.op("dve", lambda e, half=half: e.tensor_tensor(out=mg[:, half * 512:(half + 1) * 512], in0=mg[:, half * 512:(half + 1) * 512], in1=tmp[:], op=ALU.add), reads=[b_mg, b_tmp], writes=[b_mg])
        P.op("act", lambda e: e.copy(out=mb[:], in_=mg[:]), reads=[b_mg], writes=[b_mb])
        for k in range(8):
            P.op("pe", lambda e, k=k: e.transpose(pT[:, k * 128:(k + 1) * 128], mb[:, k * 128:(k + 1) * 128], ident[:]), reads=[b_mb, b_id], writes=[b_pT])
        P.op("act", lambda e: e.copy(out=mT[:].rearrange("p k t -> p (k t)"), in_=pT[:]), reads=[b_pT], writes=[b_mT])
        for half in range(2):
            for k in range(8):
                P.op("pe", lambda e, k=k, half=half: e.matmul(pC[half][:], lhsT=mT[:, k, :], rhs=Wo[:, k, half * 512:(half + 1) * 512], start=(k == 0), stop=(k == 7)), reads=[b_mT, b_Wo], writes=[b_pC[half]])
            P.op("act", lambda e, half=half, par=par: e.copy(out=ot[par][:, half * 512:(half + 1) * 512], in_=pC[half][:]), reads=[b_pC[half]], writes=[b_ot[par]])
        rs, brs = rstd_of(ot[par][:], [b_ot[par]], 1)
        P.op("dve", lambda e, par=par, rs=rs: e.scalar_tensor_tensor(out=ot[par][:], in0=ot[par][:], scalar=rs, in1=gpost_t[:], op0=ALU.mult, op1=ALU.mult), reads=[b_ot[par], brs, b_gpost], writes=[b_ot[par]])
        P.op("dve", lambda e, par=par: e.tensor_tensor(out=ot[par][:], in0=ot[par][:], in1=xt[par][:], op=ALU.add), reads=[b_ot[par], b_xt[par]], writes=[b_ot[par]])
        P.dma("pool", lambda e, ti=ti, par=par: e.dma_start(out=x_out[ti * 128:(ti + 1) * 128, :], in_=ot[par][:]), reads=[b_ot[par]], sembuf=b_ot[par])
    P.emit(); P.close()
    return nc


def build_rwkv(L):
    SEG = min(L, 512); NSEG = L // SEG; NCH = SEG // 64
    nc = bass.Bass("TRN2", target_bir_lowering=False)
    P = ProgOld(nc); B = P.buf
    def din(name, shape): return nc.dram_tensor(name, list(shape), F32, kind="ExternalInput").ap()
    zr_d = din("zr", [3, 64, 2, L + 1]); zl_d = din("zl", [64, 2, L + 1]); zg_d = din("zg", [128, L + 1])
    mu3_d = din("mu3", [64, 3, 2]); mul_d = din("mul", [64, 2]); mug_d = din("mug", [128, 1])
    pp_d = din("pp", [64, 5, 2]); wup_d = din("wup", [64, 2, 64]); aup_d = din("aup", [64, 2, 64]); gup_d = din("gup", [128, 128])
    lnwb_d = din("lnwb", [64, 2, 128]); cmask_d = din("cmask", [64, 2 * SEG]); mask5_d = din("mask5", [64, 320])
    idn_d = din("idn", [64, 64])
    y_d = nc.dram_tensor("y", [L // 64, 64, 128], F32, kind="ExternalOutput").ap()
    def T(name, shape, dt=F32):
        return P.sb(name, shape, dt), B(name)
    raw3, b_raw3 = T("raw3", [64, 3, 2, SEG + 1]); rawl, b_rawl = T("rawl", [64, 2, SEG + 1]); rawg, b_rawg = T("rawg", [128, SEG + 1])
    mu3, b_mu3 = T("mu3t", [64, 3, 2]); mul, b_mul = T("mult", [64, 2]); mug, b_mug = T("mugt", [128, 1])
    pp, b_pp = T("ppt", [64, 5, 2]); wup, b_wup = T("wupt", [64, 2, 64]); aup, b_aup = T("aupt", [64, 2, 64]); gup, b_gup = T("gupt", [128, 128])
    lnwb, b_lnwb = T("lnwbt", [64, 2, 128]); cmask, b_cmask = T("cmaskt", [64, 2 * SEG]); mask5, b_mask5 = T("mask5t", [64, 320])
    idn, b_idn = T("idnt", [64, 64]); ones, b_ones = T("onest", [64, 64])
    d3, b_d3 = T("d3", [64, 3, 2, SEG]); dl, b_dl = T("dl", [64, 2, SEG]); dg, b_dg = T("dg", [128, SEG])
    x3, b_x3 = T("x3", [64, 3, 2, SEG]); xl, b_xl = T("xl", [64, 2, SEG]); xg, b_xg = T("xg", [128, SEG])
    tw, b_tw = T("tw", [64, SEG]); sgc, b_sgc = T("sgc", [128, SEG]); sgw, b_sgw = T("sgw", [64, 2, SEG]); aa, b_aa = T("aa", [64, 2, SEG])
    t1, b_t1 = T("t1", [64, 2, SEG]); sq, b_sq = T("sq", [64, 2, SEG]); rn, b_rn = T("rn", [64, 2, SEG]); kk, b_kk = T("kk", [64, 2, SEG])
    kp, b_kp = T("kp", [64, 2, SEG]); bb, b_bb = T("bb", [64, 2, SEG]); cs, b_cs = T("cs", [64, 2, SEG])
    epos, b_epos = T("epos", [64, 2, SEG]); eneg, b_eneg = T("eneg", [64, 2, SEG]); eprev, b_eprev = T("eprev", [64, 2, SEG])
    AR, b_AR = T("AR", [64, 2, NCH, 2, 64]); Bt, b_Bt = T("Bt", [64, 2, SEG]); Kt, b_Kt = T("Kt", [64, 2, SEG]); rkr, b_rkr = T("rkr", [64, 2, SEG])
    Hs = [T(f"H{i}", [64, 2, 64]) for i in range(2)]
    Msb = [T(f"Msb{h}", [64, 320]) for h in range(2)]
    TK, b_TK = T("TK", [64, 6, 64])
    PPs = [T(f"PP{i}", [64, 2, 2, 64]) for i in range(2)]
    Xs = [T(f"X{i}", [64, 2, 64]) for i in range(2)]
    Wsb, b_Wsb = T("Wsb", [64, 128]); Usb, b_Usb = T("Usb", [64, 128]); Ysb, b_Ysb = T("Ysb", [64, 128]); yc, b_yc = T("yc", [64, 128])
    outs = [T(f"out{i}", [64, 128]) for i in range(2)]
    sm, _ = T("sm", [64, 16]); b_sm = [B() for _ in range(16)]
    junk, b_junk = T("junk", [64, 64])
    def PS(name, shape): return P.ps(name, shape), B(name)
    pM = [PS(f"pM{h}", [64, 512]) for h in range(2)]
    pK, b_pK = PS("pK", [64, 512]); pI, b_pI = PS("pI", [64, 512]); pX, b_pX = PS("pX", [64, 512])
    pW, b_pW = PS("pW", [64, 512]); pY, b_pY = PS("pY", [64, 512]); pH, b_pH = PS("pH", [64, 512])

    for (t, b, d) in [(mu3, b_mu3, mu3_d), (mul, b_mul, mul_d), (mug, b_mug, mug_d), (pp, b_pp, pp_d), (wup, b_wup, wup_d), (aup, b_aup, aup_d),
                      (gup, b_gup, gup_d), (lnwb, b_lnwb, lnwb_d), (cmask, b_cmask, cmask_d), (mask5, b_mask5, mask5_d), (idn, b_idn, idn_d)]:
        P.dma("sp", lambda e, t=t, d=d: e.dma_start(out=t[:], in_=d), writes=[b])
    P.op("dve", lambda e: e.memset(ones[:], 1.0), writes=[b_ones])
    P.op("dve", lambda e: e.memset(Hs[0][0][:], 0.0), writes=[Hs[0][1]])
    hcur = 0
    NEG = -0.6065306597126334
    oi = 0
    for sg_ in range(NSEG):
        s0 = sg_ * SEG
        for a in range(3):
            P.dma("sp", lambda e, s0=s0, a=a: e.dma_start(out=raw3[:, a, :, :], in_=zr_d[a, :, :, s0:s0 + SEG + 1]), writes=[b_raw3])
        P.dma("sp", lambda e, s0=s0: e.dma_start(out=rawl[:], in_=zl_d[:, :, s0:s0 + SEG + 1]), writes=[b_rawl])
        P.dma("sp", lambda e, s0=s0: e.dma_start(out=rawg[:], in_=zg_d[:, s0:s0 + SEG + 1]), writes=[b_rawg])
        P.op("dve", lambda e: e.tensor_tensor(out=d3[:], in0=raw3[:, :, :, 0:SEG], in1=raw3[:, :, :, 1:SEG + 1], op=ALU.subtract), reads=[b_raw3], writes=[b_d3])
        P.op("dve", lambda e: e.tensor_tensor(out=dl[:], in0=rawl[:, :, 0:SEG], in1=rawl[:, :, 1:SEG + 1], op=ALU.subtract), reads=[b_rawl], writes=[b_dl])
        P.op("dve", lambda e: e.tensor_tensor(out=dg[:], in0=rawg[:, 0:SEG], in1=rawg[:, 1:SEG + 1], op=ALU.subtract), reads=[b_rawg], writes=[b_dg])
        for a in range(3):
            for h in range(2):
                P.op("dve", lambda e, a=a, h=h: e.scalar_tensor_tensor(out=x3[:, a, h, :], in0=d3[:, a, h, :], scalar=mu3[:, a, h:h + 1], in1=raw3[:, a, h, 1:SEG + 1], op0=ALU.mult, op1=ALU.add),
                     reads=[b_d3, b_mu3, b_raw3], writes=[b_x3])
        for a in range(2):
            P.op("dve", lambda e, a=a: e.scalar_tensor_tensor(out=xl[:, a, :], in0=dl[:, a, :], scalar=mul[:, a:a + 1], in1=rawl[:, a, 1:SEG + 1], op0=ALU.mult, op1=ALU.add),
                 reads=[b_dl, b_mul, b_rawl], writes=[b_xl])
        P.op("dve", lambda e: e.scalar_tensor_tensor(out=xg[:], in0=dg[:], scalar=mug[:, 0:1], in1=rawg[:, 1:SEG + 1], op0=ALU.mult, op1=ALU.add), reads=[b_dg, b_mug, b_rawg], writes=[b_xg])
        XR = lambda h: x3[:, 0, h, :]
        XK = lambda h: x3[:, 1, h, :]
        XV = lambda h: x3[:, 2, h, :]
        P.op("act", lambda e: e.activation(out=tw[:], in_=xl[:, 0, :], func=AF.Tanh), reads=[b_xl], writes=[b_tw])
        P.op("act", lambda e: e.activation(out=sgc[:], in_=xg[:], func=AF.Sigmoid), reads=[b_xg], writes=[b_sgc])
        for h in range(2):
            P.op("pe", lambda e, h=h: e.matmul(pK[:, 0:SEG], lhsT=wup[:, h, :], rhs=tw[:], start=True, stop=True), reads=[b_wup, b_tw], writes=[b_pK])
            P.op("act", lambda e, h=h: e.activation(out=sgw[:, h, :], in_=pK[:, 0:SEG], func=AF.Sigmoid, bias=pp[:, 0, h:h + 1]), reads=[b_pK, b_pp], writes=[b_sgw])
            P.op("pe", lambda e, h=h: e.matmul(pK[:, 0:SEG], lhsT=aup[:, h, :], rhs=xl[:, 1, :], start=True, stop=True), reads=[b_aup, b_xl], writes=[b_pK])
            P.op("act", lambda e, h=h: e.activation(out=aa[:, h, :], in_=pK[:, 0:SEG], func=AF.Sigmoid, bias=pp[:, 1, h:h + 1]), reads=[b_pK, b_pp], writes=[b_aa])
        for h in range(2):
            P.op("dve", lambda e, h=h: e.tensor_scalar(out=t1[:, h, :], in0=XK(h), scalar1=pp[:, 2, h:h + 1], scalar2=None, op0=ALU.mult), reads=[b_x3, b_pp], writes=[b_t1])
        P.op("dve", lambda e: e.tensor_tensor(out=sq[:], in0=t1[:], in1=t1[:], op=ALU.mult), reads=[b_t1], writes=[b_sq])
        for h in range(2):
            P.op("pe", lambda e, h=h: e.matmul(pK[:, 0:SEG], lhsT=ones[:], rhs=sq[:, h, :], start=True, stop=True), reads=[b_ones, b_sq], writes=[b_pK])
            P.op("dve", lambda e, h=h: e.tensor_scalar(out=rn[:, h, :], in0=pK[:, 0:SEG], scalar1=1e-24, scalar2=None, op0=ALU.max), reads=[b_pK], writes=[b_rn])
        P.op("act", lambda e: e.activation(out=rn[:], in_=rn[:], func=AF.Sqrt), reads=[b_rn], writes=[b_rn])
        P.op("dve", lambda e: e.reciprocal(out=rn[:], in_=rn[:]), reads=[b_rn], writes=[b_rn])
        P.op("dve", lambda e: e.tensor_tensor(out=kk[:], in0=t1[:], in1=rn[:], op=ALU.mult), reads=[b_t1, b_rn], writes=[b_kk])
        for h in range(2):
            P.op("dve", lambda e, h=h: e.tensor_scalar(out=kp[:, h, :], in0=aa[:, h, :], scalar1=pp[:, 3, h:h + 1], scalar2=pp[:, 3, h:h + 1], op0=ALU.mult, op1=ALU.subtract), reads=[b_aa, b_pp], writes=[b_kp])
            P.op("dve", lambda e, h=h: e.scalar_tensor_tensor(out=kp[:, h, :], in0=kp[:, h, :], scalar=1.0, in1=XK(h), op0=ALU.add, op1=ALU.mult), reads=[b_kp, b_x3], writes=[b_kp])
        P.op("dve", lambda e: e.tensor_tensor(out=bb[:], in0=kk[:], in1=aa[:], op=ALU.mult), reads=[b_kk, b_aa], writes=[b_bb])
        FL = lambda t: t[:].rearrange("p h s -> p (h s)")
        P.op("dve", lambda e: e.tensor_tensor_scan(out=FL(cs), data0=cmask[:], data1=FL(sgw), initial=0.0, op0=ALU.mult, op1=ALU.add), reads=[b_cmask, b_sgw], writes=[b_cs])
        P.op("act", lambda e: e.activation(out=epos[:], in_=cs[:], func=AF.Exp, scale=NEG), reads=[b_cs], writes=[b_epos])
        P.op("act", lambda e: e.activation(out=eneg[:], in_=cs[:], func=AF.Exp, scale=-NEG), reads=[b_cs], writes=[b_eneg])
        P.op("dve", lambda e: e.tensor_tensor(out=eprev[:], in0=cs[:], in1=sgw[:], op=ALU.subtract), reads=[b_cs, b_sgw], writes=[b_eprev])
        P.op("act", lambda e: e.activation(out=eprev[:], in_=eprev[:], func=AF.Exp, scale=NEG), reads=[b_eprev], writes=[b_eprev])
        for h in range(2):
            P.op("dve", lambda e, h=h: e.scalar_tensor_tensor(out=AR[:, h, :, 0, :], in0=kk[:, h, :].rearrange("p (c t) -> p c t", t=64), scalar=-1.0, in1=eprev[:, h, :].rearrange("p (c t) -> p c t", t=64), op0=ALU.mult, op1=ALU.mult),
                 reads=[b_kk, b_eprev], writes=[b_AR])
            P.op("dve", lambda e, h=h: e.tensor_tensor(out=AR[:, h, :, 1, :], in0=XR(h).rearrange("p (c t) -> p c t", t=64), in1=epos[:, h, :].rearrange("p (c t) -> p c t", t=64), op=ALU.mult),
                 reads=[b_x3, b_epos], writes=[b_AR])
            P.op("dve", lambda e, h=h: e.scalar_tensor_tensor(out=rkr[:, h, :], in0=XR(h), scalar=pp[:, 4, h:h + 1], in1=kp[:, h, :], op0=ALU.mult, op1=ALU.mult), reads=[b_x3, b_pp, b_kp], writes=[b_rkr])
        P.op("dve", lambda e: e.tensor_tensor(out=Bt[:], in0=bb[:], in1=eneg[:], op=ALU.mult), reads=[b_bb, b_eneg], writes=[b_Bt])
        P.op("dve", lambda e: e.tensor_tensor(out=Kt[:], in0=kp[:], in1=eneg[:], op=ALU.mult), reads=[b_kp, b_eneg], writes=[b_Kt])
        for c in range(NCH):
            cs_ = slice(c * 64, (c + 1) * 64)
            for h in range(2):
                pm, bpm = pM[h]
                P.op("pe", lambda e, h=h, c=c, pm=pm, cs_=cs_: e.matmul(pm[:, 0:64], lhsT=AR[:, h, c, 0, :], rhs=Bt[:, h, cs_], start=True, stop=True), reads=[b_AR, b_Bt], writes=[bpm])
                P.op("pe", lambda e, h=h, c=c, pm=pm, cs_=cs_: e.matmul(pm[:, 64:192], lhsT=Bt[:, h, cs_], rhs=AR[:, h, c, :, :].rearrange("p a t -> p (a t)"), start=True, stop=True), reads=[b_AR, b_Bt], writes=[bpm])
                P.op("pe", lambda e, h=h, c=c, pm=pm, cs_=cs_: e.matmul(pm[:, 192:320], lhsT=Kt[:, h, cs_], rhs=AR[:, h, c, :, :].rearrange("p a t -> p (a t)"), start=True, stop=True), reads=[b_AR, b_Kt], writes=[bpm])
                P.op("dve", lambda e, h=h, pm=pm: e.tensor_tensor(out=Msb[h][0][:], in0=pm[:, 0:320], in1=mask5[:], op=ALU.mult), reads=[bpm, b_mask5], writes=[Msb[h][1]])
            for h in range(2):
                for a, (src, bsrc) in enumerate([(Bt[:, h, cs_], b_Bt), (Kt[:, h, cs_], b_Kt), (x3[:, 2, h, cs_], b_x3)]):
                    P.op("pe", lambda e, h=h, a=a, src=src: e.transpose(pK[:, (h * 3 + a) * 64:(h * 3 + a + 1) * 64], src, idn[:]), reads=[bsrc, b_idn], writes=[b_pK])
            P.op("act", lambda e: e.copy(out=TK[:].rearrange("p a t -> p (a t)"), in_=pK[:, 0:384]), reads=[b_pK], writes=[b_TK])
            pcur = 0; xcur = 0
            for h in range(2):
                P.op("act", lambda e, h=h: e.copy(out=PPs[0][0][:, h, :, :].rearrange("p a t -> p (a t)"), in_=Msb[h][0][:, 0:128]), reads=[Msb[h][1]], writes=[PPs[0][1]])
                P.op("dve", lambda e, h=h: e.tensor_tensor(out=Xs[0][0][:, h, :], in0=Msb[h][0][:, 64:128], in1=idn[:], op=ALU.add), reads=[Msb[h][1], b_idn], writes=[Xs[0][1]])
            for stp in range(5):
                pp_t, pp_b = PPs[pcur]; pn_t, pn_b = PPs[1 - pcur]
                x_t, x_b = Xs[xcur]; xn_t, xn_b = Xs[1 - xcur]
                for h in range(2):
                    P.op("pe", lambda e, h=h, pp_t=pp_t: e.matmul(pI[:, (h * 2) * 64:(h * 2 + 1) * 64], lhsT=pp_t[:, h, 1, :], rhs=pp_t[:, h, 0, :], start=True, stop=True), reads=[pp_b], writes=[b_pI])
                    P.op("pe", lambda e, h=h, pp_t=pp_t: e.matmul(pI[:, (h * 2 + 1) * 64:(h * 2 + 2) * 64], lhsT=pp_t[:, h, 0, :], rhs=pp_t[:, h, 1, :], start=True, stop=True), reads=[pp_b], writes=[b_pI])
                P.op("act", lambda e, pn_t=pn_t: e.copy(out=pn_t[:].rearrange("p h a t -> p (h a t)"), in_=pI[:, 0:256]), reads=[b_pI], writes=[pn_b])
                for h in range(2):
                    P.op("pe", lambda e, h=h, x_t=x_t: e.matmul(pX[:, h * 64:(h + 1) * 64], lhsT=idn[:], rhs=x_t[:, h, :], start=True, stop=False), reads=[b_idn, x_b], writes=[b_pX])
                    P.op("pe", lambda e, h=h, x_t=x_t, pn_t=pn_t: e.matmul(pX[:, h * 64:(h + 1) * 64], lhsT=pn_t[:, h, 0, :], rhs=x_t[:, h, :], start=False, stop=True), reads=[pn_b, x_b], writes=[b_pX])
                P.op("dve", lambda e, xn_t=xn_t: e.tensor_copy(out=xn_t[:].rearrange("p h t -> p (h t)"), in_=pX[:, 0:128]), reads=[b_pX], writes=[xn_b])
                pcur = 1 - pcur; xcur = 1 - xcur
            X_t, X_b = Xs[xcur]
            H_t, H_b = Hs[hcur]; Hn_t, Hn_b = Hs[1 - hcur]
            for h in range(2):
                P.op("pe", lambda e, h=h, c=c, H_t=H_t: e.matmul(pW[:, h * 64:(h + 1) * 64], lhsT=AR[:, h, c, 0, :], rhs=H_t[:, h, :], start=True, stop=False), reads=[b_AR, H_b], writes=[b_pW])
                P.op("pe", lambda e, h=h: e.matmul(pW[:, h * 64:(h + 1) * 64], lhsT=Msb[h][0][:, 192:256], rhs=TK[:, h * 3 + 2, :], start=False, stop=True), reads=[Msb[h][1], b_TK], writes=[b_pW])
            P.op("act", lambda e: e.copy(out=Wsb[:], in_=pW[:, 0:128]), reads=[b_pW], writes=[b_Wsb])
            for h in range(2):
                P.op("pe", lambda e, h=h, X_t=X_t: e.matmul(pW[:, 128 + h * 64:128 + (h + 1) * 64], lhsT=X_t[:, h, :], rhs=Wsb[:, h * 64:(h + 1) * 64], start=True, stop=True), reads=[X_b, b_Wsb], writes=[b_pW])
            P.op("dve", lambda e: e.tensor_copy(out=Usb[:], in_=pW[:, 128:256]), reads=[b_pW], writes=[b_Usb])
            for h in range(2):
                hs = slice(h * 64, (h + 1) * 64)
                P.op("pe", lambda e, h=h, c=c, hs=hs, H_t=H_t: e.matmul(pY[:, hs], lhsT=AR[:, h, c, 1, :], rhs=H_t[:, h, :], start=True, stop=False), reads=[b_AR, H_b], writes=[b_pY])
                P.op("pe", lambda e, h=h, hs=hs: e.matmul(pY[:, hs], lhsT=Msb[h][0][:, 128:192], rhs=Usb[:, hs], start=False, stop=False), reads=[Msb[h][1], b_Usb], writes=[b_pY])
                P.op("pe", lambda e, h=h, hs=hs: e.matmul(pY[:, hs], lhsT=Msb[h][0][:, 256:320], rhs=TK[:, h * 3 + 2, :], start=False, stop=True), reads=[Msb[h][1], b_TK], writes=[b_pY])
            P.op("pe", lambda e, cs_=cs_: e.matmul(pY[:, 128:256], lhsT=sgc[:, cs_], rhs=gup[:], start=True, stop=True), reads=[b_sgc, b_gup], writes=[b_pY])
            for h in range(2):
                P.op("pe", lambda e, h=h, cs_=cs_: e.matmul(pY[:, 256 + 2 * h:258 + 2 * h], lhsT=rkr[:, h, cs_], rhs=ones[:, 0:2], start=True, stop=True), reads=[b_rkr, b_ones], writes=[b_pY])
            for h in range(2):
                hs = slice(h * 64, (h + 1) * 64)
                P.op("pe", lambda e, h=h, hs=hs, H_t=H_t: e.matmul(pH[:, hs], lhsT=idn[:], rhs=H_t[:, h, :], start=True, stop=False), reads=[b_idn, H_b], writes=[b_pH])
                P.op("pe", lambda e, h=h, hs=hs: e.matmul(pH[:, hs], lhsT=TK[:, h * 3 + 0, :], rhs=Usb[:, hs], start=False, stop=False), reads=[b_TK, b_Usb], writes=[b_pH])
                P.op("pe", lambda e, h=h, hs=hs: e.matmul(pH[:, hs], lhsT=TK[:, h * 3 + 1, :], rhs=TK[:, h * 3 + 2, :], start=False, stop=True), reads=[b_TK], writes=[b_pH])
            for h in range(2):
                ce = c * 64 + 63
                P.op("dve", lambda e, h=h, ce=ce, Hn_t=Hn_t: e.tensor_scalar(out=Hn_t[:, h, :], in0=pH[:, h * 64:(h + 1) * 64], scalar1=epos[:, h, ce:ce + 1], scalar2=None, op0=ALU.mult), reads=[b_pH, b_epos], writes=[Hn_b])
            hcur = 1 - hcur
            P.op("act", lambda e: e.copy(out=Ysb[:], in_=pY[:, 0:128]), reads=[b_pY], writes=[b_Ysb])
            P.op("dve", lambda e: e.tensor_copy(out=sm[:, 8:12], in_=pY[:, 256:260]), reads=[b_pY], writes=[b_sm[8]])
            o_t, o_b = outs[oi % 2]; oi += 1
            for h in range(2):
                hs = slice(h * 64, (h + 1) * 64)
                mcol = sm[:, h:h + 1]; vcol = sm[:, 2 + h:3 + h]
                P.op("dve", lambda e, hs=hs, mcol=mcol: e.reduce_sum(out=mcol, in_=Ysb[:, hs], axis=AX.X), reads=[b_Ysb], writes=[b_sm[h]])
                P.op("dve", lambda e, mcol=mcol: e.tensor_scalar(out=mcol, in0=mcol, scalar1=-1.0 / 64, scalar2=None, op0=ALU.mult), reads=[b_sm[h]], writes=[b_sm[h]])
                P.op("dve", lambda e, hs=hs, mcol=mcol: e.tensor_scalar(out=yc[:, hs], in0=Ysb[:, hs], scalar1=mcol, scalar2=None, op0=ALU.add), reads=[b_Ysb, b_sm[h]], writes=[b_yc])
                P.op("act", lambda e, hs=hs, vcol=vcol: e.activation(out=junk[:], in_=yc[:, hs], func=AF.Square, scale=0.125, accum_out=vcol), reads=[b_yc], writes=[b_junk, b_sm[2 + h]])
                P.op("dve", lambda e, vcol=vcol: e.tensor_scalar(out=vcol, in0=vcol, scalar1=64e-5, scalar2=None, op0=ALU.add), reads=[b_sm[2 + h]], writes=[b_sm[2 + h]])
                P.op("act", lambda e, vcol=vcol: e.activation(out=vcol, in_=vcol, func=AF.Sqrt), reads=[b_sm[2 + h]], writes=[b_sm[2 + h]])
                P.op("dve", lambda e, vcol=vcol: e.reciprocal(out=vcol, in_=vcol), reads=[b_sm[2 + h]], writes=[b_sm[2 + h]])
                P.op("dve", lambda e, hs=hs, vcol=vcol: e.scalar_tensor_tensor(out=yc[:, hs], in0=yc[:, hs], scalar=vcol, in1=lnwb[:, 0, hs], op0=ALU.mult, op1=ALU.mult), reads=[b_yc, b_sm[2 + h], b_lnwb], writes=[b_yc])
                P.op("dve", lambda e, hs=hs: e.tensor_tensor(out=yc[:, hs], in0=yc[:, hs], in1=lnwb[:, 1, hs], op=ALU.add), reads=[b_yc, b_lnwb], writes=[b_yc])
                P.op("dve", lambda e, hs=hs, h=h: e.scalar_tensor_tensor(out=yc[:, hs], in0=TK[:, h * 3 + 2, :], scalar=sm[:, 8 + 2 * h:9 + 2 * h], in1=yc[:, hs], op0=ALU.mult, op1=ALU.add), reads=[b_TK, b_sm[8], b_yc], writes=[b_yc])
            P.op("dve", lambda e, o_t=o_t: e.tensor_tensor(out=o_t[:], in0=yc[:], in1=pY[:, 128:256], op=ALU.mult), reads=[b_yc, b_pY], writes=[o_b])
            gci = sg_ * NCH + c
            P.dma("pool", lambda e, gci=gci, o_t=o_t: e.dma_start(out=y_d[gci], in_=o_t[:]), reads=[o_b], sembuf=o_b)
    P.emit(); P.close()
    return nc


F32 = mybir.dt.float32
BF16 = mybir.dt.bfloat16
I32 = mybir.dt.int32
AF = mybir.ActivationFunctionType
ALU = mybir.AluOpType
AX = mybir.AxisListType


class Buf:
    __slots__ = ("name", "w", "r", "semidx", "cnt")

    def __init__(self, name):
        self.name = name
        self.w = None
        self.r = {}
        self.semidx = None
        self.cnt = 0


class Prog:
    ENG = ("pe", "act", "dve", "pool", "sp")

    def __init__(self, nc):
        self.nc = nc
        self.g = ExitStack()
        self.esem = {e: self.g.enter_context(nc.semaphore(f"s_{e}")) for e in self.ENG}
        self.cnt = {e: 0 for e in self.ENG}
        self.seen = {e: {} for e in self.ENG}
        self.dsem = []
        self.dfree = []
        self.ops = None
        self.ph = None
        self.live = []
        self.nbuf = 0
        self.nph = 0
        self.jreg = None

    def sb(self, name, shape, dt=F32):
        return self.ph.enter_context(self.nc.sbuf_tensor(f"{name}_p{self.nph}", list(shape), dt))

    def ps(self, name, shape, dt=F32):
        return self.ph.enter_context(self.nc.psum_tensor(f"{name}_p{self.nph}", list(shape), dt))

    def buf(self, name=None):
        self.nbuf += 1
        return Buf(name or f"b{self.nbuf}")

    def _barrier_waits(self, eng):
        need = [(e2, self.cnt[e2]) for e2 in self.ENG if self.cnt[e2] > 0]
        need += [(("d", i), c) for i, (h, c) in enumerate(self.dsem) if c > 0]
        out = []
        seen = self.seen[eng]
        for k, v in need:
            if seen.get(k, 0) >= v:
                continue
            seen[k] = v
            out.append((k, v))
        return out

    def begin(self):
        self.nph += 1
        self.ph = ExitStack()
        self.ops = {e: [] for e in self.ENG}
        self.live = []
        for e in self.ENG:
            self.ops[e].append((self._barrier_waits(e), None, None))

    def _waits(self, eng, reads, writes):
        need = {}

        def add(k, v):
            if need.get(k, 0) < v:
                need[k] = v
        for b in reads:
            if b.w is not None:
                add(*b.w)
        for b in writes:
            if b.w is not None:
                add(*b.w)
            for k, v in b.r.items():
                add(k, v)
        out = []
        seen = self.seen[eng]
        for k, v in need.items():
            if k == "pe" and eng == "pe":
                continue
            if seen.get(k, 0) >= v:
                continue
            seen[k] = v
            out.append((k, v))
        return out

    def _mark(self, tok, reads, writes):
        k, v = tok
        for b in reads:
            if b.r.get(k, 0) < v:
                b.r[k] = v
        for b in writes:
            b.w = tok
            b.r = {}

    def op(self, eng, fn, reads=(), writes=()):
        waits = self._waits(eng, reads, writes)
        self.cnt[eng] += 1
        tok = (eng, self.cnt[eng])
        self._mark(tok, reads, writes)
        self.ops[eng].append((waits, fn, (eng, 1)))

    def dma(self, q, fn, reads=(), writes=(), sembuf=None, inc=16):
        waits = self._waits(q, reads, writes)
        sbf = sembuf if sembuf is not None else (writes[0] if writes else reads[0])
        if sbf.semidx is None:
            if self.dfree:
                sbf.semidx = self.dfree.pop()
            else:
                h = self.g.enter_context(self.nc.semaphore(f"d{len(self.dsem)}"))
                self.dsem.append([h, 0])
                sbf.semidx = len(self.dsem) - 1
            sbf.cnt = self.dsem[sbf.semidx][1]
            self.live.append(sbf)
        sbf.cnt += inc
        self.dsem[sbf.semidx][1] = sbf.cnt
        key = ("d", sbf.semidx)
        tok = (key, sbf.cnt)
        self._mark(tok, reads, writes)
        self.ops[q].append((waits, fn, (key, inc)))

    def _semof(self, k):
        return self.esem[k] if isinstance(k, str) else self.dsem[k[1]][0]

    def end(self):
        nc = self.nc
        ops = self.ops
        with nc.Block() as block:
            def run(engobj, lst):
                for waits, fn, inc in lst:
                    for k, v in waits:
                        engobj.wait_ge(self._semof(k), v)
                    if fn is None:
                        continue
                    ins = fn(engobj)
                    if inc is not None:
                        ins.then_inc(self._semof(inc[0]), inc[1])

            @block.tensor
            def _(e):
                run(e, ops["pe"])

            @block.scalar
            def _(e):
                run(e, ops["act"])

            @block.vector
            def _(e):
                run(e, ops["dve"])

            @block.gpsimd
            def _(e):
                run(e, ops["pool"])

            @block.sync
            def _(e):
                run(e, ops["sp"])
        for b in self.live:
            self.dfree.append(b.semidx)
            b.semidx = None
        self.live = []
        self.ph.close()
        self.ph = None

    def finish(self):
        self.begin()
        self.end()
        self.g.close()


D = 1024; DFF = 2816; NFF = 22; EPS = 1e-6
NWIN = 43
ZR = NWIN * 128
JB = 1152
CB = 4 * JB


def emit_tok(P, TOK, xsrc, xdst, W, zdst=None):
    NT = TOK // 128
    HALF = min(1024, TOK); NH = TOK // HALF; BLK = min(512, HALF); NB = HALF // BLK; TPH = HALF // 128
    has_win = zdst is not None
    P.begin()
    B = P.buf
    g_pre, g_post, wg, wu, wd, idn_d = W["g_pre"], W["g_post"], W["wg"], W["wu"], W["wd"], W["idn"]
    ident_f = P.sb("ident_f", [128, 128]); ident = P.sb("ident", [128, 128], BF16)
    gpre_t = P.sb("gpre_t", [128, D]); gpost_t = P.sb("gpost_t", [128, D])
    hT = P.sb("hT", [128, 8, TOK], BF16)
    AT = P.sb("AT", [128, NFF, HALF], BF16)
    Wd = P.sb("Wd", [128, NFF, D], BF16)
    stg = [P.sb(f"stg{i}", [128, 2048]) for i in range(2)]
    wgb = [P.sb(f"wgb{i}", [128, 8, 128], BF16) for i in range(2)]
    wub = [P.sb(f"wub{i}", [128, 8, 128], BF16) for i in range(2)]
    xt = [P.sb(f"xt{i}", [128, D]) for i in range(2)]
    ot = [P.sb(f"ot{i}", [128, D]) for i in range(2)]
    junk = P.sb("junk", [128, D])
    hb = [P.sb(f"hb{i}", [128, D], BF16) for i in range(2)]
    sg = [P.sb(f"sg{i}", [128, 512]) for i in range(2)]
    st = P.sb("st", [128, 64])
    pA = [P.ps(f"pA{i}", [128, 512]) for i in range(2)]
    pB = [P.ps(f"pB{i}", [128, 512]) for i in range(2)]
    pC = [P.ps(f"pC{i}", [128, 512]) for i in range(2)]
    pT = [P.ps(f"pT{i}", [128, 1024], BF16) for i in range(2)]
    b_ident = B(); b_identf = B(); b_gpre = B(); b_gpost = B(); b_hT = [B() for _ in range(NT)]
    b_AT = [[B() for _ in range(NB)] for _ in range(NFF)]
    b_Wd = [B() for _ in range(NFF)]
    b_stg = [B(), B()]; b_wgb = [B(), B()]; b_wub = [B(), B()]; b_xt = [B(), B()]; b_ot = [B(), B()]
    b_junk = B(); b_hb = [B(), B()]; b_sg = [B(), B()]
    b_pA = [B(), B()]; b_pB = [B(), B()]; b_pC = [B(), B()]; b_pT = [B(), B()]
    st_next = [0]; b_st = {}

    def stcol():
        i = st_next[0] % 64; st_next[0] += 1
        if i not in b_st: b_st[i] = B()
        return st[:, i:i + 1], b_st[i]

    P.dma("sp", lambda e: e.dma_start(out=ident_f[:], in_=idn_d), writes=[b_identf])
    P.op("dve", lambda e: e.tensor_copy(out=ident[:], in_=ident_f[:]), reads=[b_identf], writes=[b_ident])
    P.dma("sp", lambda e: e.dma_start(out=gpre_t[:], in_=g_pre), writes=[b_gpre])
    P.dma("sp", lambda e: e.dma_start(out=gpost_t[:], in_=g_post), writes=[b_gpost])

    def rstd_of(src_ap, src_bufs):
        ss, bss = stcol(); rs, brs = stcol()
        P.op("act", lambda e: e.activation(out=junk[:], in_=src_ap, func=AF.Square, scale=float(D ** -0.5), accum_out=ss), reads=src_bufs, writes=[b_junk, bss])
        P.op("dve", lambda e: e.tensor_scalar(out=rs, in0=ss, scalar1=EPS, scalar2=None, op0=ALU.add), reads=[bss], writes=[brs])
        P.op("act", lambda e: e.activation(out=rs, in_=rs, func=AF.Sqrt), reads=[brs], writes=[brs])
        P.op("dve", lambda e: e.reciprocal(out=rs, in_=rs), reads=[brs], writes=[brs])
        return rs, brs

    def norm_to_hT(src_ap, src_bufs, g_t, b_g, ti, par):
        rs, brs = rstd_of(src_ap, src_bufs)
        P.op("dve", lambda e: e.scalar_tensor_tensor(out=hb[par][:], in0=src_ap, scalar=rs, in1=g_t[:], op0=ALU.mult, op1=ALU.mult), reads=src_bufs + [brs, b_g], writes=[b_hb[par]])
        for k in range(8):
            P.op("pe", lambda e, k=k: e.transpose(pT[par][:, k * 128:(k + 1) * 128], hb[par][:, k * 128:(k + 1) * 128], ident[:]), reads=[b_hb[par], b_ident], writes=[b_pT[par]])
        P.op("act", lambda e: e.copy(out=hT[:, :, ti * 128:(ti + 1) * 128], in_=pT[par][:].rearrange("p (k t) -> p k t", k=8)), reads=[b_pT[par]], writes=[b_hT[ti]])

    for ti in range(NT):
        par = ti % 2
        P.dma("sp", lambda e, ti=ti, par=par: e.dma_start(out=xt[par][:], in_=xsrc[ti * 128:(ti + 1) * 128, :]), writes=[b_xt[par]])
        norm_to_hT(xt[par][:], [b_xt[par]], gpre_t, b_gpre, ti, par)
    for c in range(NFF):
        s = c % 2
        P.dma("sp", lambda e, c=c, s=s: e.dma_start(out=stg[s][:, 0:D], in_=wd[c]), writes=[b_stg[s]])
        P.op("pool", lambda e, c=c, s=s: e.tensor_copy(out=Wd[:, c, :], in_=stg[s][:, 0:D]), reads=[b_stg[s]], writes=[b_Wd[c]])
    if has_win:
        gmix_t = P.sb("gmix_t", [128, D]); b_gmix = B()
        P.dma("sp", lambda e: e.dma_start(out=gmix_t[:], in_=W["g_mix"]), writes=[b_gmix])
    for half in range(NH):
        for c in range(NFF):
            s = c % 2
            P.dma("sp", lambda e, c=c, s=s: e.dma_start(out=stg[s][:, 0:1024], in_=wg[c].rearrange("p k f -> p (k f)")), writes=[b_stg[s]])
            P.op("pool", lambda e, s=s: e.tensor_copy(out=wgb[s][:].rearrange("p k f -> p (k f)"), in_=stg[s][:, 0:1024]), reads=[b_stg[s]], writes=[b_wgb[s]])
            P.dma("sp", lambda e, c=c, s=s: e.dma_start(out=stg[s][:, 1024:2048], in_=wu[c].rearrange("p k f -> p (k f)")), writes=[b_stg[s]])
            P.op("pool", lambda e, s=s: e.tensor_copy(out=wub[s][:].rearrange("p k f -> p (k f)"), in_=stg[s][:, 1024:2048]), reads=[b_stg[s]], writes=[b_wub[s]])
            for tb in range(NB):
                t0 = half * HALF + tb * BLK
                rd = [b_hT[(t0 // 128) + i] for i in range(BLK // 128)]
                q = tb % 2
                for k in range(8):
                    P.op("pe", lambda e, k=k, s=s, q=q, t0=t0: e.matmul(pA[q][:, 0:BLK], lhsT=wgb[s][:, k, :], rhs=hT[:, k, t0:t0 + BLK], start=(k == 0), stop=(k == 7)), reads=[b_wgb[s]] + rd, writes=[b_pA[q]])
                for k in range(8):
                    P.op("pe", lambda e, k=k, s=s, q=q, t0=t0: e.matmul(pB[q][:, 0:BLK], lhsT=wub[s][:, k, :], rhs=hT[:, k, t0:t0 + BLK], start=(k == 0), stop=(k == 7)), reads=[b_wub[s]] + rd, writes=[b_pB[q]])
                P.op("act", lambda e, q=q: e.activation(out=sg[q][:, 0:BLK], in_=pA[q][:, 0:BLK], func=AF.Silu), reads=[b_pA[q]], writes=[b_sg[q]])
                P.op("dve", lambda e, q=q, c=c, tb=tb: e.tensor_tensor(out=AT[:, c, tb * BLK:(tb + 1) * BLK], in0=sg[q][:, 0:BLK], in1=pB[q][:, 0:BLK], op=ALU.mult), reads=[b_sg[q], b_pB[q]], writes=[b_AT[c][tb]])
        for tl in range(TPH):
            ti = half * TPH + tl; par = ti % 2
            P.dma("sp", lambda e, ti=ti, par=par: e.dma_start(out=xt[par][:], in_=xsrc[ti * 128:(ti + 1) * 128, :]), writes=[b_xt[par]])
            for ch in range(2):
                for c in range(NFF):
                    P.op("pe", lambda e, c=c, ch=ch, tl=tl: e.matmul(pC[ch][:], lhsT=AT[:, c, tl * 128:(tl + 1) * 128], rhs=Wd[:, c, ch * 512:(ch + 1) * 512], start=(c == 0), stop=(c == NFF - 1)), reads=[b_AT[c][(tl * 128) // BLK], b_Wd[c]], writes=[b_pC[ch]])
                P.op("act", lambda e, ch=ch, par=par: e.copy(out=ot[par][:, ch * 512:(ch + 1) * 512], in_=pC[ch][:]), reads=[b_pC[ch]], writes=[b_ot[par]])
            rs, brs = rstd_of(ot[par][:], [b_ot[par]])
            P.op("dve", lambda e, par=par, rs=rs: e.scalar_tensor_tensor(out=ot[par][:], in0=ot[par][:], scalar=rs, in1=gpost_t[:], op0=ALU.mult, op1=ALU.mult), reads=[b_ot[par], brs, b_gpost], writes=[b_ot[par]])
            P.op("dve", lambda e, par=par: e.scalar_tensor_tensor(out=ot[par][:], in0=ot[par][:], scalar=0.5, in1=xt[par][:], op0=ALU.mult, op1=ALU.add), reads=[b_ot[par], b_xt[par]], writes=[b_ot[par]])
            P.dma("pool", lambda e, ti=ti, par=par: e.dma_start(out=xdst[ti * 128:(ti + 1) * 128, :], in_=ot[par][:]), reads=[b_ot[par]], sembuf=b_ot[par])
            if has_win:
                norm_to_hT(ot[par][:], [b_ot[par]], gmix_t, b_gmix, ti, par)
    if has_win:
        win = W["win"]
        ZW = min(1024, TOK)
        zs = [P.sb(f"zs{i}", [128, ZW]) for i in range(2)]; b_zs = [B(), B()]
        zi_n = 0
        for c in range(NWIN):
            s = c % 2
            P.dma("sp", lambda e, c=c, s=s: e.dma_start(out=stg[s][:, 0:1024], in_=win[c].rearrange("p k f -> p (k f)")), writes=[b_stg[s]])
            P.op("pool", lambda e, s=s: e.tensor_copy(out=wgb[s][:].rearrange("p k f -> p (k f)"), in_=stg[s][:, 0:1024]), reads=[b_stg[s]], writes=[b_wgb[s]])
            for z0 in range(0, TOK, ZW):
                zi = zi_n % 2; zi_n += 1
                for bi, t0 in enumerate(range(z0, z0 + ZW, BLK)):
                    q = bi % 2
                    rd = [b_hT[(t0 // 128) + i] for i in range(BLK // 128)]
                    for k in range(8):
                        P.op("pe", lambda e, k=k, s=s, q=q, t0=t0: e.matmul(pA[q][:, 0:BLK], lhsT=wgb[s][:, k, :], rhs=hT[:, k, t0:t0 + BLK], start=(k == 0), stop=(k == 7)), reads=[b_wgb[s]] + rd, writes=[b_pA[q]])
                    if bi % 2 == 0:
                        P.op("act", lambda e, q=q, zi=zi, t0=t0, z0=z0: e.copy(out=zs[zi][:, t0 - z0:t0 - z0 + BLK], in_=pA[q][:, 0:BLK]), reads=[b_pA[q]], writes=[b_zs[zi]])
                    else:
                        P.op("dve", lambda e, q=q, zi=zi, t0=t0, z0=z0: e.tensor_copy(out=zs[zi][:, t0 - z0:t0 - z0 + BLK], in_=pA[q][:, 0:BLK]), reads=[b_pA[q]], writes=[b_zs[zi]])
                P.dma("pool", lambda e, c=c, zi=zi, z0=z0: e.dma_start(out=zdst[c * 128:(c + 1) * 128, z0:z0 + ZW], in_=zs[zi][:]), reads=[b_zs[zi]], sembuf=b_zs[zi])
    P.end()


def emit_diff_h(nc, P, PFX, L):
    NQ = L // 128
    P.begin(); B = P.buf
    def din(name, shape): return nc.dram_tensor(PFX + name, list(shape), F32, kind="ExternalInput").ap()
    qk_d = din("qk", [4, 64, L])
    v_d = din("v", [128, NQ, 128])
    lam_d = din("lam", [128, 4, 64])
    cst_d = din("cst", [128, 2])
    gsub_d = din("gsub", [128, 128])
    tri_d = din("tri", [128, 128])
    y_d = nc.dram_tensor(PFX + "y", [NQ, 128, 128], F32, kind="ExternalOutput").ap()

    qkb = [P.sb(f"qkb{i}", [64, L], BF16) for i in range(4)]; b_qkb = [B() for _ in range(4)]
    vb = P.sb("vb", [128, NQ, 130], BF16); b_vb = B()
    stg = [P.sb(f"stg{i}", [128, 2048]) for i in range(2)]; b_stg = [B(), B()]
    lam_t = P.sb("lam_t", [128, 4, 64]); b_lam = B()
    cst = P.sb("cst_t", [128, 2]); b_cst = B()
    gsub = P.sb("gsub_t", [128, 128]); b_gsub = B()
    tri_f = P.sb("tri_f", [128, 128]); b_trif = B()
    tri = P.sb("tri_b", [128, 128], BF16); b_tri = B()
    ones = P.sb("ones", [128, 2], BF16); b_ones = B()
    sm = P.sb("sm", [128, 16]); b_sm = [B() for _ in range(16)]
    junk = P.sb("junk", [128, 128]); b_junk = B()
    ET = [[P.sb(f"ET{m}{i}", [128, 4, 128], BF16) for i in range(2)] for m in range(2)]
    b_ET = [[B(), B()] for _ in range(2)]
    ob = [P.sb(f"ob{i}", [128, 128]) for i in range(2)]; b_ob = [B(), B()]
    t2 = P.sb("t2", [128, 128]); b_t2 = B()
    pS = [[P.ps(f"pS{m}{i}", [128, 512]) for i in range(2)] for m in range(2)]; b_pS = [[B(), B()] for _ in range(2)]
    pO = [P.ps(f"pO{m}", [128, 512]) for m in range(2)]; b_pO = [B(), B()]

    n = 0
    for i in range(4):
        CW = min(2048, L)
        for c0 in range(0, L, CW):
            s = n % 2; n += 1
            P.dma("sp", lambda e, i=i, c0=c0, s=s: e.dma_start(out=stg[s][0:64, 0:CW], in_=qk_d[i, :, c0:c0 + CW]), writes=[b_stg[s]])
            P.op("pool", lambda e, i=i, c0=c0, s=s: e.tensor_copy(out=qkb[i][:, c0:c0 + CW], in_=stg[s][0:64, 0:CW]), reads=[b_stg[s]], writes=[b_qkb[i]])
    TW = min(16, NQ)
    for t0 in range(0, NQ, TW):
        s = n % 2; n += 1
        P.dma("sp", lambda e, t0=t0, s=s: e.dma_start(out=stg[s][:, 0:TW * 128], in_=v_d[:, t0:t0 + TW, :].rearrange("p t d -> p (t d)")), writes=[b_stg[s]])
        P.op("pool", lambda e, t0=t0, s=s: e.tensor_copy(out=vb[:, t0:t0 + TW, 0:128], in_=stg[s][:, 0:TW * 128].rearrange("p (t d) -> p t d", d=128)), reads=[b_stg[s]], writes=[b_vb])
    P.dma("sp", lambda e: e.dma_start(out=lam_t[:], in_=lam_d), writes=[b_lam])
    P.dma("sp", lambda e: e.dma_start(out=cst[:], in_=cst_d), writes=[b_cst])
    P.dma("sp", lambda e: e.dma_start(out=gsub[:], in_=gsub_d), writes=[b_gsub])
    P.dma("sp", lambda e: e.dma_start(out=tri_f[:], in_=tri_d), writes=[b_trif])
    P.op("dve", lambda e: e.tensor_copy(out=tri[:], in_=tri_f[:]), reads=[b_trif], writes=[b_tri])
    P.op("dve", lambda e: e.memset(vb[:, :, 128:130], 1.0), writes=[b_vb])
    for j in range(2):
        P.op("dve", lambda e, j=j: e.tensor_tensor(out=junk[:, 0:64], in0=lam_t[:, 2 * j, :], in1=lam_t[:, 2 * j + 1, :], op=ALU.mult), reads=[b_lam], writes=[b_junk])
        P.op("dve", lambda e, j=j: e.reduce_sum(out=sm[:, j:j + 1], in_=junk[:, 0:64], axis=AX.X), reads=[b_junk], writes=[b_sm[j]])
        P.op("act", lambda e, j=j: e.activation(out=sm[:, j:j + 1], in_=sm[:, j:j + 1], func=AF.Exp), reads=[b_sm[j]], writes=[b_sm[j]])
    P.op("dve", lambda e: e.tensor_tensor(out=sm[:, 2:3], in0=sm[:, 1:2], in1=sm[:, 0:1], op=ALU.subtract), reads=[b_sm[0], b_sm[1]], writes=[b_sm[2]])
    P.op("dve", lambda e: e.tensor_tensor(out=sm[:, 2:3], in0=sm[:, 2:3], in1=cst[:, 0:1], op=ALU.subtract), reads=[b_sm[2], b_cst], writes=[b_sm[2]])
    NEGLAM = (sm[:, 2:3], b_sm[2])

    gi = 0
    for qi in range(NQ):
        nk = qi + 1
        groups = [(g0, min(4, nk - g0)) for g0 in range(0, nk, 4)]
        for gidx, (g0, gn) in enumerate(groups):
            par = gi % 2; gi += 1
            for m in range(2):
                for j in range(gn):
                    kt = g0 + j
                    P.op("pe", lambda e, m=m, j=j, kt=kt, par=par, qi=qi: e.matmul(pS[m][par][:, j * 128:(j + 1) * 128], lhsT=qkb[2 + m][:, kt * 128:(kt + 1) * 128], rhs=qkb[m][:, qi * 128:(qi + 1) * 128], start=True, stop=True),
                         reads=[b_qkb[2 + m], b_qkb[m]], writes=[b_pS[m][par]])
                P.op("act", lambda e, m=m, par=par, gn=gn: e.activation(out=ET[m][par][:, 0:gn, :].rearrange("p g q -> p (g q)"), in_=pS[m][par][:, 0:gn * 128], func=AF.Exp, scale=0.125),
                     reads=[b_pS[m][par]], writes=[b_ET[m][par]])
                if g0 + gn == nk:
                    j = gn - 1
                    P.op("dve", lambda e, m=m, par=par, j=j: e.tensor_tensor(out=ET[m][par][:, j, :], in0=ET[m][par][:, j, :], in1=tri[:], op=ALU.mult),
                         reads=[b_ET[m][par], b_tri], writes=[b_ET[m][par]])
                for j in range(gn):
                    kt = g0 + j
                    first = (kt == 0); last = (kt == nk - 1)
                    P.op("pe", lambda e, m=m, j=j, kt=kt, par=par, first=first, last=last: e.matmul(pO[m][:, 0:130], lhsT=ET[m][par][:, j, :], rhs=vb[:, kt, :], start=first, stop=last),
                         reads=[b_ET[m][par], b_vb], writes=[b_pO[m]])
        op_ = qi % 2
        P.op("dve", lambda e: e.reciprocal(out=sm[:, 4:5], in_=pO[0][:, 128:129]), reads=[b_pO[0]], writes=[b_sm[4]])
        P.op("dve", lambda e: e.reciprocal(out=sm[:, 5:6], in_=pO[1][:, 128:129]), reads=[b_pO[1]], writes=[b_sm[5]])
        P.op("dve", lambda e: e.tensor_tensor(out=sm[:, 5:6], in0=sm[:, 5:6], in1=NEGLAM[0], op=ALU.mult), reads=[b_sm[5], NEGLAM[1]], writes=[b_sm[5]])
        P.op("dve", lambda e: e.tensor_scalar(out=t2[:], in0=pO[1][:, 0:128], scalar1=sm[:, 5:6], scalar2=None, op0=ALU.mult), reads=[b_pO[1], b_sm[5]], writes=[b_t2])
        P.op("dve", lambda e, op_=op_: e.scalar_tensor_tensor(out=ob[op_][:], in0=pO[0][:, 0:128], scalar=sm[:, 4:5], in1=t2[:], op0=ALU.mult, op1=ALU.add), reads=[b_pO[0], b_sm[4], b_t2], writes=[b_ob[op_]])
        P.op("act", lambda e, op_=op_: e.activation(out=junk[:], in_=ob[op_][:], func=AF.Square, scale=float(128 ** -0.5), accum_out=sm[:, 6:7]), reads=[b_ob[op_]], writes=[b_junk, b_sm[6]])
        P.op("dve", lambda e: e.tensor_scalar(out=sm[:, 6:7], in0=sm[:, 6:7], scalar1=1e-6, scalar2=None, op0=ALU.add), reads=[b_sm[6]], writes=[b_sm[6]])
        P.op("act", lambda e: e.activation(out=sm[:, 6:7], in_=sm[:, 6:7], func=AF.Sqrt), reads=[b_sm[6]], writes=[b_sm[6]])
        P.op("dve", lambda e: e.reciprocal(out=sm[:, 6:7], in_=sm[:, 6:7]), reads=[b_sm[6]], writes=[b_sm[6]])
        P.op("dve", lambda e: e.tensor_tensor(out=sm[:, 6:7], in0=sm[:, 6:7], in1=cst[:, 1:2], op=ALU.mult), reads=[b_sm[6], b_cst], writes=[b_sm[6]])
        P.op("dve", lambda e, op_=op_: e.scalar_tensor_tensor(out=ob[op_][:], in0=ob[op_][:], scalar=sm[:, 6:7], in1=gsub[:], op0=ALU.mult, op1=ALU.mult), reads=[b_ob[op_], b_sm[6], b_gsub], writes=[b_ob[op_]])
        P.dma("pool", lambda e, qi=qi, op_=op_: e.dma_start(out=y_d[qi], in_=ob[op_][:]), reads=[b_ob[op_]], sembuf=b_ob[op_])
    P.end()


def emit_dsa_h(nc, P, PFX, L, R=16.0, K=18):
    NQ = L // 128
    P.begin(); B = P.buf
    def din(name, shape): return nc.dram_tensor(PFX + name, list(shape), F32, kind="ExternalInput").ap()
    qk_d = din("qk", [2, 128, L])
    v_d = din("v", [128, NQ, 128])
    qi_d = din("qi", [NQ, 64, 8, 128])
    ki_d = din("ki", [64, L])
    wi_d = din("wi", [128, NQ, 8])
    negm_d = din("negm", [128, 128])
    idn_d = din("idn", [128, 128])
    y_d = nc.dram_tensor(PFX + "y", [NQ, 128, 128], F32, kind="ExternalOutput").ap()
    dbg = [P.sb(f"dbg{i}", [128, 8]) for i in range(2)]; b_dbg = [B(), B()]

    qkb = [P.sb(f"qkb{i}", [128, L], BF16) for i in range(2)]; b_qkb = [B(), B()]
    vb = P.sb("vb", [128, NQ, 130], BF16); b_vb = B()
    kiT = P.sb("kiT", [64, L]); b_ki = B()
    wi = P.sb("wi_t", [128, NQ, 8]); b_wi = B()
    negm = P.sb("negm_t", [128, 128]); b_negm = B()
    idf = P.sb("idf", [128, 128]); b_idf = B()
    idb = P.sb("idb", [128, 128], BF16); b_idb = B()
    stg = [P.sb(f"stg{i}", [128, 2048]) for i in range(2)]; b_stg = [B(), B()]
    qit = [P.sb(f"qit{i}", [64, 8, 128]) for i in range(2)]; b_qit = [B(), B()]
    score = [P.sb(f"score{i}", [128, L]) for i in range(2)]; b_score = [B(), B()]
    junkS = P.sb("junkS", [128, L], BF16); b_junkS = B()
    rl = [P.sb(f"rl{i}", [128, 512]) for i in range(2)]; b_rl = [B(), B()]
    Eb = [P.sb(f"Eb{i}", [128, 512], BF16) for i in range(2)]; b_Eb = [B(), B()]
    Pm = [P.sb(f"Pm{i}", [128, 512], BF16) for i in range(2)]; b_Pm = [B(), B()]
    PmT = [P.sb(f"PmT{i}", [128, 4, 128], BF16) for i in range(2)]; b_PmT = [B(), B()]
    ob = [P.sb(f"ob{i}", [128, 128]) for i in range(2)]; b_ob = [B(), B()]
    sm = P.sb("sm", [128, 8]); b_sm = [B() for _ in range(8)]
    pD = [P.ps(f"pD{i}", [128, 512]) for i in range(2)]; b_pD = [B(), B()]
    pS = [P.ps(f"pS{i}", [128, 512]) for i in range(2)]; b_pS = [B(), B()]
    pT = [P.ps(f"pT{i}", [128, 1024], BF16) for i in range(2)]; b_pT = [B(), B()]
    pO = P.ps("pO", [128, 512]); b_pO = B()

    n = 0
    CW = min(2048, L)
    for i in range(2):
        for c0 in range(0, L, CW):
            s = n % 2; n += 1
            P.dma("sp", lambda e, i=i, c0=c0, s=s: e.dma_start(out=stg[s][:, 0:CW], in_=qk_d[i, :, c0:c0 + CW]), writes=[b_stg[s]])
            P.op("pool", lambda e, i=i, c0=c0, s=s: e.tensor_copy(out=qkb[i][:, c0:c0 + CW], in_=stg[s][:, 0:CW]), reads=[b_stg[s]], writes=[b_qkb[i]])
    TW = min(16, NQ)
    for t0 in range(0, NQ, TW):
        s = n % 2; n += 1
        P.dma("sp", lambda e, t0=t0, s=s: e.dma_start(out=stg[s][:, 0:TW * 128], in_=v_d[:, t0:t0 + TW, :].rearrange("p t d -> p (t d)")), writes=[b_stg[s]])
        P.op("pool", lambda e, t0=t0, s=s: e.tensor_copy(out=vb[:, t0:t0 + TW, 0:128], in_=stg[s][:, 0:TW * 128].rearrange("p (t d) -> p t d", d=128)), reads=[b_stg[s]], writes=[b_vb])
    P.op("dve", lambda e: e.memset(vb[:, :, 128:130], 1.0), writes=[b_vb])
    P.dma("sp", lambda e: e.dma_start(out=kiT[:], in_=ki_d), writes=[b_ki])
    P.dma("sp", lambda e: e.dma_start(out=wi[:], in_=wi_d), writes=[b_wi])
    P.dma("sp", lambda e: e.dma_start(out=negm[:], in_=negm_d), writes=[b_negm])
    P.dma("sp", lambda e: e.dma_start(out=idf[:], in_=idn_d), writes=[b_idf])
    P.op("dve", lambda e: e.tensor_copy(out=idb[:], in_=idf[:]), reads=[b_idf], writes=[b_idb])
    SC = float((64 ** -0.5) * (8 ** -0.5))
    cnt_ = {"ci": 0, "ai": 0}
    tau = sm[:, 0:1]; mid = sm[:, 1:2]; cnt = sm[:, 2:3]; s_ = sm[:, 3:4]

    def chunks_of(qi):
        nk = qi + 1
        return [(c0, min(4, nk - c0)) for c0 in range(0, nk, 4)]

    def gen_indexer(qi):
        nk = qi + 1
        sp_ = qi % 2
        sc = score[sp_]; bsc = b_score[sp_]
        P.dma("sp", lambda e, qi=qi, sp_=sp_: e.dma_start(out=qit[sp_][:], in_=qi_d[qi]), writes=[b_qit[sp_]])
        for (c0, cn) in chunks_of(qi):
            w = cn * 128; k0 = c0 * 128
            for h in range(8):
                p = cnt_["ci"] % 2; cnt_["ci"] += 1
                P.op("pe", lambda e, h=h, p=p, k0=k0, w=w, sp_=sp_: e.matmul(pD[p][:, 0:w], lhsT=qit[sp_][:, h, :], rhs=kiT[:, k0:k0 + w], start=True, stop=True),
                     reads=[b_qit[sp_], b_ki], writes=[b_pD[p]])
                P.op("act", lambda e, p=p, w=w: e.activation(out=rl[p][:, 0:w], in_=pD[p][:, 0:w], func=AF.Relu, scale=SC), reads=[b_pD[p]], writes=[b_rl[p]])
                if h == 0:
                    P.op("dve", lambda e, p=p, w=w, k0=k0, sc=sc, qi=qi, h=h: e.tensor_scalar(out=sc[:, k0:k0 + w], in0=rl[p][:, 0:w], scalar1=wi[:, qi, h:h + 1], scalar2=None, op0=ALU.mult),
                         reads=[b_rl[p], b_wi], writes=[bsc])
                else:
                    P.op("dve", lambda e, p=p, w=w, k0=k0, sc=sc, qi=qi, h=h: e.scalar_tensor_tensor(out=sc[:, k0:k0 + w], in0=rl[p][:, 0:w], scalar=wi[:, qi, h:h + 1], in1=sc[:, k0:k0 + w], op0=ALU.mult, op1=ALU.add),
                         reads=[b_rl[p], b_wi, bsc], writes=[bsc])
                yield
        d0 = (nk - 1) * 128
        P.op("dve", lambda e, sc=sc, d0=d0: e.tensor_tensor(out=sc[:, d0:d0 + 128], in0=sc[:, d0:d0 + 128], in1=negm[:], op=ALU.add), reads=[bsc, b_negm], writes=[bsc])
        yield

    def KQ(nkeys):
        return 22 if nkeys <= 2048 else 18

    def gen_bisect(qi):
        nk = qi + 1; nkeys = nk * 128
        sc = score[qi % 2]; bsc = b_score[qi % 2]
        if nkeys <= 256:
            P.op("dve", lambda e: e.memset(tau, -R), writes=[b_sm[0]])
            yield
            return
        P.op("dve", lambda e: e.memset(mid, 0.0), writes=[b_sm[1]])
        yield
        K = KQ(nkeys)
        for it in range(K):
            P.op("dve", lambda e, sc=sc, nkeys=nkeys: e.tensor_scalar(out=junkS[:, 0:nkeys], in0=sc[:, 0:nkeys], scalar1=mid, scalar2=0.0, op0=ALU.is_ge, op1=ALU.add, accum_out=cnt),
                 reads=[bsc, b_sm[1]], writes=[b_junkS, b_sm[2]])
            yield
            if it < K - 1:
                wn = R / 2 ** (it + 1)
                P.op("dve", lambda e, wn=wn: e.tensor_scalar(out=s_, in0=cnt, scalar1=255.5, scalar2=2 * wn, op0=ALU.is_ge, op1=ALU.mult), reads=[b_sm[2]], writes=[b_sm[3]])
                yield
                P.op("dve", lambda e, wn=wn: e.scalar_tensor_tensor(out=mid, in0=s_, scalar=-wn, in1=mid, op0=ALU.add, op1=ALU.add), reads=[b_sm[3], b_sm[1]], writes=[b_sm[1]])
                yield
            else:
                wl = R / 2 ** (K - 1)
                P.op("dve", lambda e, wl=wl: e.tensor_scalar(out=s_, in0=cnt, scalar1=255.5, scalar2=wl, op0=ALU.is_ge, op1=ALU.mult), reads=[b_sm[2]], writes=[b_sm[3]])
                yield
                P.op("dve", lambda e, wl=wl: e.scalar_tensor_tensor(out=tau, in0=s_, scalar=-wl, in1=mid, op0=ALU.add, op1=ALU.add), reads=[b_sm[3], b_sm[1]], writes=[b_sm[0]])
                yield

    def attention(qi):
        nk = qi + 1
        sc = score[qi % 2]; bsc = b_score[qi % 2]
        for (c0, cn) in chunks_of(qi):
            w = cn * 128; k0 = c0 * 128
            p = cnt_["ai"] % 2; cnt_["ai"] += 1
            P.op("pe", lambda e, p=p, k0=k0, w=w, qi=qi: e.matmul(pS[p][:, 0:w], lhsT=qkb[0][:, qi * 128:(qi + 1) * 128], rhs=qkb[1][:, k0:k0 + w], start=True, stop=True),
                 reads=[b_qkb[0], b_qkb[1]], writes=[b_pS[p]])
            P.op("act", lambda e, p=p, w=w: e.activation(out=Eb[p][:, 0:w], in_=pS[p][:, 0:w], func=AF.Exp, scale=float(128 ** -0.5)), reads=[b_pS[p]], writes=[b_Eb[p]])
            P.op("dve", lambda e, p=p, w=w, k0=k0, sc=sc: e.scalar_tensor_tensor(out=Pm[p][:, 0:w], in0=sc[:, k0:k0 + w], scalar=tau, in1=Eb[p][:, 0:w], op0=ALU.is_ge, op1=ALU.mult),
                 reads=[bsc, b_sm[0], b_Eb[p]], writes=[b_Pm[p]])
            for j in range(cn):
                P.op("pe", lambda e, p=p, j=j: e.transpose(pT[p][:, j * 128:(j + 1) * 128], Pm[p][:, j * 128:(j + 1) * 128], idb[:]), reads=[b_Pm[p], b_idb], writes=[b_pT[p]])
            P.op("act", lambda e, p=p, w=w, cn=cn: e.copy(out=PmT[p][:, 0:cn, :].rearrange("p g q -> p (g q)"), in_=pT[p][:, 0:w]), reads=[b_pT[p]], writes=[b_PmT[p]])
            for j in range(cn):
                kt = c0 + j
                P.op("pe", lambda e, p=p, j=j, kt=kt, nk=nk: e.matmul(pO[:, 0:130], lhsT=PmT[p][:, j, :], rhs=vb[:, kt, :], start=(kt == 0), stop=(kt == nk - 1)),
                     reads=[b_PmT[p], b_vb], writes=[b_pO])
        op_ = qi % 2
        P.op("dve", lambda e: e.reciprocal(out=sm[:, 4:5], in_=pO[:, 128:129]), reads=[b_pO], writes=[b_sm[4]])
        P.op("dve", lambda e, op_=op_: e.tensor_scalar(out=ob[op_][:], in0=pO[:, 0:128], scalar1=sm[:, 4:5], scalar2=None, op0=ALU.mult), reads=[b_pO, b_sm[4]], writes=[b_ob[op_]])
        P.dma("pool", lambda e, qi=qi, op_=op_: e.dma_start(out=y_d[qi], in_=ob[op_][:]), reads=[b_ob[op_]], sembuf=b_ob[op_])

    for _ in gen_indexer(0):
        pass
    for qi in range(NQ):
        gb = gen_bisect(qi)
        gi_ = gen_indexer(qi + 1) if qi + 1 < NQ else iter(())
        nb = 1 if (qi + 1) * 128 <= 256 else 3 * KQ((qi + 1) * 128) + 1
        ni = 8 * len(chunks_of(qi + 1)) + 1 if qi + 1 < NQ else 0
        ratio = max(1, -(-ni // nb))
        b_done = False; i_done = (ni == 0)
        while not (b_done and i_done):
            if not b_done:
                try:
                    next(gb)
                except StopIteration:
                    b_done = True
            for _ in range(ratio):
                if i_done:
                    break
                try:
                    next(gi_)
                except StopIteration:
                    i_done = True
        attention(qi)
    P.end()


def emit_rwkv_h(nc, P, PFX, L):
    SEG = min(L, 512); NSEG = L // SEG; NCH = SEG // 64
    P.begin(); B = P.buf
    def din(name, shape): return nc.dram_tensor(PFX + name, list(shape), F32, kind="ExternalInput").ap()
    zr_d = din("zr", [3, 64, 2, L + 1]); zl_d = din("zl", [64, 2, L + 1]); zg_d = din("zg", [128, L + 1])
    mu3_d = din("mu3", [64, 3, 2]); mul_d = din("mul", [64, 2]); mug_d = din("mug", [128, 1])
    pp_d = din("pp", [64, 5, 2]); wup_d = din("wup", [64, 2, 64]); aup_d = din("aup", [64, 2, 64]); gup_d = din("gup", [128, 128])
    lnwb_d = din("lnwb", [64, 2, 128]); cmask_d = din("cmask", [64, 2 * SEG]); mask5_d = din("mask5", [64, 320])
    idn_d = din("idn", [64, 64])
    y_d = nc.dram_tensor(PFX + "y", [L // 64, 64, 128], F32, kind="ExternalOutput").ap()
    def T(name, shape, dt=F32):
        return P.sb(name, shape, dt), B(name)
    raw3, b_raw3 = T("raw3", [64, 3, 2, SEG + 1]); rawl, b_rawl = T("rawl", [64, 2, SEG + 1]); rawg, b_rawg = T("rawg", [128, SEG + 1])
    mu3, b_mu3 = T("mu3t", [64, 3, 2]); mul, b_mul = T("mult", [64, 2]); mug, b_mug = T("mugt", [128, 1])
    pp, b_pp = T("ppt", [64, 5, 2]); wup, b_wup = T("wupt", [64, 2, 64]); aup, b_aup = T("aupt", [64, 2, 64]); gup, b_gup = T("gupt", [128, 128])
    lnwb, b_lnwb = T("lnwbt", [64, 2, 128]); cmask, b_cmask = T("cmaskt", [64, 2 * SEG]); mask5, b_mask5 = T("mask5t", [64, 320])
    idn, b_idn = T("idnt", [64, 64]); ones, b_ones = T("onest", [64, 64])
    d3, b_d3 = T("d3", [64, 3, 2, SEG]); dl, b_dl = T("dl", [64, 2, SEG]); dg, b_dg = T("dg", [128, SEG])
    x3, b_x3 = T("x3", [64, 3, 2, SEG]); xl, b_xl = T("xl", [64, 2, SEG]); xg, b_xg = T("xg", [128, SEG])
    tw, b_tw = T("tw", [64, SEG]); sgc, b_sgc = T("sgc", [128, SEG]); sgw, b_sgw = T("sgw", [64, 2, SEG]); aa, b_aa = T("aa", [64, 2, SEG])
    t1, b_t1 = T("t1", [64, 2, SEG]); sq, b_sq = T("sq", [64, 2, SEG]); rn, b_rn = T("rn", [64, 2, SEG]); kk, b_kk = T("kk", [64, 2, SEG])
    kp, b_kp = T("kp", [64, 2, SEG]); bb, b_bb = T("bb", [64, 2, SEG]); cs, b_cs = T("cs", [64, 2, SEG])
    epos, b_epos = T("epos", [64, 2, SEG]); eneg, b_eneg = T("eneg", [64, 2, SEG]); eprev, b_eprev = T("eprev", [64, 2, SEG])
    AR, b_AR = T("AR", [64, 2, NCH, 2, 64]); Bt, b_Bt = T("Bt", [64, 2, SEG]); Kt, b_Kt = T("Kt", [64, 2, SEG]); rkr, b_rkr = T("rkr", [64, 2, SEG])
    Hs = [T(f"H{i}", [64, 2, 64]) for i in range(2)]
    Msb = [T(f"Msb{h}", [64, 320]) for h in range(2)]
    TK, b_TK = T("TK", [64, 6, 64])
    PPs = [T(f"PP{i}", [64, 2, 2, 64]) for i in range(2)]
    Xs = [T(f"X{i}", [64, 2, 64]) for i in range(2)]
    Wsb, b_Wsb = T("Wsb", [64, 128]); Usb, b_Usb = T("Usb", [64, 128]); Ysb, b_Ysb = T("Ysb", [64, 128]); yc, b_yc = T("yc", [64, 128])
    outs = [T(f"out{i}", [64, 128]) for i in range(2)]
    sm, _ = T("sm", [64, 16]); b_sm = [B() for _ in range(16)]
    junk, b_junk = T("junk", [64, 64])
    def PS(name, shape): return P.ps(name, shape), B(name)
    pM = [PS(f"pM{h}", [64, 512]) for h in range(2)]
    pK, b_pK = PS("pK", [64, 512]); pI, b_pI = PS("pI", [64, 512]); pX, b_pX = PS("pX", [64, 512])
    pW, b_pW = PS("pW", [64, 512]); pY, b_pY = PS("pY", [64, 512]); pH, b_pH = PS("pH", [64, 512])

    for (t, b, d) in [(mu3, b_mu3, mu3_d), (mul, b_mul, mul_d), (mug, b_mug, mug_d), (pp, b_pp, pp_d), (wup, b_wup, wup_d), (aup, b_aup, aup_d),
                      (gup, b_gup, gup_d), (lnwb, b_lnwb, lnwb_d), (cmask, b_cmask, cmask_d), (mask5, b_mask5, mask5_d), (idn, b_idn, idn_d)]:
        P.dma("sp", lambda e, t=t, d=d: e.dma_start(out=t[:], in_=d), writes=[b])
    P.op("dve", lambda e: e.memset(ones[:], 1.0), writes=[b_ones])
    P.op("dve", lambda e: e.memset(Hs[0][0][:], 0.0), writes=[Hs[0][1]])
    hcur = 0
    NEG = -0.6065306597126334
    oi = 0
    for sg_ in range(NSEG):
        s0 = sg_ * SEG
        for a in range(3):
            P.dma("sp", lambda e, s0=s0, a=a: e.dma_start(out=raw3[:, a, :, :], in_=zr_d[a, :, :, s0:s0 + SEG + 1]), writes=[b_raw3])
        P.dma("sp", lambda e, s0=s0: e.dma_start(out=rawl[:], in_=zl_d[:, :, s0:s0 + SEG + 1]), writes=[b_rawl])
        P.dma("sp", lambda e, s0=s0: e.dma_start(out=rawg[:], in_=zg_d[:, s0:s0 + SEG + 1]), writes=[b_rawg])
        P.op("dve", lambda e: e.tensor_tensor(out=d3[:], in0=raw3[:, :, :, 0:SEG], in1=raw3[:, :, :, 1:SEG + 1], op=ALU.subtract), reads=[b_raw3], writes=[b_d3])
        P.op("dve", lambda e: e.tensor_tensor(out=dl[:], in0=rawl[:, :, 0:SEG], in1=rawl[:, :, 1:SEG + 1], op=ALU.subtract), reads=[b_rawl], writes=[b_dl])
        P.op("dve", lambda e: e.tensor_tensor(out=dg[:], in0=rawg[:, 0:SEG], in1=rawg[:, 1:SEG + 1], op=ALU.subtract), reads=[b_rawg], writes=[b_dg])
        for a in range(3):
            for h in range(2):
                P.op("dve", lambda e, a=a, h=h: e.scalar_tensor_tensor(out=x3[:, a, h, :], in0=d3[:, a, h, :], scalar=mu3[:, a, h:h + 1], in1=raw3[:, a, h, 1:SEG + 1], op0=ALU.mult, op1=ALU.add),
                     reads=[b_d3, b_mu3, b_raw3], writes=[b_x3])
        for a in range(2):
            P.op("dve", lambda e, a=a: e.scalar_tensor_tensor(out=xl[:, a, :], in0=dl[:, a, :], scalar=mul[:, a:a + 1], in1=rawl[:, a, 1:SEG + 1], op0=ALU.mult, op1=ALU.add),
                 reads=[b_dl, b_mul, b_rawl], writes=[b_xl])
        P.op("dve", lambda e: e.scalar_tensor_tensor(out=xg[:], in0=dg[:], scalar=mug[:, 0:1], in1=rawg[:, 1:SEG + 1], op0=ALU.mult, op1=ALU.add), reads=[b_dg, b_mug, b_rawg], writes=[b_xg])
        XR = lambda h: x3[:, 0, h, :]
        XK = lambda h: x3[:, 1, h, :]
        XV = lambda h: x3[:, 2, h, :]
        P.op("act", lambda e: e.activation(out=tw[:], in_=xl[:, 0, :], func=AF.Tanh), reads=[b_xl], writes=[b_tw])
        P.op("act", lambda e: e.activation(out=sgc[:], in_=xg[:], func=AF.Sigmoid), reads=[b_xg], writes=[b_sgc])
        for h in range(2):
            P.op("pe", lambda e, h=h: e.matmul(pK[:, 0:SEG], lhsT=wup[:, h, :], rhs=tw[:], start=True, stop=True), reads=[b_wup, b_tw], writes=[b_pK])
            P.op("act", lambda e, h=h: e.activation(out=sgw[:, h, :], in_=pK[:, 0:SEG], func=AF.Sigmoid, bias=pp[:, 0, h:h + 1]), reads=[b_pK, b_pp], writes=[b_sgw])
            P.op("pe", lambda e, h=h: e.matmul(pK[:, 0:SEG], lhsT=aup[:, h, :], rhs=xl[:, 1, :], start=True, stop=True), reads=[b_aup, b_xl], writes=[b_pK])
            P.op("act", lambda e, h=h: e.activation(out=aa[:, h, :], in_=pK[:, 0:SEG], func=AF.Sigmoid, bias=pp[:, 1, h:h + 1]), reads=[b_pK, b_pp], writes=[b_aa])
        for h in range(2):
            P.op("dve", lambda e, h=h: e.tensor_scalar(out=t1[:, h, :], in0=XK(h), scalar1=pp[:, 2, h:h + 1], scalar2=None, op0=ALU.mult), reads=[b_x3, b_pp], writes=[b_t1])
        P.op("dve", lambda e: e.tensor_tensor(out=sq[:], in0=t1[:], in1=t1[:], op=ALU.mult), reads=[b_t1], writes=[b_sq])
        for h in range(2):
            P.op("pe", lambda e, h=h: e.matmul(pK[:, 0:SEG], lhsT=ones[:], rhs=sq[:, h, :], start=True, stop=True), reads=[b_ones, b_sq], writes=[b_pK])
            P.op("dve", lambda e, h=h: e.tensor_scalar(out=rn[:, h, :], in0=pK[:, 0:SEG], scalar1=1e-24, scalar2=None, op0=ALU.max), reads=[b_pK], writes=[b_rn])
        P.op("act", lambda e: e.activation(out=rn[:], in_=rn[:], func=AF.Sqrt), reads=[b_rn], writes=[b_rn])
        P.op("dve", lambda e: e.reciprocal(out=rn[:], in_=rn[:]), reads=[b_rn], writes=[b_rn])
        P.op("dve", lambda e: e.tensor_tensor(out=kk[:], in0=t1[:], in1=rn[:], op=ALU.mult), reads=[b_t1, b_rn], writes=[b_kk])
        for h in range(2):
            P.op("dve", lambda e, h=h: e.tensor_scalar(out=kp[:, h, :], in0=aa[:, h, :], scalar1=pp[:, 3, h:h + 1], scalar2=pp[:, 3, h:h + 1], op0=ALU.mult, op1=ALU.subtract), reads=[b_aa, b_pp], writes=[b_kp])
            P.op("dve", lambda e, h=h: e.scalar_tensor_tensor(out=kp[:, h, :], in0=kp[:, h, :], scalar=1.0, in1=XK(h), op0=ALU.add, op1=ALU.mult), reads=[b_kp, b_x3], writes=[b_kp])
        P.op("dve", lambda e: e.tensor_tensor(out=bb[:], in0=kk[:], in1=aa[:], op=ALU.mult), reads=[b_kk, b_aa], writes=[b_bb])
        FL = lambda t: t[:].rearrange("p h s -> p (h s)")
        P.op("dve", lambda e: e.tensor_tensor_scan(out=FL(cs), data0=cmask[:], data1=FL(sgw), initial=0.0, op0=ALU.mult, op1=ALU.add), reads=[b_cmask, b_sgw], writes=[b_cs])
        P.op("act", lambda e: e.activation(out=epos[:], in_=cs[:], func=AF.Exp, scale=NEG), reads=[b_cs], writes=[b_epos])
        P.op("act", lambda e: e.activation(out=eneg[:], in_=cs[:], func=AF.Exp, scale=-NEG), reads=[b_cs], writes=[b_eneg])
        P.op("dve", lambda e: e.tensor_tensor(out=eprev[:], in0=cs[:], in1=sgw[:], op=ALU.subtract), reads=[b_cs, b_sgw], writes=[b_eprev])
        P.op("act", lambda e: e.activation(out=eprev[:], in_=eprev[:], func=AF.Exp, scale=NEG), reads=[b_eprev], writes=[b_eprev])
        for h in range(2):
            P.op("dve", lambda e, h=h: e.scalar_tensor_tensor(out=AR[:, h, :, 0, :], in0=kk[:, h, :].rearrange("p (c t) -> p c t", t=64), scalar=-1.0, in1=eprev[:, h, :].rearrange("p (c t) -> p c t", t=64), op0=ALU.mult, op1=ALU.mult),
                 reads=[b_kk, b_eprev], writes=[b_AR])
            P.op("dve", lambda e, h=h: e.tensor_tensor(out=AR[:, h, :, 1, :], in0=XR(h).rearrange("p (c t) -> p c t", t=64), in1=epos[:, h, :].rearrange("p (c t) -> p c t", t=64), op=ALU.mult),
                 reads=[b_x3, b_epos], writes=[b_AR])
            P.op("dve", lambda e, h=h: e.scalar_tensor_tensor(out=rkr[:, h, :], in0=XR(h), scalar=pp[:, 4, h:h + 1], in1=kp[:, h, :], op0=ALU.mult, op1=ALU.mult), reads=[b_x3, b_pp, b_kp], writes=[b_rkr])
        P.op("dve", lambda e: e.tensor_tensor(out=Bt[:], in0=bb[:], in1=eneg[:], op=ALU.mult), reads=[b_bb, b_eneg], writes=[b_Bt])
        P.op("dve", lambda e: e.tensor_tensor(out=Kt[:], in0=kp[:], in1=eneg[:], op=ALU.mult), reads=[b_kp, b_eneg], writes=[b_Kt])
        for c in range(NCH):
            cs_ = slice(c * 64, (c + 1) * 64)
            for h in range(2):
                pm, bpm = pM[h]
                P.op("pe", lambda e, h=h, c=c, pm=pm, cs_=cs_: e.matmul(pm[:, 0:64], lhsT=AR[:, h, c, 0, :], rhs=Bt[:, h, cs_], start=True, stop=True), reads=[b_AR, b_Bt], writes=[bpm])
                P.op("pe", lambda e, h=h, c=c, pm=pm, cs_=cs_: e.matmul(pm[:, 64:192], lhsT=Bt[:, h, cs_], rhs=AR[:, h, c, :, :].rearrange("p a t -> p (a t)"), start=True, stop=True), reads=[b_AR, b_Bt], writes=[bpm])
                P.op("pe", lambda e, h=h, c=c, pm=pm, cs_=cs_: e.matmul(pm[:, 192:320], lhsT=Kt[:, h, cs_], rhs=AR[:, h, c, :, :].rearrange("p a t -> p (a t)"), start=True, stop=True), reads=[b_AR, b_Kt], writes=[bpm])
                P.op("dve", lambda e, h=h, pm=pm: e.tensor_tensor(out=Msb[h][0][:], in0=pm[:, 0:320], in1=mask5[:], op=ALU.mult), reads=[bpm, b_mask5], writes=[Msb[h][1]])
            for h in range(2):
                for a, (src, bsrc) in enumerate([(Bt[:, h, cs_], b_Bt), (Kt[:, h, cs_], b_Kt), (x3[:, 2, h, cs_], b_x3)]):
                    P.op("pe", lambda e, h=h, a=a, src=src: e.transpose(pK[:, (h * 3 + a) * 64:(h * 3 + a + 1) * 64], src, idn[:]), reads=[bsrc, b_idn], writes=[b_pK])
            P.op("act", lambda e: e.copy(out=TK[:].rearrange("p a t -> p (a t)"), in_=pK[:, 0:384]), reads=[b_pK], writes=[b_TK])
            pcur = 0; xcur = 0
            for h in range(2):
                P.op("act", lambda e, h=h: e.copy(out=PPs[0][0][:, h, :, :].rearrange("p a t -> p (a t)"), in_=Msb[h][0][:, 0:128]), reads=[Msb[h][1]], writes=[PPs[0][1]])
                P.op("dve", lambda e, h=h: e.tensor_tensor(out=Xs[0][0][:, h, :], in0=Msb[h][0][:, 64:128], in1=idn[:], op=ALU.add), reads=[Msb[h][1], b_idn], writes=[Xs[0][1]])
            for stp in range(5):
                pp_t, pp_b = PPs[pcur]; pn_t, pn_b = PPs[1 - pcur]
                x_t, x_b = Xs[xcur]; xn_t, xn_b = Xs[1 - xcur]
                for h in range(2):
                    P.op("pe", lambda e, h=h, pp_t=pp_t: e.matmul(pI[:, (h * 2) * 64:(h * 2 + 1) * 64], lhsT=pp_t[:, h, 1, :], rhs=pp_t[:, h, 0, :], start=True, stop=True), reads=[pp_b], writes=[b_pI])
                    P.op("pe", lambda e, h=h, pp_t=pp_t: e.matmul(pI[:, (h * 2 + 1) * 64:(h * 2 + 2) * 64], lhsT=pp_t[:, h, 0, :], rhs=pp_t[:, h, 1, :], start=True, stop=True), reads=[pp_b], writes=[b_pI])
                P.op("act", lambda e, pn_t=pn_t: e.copy(out=pn_t[:].rearrange("p h a t -> p (h a t)"), in_=pI[:, 0:256]), reads=[b_pI], writes=[pn_b])
                for h in range(2):
                    P.op("pe", lambda e, h=h, x_t=x_t: e.matmul(pX[:, h * 64:(h + 1) * 64], lhsT=idn[:], rhs=x_t[:, h, :], start=True, stop=False), reads=[b_idn, x_b], writes=[b_pX])
                    P.op("pe", lambda e, h=h, x_t=x_t, pn_t=pn_t: e.matmul(pX[:, h * 64:(h + 1) * 64], lhsT=pn_t[:, h, 0, :], rhs=x_t[:, h, :], start=False, stop=True), reads=[pn_b, x_b], writes=[b_pX])
                P.op("dve", lambda e, xn_t=xn_t: e.tensor_copy(out=xn_t[:].rearrange("p h t -> p (h t)"), in_=pX[:, 0:128]), reads=[b_pX], writes=[xn_b])
                pcur = 1 - pcur; xcur = 1 - xcur
            X_t, X_b = Xs[xcur]
            H_t, H_b = Hs[hcur]; Hn_t, Hn_b = Hs[1 - hcur]
            for h in range(2):
                P.op("pe", lambda e, h=h, c=c, H_t=H_t: e.matmul(pW[:, h * 64:(h + 1) * 64], lhsT=AR[:, h, c, 0, :], rhs=H_t[:, h, :], start=True, stop=False), reads=[b_AR, H_b], writes=[b_pW])
                P.op("pe", lambda e, h=h: e.matmul(pW[:, h * 64:(h + 1) * 64], lhsT=Msb[h][0][:, 192:256], rhs=TK[:, h * 3 + 2, :], start=False, stop=True), reads=[Msb[h][1], b_TK], writes=[b_pW])
            P.op("act", lambda e: e.copy(out=Wsb[:], in_=pW[:, 0:128]), reads=[b_pW], writes=[b_Wsb])
            for h in range(2):
                P.op("pe", lambda e, h=h, X_t=X_t: e.matmul(pW[:, 128 + h * 64:128 + (h + 1) * 64], lhsT=X_t[:, h, :], rhs=Wsb[:, h * 64:(h + 1) * 64], start=True, stop=True), reads=[X_b, b_Wsb], writes=[b_pW])
            P.op("dve", lambda e: e.tensor_copy(out=Usb[:], in_=pW[:, 128:256]), reads=[b_pW], writes=[b_Usb])
            for h in range(2):
                hs = slice(h * 64, (h + 1) * 64)
                P.op("pe", lambda e, h=h, c=c, hs=hs, H_t=H_t: e.matmul(pY[:, hs], lhsT=AR[:, h, c, 1, :], rhs=H_t[:, h, :], start=True, stop=False), reads=[b_AR, H_b], writes=[b_pY])
                P.op("pe", lambda e, h=h, hs=hs: e.matmul(pY[:, hs], lhsT=Msb[h][0][:, 128:192], rhs=Usb[:, hs], start=False, stop=False), reads=[Msb[h][1], b_Usb], writes=[b_pY])
                P.op("pe", lambda e, h=h, hs=hs: e.matmul(pY[:, hs], lhsT=Msb[h][0][:, 256:320], rhs=TK[:, h * 3 + 2, :], start=False, stop=True), reads=[Msb[h][1], b_TK], writes=[b_pY])
            P.op("pe", lambda e, cs_=cs_: e.matmul(pY[:, 128:256], lhsT=sgc[:, cs_], rhs=gup[:], start=True, stop=True), reads=[b_sgc, b_gup], writes=[b_pY])
            for h in range(2):
                P.op("pe", lambda e, h=h, cs_=cs_: e.matmul(pY[:, 256 + 2 * h:258 + 2 * h], lhsT=rkr[:, h, cs_], rhs=ones[:, 0:2], start=True, stop=True), reads=[b_rkr, b_ones], writes=[b_pY])
            for h in range(2):
                hs = slice(h * 64, (h + 1) * 64)
                P.op("pe", lambda e, h=h, hs=hs, H_t=H_t: e.matmul(pH[:, hs], lhsT=idn[:], rhs=H_t[:, h, :], start=True, stop=False), reads=[b_idn, H_b], writes=[b_pH])
                P.op("pe", lambda e, h=h, hs=hs: e.matmul(pH[:, hs], lhsT=TK[:, h * 3 + 0, :], rhs=Usb[:, hs], start=False, stop=False), reads=[b_TK, b_Usb], writes=[b_pH])
                P.op("pe", lambda e, h=h, hs=hs: e.matmul(pH[:, hs], lhsT=TK[:, h * 3 + 1, :], rhs=TK[:, h * 3 + 2, :], start=False, stop=True), reads=[b_TK], writes=[b_pH])
            for h in range(2):
                ce = c * 64 + 63
                P.op("dve", lambda e, h=h, ce=ce, Hn_t=Hn_t: e.tensor_scalar(out=Hn_t[:, h, :], in0=pH[:, h * 64:(h + 1) * 64], scalar1=epos[:, h, ce:ce + 1], scalar2=None, op0=ALU.mult), reads=[b_pH, b_epos], writes=[Hn_b])
            hcur = 1 - hcur
            P.op("act", lambda e: e.copy(out=Ysb[:], in_=pY[:, 0:128]), reads=[b_pY], writes=[b_Ysb])
            P.op("dve", lambda e: e.tensor_copy(out=sm[:, 8:12], in_=pY[:, 256:260]), reads=[b_pY], writes=[b_sm[8]])
            o_t, o_b = outs[oi % 2]; oi += 1
            for h in range(2):
                hs = slice(h * 64, (h + 1) * 64)
                mcol = sm[:, h:h + 1]; vcol = sm[:, 2 + h:3 + h]
                P.op("dve", lambda e, hs=hs, mcol=mcol: e.reduce_sum(out=mcol, in_=Ysb[:, hs], axis=AX.X), reads=[b_Ysb], writes=[b_sm[h]])
                P.op("dve", lambda e, mcol=mcol: e.tensor_scalar(out=mcol, in0=mcol, scalar1=-1.0 / 64, scalar2=None, op0=ALU.mult), reads=[b_sm[h]], writes=[b_sm[h]])
                P.op("dve", lambda e, hs=hs, mcol=mcol: e.tensor_scalar(out=yc[:, hs], in0=Ysb[:, hs], scalar1=mcol, scalar2=None, op0=ALU.add), reads=[b_Ysb, b_sm[h]], writes=[b_yc])
                P.op("act", lambda e, hs=hs, vcol=vcol: e.activation(out=junk[:], in_=yc[:, hs], func=AF.Square, scale=0.125, accum_out=vcol), reads=[b_yc], writes=[b_junk, b_sm[2 + h]])
                P.op("dve", lambda e, vcol=vcol: e.tensor_scalar(out=vcol, in0=vcol, scalar1=64e-5, scalar2=None, op0=ALU.add), reads=[b_sm[2 + h]], writes=[b_sm[2 + h]])
                P.op("act", lambda e, vcol=vcol: e.activation(out=vcol, in_=vcol, func=AF.Sqrt), reads=[b_sm[2 + h]], writes=[b_sm[2 + h]])
                P.op("dve", lambda e, vcol=vcol: e.reciprocal(out=vcol, in_=vcol), reads=[b_sm[2 + h]], writes=[b_sm[2 + h]])
                P.op("dve", lambda e, hs=hs, vcol=vcol: e.scalar_tensor_tensor(out=yc[:, hs], in0=yc[:, hs], scalar=vcol, in1=lnwb[:, 0, hs], op0=ALU.mult, op1=ALU.mult), reads=[b_yc, b_sm[2 + h], b_lnwb], writes=[b_yc])
                P.op("dve", lambda e, hs=hs: e.tensor_tensor(out=yc[:, hs], in0=yc[:, hs], in1=lnwb[:, 1, hs], op=ALU.add), reads=[b_yc, b_lnwb], writes=[b_yc])
                P.op("dve", lambda e, hs=hs, h=h: e.scalar_tensor_tensor(out=yc[:, hs], in0=TK[:, h * 3 + 2, :], scalar=sm[:, 8 + 2 * h:9 + 2 * h], in1=yc[:, hs], op0=ALU.mult, op1=ALU.add), reads=[b_TK, b_sm[8], b_yc], writes=[b_yc])
            P.op("dve", lambda e, o_t=o_t: e.tensor_tensor(out=o_t[:], in0=yc[:], in1=pY[:, 128:256], op=ALU.mult), reads=[b_yc, b_pY], writes=[o_b])
            gci = sg_ * NCH + c
            P.dma("pool", lambda e, gci=gci, o_t=o_t: e.dma_start(out=y_d[gci], in_=o_t[:]), reads=[o_b], sembuf=o_b)
    P.end()


TOK = 2048; NT = 16
def emit_merge_h(nc, P, PFX, x_out=None):
    P.begin(); B = P.buf
    def din(name, shape): return nc.dram_tensor(PFX + name, list(shape), F32, kind="ExternalInput").ap()
    x_in = din("x", [TOK, D]); g_mpre = din("g_mpre", [128, D]); g_mpost = din("g_mpost", [128, D])
    wgate = din("wgate", [8, 128, 3072]); wbr = din("wbr", [12, 128, D]); wo = din("wo", [8, 128, D])
    yT = din("yT", [NT, 128, 12 * 128]); idn_d = din("idn", [128, 128])
    ident_f = P.sb("ident_f", [128, 128]); ident = P.sb("ident", [128, 128], BF16)
    gpre_t = P.sb("gpre_t", [128, D]); gpost_t = P.sb("gpost_t", [128, D])
    Wg = P.sb("Wg", [128, 8, 3072], BF16); Wb = P.sb("Wb", [128, 12, D], BF16); Wo = P.sb("Wo", [128, 8, D], BF16)
    stg = [P.sb(f"stg{i}", [128, 3072]) for i in range(2)]
    xt = [P.sb(f"xt{i}", [128, D]) for i in range(2)]; ot = [P.sb(f"ot{i}", [128, D]) for i in range(2)]
    mg = P.sb("mg", [128, D]); tmp = P.sb("tmp", [128, 512]); junk = P.sb("junk", [128, D])
    hb = P.sb("hb", [128, D], BF16); mb = P.sb("mb", [128, D], BF16)
    hTt = P.sb("hTt", [128, 8, 128], BF16); mT = P.sb("mT", [128, 8, 128], BF16)
    ystg = [P.sb(f"ystg{i}", [128, 1536]) for i in range(2)]; ytb = [P.sb(f"ytb{i}", [128, 12, 128], BF16) for i in range(2)]
    sgt = [P.sb(f"sgt{i}", [128, 512]) for i in range(2)]
    st = P.sb("st", [128, 8])
    pA = [P.ps(f"pA{i}", [128, 512]) for i in range(2)]; pB = [P.ps(f"pB{i}", [128, 512]) for i in range(2)]
    pC = [P.ps(f"pC{i}", [128, 512]) for i in range(2)]; pT = P.ps("pT", [128, 1024], BF16)
    b_idf = B(); b_id = B(); b_gpre = B(); b_gpost = B(); b_Wg = B(); b_Wb = B(); b_Wo = B(); b_stg = [B(), B()]
    b_xt = [B(), B()]; b_ot = [B(), B()]; b_mg = B(); b_tmp = B(); b_junk = B(); b_hb = B(); b_mb = B(); b_hTt = B(); b_mT = B()
    b_ystg = [B(), B()]; b_ytb = [B(), B()]; b_sgt = [B(), B()]; b_st = [B() for _ in range(8)]
    b_pA = [B(), B()]; b_pB = [B(), B()]; b_pC = [B(), B()]; b_pT = B()
    P.dma("sp", lambda e: e.dma_start(out=ident_f[:], in_=idn_d), writes=[b_idf])
    P.op("dve", lambda e: e.tensor_copy(out=ident[:], in_=ident_f[:]), reads=[b_idf], writes=[b_id])
    P.dma("sp", lambda e: e.dma_start(out=gpre_t[:], in_=g_mpre), writes=[b_gpre])
    P.dma("sp", lambda e: e.dma_start(out=gpost_t[:], in_=g_mpost), writes=[b_gpost])
    n = 0
    for k in range(8):
        s = n % 2; n += 1
        P.dma("sp", lambda e, k=k, s=s: e.dma_start(out=stg[s][:, :], in_=wgate[k]), writes=[b_stg[s]])
        P.op("pool", lambda e, k=k, s=s: e.tensor_copy(out=Wg[:, k, :], in_=stg[s][:, :]), reads=[b_stg[s]], writes=[b_Wg])
    for k in range(12):
        s = n % 2; n += 1
        P.dma("sp", lambda e, k=k, s=s: e.dma_start(out=stg[s][:, 0:D], in_=wbr[k]), writes=[b_stg[s]])
        P.op("pool", lambda e, k=k, s=s: e.tensor_copy(out=Wb[:, k, :], in_=stg[s][:, 0:D]), reads=[b_stg[s]], writes=[b_Wb])
    for k in range(8):
        s = n % 2; n += 1
        P.dma("sp", lambda e, k=k, s=s: e.dma_start(out=stg[s][:, 0:D], in_=wo[k]), writes=[b_stg[s]])
        P.op("pool", lambda e, k=k, s=s: e.tensor_copy(out=Wo[:, k, :], in_=stg[s][:, 0:D]), reads=[b_stg[s]], writes=[b_Wo])

    def rstd_of(src_ap, src_bufs, col):
        ss = st[:, col:col + 1]; bss = b_st[col]
        P.op("act", lambda e: e.activation(out=junk[:], in_=src_ap, func=AF.Square, scale=float(D ** -0.5), accum_out=ss), reads=src_bufs, writes=[b_junk, bss])
        P.op("dve", lambda e: e.tensor_scalar(out=ss, in0=ss, scalar1=EPS, scalar2=None, op0=ALU.add), reads=[bss], writes=[bss])
        P.op("act", lambda e: e.activation(out=ss, in_=ss, func=AF.Sqrt), reads=[bss], writes=[bss])
        P.op("dve", lambda e: e.reciprocal(out=ss, in_=ss), reads=[bss], writes=[bss])
        return ss, bss

    qn = 0
    for ti in range(NT):
        par = ti % 2
        P.dma("sp", lambda e, ti=ti, par=par: e.dma_start(out=xt[par][:], in_=x_in[ti * 128:(ti + 1) * 128, :]), writes=[b_xt[par]])
        P.dma("sp", lambda e, ti=ti, par=par: e.dma_start(out=ystg[par][:], in_=yT[ti]), writes=[b_ystg[par]])
        P.op("pool", lambda e, par=par: e.tensor_copy(out=ytb[par][:].rearrange("p a b -> p (a b)"), in_=ystg[par][:]), reads=[b_ystg[par]], writes=[b_ytb[par]])
        rs, brs = rstd_of(xt[par][:], [b_xt[par]], 0)
        P.op("dve", lambda e, par=par, rs=rs: e.scalar_tensor_tensor(out=hb[:], in0=xt[par][:], scalar=rs, in1=gpre_t[:], op0=ALU.mult, op1=ALU.mult), reads=[b_xt[par], brs, b_gpre], writes=[b_hb])
        for k in range(8):
            P.op("pe", lambda e, k=k: e.transpose(pT[:, k * 128:(k + 1) * 128], hb[:, k * 128:(k + 1) * 128], ident[:]), reads=[b_hb, b_id], writes=[b_pT])
        P.op("act", lambda e: e.copy(out=hTt[:].rearrange("p k t -> p (k t)"), in_=pT[:]), reads=[b_pT], writes=[b_hTt])
        for half in range(2):
            for nb in range(3):
                q = qn % 2; qn += 1
                c0 = nb * 1024 + half * 512
                for k in range(8):
                    P.op("pe", lambda e, k=k, q=q, c0=c0: e.matmul(pA[q][:], lhsT=hTt[:, k, :], rhs=Wg[:, k, c0:c0 + 512], start=(k == 0), stop=(k == 7)), reads=[b_hTt, b_Wg], writes=[b_pA[q]])
                P.op("act", lambda e, q=q: e.activation(out=sgt[q][:], in_=pA[q][:], func=AF.Sigmoid), reads=[b_pA[q]], writes=[b_sgt[q]])
                for kc in range(4):
                    P.op("pe", lambda e, kc=kc, q=q, nb=nb, half=half, par=par: e.matmul(pB[q][:], lhsT=ytb[par][:, nb * 4 + kc, :], rhs=Wb[:, nb * 4 + kc, half * 512:(half + 1) * 512], start=(kc == 0), stop=(kc == 3)), reads=[b_ytb[par], b_Wb], writes=[b_pB[q]])
                if nb == 0:
                    P.op("dve", lambda e, q=q, half=half: e.tensor_tensor(out=mg[:, half * 512:(half + 1) * 512], in0=sgt[q][:], in1=pB[q][:], op=ALU.mult), reads=[b_sgt[q], b_pB[q]], writes=[b_mg])
                else:
                    P.op("dve", lambda e, q=q: e.tensor_tensor(out=tmp[:], in0=sgt[q][:], in1=pB[q][:], op=ALU.mult), reads=[b_sgt[q], b_pB[q]], writes=[b_tmp])
                    P.op("dve", lambda e, half=half: e.tensor_tensor(out=mg[:, half * 512:(half + 1) * 512], in0=mg[:, half * 512:(half + 1) * 512], in1=tmp[:], op=ALU.add), reads=[b_mg, b_tmp], writes=[b_mg])
        P.op("act", lambda e: e.copy(out=mb[:], in_=mg[:]), reads=[b_mg], writes=[b_mb])
        for k in range(8):
            P.op("pe", lambda e, k=k: e.transpose(pT[:, k * 128:(k + 1) * 128], mb[:, k * 128:(k + 1) * 128], ident[:]), reads=[b_mb, b_id], writes=[b_pT])
        P.op("act", lambda e: e.copy(out=mT[:].rearrange("p k t -> p (k t)"), in_=pT[:]), reads=[b_pT], writes=[b_mT])
        for half in range(2):
            for k in range(8):
                P.op("pe", lambda e, k=k, half=half: e.matmul(pC[half][:], lhsT=mT[:, k, :], rhs=Wo[:, k, half * 512:(half + 1) * 512], start=(k == 0), stop=(k == 7)), reads=[b_mT, b_Wo], writes=[b_pC[half]])
            P.op("act", lambda e, half=half, par=par: e.copy(out=ot[par][:, half * 512:(half + 1) * 512], in_=pC[half][:]), reads=[b_pC[half]], writes=[b_ot[par]])
        rs, brs = rstd_of(ot[par][:], [b_ot[par]], 1)
        P.op("dve", lambda e, par=par, rs=rs: e.scalar_tensor_tensor(out=ot[par][:], in0=ot[par][:], scalar=rs, in1=gpost_t[:], op0=ALU.mult, op1=ALU.mult), reads=[b_ot[par], brs, b_gpost], writes=[b_ot[par]])
        P.op("dve", lambda e, par=par: e.tensor_tensor(out=ot[par][:], in0=ot[par][:], in1=xt[par][:], op=ALU.add), reads=[b_ot[par], b_xt[par]], writes=[b_ot[par]])
        P.dma("pool", lambda e, ti=ti, par=par: e.dma_start(out=x_out[ti * 128:(ti + 1) * 128, :], in_=ot[par][:]), reads=[b_ot[par]], sembuf=b_ot[par])
    P.end()


import math as _math
_PROGS = {}
def _prog(key, fn):
    if key not in _PROGS:
        _PROGS[key] = fn()
    return _PROGS[key]

def _c(a):
    return np.ascontiguousarray(a, dtype=np.float32)

def _bc(v, p=128):
    return _c(np.broadcast_to(v[None, :], (p, v.shape[0])))

def _wl(w, nchunk):
    return _c(w.reshape(8, 128, nchunk, 128).transpose(2, 1, 0, 3))

def _pad1(a):
    return np.concatenate([np.zeros(a.shape[:-1] + (1,), np.float32), a], -1)

def _run(nc, maps):
    res = run_bass_kernel_spmd(nc, maps, core_ids=list(range(8)))
    return res.results

def _pfx(p, d):
    return {p + k: v for k, v in d.items()}

def build_mix(L):
    nc = bass.Bass("TRN2", target_bir_lowering=False)
    P = Prog(nc)
    emit_dsa_h(nc, P, "a_", L)
    emit_rwkv_h(nc, P, "b_", L)
    emit_diff_h(nc, P, "c_", L)
    P.finish()
    return nc

def build_mfa(with_a):
    nc = bass.Bass("TRN2", target_bir_lowering=False)
    P = Prog(nc)
    def din(name, shape): return nc.dram_tensor(name, list(shape), F32, kind="ExternalInput").ap()
    xb = nc.dram_tensor("xb_scr", [2048, D], F32)
    xc = nc.dram_tensor("xc_scr", [2048, D], F32)
    xo = nc.dram_tensor("xo", [2048, D], F32, kind="ExternalOutput").ap()
    emit_merge_h(nc, P, "m_", x_out=xb.ap())
    idn = din("idn", [128, 128])
    WF = {"g_pre": din("f2_gpre", [128, D]), "g_post": din("f2_gpost", [128, D]), "wg": din("f2_wg", [NFF, 128, 8, 128]),
          "wu": din("f2_wu", [NFF, 128, 8, 128]), "wd": din("f2_wd", [NFF, 128, D]), "idn": idn}
    if with_a:
        emit_tok(P, 2048, xb.ap(), xc.ap(), WF, zdst=None)
        zT = nc.dram_tensor("zT", [NWIN * 128, 2048], F32, kind="ExternalOutput").ap()
        WA = {"g_pre": din("f1_gpre", [128, D]), "g_post": din("f1_gpost", [128, D]), "wg": din("f1_wg", [NFF, 128, 8, 128]),
              "wu": din("f1_wu", [NFF, 128, 8, 128]), "wd": din("f1_wd", [NFF, 128, D]), "idn": idn,
              "g_mix": din("g_mix", [128, D]), "win": din("win", [NWIN, 128, 8, 128])}
        emit_tok(P, 2048, xc.ap(), xo, WA, zdst=zT)
    else:
        emit_tok(P, 2048, xb.ap(), xo, WF, zdst=None)
    P.finish()
    return nc

def kernel(**inp):
    inp = {k: np.asarray(v) for k, v in inp.items()}
    Bsz, L, Dm = 2, 8192, 1024
    NQ = L // 128
    x = _c(inp["x"].reshape(Bsz * L, Dm))
    idn = np.eye(128, dtype=np.float32)
    tri = np.triu(np.ones((128, 128), np.float32))
    negm = np.where(np.arange(128)[None, :] <= np.arange(128)[:, None], 0.0, -1e30).astype(np.float32)
    SEG = 512
    cmask = np.ones((64, 2 * SEG), np.float32); cmask[:, ::64] = 0
    sl = np.tril(np.ones((64, 64), np.float32), -1); su = sl.T; iu = np.triu(np.ones((64, 64), np.float32))
    mask5 = _c(np.concatenate([sl, su, iu, su, iu], 1))

    def a_weights(l):
        w_in = inp["w_in"][l]
        w_in_p = np.zeros((1024, 43 * 128), np.float32); w_in_p[:, :5448] = w_in[:, :5448]
        return {"g_pre": _bc(inp["ffn1_norm_pre"][l]), "g_post": _bc(inp["ffn1_norm_post"][l]),
                "wg": _wl(inp["ffn1_w_gate"][l], 22), "wu": _wl(inp["ffn1_w_up"][l], 22), "wd": _c(inp["ffn1_w_down"][l].reshape(22, 128, 1024)),
                "g_mix": _bc(inp["mix_norm_pre"][l]), "win": _wl(w_in_p, 43)}

    aw = a_weights(0)
    ncA = _prog("A", lambda: build_tok(False, True))
    res = _run(ncA, [dict(aw, idn=idn, x=x[c * 2048:(c + 1) * 2048]) for c in range(8)])
    x1 = np.concatenate([r["xo"] for r in res], 0)
    zT = np.concatenate([r["zT"] for r in res], 1)
    del res
    for l in range(2):
        w_in = inp["w_in"][l]
        li = 0.8 - 0.6 * _math.exp(-0.3 * l)
        lam = np.stack([inp["diff_lambda_q1"][l], inp["diff_lambda_k1"][l], inp["diff_lambda_q2"][l], inp["diff_lambda_k2"][l]])
        mu = inp["rwkv_mu"][l]
        maps = []
        for c in range(8):
            b, j = c // 4, c % 4
            zb = zT[:, b * L:(b + 1) * L]
            DO = 2120 + 1792
            zq = zb[DO:DO + 512]; zk = zb[DO + 512:DO + 1024]; zv = zb[DO + 1024:DO + 1536]
            qk = np.stack([zq[j * 128:j * 128 + 64], zq[j * 128 + 64:j * 128 + 128], zk[j * 128:j * 128 + 64], zk[j * 128 + 64:j * 128 + 128]])
            m_diff = {"qk": _c(qk), "v": _c(zv[j * 128:(j + 1) * 128].T.reshape(NQ, 128, 128).transpose(1, 0, 2)),
                      "lam": _c(np.broadcast_to(lam[None], (128, 4, 64))),
                      "cst": _c(np.broadcast_to(np.array([li, 1 - li], np.float32)[None], (128, 2))),
                      "gsub": _bc(inp["diff_subln"][l]), "tri": tri}
            q = zb[0:512][j * 128:(j + 1) * 128]; k = zb[512:1024][j * 128:(j + 1) * 128]; vv = zb[1024:1536][j * 128:(j + 1) * 128]
            qi = zb[1536:2048]; ki = zb[2048:2112]; wi = zb[2112:2120]
            m_dsa = {"qk": _c(np.stack([q, k])), "v": _c(vv.T.reshape(NQ, 128, 128).transpose(1, 0, 2)),
                     "qi": _c(qi.reshape(8, 64, NQ, 128).transpose(2, 1, 0, 3)), "ki": _c(ki),
                     "wi": _c(wi.T.reshape(NQ, 128, 8).transpose(1, 0, 2)), "negm": negm, "idn": idn}
            zrw = zb[2120:2120 + 1792]
            r_ = zrw[0:512].reshape(8, 64, L)[2 * j:2 * j + 2]; k_ = zrw[512:1024].reshape(8, 64, L)[2 * j:2 * j + 2]; v_ = zrw[1024:1536].reshape(8, 64, L)[2 * j:2 * j + 2]
            hp = lambda vec: np.ascontiguousarray(vec.reshape(8, 64)[2 * j:2 * j + 2].T)
            up = lambda w: w.reshape(64, 8, 64)[:, 2 * j:2 * j + 2, :]
            lnwb = np.stack([inp["rwkv_ln_w"][l][2 * j * 64:(2 * j + 2) * 64], inp["rwkv_ln_b"][l][2 * j * 64:(2 * j + 2) * 64]])
            m_rwkv = {"zr": _c(_pad1(np.stack([r_, k_, v_]).transpose(0, 2, 1, 3))),
                      "zl": _c(_pad1(np.stack([zrw[1536:1600], zrw[1600:1664]], 1))), "zg": _c(_pad1(zrw[1664:1792])),
                      "mu3": _c(np.stack([hp(mu[0:512]), hp(mu[512:1024]), hp(mu[1024:1536])], 1)),
                      "mul": _c(np.stack([mu[1536:1600], mu[1600:1664]], 1)), "mug": _c(mu[1664:1792][:, None]),
                      "pp": _c(np.stack([hp(inp["rwkv_w0"][l]), hp(inp["rwkv_a0"][l]), hp(inp["rwkv_k_k"][l]), hp(inp["rwkv_k_a"][l]), hp(inp["rwkv_r_k"][l])], 1)),
                      "wup": _c(up(inp["rwkv_w_up"][l])), "aup": _c(up(inp["rwkv_a_up"][l])),
                      "gup": _c(inp["rwkv_g_up"][l][:, 2 * j * 64:(2 * j + 2) * 64]),
                      "lnwb": _c(np.broadcast_to(lnwb[None], (64, 2, 128))), "cmask": cmask, "mask5": mask5, "idn": np.eye(64, dtype=np.float32)}
            m = {}
            m.update(_pfx("a_", m_dsa)); m.update(_pfx("b_", m_rwkv)); m.update(_pfx("c_", m_diff))
            maps.append(m)
        del zT
        res = _run(_prog("MIX", lambda: build_mix(L)), maps); del maps
        Y = np.zeros((3, Bsz * L, 512), np.float32)
        for c in range(8):
            b, j = c // 4, c % 4
            Y[0, b * L:(b + 1) * L, j * 128:(j + 1) * 128] = res[c]["a_y"].reshape(L, 128)
            Y[1, b * L:(b + 1) * L, j * 128:(j + 1) * 128] = res[c]["b_y"].reshape(L, 128)
            Y[2, b * L:(b + 1) * L, j * 128:(j + 1) * 128] = res[c]["c_y"].reshape(L, 128)
        del res
        cm = {"m_g_mpre": _bc(inp["mix_norm_pre"][l]), "m_g_mpost": _bc(inp["mix_norm_post"][l]),
              "m_wgate": _c(w_in[:, 5448:8520].reshape(8, 128, 3072)), "m_wbr": _c(inp["w_branch"][l].reshape(12, 128, 1024)),
              "m_wo": _c(inp["w_out"][l].reshape(8, 128, 1024)), "m_idn": idn, "idn": idn,
              "f2_gpre": _bc(inp["ffn2_norm_pre"][l]), "f2_gpost": _bc(inp["ffn2_norm_post"][l]),
              "f2_wg": _wl(inp["ffn2_w_gate"][l], 22), "f2_wu": _wl(inp["ffn2_w_up"][l], 22), "f2_wd": _c(inp["ffn2_w_down"][l].reshape(22, 128, 1024))}
        last = (l == 1)
        if not last:
            aw = a_weights(l + 1)
            cm.update({"f1_gpre": aw["g_pre"], "f1_gpost": aw["g_post"], "f1_wg": aw["wg"], "f1_wu": aw["wu"], "f1_wd": aw["wd"], "g_mix": aw["g_mix"], "win": aw["win"]})
        maps = []
        for c in range(8):
            yc_ = Y[:, c * 2048:(c + 1) * 2048, :].reshape(3, 16, 128, 4, 128)
            maps.append(dict(cm, m_x=x1[c * 2048:(c + 1) * 2048], m_yT=_c(yc_.transpose(1, 4, 0, 3, 2).reshape(16, 128, 12 * 128))))
        del Y
        res = _run(_prog("MF" if last else "MFA", (lambda: build_mfa(False)) if last else (lambda: build_mfa(True))), maps); del maps
        x1 = np.concatenate([r["xo"] for r in res], 0)
        if not last:
            zT = np.concatenate([r["zT"] for r in res], 1)
        del res
    return x1.reshape(Bsz, L, Dm).astype(np.float32)
```

```python
import numpy as np
import concourse.bass as bass
import concourse.mybir as mybir
from concourse.bass_utils import run_bass_kernel_spmd
from contextlib import ExitStack

F32 = mybir.dt.float32
BF16 = mybir.dt.bfloat16
AF = mybir.ActivationFunctionType
ALU = mybir.AluOpType
AX = mybir.AxisListType


class BufOld:
    __slots__ = ("name", "w", "r", "sem", "cnt")

    def __init__(self, name):
        self.name = name
        self.w = None
        self.r = {}
        self.sem = None
        self.cnt = 0


class ProgOld:
    ENG = ("pe", "act", "dve", "pool", "sp")

    def __init__(self, nc):
        self.nc = nc
        self.ops = {e: [] for e in self.ENG}
        self.cnt = {e: 0 for e in self.ENG}
        self.seen = {e: {} for e in self.ENG}
        self.dma_bufs = []
        self.stack = ExitStack()
        self.nbuf = 0

    def sb(self, name, shape, dt=F32):
        return self.stack.enter_context(self.nc.sbuf_tensor(name, list(shape), dt))

    def ps(self, name, shape, dt=F32):
        return self.stack.enter_context(self.nc.psum_tensor(name, list(shape), dt))

    def buf(self, name=None):
        self.nbuf += 1
        return BufOld(name or f"b{self.nbuf}")

    def _waits(self, eng, reads, writes):
        need = {}

        def add(k, v):
            if need.get(k, 0) < v:
                need[k] = v
        for b in reads:
            if b.w is not None:
                add(*b.w)
        for b in writes:
            if b.w is not None:
                add(*b.w)
            for k, v in b.r.items():
                add(k, v)
        out = []
        seen = self.seen[eng]
        for k, v in need.items():
            if k == "pe" and eng == "pe":
                continue
            if seen.get(k, 0) >= v:
                continue
            seen[k] = v
            out.append((k, v))
        return out

    def _mark(self, tok, reads, writes):
        k, v = tok
        for b in reads:
            if b.r.get(k, 0) < v:
                b.r[k] = v
        for b in writes:
            b.w = tok
            b.r = {}

    def op(self, eng, fn, reads=(), writes=()):
        waits = self._waits(eng, reads, writes)
        self.cnt[eng] += 1
        tok = (eng, self.cnt[eng])
        self._mark(tok, reads, writes)
        self.ops[eng].append((waits, fn, (eng, 1)))

    def dma(self, q, fn, reads=(), writes=(), sembuf=None):
        waits = self._waits(q, reads, writes)
        sbf = sembuf if sembuf is not None else (writes[0] if writes else reads[0])
        if sbf.sem is None:
            sbf.sem = self.stack.enter_context(self.nc.semaphore(f"d_{sbf.name}_{len(self.dma_bufs)}"))
            self.dma_bufs.append(sbf)
        sbf.cnt += 16
        tok = (sbf, sbf.cnt)
        self._mark(tok, reads, writes)
        self.ops[q].append((waits, fn, (sbf, 16)))

    def emit(self):
        nc = self.nc
        sems = {e: self.stack.enter_context(nc.semaphore(f"s_{e}")) for e in self.ENG}

        def semof(k):
            return sems[k] if isinstance(k, str) else k.sem
        final_waits = [(b, b.cnt) for b in self.dma_bufs if self.seen["sp"].get(b, 0) < b.cnt]
        for e in ("pe", "act", "dve", "pool"):
            if self.cnt[e] > 0:
                final_waits.append((e, self.cnt[e]))
        self.ops["sp"].append((final_waits, None, None))
        block = self.stack.enter_context(nc.Block())

        def run(engobj, lst):
            for waits, fn, inc in lst:
                for k, v in waits:
                    engobj.wait_ge(semof(k), v)
                if fn is None:
                    continue
                ins = fn(engobj)
                if inc is not None:
                    ins.then_inc(semof(inc[0]), inc[1])

        @block.tensor
        def _(e):
            run(e, self.ops["pe"])

        @block.scalar
        def _(e):
            run(e, self.ops["act"])

        @block.vector
        def _(e):
            run(e, self.ops["dve"])

        @block.gpsimd
        def _(e):
            run(e, self.ops["pool"])

        @block.sync
        def _(e):
            run(e, self.ops["sp"])

    def close(self):
        self.stack.close()


D = 1024; DFF = 2816; NFF = 22; TOK = 2048; NT = 16; EPS = 1e-6
NWIN = 43

def build_tok(has_merge, has_win):
    nc = bass.Bass("TRN2", target_bir_lowering=False)
    P = ProgOld(nc)
    def din(name, shape): return nc.dram_tensor(name, list(shape), F32, kind="ExternalInput").ap()
    def dout(name, shape): return nc.dram_tensor(name, list(shape), F32, kind="ExternalOutput").ap()
    x_in = din("x", [TOK, D])
    g_pre = din("g_pre", [128, D]); g_post = din("g_post", [128, D])
    wg = din("wg", [NFF, 128, 8, 128]); wu = din("wu", [NFF, 128, 8, 128]); wd = din("wd", [NFF, 128, D])
    idn_d = din("idn", [128, 128])
    x_out = dout("xo", [TOK, D])
    if has_win:
        g_mix = din("g_mix", [128, D]); win = din("win", [NWIN, 128, 8, 128]); zT = dout("zT", [NWIN * 128, TOK])
    if has_merge:
        g_mpre = din("g_mpre", [128, D]); g_mpost = din("g_mpost", [128, D])
        wgate = din("wgate", [8, 128, 3072]); wbr = din("wbr", [12, 128, D]); wo = din("wo", [8, 128, D])
        yT = din("yT", [NT, 128, 12, 128])
        x2d = dout("x2", [TOK, D])

    ident_f = P.sb("ident_f", [128, 128]); ident = P.sb("ident", [128, 128], BF16)
    gpre_t = P.sb("gpre_t", [128, D]); gpost_t = P.sb("gpost_t", [128, D])
    hT = P.sb("hT", [128, 8, TOK], BF16)
    AT = P.sb("AT", [128, NFF, 1024], BF16)
    Wd = P.sb("Wd", [128, NFF, D], BF16)
    stg = [P.sb(f"stg{i}", [128, 3072]) for i in range(2)]
    wgb = [P.sb(f"wgb{i}", [128, 8, 128], BF16) for i in range(2)]
    wub = [P.sb(f"wub{i}", [128, 8, 128], BF16) for i in range(2)]
    xt = [P.sb(f"xt{i}", [128, D]) for i in range(2)]
    ot = [P.sb(f"ot{i}", [128, D]) for i in range(2)]
    junk = P.sb("junk", [128, D])
    hb = [P.sb(f"hb{i}", [128, D], BF16) for i in range(2)]
    sg = [P.sb(f"sg{i}", [128, 512]) for i in range(2)]
    st = P.sb("st", [128, 64])
    pA = [P.ps(f"pA{i}", [128, 512]) for i in range(2)]
    pB = [P.ps(f"pB{i}", [128, 512]) for i in range(2)]
    pC = [P.ps(f"pC{i}", [128, 512]) for i in range(2)]
    pT = [P.ps(f"pT{i}", [128, 1024], BF16) for i in range(2)]
    B = P.buf
    b_ident = B(); b_identf = B(); b_gpre = B(); b_gpost = B(); b_hT = [B() for _ in range(NT)]
    b_AT = [[B() for _ in range(2)] for _ in range(NFF)]
    b_Wd = [B() for _ in range(NFF)]
    b_stg = [B(), B()]; b_wgb = [B(), B()]; b_wub = [B(), B()]; b_xt = [B(), B()]; b_ot = [B(), B()]
    b_junk = B(); b_hb = [B(), B()]; b_sg = [B(), B()]
    b_pA = [B(), B()]; b_pB = [B(), B()]; b_pC = [B(), B()]; b_pT = [B(), B()]
    st_next = [0]
    b_st = {}
    def stcol():
        i = st_next[0] % 64; st_next[0] += 1
        if i not in b_st: b_st[i] = B()
        return st[:, i:i + 1], b_st[i]

    P.dma("sp", lambda e: e.dma_start(out=ident_f[:], in_=idn_d), writes=[b_identf])
    P.op("dve", lambda e: e.tensor_copy(out=ident[:], in_=ident_f[:]), reads=[b_identf], writes=[b_ident])
    P.dma("sp", lambda e: e.dma_start(out=gpre_t[:], in_=g_pre), writes=[b_gpre])
    P.dma("sp", lambda e: e.dma_start(out=gpost_t[:], in_=g_post), writes=[b_gpost])

    def rstd_of(src_ap, src_bufs):
        ss, bss = stcol(); rs, brs = stcol()
        P.op("act", lambda e: e.activation(out=junk[:], in_=src_ap, func=AF.Square, scale=float(D ** -0.5), accum_out=ss),
             reads=src_bufs, writes=[b_junk, bss])
        P.op("dve", lambda e: e.tensor_scalar(out=rs, in0=ss, scalar1=EPS, scalar2=None, op0=ALU.add),
             reads=[bss], writes=[brs])
        P.op("act", lambda e: e.activation(out=rs, in_=rs, func=AF.Sqrt), reads=[brs], writes=[brs])
        P.op("dve", lambda e: e.reciprocal(out=rs, in_=rs), reads=[brs], writes=[brs])
        return rs, brs

    def norm_to_hT(src_ap, src_bufs, g_t, b_g, ti, par):
        rs, brs = rstd_of(src_ap, src_bufs)
        P.op("dve", lambda e: e.scalar_tensor_tensor(out=hb[par][:], in0=src_ap, scalar=rs, in1=g_t[:], op0=ALU.mult, op1=ALU.mult),
             reads=src_bufs + [brs, b_g], writes=[b_hb[par]])
        for k in range(8):
            P.op("pe", lambda e, k=k: e.transpose(pT[par][:, k * 128:(k + 1) * 128], hb[par][:, k * 128:(k + 1) * 128], ident[:]),
                 reads=[b_hb[par], b_ident], writes=[b_pT[par]])
        P.op("act", lambda e: e.copy(out=hT[:, :, ti * 128:(ti + 1) * 128], in_=pT[par][:].rearrange("p (k t) -> p k t", k=8)),
             reads=[b_pT[par]], writes=[b_hT[ti]])

    def ffn_stage(xsrc, xsrc_bufs, xdst, post_tile=None):
        dst_bufs = [B() for _ in range(NT)]
        for ti in range(NT):
            par = ti % 2
            rb = [xsrc_bufs[ti]] if xsrc_bufs[ti] is not None else []
            P.dma("sp", lambda e, ti=ti, par=par: e.dma_start(out=xt[par][:], in_=xsrc[ti * 128:(ti + 1) * 128, :]),
                  reads=rb, writes=[b_xt[par]])
            norm_to_hT(xt[par][:], [b_xt[par]], gpre_t, b_gpre, ti, par)
        for c in range(NFF):
            s = c % 2
            P.dma("sp", lambda e, c=c, s=s: e.dma_start(out=stg[s][:, 0:D], in_=wd[c]), writes=[b_stg[s]])
            P.op("pool", lambda e, c=c, s=s: e.tensor_copy(out=Wd[:, c, :], in_=stg[s][:, 0:D]), reads=[b_stg[s]], writes=[b_Wd[c]])
        for half in range(2):
            for c in range(NFF):
                s = c % 2
                P.dma("sp", lambda e, c=c, s=s: e.dma_start(out=stg[s][:, 0:1024], in_=wg[c].rearrange("p k f -> p (k f)")), writes=[b_stg[s]])
                P.op("pool", lambda e, s=s: e.tensor_copy(out=wgb[s][:].rearrange("p k f -> p (k f)"), in_=stg[s][:, 0:1024]), reads=[b_stg[s]], writes=[b_wgb[s]])
                P.dma("sp", lambda e, c=c, s=s: e.dma_start(out=stg[s][:, 1024:2048], in_=wu[c].rearrange("p k f -> p (k f)")), writes=[b_stg[s]])
                P.op("pool", lambda e, s=s: e.tensor_copy(out=wub[s][:].rearrange("p k f -> p (k f)"), in_=stg[s][:, 1024:2048]), reads=[b_stg[s]], writes=[b_wub[s]])
                for tb in range(2):
                    t0 = half * 1024 + tb * 512
                    rd = [b_hT[(t0 // 128) + i] for i in range(4)]
                    q = tb
                    for k in range(8):
                        P.op("pe", lambda e, k=k, s=s, q=q, t0=t0: e.matmul(pA[q][:], lhsT=wgb[s][:, k, :], rhs=hT[:, k, t0:t0 + 512], start=(k == 0), stop=(k == 7)),
                             reads=[b_wgb[s]] + rd, writes=[b_pA[q]])
                    for k in range(8):
                        P.op("pe", lambda e, k=k, s=s, q=q, t0=t0: e.matmul(pB[q][:], lhsT=wub[s][:, k, :], rhs=hT[:, k, t0:t0 + 512], start=(k == 0), stop=(k == 7)),
                             reads=[b_wub[s]] + rd, writes=[b_pB[q]])
                    P.op("act", lambda e, q=q: e.activation(out=sg[q][:], in_=pA[q][:], func=AF.Silu), reads=[b_pA[q]], writes=[b_sg[q]])
                    P.op("dve", lambda e, q=q, c=c, tb=tb: e.tensor_tensor(out=AT[:, c, tb * 512:(tb + 1) * 512], in0=sg[q][:], in1=pB[q][:], op=ALU.mult),
                         reads=[b_sg[q], b_pB[q]], writes=[b_AT[c][tb]])
            for tl in range(8):
                ti = half * 8 + tl; par = ti % 2
                rb = [xsrc_bufs[ti]] if xsrc_bufs[ti] is not None else []
                P.dma("sp", lambda e, ti=ti, par=par: e.dma_start(out=xt[par][:], in_=xsrc[ti * 128:(ti + 1) * 128, :]),
                      reads=rb, writes=[b_xt[par]])
                for ch in range(2):
                    for c in range(NFF):
                        P.op("pe", lambda e, c=c, ch=ch, tl=tl: e.matmul(pC[ch][:], lhsT=AT[:, c, tl * 128:(tl + 1) * 128], rhs=Wd[:, c, ch * 512:(ch + 1) * 512], start=(c == 0), stop=(c == NFF - 1)),
                             reads=[b_AT[c][tl // 4], b_Wd[c]], writes=[b_pC[ch]])
                    P.op("act", lambda e, ch=ch, par=par: e.copy(out=ot[par][:, ch * 512:(ch + 1) * 512], in_=pC[ch][:]), reads=[b_pC[ch]], writes=[b_ot[par]])
                rs, brs = rstd_of(ot[par][:], [b_ot[par]])
                P.op("dve", lambda e, par=par, rs=rs: e.scalar_tensor_tensor(out=ot[par][:], in0=ot[par][:], scalar=rs, in1=gpost_t[:], op0=ALU.mult, op1=ALU.mult),
                     reads=[b_ot[par], brs, b_gpost], writes=[b_ot[par]])
                P.op("dve", lambda e, par=par: e.scalar_tensor_tensor(out=ot[par][:], in0=ot[par][:], scalar=0.5, in1=xt[par][:], op0=ALU.mult, op1=ALU.add),
                     reads=[b_ot[par], b_xt[par]], writes=[b_ot[par]])
                P.dma("pool", lambda e, ti=ti, par=par: e.dma_start(out=xdst[ti * 128:(ti + 1) * 128, :], in_=ot[par][:]),
                      reads=[b_ot[par]], writes=[dst_bufs[ti]], sembuf=b_ot[par])
                if post_tile is not None:
                    post_tile(ti, par)
        return dst_bufs

    src_bufs = [None] * NT
    xsrc = x_in
    if has_merge:
        raise NotImplementedError
    if has_win:
        gmix_t = P.sb("gmix_t", [128, D]); b_gmix = B()
        P.dma("sp", lambda e: e.dma_start(out=gmix_t[:], in_=g_mix), writes=[b_gmix])
        hT2 = hT; b_hT2 = b_hT
        def post(ti, par):
            rs, brs = rstd_of(ot[par][:], [b_ot[par]])
            P.op("dve", lambda e: e.scalar_tensor_tensor(out=hb[par][:], in0=ot[par][:], scalar=rs, in1=gmix_t[:], op0=ALU.mult, op1=ALU.mult),
                 reads=[b_ot[par], brs, b_gmix], writes=[b_hb[par]])
            for k in range(8):
                P.op("pe", lambda e, k=k: e.transpose(pT[par][:, k * 128:(k + 1) * 128], hb[par][:, k * 128:(k + 1) * 128], ident[:]),
                     reads=[b_hb[par], b_ident], writes=[b_pT[par]])
            P.op("act", lambda e: e.copy(out=hT2[:, :, ti * 128:(ti + 1) * 128], in_=pT[par][:].rearrange("p (k t) -> p k t", k=8)),
                 reads=[b_pT[par]], writes=[b_hT2[ti]])
        ffn_stage(xsrc, src_bufs, x_out, post)
        zs = [P.sb(f"zs{i}", [128, 1024]) for i in range(2)]; b_zs = [B(), B()]
        for c in range(NWIN):
            s = c % 2
            P.dma("sp", lambda e, c=c, s=s: e.dma_start(out=stg[s][:, 0:1024], in_=win[c].rearrange("p k f -> p (k f)")), writes=[b_stg[s]])
            P.op("pool", lambda e, s=s: e.tensor_copy(out=wgb[s][:].rearrange("p k f -> p (k f)"), in_=stg[s][:, 0:1024]), reads=[b_stg[s]], writes=[b_wgb[s]])
            for tb in range(4):
                q = tb % 2; t0 = tb * 512; zi = tb // 2
                rd = [b_hT2[(t0 // 128) + i] for i in range(4)]
                for k in range(8):
                    P.op("pe", lambda e, k=k, s=s, q=q, t0=t0: e.matmul(pA[q][:], lhsT=wgb[s][:, k, :], rhs=hT2[:, k, t0:t0 + 512], start=(k == 0), stop=(k == 7)),
                         reads=[b_wgb[s]] + rd, writes=[b_pA[q]])
                if tb % 2 == 0:
                    P.op("act", lambda e, q=q, zi=zi: e.copy(out=zs[zi][:, 0:512], in_=pA[q][:]), reads=[b_pA[q]], writes=[b_zs[zi]])
                else:
                    P.op("dve", lambda e, q=q, zi=zi: e.tensor_copy(out=zs[zi][:, 512:1024], in_=pA[q][:]), reads=[b_pA[q]], writes=[b_zs[zi]])
                    P.dma("pool", lambda e, c=c, zi=zi: e.dma_start(out=zT[c * 128:(c + 1) * 128, zi * 1024:(zi + 1) * 1024], in_=zs[zi][:]), reads=[b_zs[zi]], sembuf=b_zs[zi])
    else:
        ffn_stage(xsrc, src_bufs, x_out, None)
    P.emit(); P.close()
    return nc


def build_diff(L):
    NQ = L // 128
    nc = bass.Bass("TRN2", target_bir_lowering=False)
    P = ProgOld(nc); B = P.buf
    def din(name, shape): return nc.dram_tensor(name, list(shape), F32, kind="ExternalInput").ap()
    qk_d = din("qk", [4, 64, L])
    v_d = din("v", [128, NQ, 128])
    lam_d = din("lam", [128, 4, 64])
    cst_d = din("cst", [128, 2])
    gsub_d = din("gsub", [128, 128])
    tri_d = din("tri", [128, 128])
    y_d = nc.dram_tensor("y", [NQ, 128, 128], F32, kind="ExternalOutput").ap()

    qkb = [P.sb(f"qkb{i}", [64, L], BF16) for i in range(4)]; b_qkb = [B() for _ in range(4)]
    vb = P.sb("vb", [128, NQ, 130], BF16); b_vb = B()
    stg = [P.sb(f"stg{i}", [128, 2048]) for i in range(2)]; b_stg = [B(), B()]
    lam_t = P.sb("lam_t", [128, 4, 64]); b_lam = B()
    cst = P.sb("cst_t", [128, 2]); b_cst = B()
    gsub = P.sb("gsub_t", [128, 128]); b_gsub = B()
    tri_f = P.sb("tri_f", [128, 128]); b_trif = B()
    tri = P.sb("tri_b", [128, 128], BF16); b_tri = B()
    ones = P.sb("ones", [128, 2], BF16); b_ones = B()
    sm = P.sb("sm", [128, 16]); b_sm = [B() for _ in range(16)]
    junk = P.sb("junk", [128, 128]); b_junk = B()
    ET = [[P.sb(f"ET{m}{i}", [128, 4, 128], BF16) for i in range(2)] for m in range(2)]
    b_ET = [[B(), B()] for _ in range(2)]
    ob = [P.sb(f"ob{i}", [128, 128]) for i in range(2)]; b_ob = [B(), B()]
    t2 = P.sb("t2", [128, 128]); b_t2 = B()
    pS = [[P.ps(f"pS{m}{i}", [128, 512]) for i in range(2)] for m in range(2)]; b_pS = [[B(), B()] for _ in range(2)]
    pO = [P.ps(f"pO{m}", [128, 512]) for m in range(2)]; b_pO = [B(), B()]

    n = 0
    for i in range(4):
        CW = min(2048, L)
        for c0 in range(0, L, CW):
            s = n % 2; n += 1
            P.dma("sp", lambda e, i=i, c0=c0, s=s: e.dma_start(out=stg[s][0:64, 0:CW], in_=qk_d[i, :, c0:c0 + CW]), writes=[b_stg[s]])
            P.op("pool", lambda e, i=i, c0=c0, s=s: e.tensor_copy(out=qkb[i][:, c0:c0 + CW], in_=stg[s][0:64, 0:CW]), reads=[b_stg[s]], writes=[b_qkb[i]])
    TW = min(16, NQ)
    for t0 in range(0, NQ, TW):
        s = n % 2; n += 1
        P.dma("sp", lambda e, t0=t0, s=s: e.dma_start(out=stg[s][:, 0:TW * 128], in_=v_d[:, t0:t0 + TW, :].rearrange("p t d -> p (t d)")), writes=[b_stg[s]])
        P.op("pool", lambda e, t0=t0, s=s: e.tensor_copy(out=vb[:, t0:t0 + TW, 0:128], in_=stg[s][:, 0:TW * 128].rearrange("p (t d) -> p t d", d=128)), reads=[b_stg[s]], writes=[b_vb])
    P.dma("sp", lambda e: e.dma_start(out=lam_t[:], in_=lam_d), writes=[b_lam])
    P.dma("sp", lambda e: e.dma_start(out=cst[:], in_=cst_d), writes=[b_cst])
    P.dma("sp", lambda e: e.dma_start(out=gsub[:], in_=gsub_d), writes=[b_gsub])
    P.dma("sp", lambda e: e.dma_start(out=tri_f[:], in_=tri_d), writes=[b_trif])
    P.op("dve", lambda e: e.tensor_copy(out=tri[:], in_=tri_f[:]), reads=[b_trif], writes=[b_tri])
    P.op("dve", lambda e: e.memset(vb[:, :, 128:130], 1.0), writes=[b_vb])
    for j in range(2):
        P.op("dve", lambda e, j=j: e.tensor_tensor(out=junk[:, 0:64], in0=lam_t[:, 2 * j, :], in1=lam_t[:, 2 * j + 1, :], op=ALU.mult), reads=[b_lam], writes=[b_junk])
        P.op("dve", lambda e, j=j: e.reduce_sum(out=sm[:, j:j + 1], in_=junk[:, 0:64], axis=AX.X), reads=[b_junk], writes=[b_sm[j]])
        P.op("act", lambda e, j=j: e.activation(out=sm[:, j:j + 1], in_=sm[:, j:j + 1], func=AF.Exp), reads=[b_sm[j]], writes=[b_sm[j]])
    P.op("dve", lambda e: e.tensor_tensor(out=sm[:, 2:3], in0=sm[:, 1:2], in1=sm[:, 0:1], op=ALU.subtract), reads=[b_sm[0], b_sm[1]], writes=[b_sm[2]])
    P.op("dve", lambda e: e.tensor_tensor(out=sm[:, 2:3], in0=sm[:, 2:3], in1=cst[:, 0:1], op=ALU.subtract), reads=[b_sm[2], b_cst], writes=[b_sm[2]])
    NEGLAM = (sm[:, 2:3], b_sm[2])

    gi = 0
    for qi in range(NQ):
        nk = qi + 1
        groups = [(g0, min(4, nk - g0)) for g0 in range(0, nk, 4)]
        for gidx, (g0, gn) in enumerate(groups):
            par = gi % 2; gi += 1
            for m in range(2):
                for j in range(gn):
                    kt = g0 + j
                    P.op("pe", lambda e, m=m, j=j, kt=kt, par=par, qi=qi: e.matmul(pS[m][par][:, j * 128:(j + 1) * 128], lhsT=qkb[2 + m][:, kt * 128:(kt + 1) * 128], rhs=qkb[m][:, qi * 128:(qi + 1) * 128], start=True, stop=True),
                         reads=[b_qkb[2 + m], b_qkb[m]], writes=[b_pS[m][par]])
                P.op("act", lambda e, m=m, par=par, gn=gn: e.activation(out=ET[m][par][:, 0:gn, :].rearrange("p g q -> p (g q)"), in_=pS[m][par][:, 0:gn * 128], func=AF.Exp, scale=0.125),
                     reads=[b_pS[m][par]], writes=[b_ET[m][par]])
                if g0 + gn == nk:
                    j = gn - 1
                    P.op("dve", lambda e, m=m, par=par, j=j: e.tensor_tensor(out=ET[m][par][:, j, :], in0=ET[m][par][:, j, :], in1=tri[:], op=ALU.mult),
                         reads=[b_ET[m][par], b_tri], writes=[b_ET[m][par]])
                for j in range(gn):
                    kt = g0 + j
                    first = (kt == 0); last = (kt == nk - 1)
                    P.op("pe", lambda e, m=m, j=j, kt=kt, par=par, first=first, last=last: e.matmul(pO[m][:, 0:130], lhsT=ET[m][par][:, j, :], rhs=vb[:, kt, :], start=first, stop=last),
                         reads=[b_ET[m][par], b_vb], writes=[b_pO[m]])
        op_ = qi % 2
        P.op("dve", lambda e: e.reciprocal(out=sm[:, 4:5], in_=pO[0][:, 128:129]), reads=[b_pO[0]], writes=[b_sm[4]])
        P.op("dve", lambda e: e.reciprocal(out=sm[:, 5:6], in_=pO[1][:, 128:129]), reads=[b_pO[1]], writes=[b_sm[5]])
        P.op("dve", lambda e: e.tensor_tensor(out=sm[:, 5:6], in0=sm[:, 5:6], in1=NEGLAM[0], op=ALU.mult), reads=[b_sm[5], NEGLAM[1]], writes=[b_sm[5]])
        P.op("dve", lambda e: e.tensor_scalar(out=t2[:], in0=pO[1][:, 0:128], scalar1=sm[:, 5:6], scalar2=None, op0=ALU.mult), reads=[b_pO[1], b_sm[5]], writes=[b_t2])
        P.op("dve", lambda e, op_=op_: e.scalar_tensor_tensor(out=ob[op_][:], in0=pO[0][:, 0:128], scalar=sm[:, 4:5], in1=t2[:], op0=ALU.mult, op1=ALU.add), reads=[b_pO[0], b_sm[4], b_t2], writes=[b_ob[op_]])
        P.op("act", lambda e, op_=op_: e.activation(out=junk[:], in_=ob[op_][:], func=AF.Square, scale=float(128 ** -0.5), accum_out=sm[:, 6:7]), reads=[b_ob[op_]], writes=[b_junk, b_sm[6]])
        P.op("dve", lambda e: e.tensor_scalar(out=sm[:, 6:7], in0=sm[:, 6:7], scalar1=1e-6, scalar2=None, op0=ALU.add), reads=[b_sm[6]], writes=[b_sm[6]])
        P.op("act", lambda e: e.activation(out=sm[:, 6:7], in_=sm[:, 6:7], func=AF.Sqrt), reads=[b_sm[6]], writes=[b_sm[6]])
        P.op("dve", lambda e: e.reciprocal(out=sm[:, 6:7], in_=sm[:, 6:7]), reads=[b_sm[6]], writes=[b_sm[6]])
        P.op("dve", lambda e: e.tensor_tensor(out=sm[:, 6:7], in0=sm[:, 6:7], in1=cst[:, 1:2], op=ALU.mult), reads=[b_sm[6], b_cst], writes=[b_sm[6]])
        P.op("dve", lambda e, op_=op_: e.scalar_tensor_tensor(out=ob[op_][:], in0=ob[op_][:], scalar=sm[:, 6:7], in1=gsub[:], op0=ALU.mult, op1=ALU.mult), reads=[b_ob[op_], b_sm[6], b_gsub], writes=[b_ob[op_]])
        P.dma("pool", lambda e, qi=qi, op_=op_: e.dma_start(out=y_d[qi], in_=ob[op_][:]), reads=[b_ob[op_]], sembuf=b_ob[op_])
    P.emit(); P.close()
    return nc


def build_dsa(L, R=32.0, K=22):
    NQ = L // 128
    nc = bass.Bass("TRN2", target_bir_lowering=False)
    P = ProgOld(nc); B = P.buf
    def din(name, shape): return nc.dram_tensor(name, list(shape), F32, kind="ExternalInput").ap()
    qk_d = din("qk", [2, 128, L])
    v_d = din("v", [128, NQ, 128])
    qi_d = din("qi", [NQ, 64, 8, 128])
    ki_d = din("ki", [64, L])
    wi_d = din("wi", [128, NQ, 8])
    negm_d = din("negm", [128, 128])
    idn_d = din("idn", [128, 128])
    y_d = nc.dram_tensor("y", [NQ, 128, 128], F32, kind="ExternalOutput").ap()
    dbg_d = nc.dram_tensor("dbg", [NQ, 128, 8], F32, kind="ExternalOutput").ap()
    dbg = [P.sb(f"dbg{i}", [128, 8]) for i in range(2)]; b_dbg = [B(), B()]

    qkb = [P.sb(f"qkb{i}", [128, L], BF16) for i in range(2)]; b_qkb = [B(), B()]
    vb = P.sb("vb", [128, NQ, 130], BF16); b_vb = B()
    kiT = P.sb("kiT", [64, L]); b_ki = B()
    wi = P.sb("wi_t", [128, NQ, 8]); b_wi = B()
    negm = P.sb("negm_t", [128, 128]); b_negm = B()
    idf = P.sb("idf", [128, 128]); b_idf = B()
    idb = P.sb("idb", [128, 128], BF16); b_idb = B()
    stg = [P.sb(f"stg{i}", [128, 2048]) for i in range(2)]; b_stg = [B(), B()]
    qit = [P.sb(f"qit{i}", [64, 8, 128]) for i in range(2)]; b_qit = [B(), B()]
    score = [P.sb(f"score{i}", [128, L]) for i in range(2)]; b_score = [B(), B()]
    junkS = P.sb("junkS", [128, L], BF16); b_junkS = B()
    rl = [P.sb(f"rl{i}", [128, 512]) for i in range(2)]; b_rl = [B(), B()]
    Eb = [P.sb(f"Eb{i}", [128, 512], BF16) for i in range(2)]; b_Eb = [B(), B()]
    Pm = [P.sb(f"Pm{i}", [128, 512], BF16) for i in range(2)]; b_Pm = [B(), B()]
    PmT = [P.sb(f"PmT{i}", [128, 4, 128], BF16) for i in range(2)]; b_PmT = [B(), B()]
    ob = [P.sb(f"ob{i}", [128, 128]) for i in range(2)]; b_ob = [B(), B()]
    sm = P.sb("sm", [128, 8]); b_sm = [B() for _ in range(8)]
    pD = [P.ps(f"pD{i}", [128, 512]) for i in range(2)]; b_pD = [B(), B()]
    pS = [P.ps(f"pS{i}", [128, 512]) for i in range(2)]; b_pS = [B(), B()]
    pT = [P.ps(f"pT{i}", [128, 1024], BF16) for i in range(2)]; b_pT = [B(), B()]
    pO = P.ps("pO", [128, 512]); b_pO = B()

    n = 0
    CW = min(2048, L)
    for i in range(2):
        for c0 in range(0, L, CW):
            s = n % 2; n += 1
            P.dma("sp", lambda e, i=i, c0=c0, s=s: e.dma_start(out=stg[s][:, 0:CW], in_=qk_d[i, :, c0:c0 + CW]), writes=[b_stg[s]])
            P.op("pool", lambda e, i=i, c0=c0, s=s: e.tensor_copy(out=qkb[i][:, c0:c0 + CW], in_=stg[s][:, 0:CW]), reads=[b_stg[s]], writes=[b_qkb[i]])
    TW = min(16, NQ)
    for t0 in range(0, NQ, TW):
        s = n % 2; n += 1
        P.dma("sp", lambda e, t0=t0, s=s: e.dma_start(out=stg[s][:, 0:TW * 128], in_=v_d[:, t0:t0 + TW, :].rearrange("p t d -> p (t d)")), writes=[b_stg[s]])
        P.op("pool", lambda e, t0=t0, s=s: e.tensor_copy(out=vb[:, t0:t0 + TW, 0:128], in_=stg[s][:, 0:TW * 128].rearrange("p (t d) -> p t d", d=128)), reads=[b_stg[s]], writes=[b_vb])
    P.op("dve", lambda e: e.memset(vb[:, :, 128:130], 1.0), writes=[b_vb])
    P.dma("sp", lambda e: e.dma_start(out=kiT[:], in_=ki_d), writes=[b_ki])
    P.dma("sp", lambda e: e.dma_start(out=wi[:], in_=wi_d), writes=[b_wi])
    P.dma("sp", lambda e: e.dma_start(out=negm[:], in_=negm_d), writes=[b_negm])
    P.dma("sp", lambda e: e.dma_start(out=idf[:], in_=idn_d), writes=[b_idf])
    P.op("dve", lambda e: e.tensor_copy(out=idb[:], in_=idf[:]), reads=[b_idf], writes=[b_idb])
    SC = float((64 ** -0.5) * (8 ** -0.5))
    ci = 0; ai = 0
    for qi in range(NQ):
        nk = qi + 1; nkeys = nk * 128
        sp_ = qi % 2
        sc = score[sp_]; bsc = b_score[sp_]
        P.dma("sp", lambda e, qi=qi, sp_=sp_: e.dma_start(out=qit[sp_][:], in_=qi_d[qi]), writes=[b_qit[sp_]])
        chunks = [(c0, min(4, nk - c0)) for c0 in range(0, nk, 4)]
        for (c0, cn) in chunks:
            w = cn * 128; k0 = c0 * 128
            for h in range(8):
                p = ci % 2; ci += 1
                P.op("pe", lambda e, h=h, p=p, k0=k0, w=w, sp_=sp_: e.matmul(pD[p][:, 0:w], lhsT=qit[sp_][:, h, :], rhs=kiT[:, k0:k0 + w], start=True, stop=True),
                     reads=[b_qit[sp_], b_ki], writes=[b_pD[p]])
                P.op("act", lambda e, p=p, w=w: e.activation(out=rl[p][:, 0:w], in_=pD[p][:, 0:w], func=AF.Relu, scale=SC), reads=[b_pD[p]], writes=[b_rl[p]])
                if h == 0:
                    P.op("dve", lambda e, p=p, w=w, k0=k0, sc=sc, qi=qi, h=h: e.tensor_scalar(out=sc[:, k0:k0 + w], in0=rl[p][:, 0:w], scalar1=wi[:, qi, h:h + 1], scalar2=None, op0=ALU.mult),
                         reads=[b_rl[p], b_wi], writes=[bsc])
                else:
                    P.op("dve", lambda e, p=p, w=w, k0=k0, sc=sc, qi=qi, h=h: e.scalar_tensor_tensor(out=sc[:, k0:k0 + w], in0=rl[p][:, 0:w], scalar=wi[:, qi, h:h + 1], in1=sc[:, k0:k0 + w], op0=ALU.mult, op1=ALU.add),
                         reads=[b_rl[p], b_wi, bsc], writes=[bsc])
        d0 = (nk - 1) * 128
        P.op("dve", lambda e, sc=sc, d0=d0: e.tensor_tensor(out=sc[:, d0:d0 + 128], in0=sc[:, d0:d0 + 128], in1=negm[:], op=ALU.add), reads=[bsc, b_negm], writes=[bsc])
        tau = sm[:, 0:1]; mid = sm[:, 1:2]; cnt = sm[:, 2:3]; s_ = sm[:, 3:4]
        if nkeys <= 256:
            P.op("dve", lambda e: e.memset(tau, -R), writes=[b_sm[0]])
        else:
            P.op("dve", lambda e: e.memset(mid, 0.0), writes=[b_sm[1]])
            for it in range(K):
                P.op("dve", lambda e, sc=sc, nkeys=nkeys: e.tensor_scalar(out=junkS[:, 0:nkeys], in0=sc[:, 0:nkeys], scalar1=mid, scalar2=0.0, op0=ALU.is_ge, op1=ALU.add, accum_out=cnt),
                     reads=[bsc, b_sm[1]], writes=[b_junkS, b_sm[2]])
                if it < K - 1:
                    wn = R / 2 ** (it + 1)
                    P.op("dve", lambda e, wn=wn: e.tensor_scalar(out=s_, in0=cnt, scalar1=255.5, scalar2=2 * wn, op0=ALU.is_ge, op1=ALU.mult), reads=[b_sm[2]], writes=[b_sm[3]])
                    P.op("dve", lambda e, wn=wn: e.scalar_tensor_tensor(out=mid, in0=s_, scalar=-wn, in1=mid, op0=ALU.add, op1=ALU.add), reads=[b_sm[3], b_sm[1]], writes=[b_sm[1]])
                else:
                    wl = R / 2 ** (K - 1)
                    P.op("dve", lambda e, wl=wl: e.tensor_scalar(out=s_, in0=cnt, scalar1=255.5, scalar2=wl, op0=ALU.is_ge, op1=ALU.mult), reads=[b_sm[2]], writes=[b_sm[3]])
                    P.op("dve", lambda e, wl=wl: e.scalar_tensor_tensor(out=tau, in0=s_, scalar=-wl, in1=mid, op0=ALU.add, op1=ALU.add), reads=[b_sm[3], b_sm[1]], writes=[b_sm[0]])
        P.op("dve", lambda e, sp_=sp_: e.tensor_copy(out=dbg[sp_][:], in_=sm[:]), reads=b_sm, writes=[b_dbg[sp_]])
        P.dma("pool", lambda e, qi=qi, sp_=sp_: e.dma_start(out=dbg_d[qi], in_=dbg[sp_][:]), reads=[b_dbg[sp_]], sembuf=b_dbg[sp_])
        for (c0, cn) in chunks:
            w = cn * 128; k0 = c0 * 128
            p = ai % 2; ai += 1
            P.op("pe", lambda e, p=p, k0=k0, w=w, qi=qi: e.matmul(pS[p][:, 0:w], lhsT=qkb[0][:, qi * 128:(qi + 1) * 128], rhs=qkb[1][:, k0:k0 + w], start=True, stop=True),
                 reads=[b_qkb[0], b_qkb[1]], writes=[b_pS[p]])
            P.op("act", lambda e, p=p, w=w: e.activation(out=Eb[p][:, 0:w], in_=pS[p][:, 0:w], func=AF.Exp, scale=float(128 ** -0.5)), reads=[b_pS[p]], writes=[b_Eb[p]])
            P.op("dve", lambda e, p=p, w=w, k0=k0, sc=sc: e.scalar_tensor_tensor(out=Pm[p][:, 0:w], in0=sc[:, k0:k0 + w], scalar=tau, in1=Eb[p][:, 0:w], op0=ALU.is_ge, op1=ALU.mult),
                 reads=[bsc, b_sm[0], b_Eb[p]], writes=[b_Pm[p]])
            for j in range(cn):
                P.op("pe", lambda e, p=p, j=j: e.transpose(pT[p][:, j * 128:(j + 1) * 128], Pm[p][:, j * 128:(j + 1) * 128], idb[:]), reads=[b_Pm[p], b_idb], writes=[b_pT[p]])
            P.op("act", lambda e, p=p, w=w, cn=cn: e.copy(out=PmT[p][:, 0:cn, :].rearrange("p g q -> p (g q)"), in_=pT[p][:, 0:w]), reads=[b_pT[p]], writes=[b_PmT[p]])
            for j in range(cn):
                kt = c0 + j
                P.op("pe", lambda e, p=p, j=j, kt=kt, nk=nk: e.matmul(pO[:, 0:130], lhsT=PmT[p][:, j, :], rhs=vb[:, kt, :], start=(kt == 0), stop=(kt == nk - 1)),
                     reads=[b_PmT[p], b_vb], writes=[b_pO])
        op_ = qi % 2
        P.op("dve", lambda e: e.reciprocal(out=sm[:, 4:5], in_=pO[:, 128:129]), reads=[b_pO], writes=[b_sm[4]])
        P.op("dve", lambda e, op_=op_: e.tensor_scalar(out=ob[op_][:], in0=pO[:, 0:128], scalar1=sm[:, 4:5], scalar2=None, op0=ALU.mult), reads=[b_pO, b_sm[4]], writes=[b_ob[op_]])
        P.dma("pool", lambda e, qi=qi, op_=op_: e.dma_start(out=y_d[qi], in_=ob[op_][:]), reads=[b_ob[op_]], sembuf=b_ob[op_])
    P.emit(); P.close()
    return nc

D = 1024; TOK = 2048; NT = 16; EPS = 1e-6

def build_merge():
    nc = bass.Bass("TRN2", target_bir_lowering=False)
    P = ProgOld(nc); B = P.buf
    def din(name, shape): return nc.dram_tensor(name, list(shape), F32, kind="ExternalInput").ap()
    x_in = din("x", [TOK, D]); g_mpre = din("g_mpre", [128, D]); g_mpost = din("g_mpost", [128, D])
    wgate = din("wgate", [8, 128, 3072]); wbr = din("wbr", [12, 128, D]); wo = din("wo", [8, 128, D])
    yT = din("yT", [NT, 128, 12 * 128]); idn_d = din("idn", [128, 128])
    x_out = nc.dram_tensor("xo", [TOK, D], F32, kind="ExternalOutput").ap()
    ident_f = P.sb("ident_f", [128, 128]); ident = P.sb("ident", [128, 128], BF16)
    gpre_t = P.sb("gpre_t", [128, D]); gpost_t = P.sb("gpost_t", [128, D])
    Wg = P.sb("Wg", [128, 8, 3072], BF16); Wb = P.sb("Wb", [128, 12, D], BF16); Wo = P.sb("Wo", [128, 8, D], BF16)
    stg = [P.sb(f"stg{i}", [128, 3072]) for i in range(2)]
    xt = [P.sb(f"xt{i}", [128, D]) for i in range(2)]; ot = [P.sb(f"ot{i}", [128, D]) for i in range(2)]
    mg = P.sb("mg", [128, D]); tmp = P.sb("tmp", [128, 512]); junk = P.sb("junk", [128, D])
    hb = P.sb("hb", [128, D], BF16); mb = P.sb("mb", [128, D], BF16)
    hTt = P.sb("hTt", [128, 8, 128], BF16); mT = P.sb("mT", [128, 8, 128], BF16)
    ystg = [P.sb(f"ystg{i}", [128, 1536]) for i in range(2)]; ytb = [P.sb(f"ytb{i}", [128, 12, 128], BF16) for i in range(2)]
    sgt = [P.sb(f"sgt{i}", [128, 512]) for i in range(2)]
    st = P.sb("st", [128, 8])
    pA = [P.ps(f"pA{i}", [128, 512]) for i in range(2)]; pB = [P.ps(f"pB{i}", [128, 512]) for i in range(2)]
    pC = [P.ps(f"pC{i}", [128, 512]) for i in range(2)]; pT = P.ps("pT", [128, 1024], BF16)
    b_idf = B(); b_id = B(); b_gpre = B(); b_gpost = B(); b_Wg = B(); b_Wb = B(); b_Wo = B(); b_stg = [B(), B()]
    b_xt = [B(), B()]; b_ot = [B(), B()]; b_mg = B(); b_tmp = B(); b_junk = B(); b_hb = B(); b_mb = B(); b_hTt = B(); b_mT = B()
    b_ystg = [B(), B()]; b_ytb = [B(), B()]; b_sgt = [B(), B()]; b_st = [B() for _ in range(8)]
    b_pA = [B(), B()]; b_pB = [B(), B()]; b_pC = [B(), B()]; b_pT = B()
    P.dma("sp", lambda e: e.dma_start(out=ident_f[:], in_=idn_d), writes=[b_idf])
    P.op("dve", lambda e: e.tensor_copy(out=ident[:], in_=ident_f[:]), reads=[b_idf], writes=[b_id])
    P.dma("sp", lambda e: e.dma_start(out=gpre_t[:], in_=g_mpre), writes=[b_gpre])
    P.dma("sp", lambda e: e.dma_start(out=gpost_t[:], in_=g_mpost), writes=[b_gpost])
    n = 0
    for k in range(8):
        s = n % 2; n += 1
        P.dma("sp", lambda e, k=k, s=s: e.dma_start(out=stg[s][:, :], in_=wgate[k]), writes=[b_stg[s]])
        P.op("pool", lambda e, k=k, s=s: e.tensor_copy(out=Wg[:, k, :], in_=stg[s][:, :]), reads=[b_stg[s]], writes=[b_Wg])
    for k in range(12):
        s = n % 2; n += 1
        P.dma("sp", lambda e, k=k, s=s: e.dma_start(out=stg[s][:, 0:D], in_=wbr[k]), writes=[b_stg[s]])
        P.op("pool", lambda e, k=k, s=s: e.tensor_copy(out=Wb[:, k, :], in_=stg[s][:, 0:D]), reads=[b_stg[s]], writes=[b_Wb])
    for k in range(8):
        s = n % 2; n += 1
        P.dma("sp", lambda e, k=k, s=s: e.dma_start(out=stg[s][:, 0:D], in_=wo[k]), writes=[b_stg[s]])
        P.op("pool", lambda e, k=k, s=s: e.tensor_copy(out=Wo[:, k, :], in_=stg[s][:, 0:D]), reads=[b_stg[s]], writes=[b_Wo])

    def rstd_of(src_ap, src_bufs, col):
        ss = st[:, col:col + 1]; bss = b_st[col]
        P.op("act", lambda e: e.activation(out=junk[:], in_=src_ap, func=AF.Square, scale=float(D ** -0.5), accum_out=ss), reads=src_bufs, writes=[b_junk, bss])
        P.op("dve", lambda e: e.tensor_scalar(out=ss, in0=ss, scalar1=EPS, scalar2=None, op0=ALU.add), reads=[bss], writes=[bss])
        P.op("act", lambda e: e.activation(out=ss, in_=ss, func=AF.Sqrt), reads=[bss], writes=[bss])
        P.op("dve", lambda e: e.reciprocal(out=ss, in_=ss), reads=[bss], writes=[bss])
        return ss, bss

    qn = 0
    for ti in range(NT):
        par = ti % 2
        P.dma("sp", lambda e, ti=ti, par=par: e.dma_start(out=xt[par][:], in_=x_in[ti * 128:(ti + 1) * 128, :]), writes=[b_xt[par]])
        P.dma("sp", lambda e, ti=ti, par=par: e.dma_start(out=ystg[par][:], in_=yT[ti]), writes=[b_ystg[par]])
        P.op("pool", lambda e, par=par: e.tensor_copy(out=ytb[par][:].rearrange("p a b -> p (a b)"), in_=ystg[par][:]), reads=[b_ystg[par]], writes=[b_ytb[par]])
        rs, brs = rstd_of(xt[par][:], [b_xt[par]], 0)
        P.op("dve", lambda e, par=par, rs=rs: e.scalar_tensor_tensor(out=hb[:], in0=xt[par][:], scalar=rs, in1=gpre_t[:], op0=ALU.mult, op1=ALU.mult), reads=[b_xt[par], brs, b_gpre], writes=[b_hb])
        for k in range(8):
            P.op("pe", lambda e, k=k: e.transpose(pT[:, k * 128:(k + 1) * 128], hb[:, k * 128:(k + 1) * 128], ident[:]), reads=[b_hb, b_id], writes=[b_pT])
        P.op("act", lambda e: e.copy(out=hTt[:].rearrange("p k t -> p (k t)"), in_=pT[:]), reads=[b_pT], writes=[b_hTt])
        for half in range(2):
            for nb in range(3):
                q = qn % 2; qn += 1
                c0 = nb * 1024 + half * 512
                for k in range(8):
                    P.op("pe", lambda e, k=k, q=q, c0=c0: e.matmul(pA[q][:], lhsT=hTt[:, k, :], rhs=Wg[:, k, c0:c0 + 512], start=(k == 0), stop=(k == 7)), reads=[b_hTt, b_Wg], writes=[b_pA[q]])
                P.op("act", lambda e, q=q: e.activation(out=sgt[q][:], in_=pA[q][:], func=AF.Sigmoid), reads=[b_pA[q]], writes=[b_sgt[q]])
                for kc in range(4):
                    P.op("pe", lambda e, kc=kc, q=q, nb=nb, half=half, par=par: e.matmul(pB[q][:], lhsT=ytb[par][:, nb * 4 + kc, :], rhs=Wb[:, nb * 4 + kc, half * 512:(half + 1) * 512], start=(kc == 0), stop=(kc == 3)), reads=[b_ytb[par], b_Wb], writes=[b_pB[q]])
                if nb == 0:
                    P.op("dve", lambda e, q=q, half=half: e.tensor_tensor(out=mg[:, half * 512:(half + 1) * 512], in0=sgt[q][:], in1=pB[q][:], op=ALU.mult), reads=[b_sgt[q], b_pB[q]], writes=[b_mg])
                else:
                    P.op("dve", lambda e, q=q: e.tensor_tensor(out=tmp[:], in0=sgt[q][:], in1=pB[q][:], op=ALU.mult), reads=[b_sgt[q], b_pB[q]], writes=[b_tmp])
                    P.op("dve", lambda e, half=half: e.tensor_tensor(out=mg[:, half * 512:(half + 1) * 512], in0=mg[:, half * 512:(half + 1) * 512], in1=tmp[:], op=ALU.add), reads=[b_mg, b_tmp], writes=[b_mg])
        P.op("act", lambda e: e.copy(out=mb[:], in_=mg[:]), reads=[b_mg], writes=[b_mb])
        for k in range(8):
            P.op("pe", lambda e, k=k: e.transpose(pT[:, k * 128:(k + 1) * 128], mb[:, k * 128:(k + 1) * 128], ident[:]), reads=[b_mb, b_id], writes=[b_pT])
        P.op("act", lambda e: e.copy(out=mT[:].rearrange("p k t -> p (k t)"), in_=pT[:]), reads=[b_pT], writes=[b_mT])
        for half in range(2):
            for k in range(8):
                P.op("pe", lambda e, k=k, half=half: e.matmul(pC[half][:], lhsT=mT[:, k, :], rhs=Wo[:, k, half * 512:(half + 1) * 512], start=(k == 0), stop=(k == 7)), reads=[b_mT, b_Wo], writes=[b_pC[half]])
            P.op("act", lambda e, half=half, par=par: e.copy(out=ot[par][:, half * 512:(half + 1) * 512], in_=pC[half][:]), reads=[b_pC[half]], writes=[b_ot[par]])
        rs, brs = rstd_of(ot[par][:], [b_ot[par]], 1)
        P.op("dve", lambda e, par=par, rs=rs: e.scalar_tensor_tensor(out=ot[par][:], in0=ot[par][:], scalar=rs, in1=gpost_t[:], op0=ALU.mult, op1=ALU.mult), reads=[b_ot[par], brs, b_gpost], writes=[b_ot[par]])
        P.op("dve", lambda e, par=par: e.tensor_tensor(out=ot[par][:], in0=ot[par][:], in1=xt[par][:], op=ALU.add), reads=[b_ot[par], b_xt[par]], writes=[b_ot[par]])
        P.dma("pool", lambda e, ti=ti, par=par: e.dma_start(out=x_out[ti * 128:(ti + 1) * 128, :], in_=ot[par][:]), reads=[b_ot[par]], sembuf=b_ot[par])
    P.emit(); P.close()
    return nc


def build_rwkv(L):
    SEG = min(L, 512); NSEG = L // SEG; NCH = SEG // 64
    nc = bass.Bass("TRN2", target_bir_lowering=False)
    P = ProgOld(nc); B = P.buf
    def din(name, shape): return nc.dram_tensor(name, list(shape), F32, kind="ExternalInput").ap()
    zr_d = din("zr", [3, 64, 2, L + 1]); zl_d = din("zl", [64, 2, L + 1]); zg_d = din("zg", [128, L + 1])
    mu3_d = din("mu3", [64, 3, 2]); mul_d = din("mul", [64, 2]); mug_d = din("mug", [128, 1])
    pp_d = din("pp", [64, 5, 2]); wup_d = din("wup", [64, 2, 64]); aup_d = din("aup", [64, 2, 64]); gup_d = din("gup", [128, 128])
    lnwb_d = din("lnwb", [64, 2, 128]); cmask_d = din("cmask", [64, 2 * SEG]); mask5_d = din("mask5", [64, 320])
    idn_d = din("idn", [64, 64])
    y_d = nc.dram_tensor("y", [L // 64, 64, 128], F32, kind="ExternalOutput").ap()
    def T(name, shape, dt=F32):
        return P.sb(name, shape, dt), B(name)
    raw3, b_raw3 = T("raw3", [64, 3, 2, SEG + 1]); rawl, b_rawl = T("rawl", [64, 2, SEG + 1]); rawg, b_rawg = T("rawg", [128, SEG + 1])
    mu3, b_mu3 = T("mu3t", [64, 3, 2]); mul, b_mul = T("mult", [64, 2]); mug, b_mug = T("mugt", [128, 1])
    pp, b_pp = T("ppt", [64, 5, 2]); wup, b_wup = T("wupt", [64, 2, 64]); aup, b_aup = T("aupt", [64, 2, 64]); gup, b_gup = T("gupt", [128, 128])
    lnwb, b_lnwb = T("lnwbt", [64, 2, 128]); cmask, b_cmask = T("cmaskt", [64, 2 * SEG]); mask5, b_mask5 = T("mask5t", [64, 320])
    idn, b_idn = T("idnt", [64, 64]); ones, b_ones = T("onest", [64, 64])
    d3, b_d3 = T("d3", [64, 3, 2, SEG]); dl, b_dl = T("dl", [64, 2, SEG]); dg, b_dg = T("dg", [128, SEG])
    x3, b_x3 = T("x3", [64, 3, 2, SEG]); xl, b_xl = T("xl", [64, 2, SEG]); xg, b_xg = T("xg", [128, SEG])
    tw, b_tw = T("tw", [64, SEG]); sgc, b_sgc = T("sgc", [128, SEG]); sgw, b_sgw = T("sgw", [64, 2, SEG]); aa, b_aa = T("aa", [64, 2, SEG])
    t1, b_t1 = T("t1", [64, 2, SEG]); sq, b_sq = T("sq", [64, 2, SEG]); rn, b_rn = T("rn", [64, 2, SEG]); kk, b_kk = T("kk", [64, 2, SEG])
    kp, b_kp = T("kp", [64, 2, SEG]); bb, b_bb = T("bb", [64, 2, SEG]); cs, b_cs = T("cs", [64, 2, SEG])
    epos, b_epos = T("epos", [64, 2, SEG]); eneg, b_eneg = T("eneg", [64, 2, SEG]); eprev, b_eprev = T("eprev", [64, 2, SEG])
    AR, b_AR = T("AR", [64, 2, NCH, 2, 64]); Bt, b_Bt = T("Bt", [64, 2, SEG]); Kt, b_Kt = T("Kt", [64, 2, SEG]); rkr, b_rkr = T("rkr", [64, 2, SEG])
    Hs = [T(f"H{i}", [64, 2, 64]) for i in range(2)]
    Msb = [T(f"Msb{h}", [64, 320]) for h in range(2)]
    TK, b_TK = T("TK", [64, 6, 64])
    PPs = [T(f"PP{i}", [64, 2, 2, 64]) for i in range(2)]
    Xs = [T(f"X{i}", [64, 2, 64]) for i in range(2)]
    Wsb, b_Wsb = T("Wsb", [64, 128]); Usb, b_Usb = T("Usb", [64, 128]); Ysb, b_Ysb = T("Ysb", [64, 128]); yc, b_yc = T("yc", [64, 128])
    outs = [T(f"out{i}", [64, 128]) for i in range(2)]
    sm, _ = T("sm", [64, 16]); b_sm = [B() for _ in range(16)]
    junk, b_junk = T("junk", [64, 64])
    def PS(name, shape): return P.ps(name, shape), B(name)
    pM = [PS(f"pM{h}", [64, 512]) for h in range(2)]
    pK, b_pK = PS("pK", [64, 512]); pI, b_pI = PS("pI", [64, 512]); pX, b_pX = PS("pX", [64, 512])
    pW, b_pW = PS("pW", [64, 512]); pY, b_pY = PS("pY", [64, 512]); pH, b_pH = PS("pH", [64, 512])

    for (t, b, d) in [(mu3, b_mu3, mu3_d), (mul, b_mul, mul_d), (mug, b_mug, mug_d), (pp, b_pp, pp_d), (wup, b_wup, wup_d), (aup, b_aup, aup_d),
                      (gup, b_gup, gup_d), (lnwb, b_lnwb, lnwb_d), (cmask, b_cmask, cmask_d), (mask5, b_mask5, mask5_d), (idn, b_idn, idn_d)]:
        P.dma("sp", lambda e, t=t, d=d: e.dma_start(out=t[:], in_=d), writes=[b])
    P.op("dve", lambda e: e.memset(ones[:], 1.0), writes=[b_ones])
    P.op("dve", lambda e: e.memset(Hs[0][0][:], 0.0), writes=[Hs[0][1]])
    hcur = 0
    NEG = -0.6065306597126334
    oi = 0
    for sg_ in range(NSEG):
        s0 = sg_ * SEG
        for a in range(3):
            P.dma("sp", lambda e, s0=s0, a=a: e.dma_start(out=raw3[:, a, :, :], in_=zr_d[a, :, :, s0:s0 + SEG + 1]), writes=[b_raw3])
        P.dma("sp", lambda e, s0=s0: e.dma_start(out=rawl[:], in_=zl_d[:, :, s0:s0 + SEG + 1]), writes=[b_rawl])
        P.dma("sp", lambda e, s0=s0: e.dma_start(out=rawg[:], in_=zg_d[:, s0:s0 + SEG + 1]), writes=[b_rawg])
        P.op("dve", lambda e: e.tensor_tensor(out=d3[:], in0=raw3[:, :, :, 0:SEG], in1=raw3[:, :, :, 1:SEG + 1], op=ALU.subtract), reads=[b_raw3], writes=[b_d3])
        P.op("dve", lambda e: e.tensor_tensor(out=dl[:], in0=rawl[:, :, 0:SEG], in1=rawl[:, :, 1:SEG + 1], op=ALU.subtract), reads=[b_rawl], writes=[b_dl])
        P.op("dve", lambda e: e.tensor_tensor(out=dg[:], in0=rawg[:, 0:SEG], in1=rawg[:, 1:SEG + 1], op=ALU.subtract), reads=[b_rawg], writes=[b_dg])
        for a in range(3):
            for h in range(2):
                P.op("dve", lambda e, a=a, h=h: e.scalar_tensor_tensor(out=x3[:, a, h, :], in0=d3[:, a, h, :], scalar=mu3[:, a, h:h + 1], in1=raw3[:, a, h, 1:SEG + 1], op0=ALU.mult, op1=ALU.add),
                     reads=[b_d3, b_mu3, b_raw3], writes=[b_x3])
        for a in range(2):
            P.op("dve", lambda e, a=a: e.scalar_tensor_tensor(out=xl[:, a, :], in0=dl[:, a, :], scalar=mul[:, a:a + 1], in1=rawl[:, a, 1:SEG + 1], op0=ALU.mult, op1=ALU.add),
                 reads=[b_dl, b_mul, b_rawl], writes=[b_xl])
        P.op("dve", lambda e: e.scalar_tensor_tensor(out=xg[:], in0=dg[:], scalar=mug[:, 0:1], in1=rawg[:, 1:SEG + 1], op0=ALU.mult, op1=ALU.add), reads=[b_dg, b_mug, b_rawg], writes=[b_xg])
        XR = lambda h: x3[:, 0, h, :]
        XK = lambda h: x3[:, 1, h, :]
        XV = lambda h: x3[:, 2, h, :]
        P.op("act", lambda e: e.activation(out=tw[:], in_=xl[:, 0, :], func=AF.Tanh), reads=[b_xl], writes=[b_tw])
        P.op("act", lambda e: e.activation(out=sgc[:], in_=xg[:], func=AF.Sigmoid), reads=[b_xg], writes=[b_sgc])
        for h in range(2):
            P.op("pe", lambda e, h=h: e.matmul(pK[:, 0:SEG], lhsT=wup[:, h, :], rhs=tw[:], start=True, stop=True), reads=[b_wup, b_tw], writes=[b_pK])
            P.op("act", lambda e, h=h: e.activation(out=sgw[:, h, :], in_=pK[:, 0:SEG], func=AF.Sigmoid, bias=pp[:, 0, h:h + 1]), reads=[b_pK, b_pp], writes=[b_sgw])
            P.op("pe", lambda e, h=h: e.matmul(pK[:, 0:SEG], lhsT=aup[:, h, :], rhs=xl[:, 1, :], start=True, stop=True), reads=[b_aup, b_xl], writes=[b_pK])
            P.op("act", lambda e, h=h: e.activation(out=aa[:, h, :], in_=pK[:, 0:SEG], func=AF.Sigmoid, bias=pp[:, 1, h:h + 1]), reads=[b_pK, b_pp], writes=[b_aa])
        for h in range(2):
            P.op("dve", lambda e, h=h: e.tensor_scalar(out=t1[:, h, :], in0=XK(h), scalar1=pp[:, 2, h:h + 1], scalar2=None, op0=ALU.mult), reads=[b_x3, b_pp], writes=[b_t1])
        P.op("dve", lambda e: e.tensor_tensor(out=sq[:], in0=t1[:], in1=t1[:], op=ALU.mult), reads=[b_t1], writes=[b_sq])
        for h in range(2):
            P.op("pe", lambda e, h=h: e.matmul(pK[:, 0:SEG], lhsT=ones[:], rhs=sq[:, h, :], start=True, stop=True), reads=[b_ones, b_sq], writes=[b_pK])
            P.op("dve", lambda e, h=h: e.tensor_scalar(out=rn[:, h, :], in0=pK[:, 0:SEG], scalar1=1e-24, scalar2=None, op0=ALU.max), reads=[b_pK], writes=[b_rn])
        P.op("act", lambda e: e.activation(out=rn[:], in_=rn[:], func=AF.Sqrt), reads=[b_rn], writes=[b_rn])
        P.op("dve", lambda e: e.reciprocal(out=rn[:], in_=rn[:]), reads=[b_rn], writes=[b_rn])
        P.op("dve", lambda e: e.tensor_tensor(out=kk[:], in0=t1[:], in1=rn[:], op=ALU.mult), reads=[b_t1, b_rn], writes=[b_kk])
        for h in range(2):
            P.op("dve", lambda e, h=h: e.tensor_scalar(out=kp[:, h, :], in0=aa[:, h, :], scalar1=pp[:, 3, h:h + 1], scalar2=pp[:, 3, h:h + 1], op0=ALU.mult, op1=ALU.subtract), reads=[b_aa, b_pp], writes=[b_kp])
            P.op("dve", lambda e, h=h: e.scalar_tensor_tensor(out=kp[:, h, :], in0=kp[:, h, :], scalar=1.0, in1=XK(h), op0=ALU.add, op1=ALU.mult), reads=[b_kp, b_x3], writes=[b_kp])
        P.op("dve", lambda e: e.tensor_tensor(out=bb[:], in0=kk[:], in1=aa[:], op=ALU.mult), reads=[b_kk, b_aa], writes=[b_bb])
        FL = lambda t: t[:].rearrange("p h s -> p (h s)")
        P.op("dve", lambda e: e.tensor_tensor_scan(out=FL(cs), data0=cmask[:], data1=FL(sgw), initial=0.0, op0=ALU.mult, op1=ALU.add), reads=[b_cmask, b_sgw], writes=[b_cs])
        P.op("act", lambda e: e.activation(out=epos[:], in_=cs[:], func=AF.Exp, scale=NEG), reads=[b_cs], writes=[b_epos])
        P.op("act", lambda e: e.activation(out=eneg[:], in_=cs[:], func=AF.Exp, scale=-NEG), reads=[b_cs], writes=[b_eneg])
        P.op("dve", lambda e: e.tensor_tensor(out=eprev[:], in0=cs[:], in1=sgw[:], op=ALU.subtract), reads=[b_cs, b_sgw], writes=[b_eprev])
        P.op("act", lambda e: e.activation(out=eprev[:], in_=eprev[:], func=AF.Exp, scale=NEG), reads=[b_eprev], writes=[b_eprev])
        for h in range(2):
            P.op("dve", lambda e, h=h: e.scalar_tensor_tensor(out=AR[:, h, :, 0, :], in0=kk[:, h, :].rearrange("p (c t) -> p c t", t=64), scalar=-1.0, in1=eprev[:, h, :].rearrange("p (c t) -> p c t", t=64), op0=ALU.mult, op1=ALU.mult),
                 reads=[b_kk, b_eprev], writes=[b_AR])
            P.op("dve", lambda e, h=h: e.tensor_tensor(out=AR[:, h, :, 1, :], in0=XR(h).rearrange("p (c t) -> p c t", t=64), in1=epos[:, h, :].rearrange("p (c t) -> p c t", t=64), op=ALU.mult),
                 reads=[b_x3, b_epos], writes=[b_AR])
            P.op("dve", lambda e, h=h: e.scalar_tensor_tensor(out=rkr[:, h, :], in0=XR(h), scalar=pp[:, 4, h:h + 1], in1=kp[:, h, :], op0=ALU.mult, op1=ALU.mult), reads=[b_x3, b_pp, b_kp], writes=[b_rkr])
        P.op("dve", lambda e: e.tensor_tensor(out=Bt[:], in0=bb[:], in1=eneg[:], op=ALU.mult), reads=[b_bb, b_eneg], writes=[b_Bt])
        P.op("dve", lambda e: e.tensor_tensor(out=Kt[:], in0=kp[:], in1=eneg[:], op=ALU.mult), reads=[b_kp, b_eneg], writes=[b_Kt])
        for c in range(NCH):
            cs_ = slice(c * 64, (c + 1) * 64)
            for h in range(2):
                pm, bpm = pM[h]
                P.op("pe", lambda e, h=h, c=c, pm=pm, cs_=cs_: e.matmul(pm[:, 0:64], lhsT=AR[:, h, c, 0, :], rhs=Bt[:, h, cs_], start=True, stop=True), reads=[b_AR, b_Bt], writes=[bpm])
                P.op("pe", lambda e, h=h, c=c, pm=pm, cs_=cs_: e.matmul(pm[:, 64:192], lhsT=Bt[:, h, cs_], rhs=AR[:, h, c, :, :].rearrange("p a t -> p (a t)"), start=True, stop=True), reads=[b_AR, b_Bt], writes=[bpm])
                P.op("pe", lambda e, h=h, c=c, pm=pm, cs_=cs_: e.matmul(pm[:, 192:320], lhsT=Kt[:, h, cs_], rhs=AR[:, h, c, :, :].rearrange("p a t -> p (a t)"), start=True, stop=True), reads=[b_AR, b_Kt], writes=[bpm])
                P.op("dve", lambda e, h=h, pm=pm: e.tensor_tensor(out=Msb[h][0][:], in0=pm[:, 0:320], in1=mask5[:], op=ALU.mult), reads=[bpm, b_mask5], writes=[Msb[h][1]])
            for h in range(2):
                for a, (src, bsrc) in enumerate([(Bt[:, h, cs_], b_Bt), (Kt[:, h, cs_], b_Kt), (x3[:, 2, h, cs_], b_x3)]):
                    P.op("pe", lambda e, h=h, a=a, src=src: e.transpose(pK[:, (h * 3 + a) * 64:(h * 3 + a + 1) * 64], src, idn[:]), reads=[bsrc, b_idn], writes=[b_pK])
            P.op("act", lambda e: e.copy(out=TK[:].rearrange("p a t -> p (a t)"), in_=pK[:, 0:384]), reads=[b_pK], writes=[b_TK])
            pcur = 0; xcur = 0
            for h in range(2):
                P.op("act", lambda e, h=h: e.copy(out=PPs[0][0][:, h, :, :].rearrange("p a t -> p (a t)"), in_=Msb[h][0][:, 0:128]), reads=[Msb[h][1]], writes=[PPs[0][1]])
                P.op("dve", lambda e, h=h: e.tensor_tensor(out=Xs[0][0][:, h, :], in0=Msb[h][0][:, 64:128], in1=idn[:], op=ALU.add), reads=[Msb[h][1], b_idn], writes=[Xs[0][1]])
            for stp in range(5):
                pp_t, pp_b = PPs[pcur]; pn_t, pn_b = PPs[1 - pcur]
                x_t, x_b = Xs[xcur]; xn_t, xn_b = Xs[1 - xcur]
                for h in range(2):
                    P.op("pe", lambda e, h=h, pp_t=pp_t: e.matmul(pI[:, (h * 2) * 64:(h * 2 + 1) * 64], lhsT=pp_t[:, h, 1, :], rhs=pp_t[:, h, 0, :], start=True, stop=True), reads=[pp_b], writes=[b_pI])
                    P.op("pe", lambda e, h=h, pp_t=pp_t: e.matmul(pI[:, (h * 2 + 1) * 64:(h * 2 + 2) * 64], lhsT=pp_t[:, h, 0, :], rhs=pp_t[:, h, 1, :], start=True, stop=True), reads=[pp_b], writes=[b_pI])
                P.op("act", lambda e, pn_t=pn_t: e.copy(out=pn_t[:].rearrange("p h a t -> p (h a t)"), in_=pI[:, 0:256]), reads=[b_pI], writes=[pn_b])
                for h in range(2):
                    P.op("pe", lambda e, h=h, x_t=x_t: e.matmul(pX[:, h * 64:(h + 1) * 64], lhsT=idn[:], rhs=x_t[:, h, :], start=True, stop=False), reads=[b_idn, x_b], writes=[b_pX])
                    P.op("pe", lambda e, h=h, x_t=x_t, pn_t=pn_t: e.matmul(pX[:, h * 64:(h + 1) * 64], lhsT=pn_t[:, h, 0, :], rhs=x_t[:, h, :], start=False, stop=True), reads=[pn_b, x_b], writes=[b_pX])
                P.op("dve", lambda e, xn_t=xn_t: e.tensor_copy(out=xn_t[:].rearrange("p h t -> p (h t)"), in_=pX[:, 0:128]), reads=[b_pX], writes=[xn_b])
                pcur = 1 - pcur; xcur = 1 - xcur
            X_t, X_b = Xs[xcur]
            H_t, H_b = Hs[hcur]; Hn_t, Hn_b = Hs[1 - hcur]
            for h in range(2):
                P.op("pe", lambda e, h=h, c=c, H_t=H_t: e.matmul(pW[:, h * 64:(h + 1) * 64], lhsT=AR[:, h, c, 0, :], rhs=H_t[:, h, :], start=True, stop=False), reads=[b_AR, H_b], writes=[b_pW])
                P.op("pe", lambda e, h=h: e.matmul(pW[:, h * 64:(h + 1) * 64], lhsT=Msb[h][0][:, 192:256], rhs=TK[:, h * 3 + 2, :], start=False, stop=True), reads=[Msb[h][1], b_TK], writes=[b_pW])
            P.op("act", lambda e: e.copy(out=Wsb[:], in_=pW[:, 0:128]), reads=[b_pW], writes=[b_Wsb])
            for h in range(2):
                P.op("pe", lambda e, h=h, X_t=X_t: e.matmul(pW[:, 128 + h * 64:128 + (h + 1) * 64], lhsT=X_t[:, h, :], rhs=Wsb[:, h * 64:(h + 1) * 64], start=True, stop=True), reads=[X_b, b_Wsb], writes=[b_pW])
            P.op("dve", lambda e: e.tensor_copy(out=Usb[:], in_=pW[:, 128:256]), reads=[b_pW], writes=[b_Usb])
            for h in range(2):
                hs = slice(h * 64, (h + 1) * 64)
                P.op("pe", lambda e, h=h, c=c, hs=hs, H_t=H_t: e.matmul(pY[:, hs], lhsT=AR[:, h, c, 1, :], rhs=H_t[:, h, :], start=True, stop=False), reads=[b_AR, H_b], writes=[b_pY])
                P.op("pe", lambda e, h=h, hs=hs: e.matmul(pY[:, hs], lhsT=Msb[h][0][:, 128:192], rhs=Usb[:, hs], start=False, stop=False), reads=[Msb[h][1], b_Usb], writes=[b_pY])
                P.op("pe", lambda e, h=h, hs=hs: e.matmul(pY[:, hs], lhsT=Msb[h][0][:, 256:320], rhs=TK[:, h * 3 + 2, :], start=False, stop=True), reads=[Msb[h][1], b_TK], writes=[b_pY])
            P.op("pe", lambda e, cs_=cs_: e.matmul(pY[:, 128:256], lhsT=sgc[:, cs_], rhs=gup[:], start=True, stop=True), reads=[b_sgc, b_gup], writes=[b_pY])
            for h in range(2):
                P.op("pe", lambda e, h=h, cs_=cs_: e.matmul(pY[:, 256 + 2 * h:258 + 2 * h], lhsT=rkr[:, h, cs_], rhs=ones[:, 0:2], start=True, stop=True), reads=[b_rkr, b_ones], writes=[b_pY])
            for h in range(2):
                hs = slice(h * 64, (h + 1) * 64)
                P.op("pe", lambda e, h=h, hs=hs, H_t=H_t: e.matmul(pH[:, hs], lhsT=idn[:], rhs=H_t[:, h, :], start=True, stop=False), reads=[b_idn, H_b], writes=[b_pH])
                P.op("pe", lambda e, h=h, hs=hs: e.matmul(pH[:, hs], lhsT=TK[:, h * 3 + 0, :], rhs=Usb[:, hs], start=False, stop=False), reads=[b_TK, b_Usb], writes=[b_pH])
                P.op("pe", lambda e, h=h, hs=hs: e.matmul(pH[:, hs], lhsT=TK[:, h * 3 + 1, :], rhs=TK[:, h * 3 + 2, :], start=False, stop=True), reads=[b_TK], writes=[b_pH])
            for h in range(2):
                ce = c * 64 + 63
                P.op("dve", lambda e, h=h, ce=ce, Hn_t=Hn_t: e.tensor_scalar(out=Hn_t[:, h, :], in0=pH[:, h * 64:(h + 1) * 64], scalar1=epos[:, h, ce:ce + 1], scalar2=None, op0=ALU.mult), reads=[b_pH, b_epos], writes=[Hn_b])
            hcur = 1 - hcur
            P.op("act", lambda e: e.copy(out=Ysb[:], in_=pY[:, 0:128]), reads=[b_pY], writes=[b_Ysb])
            P.op("dve", lambda e: e.tensor_copy(out=sm[:, 8:12], in_=pY[:, 256:260]), reads=[b_pY], writes=[b_sm[8]])
            o_t, o_b = outs[oi % 2]; oi += 1
            for h in range(2):
                hs = slice(h * 64, (h + 1) * 64)
                mcol = sm[:, h:h + 1]; vcol = sm[:, 2 + h:3 + h]
                P.op("dve", lambda e, hs=hs, mcol=mcol: e.reduce_sum(out=mcol, in_=Ysb[:, hs], axis=AX.X), reads=[b_Ysb], writes=[b_sm[h]])
                P.op("dve", lambda e, mcol=mcol: e.tensor_scalar(out=mcol, in0=mcol, scalar1=-1.0 / 64, scalar2=None, op0=ALU.mult), reads=[b_sm[h]], writes=[b_sm[h]])
                P.op("dve", lambda e, hs=hs, mcol=mcol: e.tensor_scalar(out=yc[:, hs], in0=Ysb[:, hs], scalar1=mcol, scalar2=None, op0=ALU.add), reads=[b_Ysb, b_sm[h]], writes=[b_yc])
                P.op("act", lambda e, hs=hs, vcol=vcol: e.activation(out=junk[:], in_=yc[:, hs], func=AF.Square, scale=0.125, accum_out=vcol), reads=[b_yc], writes=[b_junk, b_sm[2 + h]])
                P.op("dve", lambda e, vcol=vcol: e.tensor_scalar(out=vcol, in0=vcol, scalar1=64e-5, scalar2=None, op0=ALU.add), reads=[b_sm[2 + h]], writes=[b_sm[2 + h]])
                P.op("act", lambda e, vcol=vcol: e.activation(out=vcol, in_=vcol, func=AF.Sqrt), reads=[b_sm[2 + h]], writes=[b_sm[2 + h]])
                P.op("dve", lambda e, vcol=vcol: e.reciprocal(out=vcol, in_=vcol), reads=[b_sm[2 + h]], writes=[b_sm[2 + h]])
                P.op("dve", lambda e, hs=hs, vcol=vcol: e.scalar_tensor_tensor(out=yc[:, hs], in0=yc[:, hs], scalar=vcol, in1=lnwb[:, 0, hs], op0=ALU.mult, op1=ALU.mult), reads=[b_yc, b_sm[2 + h], b_lnwb], writes=[b_yc])
                P.op("dve", lambda e, hs=hs: e.tensor_tensor(out=yc[:, hs], in0=yc[:, hs], in1=lnwb[:, 1, hs], op=ALU.add), reads=[b_yc, b_lnwb], writes=[b_yc])
                P.op("dve", lambda e, hs=hs, h=h: e.scalar_tensor_tensor(out=yc[:, hs], in0=TK[:, h * 3 + 2, :], scalar=sm[:, 8 + 2 * h:9 + 2 * h], in1=yc[:, hs], op0=ALU.mult, op1=ALU.add), reads=[b_TK, b_sm[8], b_yc], writes=[b_yc])
            P.op("dve", lambda e, o_t=o_t: e.tensor_tensor(out=o_t[:], in0=yc[:], in1=pY[:, 128:256], op=ALU.mult), reads=[b_yc, b_pY], writes=[o_b])
            gci = sg_ * NCH + c
            P.dma("pool", lambda e, gci=gci, o_t=o_t: e.dma_start(out=y_d[gci], in_=o_t[:]), reads=[o_b], sembuf=o_b)
    P.emit(); P.close()
    return nc


F32 = mybir.dt.float32
BF16 = mybir.dt.bfloat16
I32 = mybir.dt.int32
AF = mybir.ActivationFunctionType
ALU = mybir.AluOpType
AX = mybir.AxisListType


class Buf:
    __slots__ = ("name", "w", "r", "semidx", "cnt")

    def __init__(self, name):
        self.name = name
        self.w = None
        self.r = {}
        self.semidx = None
        self.cnt = 0


class Prog:
    ENG = ("pe", "act", "dve", "pool", "sp")

    def __init__(self, nc):
        self.nc = nc
        self.g = ExitStack()
        self.esem = {e: self.g.enter_context(nc.semaphore(f"s_{e}")) for e in self.ENG}
        self.cnt = {e: 0 for e in self.ENG}
        self.seen = {e: {} for e in self.ENG}
        self.dsem = []
        self.dfree = []
        self.ops = None
        self.ph = None
        self.live = []
        self.nbuf = 0
        self.nph = 0
        self.jreg = None

    def sb(self, name, shape, dt=F32):
        return self.ph.enter_context(self.nc.sbuf_tensor(f"{name}_p{self.nph}", list(shape), dt))

    def ps(self, name, shape, dt=F32):
        return self.ph.enter_context(self.nc.psum_tensor(f"{name}_p{self.nph}", list(shape), dt))

    def buf(self, name=None):
        self.nbuf += 1
        return Buf(name or f"b{self.nbuf}")

    def _barrier_waits(self, eng):
        need = [(e2, self.cnt[e2]) for e2 in self.ENG if self.cnt[e2] > 0]
        need += [(("d", i), c) for i, (h, c) in enumerate(self.dsem) if c > 0]
        out = []
        seen = self.seen[eng]
        for k, v in need:
            if seen.get(k, 0) >= v:
                continue
            seen[k] = v
            out.append((k, v))
        return out

    def begin(self):
        self.nph += 1
        self.ph = ExitStack()
        self.ops = {e: [] for e in self.ENG}
        self.live = []
        for e in self.ENG:
            self.ops[e].append((self._barrier_waits(e), None, None))

    def _waits(self, eng, reads, writes):
        need = {}

        def add(k, v):
            if need.get(k, 0) < v:
                need[k] = v
        for b in reads:
            if b.w is not None:
                add(*b.w)
        for b in writes:
            if b.w is not None:
                add(*b.w)
            for k, v in b.r.items():
                add(k, v)
        out = []
        seen = self.seen[eng]
        for k, v in need.items():
            if k == "pe" and eng == "pe":
                continue
            if seen.get(k, 0) >= v:
                continue
            seen[k] = v
            out.append((k, v))
        return out

    def _mark(self, tok, reads, writes):
        k, v = tok
        for b in reads:
            if b.r.get(k, 0) < v:
                b.r[k] = v
        for b in writes:
            b.w = tok
            b.r = {}

    def op(self, eng, fn, reads=(), writes=()):
        waits = self._waits(eng, reads, writes)
        self.cnt[eng] += 1
        tok = (eng, self.cnt[eng])
        self._mark(tok, reads, writes)
        self.ops[eng].append((waits, fn, (eng, 1)))

    def dma(self, q, fn, reads=(), writes=(), sembuf=None, inc=16):
        waits = self._waits(q, reads, writes)
        sbf = sembuf if sembuf is not None else (writes[0] if writes else reads[0])
        if sbf.semidx is None:
            if self.dfree:
                sbf.semidx = self.dfree.pop()
            else:
                h = self.g.enter_context(self.nc.semaphore(f"d{len(self.dsem)}"))
                self.dsem.append([h, 0])
                sbf.semidx = len(self.dsem) - 1
            sbf.cnt = self.dsem[sbf.semidx][1]
            self.live.append(sbf)
        sbf.cnt += inc
        self.dsem[sbf.semidx][1] = sbf.cnt
        key = ("d", sbf.semidx)
        tok = (key, sbf.cnt)
        self._mark(tok, reads, writes)
        self.ops[q].append((waits, fn, (key, inc)))

    def _semof(self, k):
        return self.esem[k] if isinstance(k, str) else self.dsem[k[1]][0]

    def end(self):
        nc = self.nc
        ops = self.ops
        with nc.Block() as block:
            def run(engobj, lst):
                for waits, fn, inc in lst:
                    for k, v in waits:
                        engobj.wait_ge(self._semof(k), v)
                    if fn is None:
                        continue
                    ins = fn(engobj)
                    if inc is not None:
                        ins.then_inc(self._semof(inc[0]), inc[1])

            @block.tensor
            def _(e):
                run(e, ops["pe"])

            @block.scalar
            def _(e):
                run(e, ops["act"])

            @block.vector
            def _(e):
                run(e, ops["dve"])

            @block.gpsimd
            def _(e):
                run(e, ops["pool"])

            @block.sync
            def _(e):
                run(e, ops["sp"])
        for b in self.live:
            self.dfree.append(b.semidx)
            b.semidx = None
        self.live = []
        self.ph.close()
        self.ph = None

    def finish(self):
        self.begin()
        self.end()
        self.g.close()


D = 1024; DFF = 2816; NFF = 22; EPS = 1e-6
NWIN = 43
ZR = NWIN * 128
JB = 1152
CB = 4 * JB


def emit_tok(P, TOK, xsrc, xdst, W, zdst=None):
    NT = TOK // 128
    HALF = min(1024, TOK); NH = TOK // HALF; BLK = min(512, HALF); NB = HALF // BLK; TPH = HALF // 128
    has_win = zdst is not None
    P.begin()
    B = P.buf
    g_pre, g_post, wg, wu, wd, idn_d = W["g_pre"], W["g_post"], W["wg"], W["wu"], W["wd"], W["idn"]
    ident_f = P.sb("ident_f", [128, 128]); ident = P.sb("ident", [128, 128], BF16)
    gpre_t = P.sb("gpre_t", [128, D]); gpost_t = P.sb("gpost_t", [128, D])
    hT = P.sb("hT", [128, 8, TOK], BF16)
    AT = P.sb("AT", [128, NFF, HALF], BF16)
    Wd = P.sb("Wd", [128, NFF, D], BF16)
    stg = [P.sb(f"stg{i}", [128, 2048]) for i in range(2)]
    wgb = [P.sb(f"wgb{i}", [128, 8, 128], BF16) for i in range(2)]
    wub = [P.sb(f"wub{i}", [128, 8, 128], BF16) for i in range(2)]
    xt = [P.sb(f"xt{i}", [128, D]) for i in range(2)]
    ot = [P.sb(f"ot{i}", [128, D]) for i in range(2)]
    junk = P.sb("junk", [128, D])
    hb = [P.sb(f"hb{i}", [128, D], BF16) for i in range(2)]
    sg = [P.sb(f"sg{i}", [128, 512]) for i in range(2)]
    st = P.sb("st", [128, 64])
    pA = [P.ps(f"pA{i}", [128, 512]) for i in range(2)]
    pB = [P.ps(f"pB{i}", [128, 512]) for i in range(2)]
    pC = [P.ps(f"pC{i}", [128, 512]) for i in range(2)]
    pT = [P.ps(f"pT{i}", [128, 1024], BF16) for i in range(2)]
    b_ident = B(); b_identf = B(); b_gpre = B(); b_gpost = B(); b_hT = [B() for _ in range(NT)]
    b_AT = [[B() for _ in range(NB)] for _ in range(NFF)]
    b_Wd = [B() for _ in range(NFF)]
    b_stg = [B(), B()]; b_wgb = [B(), B()]; b_wub = [B(), B()]; b_xt = [B(), B()]; b_ot = [B(), B()]
    b_junk = B(); b_hb = [B(), B()]; b_sg = [B(), B()]
    b_pA = [B(), B()]; b_pB = [B(), B()]; b_pC = [B(), B()]; b_pT = [B(), B()]
    st_next = [0]; b_st = {}

    def stcol():
        i = st_next[0] % 64; st_next[0] += 1
        if i not in b_st: b_st[i] = B()
        return st[:, i:i + 1], b_st[i]

    P.dma("sp", lambda e: e.dma_start(out=ident_f[:], in_=idn_d), writes=[b_identf])
    P.op("dve", lambda e: e.tensor_copy(out=ident[:], in_=ident_f[:]), reads=[b_identf], writes=[b_ident])
    P.dma("sp", lambda e: e.dma_start(out=gpre_t[:], in_=g_pre), writes=[b_gpre])
    P.dma("sp", lambda e: e.dma_start(out=gpost_t[:], in_=g_post), writes=[b_gpost])

    def rstd_of(src_ap, src_bufs):
        ss, bss = stcol(); rs, brs = stcol()
        P.op("act", lambda e: e.activation(out=junk[:], in_=src_ap, func=AF.Square, scale=float(D ** -0.5), accum_out=ss), reads=src_bufs, writes=[b_junk, bss])
        P.op("dve", lambda e: e.tensor_scalar(out=rs, in0=ss, scalar1=EPS, scalar2=None, op0=ALU.add), reads=[bss], writes=[brs])
        P.op("act", lambda e: e.activation(out=rs, in_=rs, func=AF.Sqrt), reads=[brs], writes=[brs])
        P.op("dve", lambda e: e.reciprocal(out=rs, in_=rs), reads=[brs], writes=[brs])
        return rs, brs

    def norm_to_hT(src_ap, src_bufs, g_t, b_g, ti, par):
        rs, brs = rstd_of(src_ap, src_bufs)
        P.op("dve", lambda e: e.scalar_tensor_tensor(out=hb[par][:], in0=src_ap, scalar=rs, in1=g_t[:], op0=ALU.mult, op1=ALU.mult), reads=src_bufs + [brs, b_g], writes=[b_hb[par]])
        for k in range(8):
            P.op("pe", lambda e, k=k: e.transpose(pT[par][:, k * 128:(k + 1) * 128], hb[par][:, k * 128:(k + 1) * 128], ident[:]), reads=[b_hb[par], b_ident], writes=[b_pT[par]])
        P.op("act", lambda e: e.copy(out=hT[:, :, ti * 128:(ti + 1) * 128], in_=pT[par][:].rearrange("p (k t) -> p k t", k=8)), reads=[b_pT[par]], writes=[b_hT[ti]])

    for ti in range(NT):
        par = ti % 2
        P.dma("sp", lambda e, ti=ti, par=par: e.dma_start(out=xt[par][:], in_=xsrc[ti * 128:(ti + 1) * 128, :]), writes=[b_xt[par]])
        norm_to_hT(xt[par][:], [b_xt[par]], gpre_t, b_gpre, ti, par)
    for c in range(NFF):
        s = c % 2
        P.dma("sp", lambda e, c=c, s=s: e.dma_start(out=stg[s][:, 0:D], in_=wd[c]), writes=[b_stg[s]])
        P.op("pool", lambda e, c=c, s=s: e.tensor_copy(out=Wd[:, c, :], in_=stg[s][:, 0:D]), reads=[b_stg[s]], writes=[b_Wd[c]])
    if has_win:
        gmix_t = P.sb("gmix_t", [128, D]); b_gmix = B()
        P.dma("sp", lambda e: e.dma_start(out=gmix_t[:], in_=W["g_mix"]), writes=[b_gmix])
    for half in range(NH):
        for c in range(NFF):
            s = c % 2
            P.dma("sp", lambda e, c=c, s=s: e.dma_start(out=stg[s][:, 0:1024], in_=wg[c].rearrange("p k f -> p (k f)")), writes=[b_stg[s]])
            P.op("pool", lambda e, s=s: e.tensor_copy(out=wgb[s][:].rearrange("p k f -> p (k f)"), in_=stg[s][:, 0:1024]), reads=[b_stg[s]], writes=[b_wgb[s]])
            P.dma("sp", lambda e, c=c, s=s: e.dma_start(out=stg[s][:, 1024:2048], in_=wu[c].rearrange("p k f -> p (k f)")), writes=[b_stg[s]])
            P.op("pool", lambda e, s=s: e.tensor_copy(out=wub[s][:].rearrange("p k f -> p (k f)"), in_=stg[s][:, 1024:2048]), reads=[b_stg[s]], writes=[b_wub[s]])
            for tb in range(NB):
                t0 = half * HALF + tb * BLK
                rd = [b_hT[(t0 // 128) + i] for i in range(BLK // 128)]
                q = tb % 2
                for k in range(8):
                    P.op("pe", lambda e, k=k, s=s, q=q, t0=t0: e.matmul(pA[q][:, 0:BLK], lhsT=wgb[s][:, k, :], rhs=hT[:, k, t0:t0 + BLK], start=(k == 0), stop=(k == 7)), reads=[b_wgb[s]] + rd, writes=[b_pA[q]])
                for k in range(8):
                    P.op("pe", lambda e, k=k, s=s, q=q, t0=t0: e.matmul(pB[q][:, 0:BLK], lhsT=wub[s][:, k, :], rhs=hT[:, k, t0:t0 + BLK], start=(k == 0), stop=(k == 7)), reads=[b_wub[s]] + rd, writes=[b_pB[q]])
                P.op("act", lambda e, q=q: e.activation(out=sg[q][:, 0:BLK], in_=pA[q][:, 0:BLK], func=AF.Silu), reads=[b_pA[q]], writes=[b_sg[q]])
                P.op("dve", lambda e, q=q, c=c, tb=tb: e.tensor_tensor(out=AT[:, c, tb * BLK:(tb + 1) * BLK], in0=sg[q][:, 0:BLK], in1=pB[q][:, 0:BLK], op=ALU.mult), reads=[b_sg[q], b_pB[q]], writes=[b_AT[c][tb]])
        for tl in range(TPH):
            ti = half * TPH + tl; par = ti % 2
            P.dma("sp", lambda e, ti=ti, par=par: e.dma_start(out=xt[par][:], in_=xsrc[ti * 128:(ti + 1) * 128, :]), writes=[b_xt[par]])
            for ch in range(2):
                for c in range(NFF):
                    P.op("pe", lambda e, c=c, ch=ch, tl=tl: e.matmul(pC[ch][:], lhsT=AT[:, c, tl * 128:(tl + 1) * 128], rhs=Wd[:, c, ch * 512:(ch + 1) * 512], start=(c == 0), stop=(c == NFF - 1)), reads=[b_AT[c][(tl * 128) // BLK], b_Wd[c]], writes=[b_pC[ch]])
                P.op("act", lambda e, ch=ch, par=par: e.copy(out=ot[par][:, ch * 512:(ch + 1) * 512], in_=pC[ch][:]), reads=[b_pC[ch]], writes=[b_ot[par]])
            rs, brs = rstd_of(ot[par][:], [b_ot[par]])
            P.op("dve", lambda e, par=par, rs=rs: e.scalar_tensor_tensor(out=ot[par][:], in0=ot[par][:], scalar=rs, in1=gpost_t[:], op0=ALU.mult, op1=ALU.mult), reads=[b_ot[par], brs, b_gpost], writes=[b_ot[par]])
            P.op("dve", lambda e, par=par: e.scalar_tensor_tensor(out=ot[par][:], in0=ot[par][:], scalar=0.5, in1=xt[par][:], op0=ALU.mult, op1=ALU.add), reads=[b_ot[par], b_xt[par]], writes=[b_ot[par]])
            P.dma("pool", lambda e, ti=ti, par=par: e.dma_start(out=xdst[ti * 128:(ti + 1) * 128, :], in_=ot[par][:]), reads=[b_ot[par]], sembuf=b_ot[par])
            if has_win:
                norm_to_hT(ot[par][:], [b_ot[par]], gmix_t, b_gmix, ti, par)
    if has_win:
        win = W["win"]
        ZW = min(1024, TOK)
        zs = [P.sb(f"zs{i}", [128, ZW]) for i in range(2)]; b_zs = [B(), B()]
        zi_n = 0
        for c in range(NWIN):
            s = c % 2
            P.dma("sp", lambda e, c=c, s=s: e.dma_start(out=stg[s][:, 0:1024], in_=win[c].rearrange("p k f -> p (k f)")), writes=[b_stg[s]])
            P.op("pool", lambda e, s=s: e.tensor_copy(out=wgb[s][:].rearrange("p k f -> p (k f)"), in_=stg[s][:, 0:1024]), reads=[b_stg[s]], writes=[b_wgb[s]])
            for z0 in range(0, TOK, ZW):
                zi = zi_n % 2; zi_n += 1
                for bi, t0 in enumerate(range(z0, z0 + ZW, BLK)):
                    q = bi % 2
                    rd = [b_hT[(t0 // 128) + i] for i in range(BLK // 128)]
                    for k in range(8):
                        P.op("pe", lambda e, k=k, s=s, q=q, t0=t0: e.matmul(pA[q][:, 0:BLK], lhsT=wgb[s][:, k, :], rhs=hT[:, k, t0:t0 + BLK], start=(k == 0), stop=(k == 7)), reads=[b_wgb[s]] + rd, writes=[b_pA[q]])
                    if bi % 2 == 0:
                        P.op("act", lambda e, q=q, zi=zi, t0=t0, z0=z0: e.copy(out=zs[zi][:, t0 - z0:t0 - z0 + BLK], in_=pA[q][:, 0:BLK]), reads=[b_pA[q]], writes=[b_zs[zi]])
                    else:
                        P.op("dve", lambda e, q=q, zi=zi, t0=t0, z0=z0: e.tensor_copy(out=zs[zi][:, t0 - z0:t0 - z0 + BLK], in_=pA[q][:, 0:BLK]), reads=[b_pA[q]], writes=[b_zs[zi]])
                P.dma("pool", lambda e, c=c, zi=zi, z0=z0: e.dma_start(out=zdst[c * 128:(c + 1) * 128, z0:z0 + ZW], in_=zs[zi][:]), reads=[b_zs[zi]], sembuf=b_zs[zi])
    P.end()


def emit_diff_h(nc, P, PFX, L):
    NQ = L // 128
    P.begin(); B = P.buf
    def din(name, shape): return nc.dram_tensor(PFX + name, list(shape), F32, kind="ExternalInput").ap()
    qk_d = din("qk", [4, 64, L])
    v_d = din("v", [128, NQ, 128])
    lam_d = din("lam", [128, 4, 64])
    cst_d = din("cst", [128, 2])
    gsub_d = din("gsub", [128, 128])
    tri_d = din("tri", [128, 128])
    y_d = nc.dram_tensor(PFX + "y", [NQ, 128, 128], F32, kind="ExternalOutput").ap()

    qkb = [P.sb(f"qkb{i}", [64, L], BF16) for i in range(4)]; b_qkb = [B() for _ in range(4)]
    vb = P.sb("vb", [128, NQ, 130], BF16); b_vb = B()
    stg = [P.sb(f"stg{i}", [128, 2048]) for i in range(2)]; b_stg = [B(), B()]
    lam_t = P.sb("lam_t", [128, 4, 64]); b_lam = B()
    cst = P.sb("cst_t", [128, 2]); b_cst = B()
    gsub = P.sb("gsub_t", [128, 128]); b_gsub = B()
    tri_f = P.sb("tri_f", [128, 128]); b_trif = B()
    tri = P.sb("tri_b", [128, 128], BF16); b_tri = B()
    ones = P.sb("ones", [128, 2], BF16); b_ones = B()
    sm = P.sb("sm", [128, 16]); b_sm = [B() for _ in range(16)]
    junk = P.sb("junk", [128, 128]); b_junk = B()
    ET = [[P.sb(f"ET{m}{i}", [128, 4, 128], BF16) for i in range(2)] for m in range(2)]
    b_ET = [[B(), B()] for _ in range(2)]
    ob = [P.sb(f"ob{i}", [128, 128]) for i in range(2)]; b_ob = [B(), B()]
    t2 = P.sb("t2", [128, 128]); b_t2 = B()
    pS = [[P.ps(f"pS{m}{i}", [128, 512]) for i in range(2)] for m in range(2)]; b_pS = [[B(), B()] for _ in range(2)]
    pO = [P.ps(f"pO{m}", [128, 512]) for m in range(2)]; b_pO = [B(), B()]

    n = 0
    for i in range(4):
        CW = min(2048, L)
        for c0 in range(0, L, CW):
            s = n % 2; n += 1
            P.dma("sp", lambda e, i=i, c0=c0, s=s: e.dma_start(out=stg[s][0:64, 0:CW], in_=qk_d[i, :, c0:c0 + CW]), writes=[b_stg[s]])
            P.op("pool", lambda e, i=i, c0=c0, s=s: e.tensor_copy(out=qkb[i][:, c0:c0 + CW], in_=stg[s][0:64, 0:CW]), reads=[b_stg[s]], writes=[b_qkb[i]])
    TW = min(16, NQ)
    for t0 in range(0, NQ, TW):
        s = n % 2; n += 1
        P.dma("sp", lambda e, t0=t0, s=s: e.dma_start(out=stg[s][:, 0:TW * 128], in_=v_d[:, t0:t0 + TW, :].rearrange("p t d -> p (t d)")), writes=[b_stg[s]])
        P.op("pool", lambda e, t0=t0, s=s: e.tensor_copy(out=vb[:, t0:t0 + TW, 0:128], in_=stg[s][:, 0:TW * 128].rearrange("p (t d) -> p t d", d=128)), reads=[b_stg[s]], writes=[b_vb])
    P.dma("sp", lambda e: e.dma_start(out=lam_t[:], in_=lam_d), writes=[b_lam])
    P.dma("sp", lambda e: e.dma_start(out=cst[:], in_=cst_d), writes=[b_cst])
    P.dma("sp", lambda e: e.dma_start(out=gsub[:], in_=gsub_d), writes=[b_gsub])
    P.dma("sp", lambda e: e.dma_start(out=tri_f[:], in_=tri_d), writes=[b_trif])
    P.op("dve", lambda e: e.tensor_copy(out=tri[:], in_=tri_f[:]), reads=[b_trif], writes=[b_tri])
    P.op("dve", lambda e: e.memset(vb[:, :, 128:130], 1.0), writes=[b_vb])
    for j in range(2):
        P.op("dve", lambda e, j=j: e.tensor_tensor(out=junk[:, 0:64], in0=lam_t[:, 2 * j, :], in1=lam_t[:, 2 * j + 1, :], op=ALU.mult), reads=[b_lam], writes=[b_junk])
        P.op("dve", lambda e, j=j: e.reduce_sum(out=sm[:, j:j + 1], in_=junk[:, 0:64], axis=AX.X), reads=[b_junk], writes=[b_sm[j]])
        P.op("act", lambda e, j=j: e.activation(out=sm[:, j:j + 1], in_=sm[:, j:j + 1], func=AF.Exp), reads=[b_sm[j]], writes=[b_sm[j]])
    P.op("dve", lambda e: e.tensor_tensor(out=sm[:, 2:3], in0=sm[:, 1:2], in1=sm[:, 0:1], op=ALU.subtract), reads=[b_sm[0], b_sm[1]], writes=[b_sm[2]])
    P.op("dve", lambda e: e.tensor_tensor(out=sm[:, 2:3], in0=sm[:, 2:3], in1=cst[:, 0:1], op=ALU.subtract), reads=[b_sm[2], b_cst], writes=[b_sm[2]])
    NEGLAM = (sm[:, 2:3], b_sm[2])

    gi = 0
    for qi in range(NQ):
        nk = qi + 1
        groups = [(g0, min(4, nk - g0)) for g0 in range(0, nk, 4)]
        for gidx, (g0, gn) in enumerate(groups):
            par = gi % 2; gi += 1
            for m in range(2):
                for j in range(gn):
                    kt = g0 + j
                    P.op("pe", lambda e, m=m, j=j, kt=kt, par=par, qi=qi: e.matmul(pS[m][par][:, j * 128:(j + 1) * 128], lhsT=qkb[2 + m][:, kt * 128:(kt + 1) * 128], rhs=qkb[m][:, qi * 128:(qi + 1) * 128], start=True, stop=True),
                         reads=[b_qkb[2 + m], b_qkb[m]], writes=[b_pS[m][par]])
                P.op("act", lambda e, m=m, par=par, gn=gn: e.activation(out=ET[m][par][:, 0:gn, :].rearrange("p g q -> p (g q)"), in_=pS[m][par][:, 0:gn * 128], func=AF.Exp, scale=0.125),
                     reads=[b_pS[m][par]], writes=[b_ET[m][par]])
                if g0 + gn == nk:
                    j = gn - 1
                    P.op("dve", lambda e, m=m, par=par, j=j: e.tensor_tensor(out=ET[m][par][:, j, :], in0=ET[m][par][:, j, :], in1=tri[:], op=ALU.mult),
                         reads=[b_ET[m][par], b_tri], writes=[b_ET[m][par]])
                for j in range(gn):
                    kt = g0 + j
                    first = (kt == 0); last = (kt == nk - 1)
                    P.op("pe", lambda e, m=m, j=j, kt=kt, par=par, first=first, last=last: e.matmul(pO[m][:, 0:130], lhsT=ET[m][par][:, j, :], rhs=vb[:, kt, :], start=first, stop=last),
                         reads=[b_ET[m][par], b_vb], writes=[b_pO[m]])
        op_ = qi % 2
        P.op("dve", lambda e: e.reciprocal(out=sm[:, 4:5], in_=pO[0][:, 128:129]), reads=[b_pO[0]], writes=[b_sm[4]])
        P.op("dve", lambda e: e.reciprocal(out=sm[:, 5:6], in_=pO[1][:, 128:129]), reads=[b_pO[1]], writes=[b_sm[5]])
        P.op("dve", lambda e: e.tensor_tensor(out=sm[:, 5:6], in0=sm[:, 5:6], in1=NEGLAM[0], op=ALU.mult), reads=[b_sm[5], NEGLAM[1]], writes=[b_sm[5]])
        P.op("dve", lambda e: e.tensor_scalar(out=t2[:], in0=pO[1][:, 0:128], scalar1=sm[:, 5:6], scalar2=None, op0=ALU.mult), reads=[b_pO[1], b_sm[5]], writes=[b_t2])
        P.op("dve", lambda e, op_=op_: e.scalar_tensor_tensor(out=ob[op_][:], in0=pO[0][:, 0:128], scalar=sm[:, 4:5], in1=t2[:], op0=ALU.mult, op1=ALU.add), reads=[b_pO[0], b_sm[4], b_t2], writes=[b_ob[op_]])
        P.op("act", lambda e, op_=op_: e.activation(out=junk[:], in_=ob[op_][:], func=AF.Square, scale=float(128 ** -0.5), accum_out=sm[:, 6:7]), reads=[b_ob[op_]], writes=[b_junk, b_sm[6]])
        P.op("dve", lambda e: e.tensor_scalar(out=sm[:, 6:7], in0=sm[:, 6:7], scalar1=1e-6, scalar2=None, op0=ALU.add), reads=[b_sm[6]], writes=[b_sm[6]])
        P.op("act", lambda e: e.activation(out=sm[:, 6:7], in_=sm[:, 6:7], func=AF.Sqrt), reads=[b_sm[6]], writes=[b_sm[6]])
        P.op("dve", lambda e: e.reciprocal(out=sm[:, 6:7], in_=sm[:, 6:7]), reads=[b_sm[6]], writes=[b_sm[6]])
        P.op("dve", lambda e: e.tensor_tensor(out=sm[:, 6:7], in0=sm[:, 6:7], in1=cst[:, 1:2], op=ALU.mult), reads=[b_sm[6], b_cst], writes=[b_sm[6]])
        P.op("dve", lambda e, op_=op_: e.scalar_tensor_tensor(out=ob[op_][:], in0=ob[op_][:], scalar=sm[:, 6:7], in1=gsub[:], op0=ALU.mult, op1=ALU.mult), reads=[b_ob[op_], b_sm[6], b_gsub], writes=[b_ob[op_]])
        P.dma("pool", lambda e, qi=qi, op_=op_: e.dma_start(out=y_d[qi], in_=ob[op_][:]), reads=[b_ob[op_]], sembuf=b_ob[op_])
    P.end()


def emit_dsa_h(nc, P, PFX, L, R=16.0, K=18):
    NQ = L // 128
    P.begin(); B = P.buf
    def din(name, shape): return nc.dram_tensor(PFX + name, list(shape), F32, kind="ExternalInput").ap()
    qk_d = din("qk", [2, 128, L])
    v_d = din("v", [128, NQ, 128])
    qi_d = din("qi", [NQ, 64, 8, 128])
    ki_d = din("ki", [64, L])
    wi_d = din("wi", [128, NQ, 8])
    negm_d = din("negm", [128, 128])
    idn_d = din("idn", [128, 128])
    y_d = nc.dram_tensor(PFX + "y", [NQ, 128, 128], F32, kind="ExternalOutput").ap()
    dbg = [P.sb(f"dbg{i}", [128, 8]) for i in range(2)]; b_dbg = [B(), B()]

    qkb = [P.sb(f"qkb{i}", [128, L], BF16) for i in range(2)]; b_qkb = [B(), B()]
    vb = P.sb("vb", [128, NQ, 130], BF16); b_vb = B()
    kiT = P.sb("kiT", [64, L]); b_ki = B()
    wi = P.sb("wi_t", [128, NQ, 8]); b_wi = B()
    negm = P.sb("negm_t", [128, 128]); b_negm = B()
    idf = P.sb("idf", [128, 128]); b_idf = B()
    idb = P.sb("idb", [128, 128], BF16); b_idb = B()
    stg = [P.sb(f"stg{i}", [128, 2048]) for i in range(2)]; b_stg = [B(), B()]
    qit = [P.sb(f"qit{i}", [64, 8, 128]) for i in range(2)]; b_qit = [B(), B()]
    score = [P.sb(f"score{i}", [128, L]) for i in range(2)]; b_score = [B(), B()]
    junkS = P.sb("junkS", [128, L], BF16); b_junkS = B()
    rl = [P.sb(f"rl{i}", [128, 512]) for i in range(2)]; b_rl = [B(), B()]
    Eb = [P.sb(f"Eb{i}", [128, 512], BF16) for i in range(2)]; b_Eb = [B(), B()]
    Pm = [P.sb(f"Pm{i}", [128, 512], BF16) for i in range(2)]; b_Pm = [B(), B()]
    PmT = [P.sb(f"PmT{i}", [128, 4, 128], BF16) for i in range(2)]; b_PmT = [B(), B()]
    ob = [P.sb(f"ob{i}", [128, 128]) for i in range(2)]; b_ob = [B(), B()]
    sm = P.sb("sm", [128, 8]); b_sm = [B() for _ in range(8)]
    pD = [P.ps(f"pD{i}", [128, 512]) for i in range(2)]; b_pD = [B(), B()]
    pS = [P.ps(f"pS{i}", [128, 512]) for i in range(2)]; b_pS = [B(), B()]
    pT = [P.ps(f"pT{i}", [128, 1024], BF16) for i in range(2)]; b_pT = [B(), B()]
    pO = P.ps("pO", [128, 512]); b_pO = B()

    n = 0
    CW = min(2048, L)
    for i in range(2):
        for c0 in range(0, L, CW):
            s = n % 2; n += 1
            P.dma("sp", lambda e, i=i, c0=c0, s=s: e.dma_start(out=stg[s][:, 0:CW], in_=qk_d[i, :, c0:c0 + CW]), writes=[b_stg[s]])
            P.op("pool", lambda e, i=i, c0=c0, s=s: e.tensor_copy(out=qkb[i][:, c0:c0 + CW], in_=stg[s][:, 0:CW]), reads=[b_stg[s]], writes=[b_qkb[i]])
    TW = min(16, NQ)
    for t0 in range(0, NQ, TW):
        s = n % 2; n += 1
        P.dma("sp", lambda e, t0=t0, s=s: e.dma_start(out=stg[s][:, 0:TW * 128], in_=v_d[:, t0:t0 + TW, :].rearrange("p t d -> p (t d)")), writes=[b_stg[s]])
        P.op("pool", lambda e, t0=t0, s=s: e.tensor_copy(out=vb[:, t0:t0 + TW, 0:128], in_=stg[s][:, 0:TW * 128].rearrange("p (t d) -> p t d", d=128)), reads=[b_stg[s]], writes=[b_vb])
    P.op("dve", lambda e: e.memset(vb[:, :, 128:130], 1.0), writes=[b_vb])
    P.dma("sp", lambda e: e.dma_start(out=kiT[:], in_=ki_d), writes=[b_ki])
    P.dma("sp", lambda e: e.dma_start(out=wi[:], in_=wi_d), writes=[b_wi])
    P.dma("sp", lambda e: e.dma_start(out=negm[:], in_=negm_d), writes=[b_negm])
    P.dma("sp", lambda e: e.dma_start(out=idf[:], in_=idn_d), writes=[b_idf])
    P.op("dve", lambda e: e.tensor_copy(out=idb[:], in_=idf[:]), reads=[b_idf], writes=[b_idb])
    SC = float((64 ** -0.5) * (8 ** -0.5))
    cnt_ = {"ci": 0, "ai": 0}
    tau = sm[:, 0:1]; mid = sm[:, 1:2]; cnt = sm[:, 2:3]; s_ = sm[:, 3:4]

    def chunks_of(qi):
        nk = qi + 1
        return [(c0, min(4, nk - c0)) for c0 in range(0, nk, 4)]

    def gen_indexer(qi):
        nk = qi + 1
        sp_ = qi % 2
        sc = score[sp_]; bsc = b_score[sp_]
        P.dma("sp", lambda e, qi=qi, sp_=sp_: e.dma_start(out=qit[sp_][:], in_=qi_d[qi]), writes=[b_qit[sp_]])
        for (c0, cn) in chunks_of(qi):
            w = cn * 128; k0 = c0 * 128
            for h in range(8):
                p = cnt_["ci"] % 2; cnt_["ci"] += 1
                P.op("pe", lambda e, h=h, p=p, k0=k0, w=w, sp_=sp_: e.matmul(pD[p][:, 0:w], lhsT=qit[sp_][:, h, :], rhs=kiT[:, k0:k0 + w], start=True, stop=True),
                     reads=[b_qit[sp_], b_ki], writes=[b_pD[p]])
                P.op("act", lambda e, p=p, w=w: e.activation(out=rl[p][:, 0:w], in_=pD[p][:, 0:w], func=AF.Relu, scale=SC), reads=[b_pD[p]], writes=[b_rl[p]])
                if h == 0:
                    P.op("dve", lambda e, p=p, w=w, k0=k0, sc=sc, qi=qi, h=h: e.tensor_scalar(out=sc[:, k0:k0 + w], in0=rl[p][:, 0:w], scalar1=wi[:, qi, h:h + 1], scalar2=None, op0=ALU.mult),
                         reads=[b_rl[p], b_wi], writes=[bsc])
                else:
                    P.op("dve", lambda e, p=p, w=w, k0=k0, sc=sc, qi=qi, h=h: e.scalar_tensor_tensor(out=sc[:, k0:k0 + w], in0=rl[p][:, 0:w], scalar=wi[:, qi, h:h + 1], in1=sc[:, k0:k0 + w], op0=ALU.mult, op1=ALU.add),
                         reads=[b_rl[p], b_wi, bsc], writes=[bsc])
                yield
        d0 = (nk - 1) * 128
        P.op("dve", lambda e, sc=sc, d0=d0: e.tensor_tensor(out=sc[:, d0:d0 + 128], in0=sc[:, d0:d0 + 128], in1=negm[:], op=ALU.add), reads=[bsc, b_negm], writes=[bsc])
        yield

    def KQ(nkeys):
        return 22 if nkeys <= 2048 else (18 if nkeys <= 4096 else 16)

    def gen_bisect(qi):
        nk = qi + 1; nkeys = nk * 128
        sc = score[qi % 2]; bsc = b_score[qi % 2]
        if nkeys <= 256:
            P.op("dve", lambda e: e.memset(tau, -R), writes=[b_sm[0]])
            yield
            return
        P.op("dve", lambda e: e.memset(mid, 0.0), writes=[b_sm[1]])
        yield
        K = KQ(nkeys)
        for it in range(K):
            P.op("dve", lambda e, sc=sc, nkeys=nkeys: e.tensor_scalar(out=junkS[:, 0:nkeys], in0=sc[:, 0:nkeys], scalar1=mid, scalar2=0.0, op0=ALU.is_ge, op1=ALU.add, accum_out=cnt),
                 reads=[bsc, b_sm[1]], writes=[b_junkS, b_sm[2]])
            yield
            if it < K - 1:
                wn = R / 2 ** (it + 1)
                P.op("dve", lambda e, wn=wn: e.tensor_scalar(out=s_, in0=cnt, scalar1=255.5, scalar2=2 * wn, op0=ALU.is_ge, op1=ALU.mult), reads=[b_sm[2]], writes=[b_sm[3]])
                yield
                P.op("dve", lambda e, wn=wn: e.scalar_tensor_tensor(out=mid, in0=s_, scalar=-wn, in1=mid, op0=ALU.add, op1=ALU.add), reads=[b_sm[3], b_sm[1]], writes=[b_sm[1]])
                yield
            else:
                wl = R / 2 ** (K - 1)
                P.op("dve", lambda e, wl=wl: e.tensor_scalar(out=s_, in0=cnt, scalar1=255.5, scalar2=wl, op0=ALU.is_ge, op1=ALU.mult), reads=[b_sm[2]], writes=[b_sm[3]])
                yield
                P.op("dve", lambda e, wl=wl: e.scalar_tensor_tensor(out=tau, in0=s_, scalar=-wl, in1=mid, op0=ALU.add, op1=ALU.add), reads=[b_sm[3], b_sm[1]], writes=[b_sm[0]])
                yield

    def attention(qi):
        nk = qi + 1
        sc = score[qi % 2]; bsc = b_score[qi % 2]
        for (c0, cn) in chunks_of(qi):
            w = cn * 128; k0 = c0 * 128
            p = cnt_["ai"] % 2; cnt_["ai"] += 1
            P.op("pe", lambda e, p=p, k0=k0, w=w, qi=qi: e.matmul(pS[p][:, 0:w], lhsT=qkb[0][:, qi * 128:(qi + 1) * 128], rhs=qkb[1][:, k0:k0 + w], start=True, stop=True),
                 reads=[b_qkb[0], b_qkb[1]], writes=[b_pS[p]])
            P.op("act", lambda e, p=p, w=w: e.activation(out=Eb[p][:, 0:w], in_=pS[p][:, 0:w], func=AF.Exp, scale=float(128 ** -0.5)), reads=[b_pS[p]], writes=[b_Eb[p]])
            P.op("dve", lambda e, p=p, w=w, k0=k0, sc=sc: e.scalar_tensor_tensor(out=Pm[p][:, 0:w], in0=sc[:, k0:k0 + w], scalar=tau, in1=Eb[p][:, 0:w], op0=ALU.is_ge, op1=ALU.mult),
                 reads=[bsc, b_sm[0], b_Eb[p]], writes=[b_Pm[p]])
            for j in range(cn):
                P.op("pe", lambda e, p=p, j=j: e.transpose(pT[p][:, j * 128:(j + 1) * 128], Pm[p][:, j * 128:(j + 1) * 128], idb[:]), reads=[b_Pm[p], b_idb], writes=[b_pT[p]])
            P.op("act", lambda e, p=p, w=w, cn=cn: e.copy(out=PmT[p][:, 0:cn, :].rearrange("p g q -> p (g q)"), in_=pT[p][:, 0:w]), reads=[b_pT[p]], writes=[b_PmT[p]])
            for j in range(cn):
                kt = c0 + j
                P.op("pe", lambda e, p=p, j=j, kt=kt, nk=nk: e.matmul(pO[:, 0:130], lhsT=PmT[p][:, j, :], rhs=vb[:, kt, :], start=(kt == 0), stop=(kt == nk - 1)),
                     reads=[b_PmT[p], b_vb], writes=[b_pO])
        op_ = qi % 2
        P.op("dve", lambda e: e.reciprocal(out=sm[:, 4:5], in_=pO[:, 128:129]), reads=[b_pO], writes=[b_sm[4]])
        P.op("dve", lambda e, op_=op_: e.tensor_scalar(out=ob[op_][:], in0=pO[:, 0:128], scalar1=sm[:, 4:5], scalar2=None, op0=ALU.mult), reads=[b_pO, b_sm[4]], writes=[b_ob[op_]])
        P.dma("pool", lambda e, qi=qi, op_=op_: e.dma_start(out=y_d[qi], in_=ob[op_][:]), reads=[b_ob[op_]], sembuf=b_ob[op_])

    for _ in gen_indexer(0):
        pass
    for qi in range(NQ):
        gb = gen_bisect(qi)
        gi_ = gen_indexer(qi + 1) if qi + 1 < NQ else iter(())
        nb = 1 if (qi + 1) * 128 <= 256 else 3 * KQ((qi + 1) * 128) + 1
        ni = 8 * len(chunks_of(qi + 1)) + 1 if qi + 1 < NQ else 0
        ratio = max(1, -(-ni // nb))
        b_done = False; i_done = (ni == 0)
        while not (b_done and i_done):
            if not b_done:
                try:
                    next(gb)
                except StopIteration:
                    b_done = True
            for _ in range(ratio):
                if i_done:
                    break
                try:
                    next(gi_)
                except StopIteration:
                    i_done = True
        attention(qi)
    P.end()


def emit_rwkv_h(nc, P, PFX, L):
    SEG = min(L, 512); NSEG = L // SEG; NCH = SEG // 64
    P.begin(); B = P.buf
    def din(name, shape): return nc.dram_tensor(PFX + name, list(shape), F32, kind="ExternalInput").ap()
    zr_d = din("zr", [3, 64, 2, L + 1]); zl_d = din("zl", [64, 2, L + 1]); zg_d = din("zg", [128, L + 1])
    mu3_d = din("mu3", [64, 3, 2]); mul_d = din("mul", [64, 2]); mug_d = din("mug", [128, 1])
    pp_d = din("pp", [64, 5, 2]); wup_d = din("wup", [64, 2, 64]); aup_d = din("aup", [64, 2, 64]); gup_d = din("gup", [128, 128])
    lnwb_d = din("lnwb", [64, 2, 128]); cmask_d = din("cmask", [64, 2 * SEG]); mask5_d = din("mask5", [64, 320])
    idn_d = din("idn", [64, 64])
    y_d = nc.dram_tensor(PFX + "y", [L // 64, 64, 128], F32, kind="ExternalOutput").ap()
    def T(name, shape, dt=F32):
        return P.sb(name, shape, dt), B(name)
    raw3, b_raw3 = T("raw3", [64, 3, 2, SEG + 1]); rawl, b_rawl = T("rawl", [64, 2, SEG + 1]); rawg, b_rawg = T("rawg", [128, SEG + 1])
    mu3, b_mu3 = T("mu3t", [64, 3, 2]); mul, b_mul = T("mult", [64, 2]); mug, b_mug = T("mugt", [128, 1])
    pp, b_pp = T("ppt", [64, 5, 2]); wup, b_wup = T("wupt", [64, 2, 64]); aup, b_aup = T("aupt", [64, 2, 64]); gup, b_gup = T("gupt", [128, 128])
    lnwb, b_lnwb = T("lnwbt", [64, 2, 128]); cmask, b_cmask = T("cmaskt", [64, 2 * SEG]); mask5, b_mask5 = T("mask5t", [64, 320])
    idn, b_idn = T("idnt", [64, 64]); ones, b_ones = T("onest", [64, 64])
    d3, b_d3 = T("d3", [64, 3, 2, SEG]); dl, b_dl = T("dl", [64, 2, SEG]); dg, b_dg = T("dg", [128, SEG])
    x3, b_x3 = T("x3", [64, 3, 2, SEG]); xl, b_xl = T("xl", [64, 2, SEG]); xg, b_xg = T("xg", [128, SEG])
    tw, b_tw = T("tw", [64, SEG]); sgc, b_sgc = T("sgc", [128, SEG]); sgw, b_sgw = T("sgw", [64, 2, SEG]); aa, b_aa = T("aa", [64, 2, SEG])
    t1, b_t1 = T("t1", [64, 2, SEG]); sq, b_sq = T("sq", [64, 2, SEG]); rn, b_rn = T("rn", [64, 2, SEG]); kk, b_kk = T("kk", [64, 2, SEG])
    kp, b_kp = T("kp", [64, 2, SEG]); bb, b_bb = T("bb", [64, 2, SEG]); cs, b_cs = T("cs", [64, 2, SEG])
    epos, b_epos = T("epos", [64, 2, SEG]); eneg, b_eneg = T("eneg", [64, 2, SEG]); eprev, b_eprev = T("eprev", [64, 2, SEG])
    AR, b_AR = T("AR", [64, 2, NCH, 2, 64]); Bt, b_Bt = T("Bt", [64, 2, SEG]); Kt, b_Kt = T("Kt", [64, 2, SEG]); rkr, b_rkr = T("rkr", [64, 2, SEG])
    Hs = [T(f"H{i}", [64, 2, 64]) for i in range(2)]
    Msb2 = [[T(f"Msb{p}{h}", [64, 320]) for h in range(2)] for p in range(2)]
    TK2 = [T(f"TK{p}", [64, 6, 64]) for p in range(2)]
    Xfin = [T(f"Xf{p}", [64, 2, 64]) for p in range(2)]
    st_ = {"hcur": 0, "oi": 0}
    PPs = [T(f"PP{i}", [64, 2, 2, 64]) for i in range(2)]
    Xs = [T(f"X{i}", [64, 2, 64]) for i in range(2)]
    Wsb, b_Wsb = T("Wsb", [64, 128]); Usb, b_Usb = T("Usb", [64, 128]); Ysb, b_Ysb = T("Ysb", [64, 128]); yc, b_yc = T("yc", [64, 128])
    outs = [T(f"out{i}", [64, 128]) for i in range(2)]
    sm, _ = T("sm", [64, 16]); b_sm = [B() for _ in range(16)]
    junk, b_junk = T("junk", [64, 64])
    def PS(name, shape): return P.ps(name, shape), B(name)
    pM = [PS(f"pM{h}", [64, 512]) for h in range(2)]
    pK, b_pK = PS("pK", [64, 512]); pI, b_pI = PS("pI", [64, 512]); pX, b_pX = PS("pX", [64, 512])
    pW, b_pW = PS("pW", [64, 512]); pY, b_pY = PS("pY", [64, 512]); pH, b_pH = PS("pH", [64, 512])

    for (t, b, d) in [(mu3, b_mu3, mu3_d), (mul, b_mul, mul_d), (mug, b_mug, mug_d), (pp, b_pp, pp_d), (wup, b_wup, wup_d), (aup, b_aup, aup_d),
                      (gup, b_gup, gup_d), (lnwb, b_lnwb, lnwb_d), (cmask, b_cmask, cmask_d), (mask5, b_mask5, mask5_d), (idn, b_idn, idn_d)]:
        P.dma("sp", lambda e, t=t, d=d: e.dma_start(out=t[:], in_=d), writes=[b])
    P.op("dve", lambda e: e.memset(ones[:], 1.0), writes=[b_ones])
    P.op("dve", lambda e: e.memset(Hs[0][0][:], 0.0), writes=[Hs[0][1]])
    NEG = -0.6065306597126334
    for sg_ in range(NSEG):
        s0 = sg_ * SEG
        for a in range(3):
            P.dma("sp", lambda e, s0=s0, a=a: e.dma_start(out=raw3[:, a, :, :], in_=zr_d[a, :, :, s0:s0 + SEG + 1]), writes=[b_raw3])
        P.dma("sp", lambda e, s0=s0: e.dma_start(out=rawl[:], in_=zl_d[:, :, s0:s0 + SEG + 1]), writes=[b_rawl])
        P.dma("sp", lambda e, s0=s0: e.dma_start(out=rawg[:], in_=zg_d[:, s0:s0 + SEG + 1]), writes=[b_rawg])
        P.op("dve", lambda e: e.tensor_tensor(out=d3[:], in0=raw3[:, :, :, 0:SEG], in1=raw3[:, :, :, 1:SEG + 1], op=ALU.subtract), reads=[b_raw3], writes=[b_d3])
        P.op("dve", lambda e: e.tensor_tensor(out=dl[:], in0=rawl[:, :, 0:SEG], in1=rawl[:, :, 1:SEG + 1], op=ALU.subtract), reads=[b_rawl], writes=[b_dl])
        P.op("dve", lambda e: e.tensor_tensor(out=dg[:], in0=rawg[:, 0:SEG], in1=rawg[:, 1:SEG + 1], op=ALU.subtract), reads=[b_rawg], writes=[b_dg])
        for a in range(3):
            for h in range(2):
                P.op("dve", lambda e, a=a, h=h: e.scalar_tensor_tensor(out=x3[:, a, h, :], in0=d3[:, a, h, :], scalar=mu3[:, a, h:h + 1], in1=raw3[:, a, h, 1:SEG + 1], op0=ALU.mult, op1=ALU.add),
                     reads=[b_d3, b_mu3, b_raw3], writes=[b_x3])
        for a in range(2):
            P.op("dve", lambda e, a=a: e.scalar_tensor_tensor(out=xl[:, a, :], in0=dl[:, a, :], scalar=mul[:, a:a + 1], in1=rawl[:, a, 1:SEG + 1], op0=ALU.mult, op1=ALU.add),
                 reads=[b_dl, b_mul, b_rawl], writes=[b_xl])
        P.op("dve", lambda e: e.scalar_tensor_tensor(out=xg[:], in0=dg[:], scalar=mug[:, 0:1], in1=rawg[:, 1:SEG + 1], op0=ALU.mult, op1=ALU.add), reads=[b_dg, b_mug, b_rawg], writes=[b_xg])
        XR = lambda h: x3[:, 0, h, :]
        XK = lambda h: x3[:, 1, h, :]
        XV = lambda h: x3[:, 2, h, :]
        P.op("act", lambda e: e.activation(out=tw[:], in_=xl[:, 0, :], func=AF.Tanh), reads=[b_xl], writes=[b_tw])
        P.op("act", lambda e: e.activation(out=sgc[:], in_=xg[:], func=AF.Sigmoid), reads=[b_xg], writes=[b_sgc])
        for h in range(2):
            P.op("pe", lambda e, h=h: e.matmul(pK[:, 0:SEG], lhsT=wup[:, h, :], rhs=tw[:], start=True, stop=True), reads=[b_wup, b_tw], writes=[b_pK])
            P.op("act", lambda e, h=h: e.activation(out=sgw[:, h, :], in_=pK[:, 0:SEG], func=AF.Sigmoid, bias=pp[:, 0, h:h + 1]), reads=[b_pK, b_pp], writes=[b_sgw])
            P.op("pe", lambda e, h=h: e.matmul(pK[:, 0:SEG], lhsT=aup[:, h, :], rhs=xl[:, 1, :], start=True, stop=True), reads=[b_aup, b_xl], writes=[b_pK])
            P.op("act", lambda e, h=h: e.activation(out=aa[:, h, :], in_=pK[:, 0:SEG], func=AF.Sigmoid, bias=pp[:, 1, h:h + 1]), reads=[b_pK, b_pp], writes=[b_aa])
        for h in range(2):
            P.op("dve", lambda e, h=h: e.tensor_scalar(out=t1[:, h, :], in0=XK(h), scalar1=pp[:, 2, h:h + 1], scalar2=None, op0=ALU.mult), reads=[b_x3, b_pp], writes=[b_t1])
        P.op("dve", lambda e: e.tensor_tensor(out=sq[:], in0=t1[:], in1=t1[:], op=ALU.mult), reads=[b_t1], writes=[b_sq])
        for h in range(2):
            P.op("pe", lambda e, h=h: e.matmul(pK[:, 0:SEG], lhsT=ones[:], rhs=sq[:, h, :], start=True, stop=True), reads=[b_ones, b_sq], writes=[b_pK])
            P.op("dve", lambda e, h=h: e.tensor_scalar(out=rn[:, h, :], in0=pK[:, 0:SEG], scalar1=1e-24, scalar2=None, op0=ALU.max), reads=[b_pK], writes=[b_rn])
        P.op("act", lambda e: e.activation(out=rn[:], in_=rn[:], func=AF.Sqrt), reads=[b_rn], writes=[b_rn])
        P.op("dve", lambda e: e.reciprocal(out=rn[:], in_=rn[:]), reads=[b_rn], writes=[b_rn])
        P.op("dve", lambda e: e.tensor_tensor(out=kk[:], in0=t1[:], in1=rn[:], op=ALU.mult), reads=[b_t1, b_rn], writes=[b_kk])
        for h in range(2):
            P.op("dve", lambda e, h=h: e.tensor_scalar(out=kp[:, h, :], in0=aa[:, h, :], scalar1=pp[:, 3, h:h + 1], scalar2=pp[:, 3, h:h + 1], op0=ALU.mult, op1=ALU.subtract), reads=[b_aa, b_pp], writes=[b_kp])
            P.op("dve", lambda e, h=h: e.scalar_tensor_tensor(out=kp[:, h, :], in0=kp[:, h, :], scalar=1.0, in1=XK(h), op0=ALU.add, op1=ALU.mult), reads=[b_kp, b_x3], writes=[b_kp])
        P.op("dve", lambda e: e.tensor_tensor(out=bb[:], in0=kk[:], in1=aa[:], op=ALU.mult), reads=[b_kk, b_aa], writes=[b_bb])
        FL = lambda t: t[:].rearrange("p h s -> p (h s)")
        P.op("dve", lambda e: e.tensor_tensor_scan(out=FL(cs), data0=cmask[:], data1=FL(sgw), initial=0.0, op0=ALU.mult, op1=ALU.add), reads=[b_cmask, b_sgw], writes=[b_cs])
        P.op("act", lambda e: e.activation(out=epos[:], in_=cs[:], func=AF.Exp, scale=NEG), reads=[b_cs], writes=[b_epos])
        P.op("act", lambda e: e.activation(out=eneg[:], in_=cs[:], func=AF.Exp, scale=-NEG), reads=[b_cs], writes=[b_eneg])
        P.op("dve", lambda e: e.tensor_tensor(out=eprev[:], in0=cs[:], in1=sgw[:], op=ALU.subtract), reads=[b_cs, b_sgw], writes=[b_eprev])
        P.op("act", lambda e: e.activation(out=eprev[:], in_=eprev[:], func=AF.Exp, scale=NEG), reads=[b_eprev], writes=[b_eprev])
        for h in range(2):
            P.op("dve", lambda e, h=h: e.scalar_tensor_tensor(out=AR[:, h, :, 0, :], in0=kk[:, h, :].rearrange("p (c t) -> p c t", t=64), scalar=-1.0, in1=eprev[:, h, :].rearrange("p (c t) -> p c t", t=64), op0=ALU.mult, op1=ALU.mult),
                 reads=[b_kk, b_eprev], writes=[b_AR])
            P.op("dve", lambda e, h=h: e.tensor_tensor(out=AR[:, h, :, 1, :], in0=XR(h).rearrange("p (c t) -> p c t", t=64), in1=epos[:, h, :].rearrange("p (c t) -> p c t", t=64), op=ALU.mult),
                 reads=[b_x3, b_epos], writes=[b_AR])
            P.op("dve", lambda e, h=h: e.scalar_tensor_tensor(out=rkr[:, h, :], in0=XR(h), scalar=pp[:, 4, h:h + 1], in1=kp[:, h, :], op0=ALU.mult, op1=ALU.mult), reads=[b_x3, b_pp, b_kp], writes=[b_rkr])
        P.op("dve", lambda e: e.tensor_tensor(out=Bt[:], in0=bb[:], in1=eneg[:], op=ALU.mult), reads=[b_bb, b_eneg], writes=[b_Bt])
        P.op("dve", lambda e: e.tensor_tensor(out=Kt[:], in0=kp[:], in1=eneg[:], op=ALU.mult), reads=[b_kp, b_eneg], writes=[b_Kt])
        def pre(c):
            cs_ = slice(c * 64, (c + 1) * 64)
            for h in range(2):
                pm, bpm = pM[h]
                P.op("pe", lambda e, h=h, c=c, pm=pm, cs_=cs_: e.matmul(pm[:, 0:64], lhsT=AR[:, h, c, 0, :], rhs=Bt[:, h, cs_], start=True, stop=True), reads=[b_AR, b_Bt], writes=[bpm])
                P.op("pe", lambda e, h=h, c=c, pm=pm, cs_=cs_: e.matmul(pm[:, 64:192], lhsT=Bt[:, h, cs_], rhs=AR[:, h, c, :, :].rearrange("p a t -> p (a t)"), start=True, stop=True), reads=[b_AR, b_Bt], writes=[bpm])
                P.op("pe", lambda e, h=h, c=c, pm=pm, cs_=cs_: e.matmul(pm[:, 192:320], lhsT=Kt[:, h, cs_], rhs=AR[:, h, c, :, :].rearrange("p a t -> p (a t)"), start=True, stop=True), reads=[b_AR, b_Kt], writes=[bpm])
                P.op("dve", lambda e, h=h, pm=pm: e.tensor_tensor(out=Msb2[c % 2][h][0][:], in0=pm[:, 0:320], in1=mask5[:], op=ALU.mult), reads=[bpm, b_mask5], writes=[Msb2[c % 2][h][1]])
            yield
            for h in range(2):
                for a, (src, bsrc) in enumerate([(Bt[:, h, cs_], b_Bt), (Kt[:, h, cs_], b_Kt), (x3[:, 2, h, cs_], b_x3)]):
                    P.op("pe", lambda e, h=h, a=a, src=src: e.transpose(pK[:, (h * 3 + a) * 64:(h * 3 + a + 1) * 64], src, idn[:]), reads=[bsrc, b_idn], writes=[b_pK])
            yield
            P.op("act", lambda e: e.copy(out=TK2[c % 2][0][:].rearrange("p a t -> p (a t)"), in_=pK[:, 0:384]), reads=[b_pK], writes=[TK2[c % 2][1]])
            yield
            pcur = 0; xcur = 0
            yield
            for h in range(2):
                P.op("act", lambda e, h=h: e.copy(out=PPs[0][0][:, h, :, :].rearrange("p a t -> p (a t)"), in_=Msb2[c % 2][h][0][:, 0:128]), reads=[Msb2[c % 2][h][1]], writes=[PPs[0][1]])
                P.op("dve", lambda e, h=h: e.tensor_tensor(out=Xs[0][0][:, h, :], in0=Msb2[c % 2][h][0][:, 64:128], in1=idn[:], op=ALU.add), reads=[Msb2[c % 2][h][1], b_idn], writes=[Xs[0][1]])
            yield
            for stp in range(5):
                pp_t, pp_b = PPs[pcur]; pn_t, pn_b = PPs[1 - pcur]
                x_t, x_b = Xs[xcur]; xn_t, xn_b = Xs[1 - xcur]
                for h in range(2):
                    P.op("pe", lambda e, h=h, pp_t=pp_t: e.matmul(pI[:, (h * 2) * 64:(h * 2 + 1) * 64], lhsT=pp_t[:, h, 1, :], rhs=pp_t[:, h, 0, :], start=True, stop=True), reads=[pp_b], writes=[b_pI])
                    P.op("pe", lambda e, h=h, pp_t=pp_t: e.matmul(pI[:, (h * 2 + 1) * 64:(h * 2 + 2) * 64], lhsT=pp_t[:, h, 0, :], rhs=pp_t[:, h, 1, :], start=True, stop=True), reads=[pp_b], writes=[b_pI])
                P.op("act", lambda e, pn_t=pn_t: e.copy(out=pn_t[:].rearrange("p h a t -> p (h a t)"), in_=pI[:, 0:256]), reads=[b_pI], writes=[pn_b])
                for h in range(2):
                    P.op("pe", lambda e, h=h, x_t=x_t: e.matmul(pX[:, h * 64:(h + 1) * 64], lhsT=idn[:], rhs=x_t[:, h, :], start=True, stop=False), reads=[b_idn, x_b], writes=[b_pX])
                    P.op("pe", lambda e, h=h, x_t=x_t, pn_t=pn_t: e.matmul(pX[:, h * 64:(h + 1) * 64], lhsT=pn_t[:, h, 0, :], rhs=x_t[:, h, :], start=False, stop=True), reads=[pn_b, x_b], writes=[b_pX])
                P.op("dve", lambda e, xn_t=xn_t: e.tensor_copy(out=xn_t[:].rearrange("p h t -> p (h t)"), in_=pX[:, 0:128]), reads=[b_pX], writes=[xn_b])
                pcur = 1 - pcur; xcur = 1 - xcur
            yield

            Xf_t, Xf_b = Xfin[c % 2]
            X_t, X_b = Xs[xcur]
            P.op("act", lambda e, Xf_t=Xf_t, X_t=X_t: e.copy(out=Xf_t[:].rearrange("p h t -> p (h t)"), in_=X_t[:].rearrange("p h t -> p (h t)")), reads=[X_b], writes=[Xf_b])
            yield

        def seq(c):
            cs_ = slice(c * 64, (c + 1) * 64)
            X_t, X_b = Xfin[c % 2]
            H_t, H_b = Hs[st_['hcur']]; Hn_t, Hn_b = Hs[1 - st_['hcur']]
            for h in range(2):
                P.op("pe", lambda e, h=h, c=c, H_t=H_t: e.matmul(pW[:, h * 64:(h + 1) * 64], lhsT=AR[:, h, c, 0, :], rhs=H_t[:, h, :], start=True, stop=False), reads=[b_AR, H_b], writes=[b_pW])
                P.op("pe", lambda e, h=h: e.matmul(pW[:, h * 64:(h + 1) * 64], lhsT=Msb2[c % 2][h][0][:, 192:256], rhs=TK2[c % 2][0][:, h * 3 + 2, :], start=False, stop=True), reads=[Msb2[c % 2][h][1], TK2[c % 2][1]], writes=[b_pW])
            yield
            P.op("act", lambda e: e.copy(out=Wsb[:], in_=pW[:, 0:128]), reads=[b_pW], writes=[b_Wsb])
            yield
            for h in range(2):
                P.op("pe", lambda e, h=h, X_t=X_t: e.matmul(pW[:, 128 + h * 64:128 + (h + 1) * 64], lhsT=X_t[:, h, :], rhs=Wsb[:, h * 64:(h + 1) * 64], start=True, stop=True), reads=[X_b, b_Wsb], writes=[b_pW])
            yield
            P.op("dve", lambda e: e.tensor_copy(out=Usb[:], in_=pW[:, 128:256]), reads=[b_pW], writes=[b_Usb])
            yield
            for h in range(2):
                hs = slice(h * 64, (h + 1) * 64)
                P.op("pe", lambda e, h=h, c=c, hs=hs, H_t=H_t: e.matmul(pY[:, hs], lhsT=AR[:, h, c, 1, :], rhs=H_t[:, h, :], start=True, stop=False), reads=[b_AR, H_b], writes=[b_pY])
                P.op("pe", lambda e, h=h, hs=hs: e.matmul(pY[:, hs], lhsT=Msb2[c % 2][h][0][:, 128:192], rhs=Usb[:, hs], start=False, stop=False), reads=[Msb2[c % 2][h][1], b_Usb], writes=[b_pY])
                P.op("pe", lambda e, h=h, hs=hs: e.matmul(pY[:, hs], lhsT=Msb2[c % 2][h][0][:, 256:320], rhs=TK2[c % 2][0][:, h * 3 + 2, :], start=False, stop=True), reads=[Msb2[c % 2][h][1], TK2[c % 2][1]], writes=[b_pY])
            yield
            P.op("pe", lambda e, cs_=cs_: e.matmul(pY[:, 128:256], lhsT=sgc[:, cs_], rhs=gup[:], start=True, stop=True), reads=[b_sgc, b_gup], writes=[b_pY])
            yield
            for h in range(2):
                P.op("pe", lambda e, h=h, cs_=cs_: e.matmul(pY[:, 256 + 2 * h:258 + 2 * h], lhsT=rkr[:, h, cs_], rhs=ones[:, 0:2], start=True, stop=True), reads=[b_rkr, b_ones], writes=[b_pY])
            yield
            for h in range(2):
                hs = slice(h * 64, (h + 1) * 64)
                P.op("pe", lambda e, h=h, hs=hs, H_t=H_t: e.matmul(pH[:, hs], lhsT=idn[:], rhs=H_t[:, h, :], start=True, stop=False), reads=[b_idn, H_b], writes=[b_pH])
                P.op("pe", lambda e, h=h, hs=hs: e.matmul(pH[:, hs], lhsT=TK2[c % 2][0][:, h * 3 + 0, :], rhs=Usb[:, hs], start=False, stop=False), reads=[TK2[c % 2][1], b_Usb], writes=[b_pH])
                P.op("pe", lambda e, h=h, hs=hs: e.matmul(pH[:, hs], lhsT=TK2[c % 2][0][:, h * 3 + 1, :], rhs=TK2[c % 2][0][:, h * 3 + 2, :], start=False, stop=True), reads=[TK2[c % 2][1]], writes=[b_pH])
            yield
            for h in range(2):
                ce = c * 64 + 63
                P.op("dve", lambda e, h=h, ce=ce, Hn_t=Hn_t: e.tensor_scalar(out=Hn_t[:, h, :], in0=pH[:, h * 64:(h + 1) * 64], scalar1=epos[:, h, ce:ce + 1], scalar2=None, op0=ALU.mult), reads=[b_pH, b_epos], writes=[Hn_b])
            yield
            st_['hcur'] = 1 - st_['hcur']
            P.op("act", lambda e: e.copy(out=Ysb[:], in_=pY[:, 0:128]), reads=[b_pY], writes=[b_Ysb])
            yield
            P.op("dve", lambda e: e.tensor_copy(out=sm[:, 8:12], in_=pY[:, 256:260]), reads=[b_pY], writes=[b_sm[8]])
            yield
            o_t, o_b = outs[st_['oi'] % 2]; st_['oi'] += 1
            for h in range(2):
                hs = slice(h * 64, (h + 1) * 64)
                mcol = sm[:, h:h + 1]; vcol = sm[:, 2 + h:3 + h]
                P.op("dve", lambda e, hs=hs, mcol=mcol: e.reduce_sum(out=mcol, in_=Ysb[:, hs], axis=AX.X), reads=[b_Ysb], writes=[b_sm[h]])
                P.op("dve", lambda e, mcol=mcol: e.tensor_scalar(out=mcol, in0=mcol, scalar1=-1.0 / 64, scalar2=None, op0=ALU.mult), reads=[b_sm[h]], writes=[b_sm[h]])
                P.op("dve", lambda e, hs=hs, mcol=mcol: e.tensor_scalar(out=yc[:, hs], in0=Ysb[:, hs], scalar1=mcol, scalar2=None, op0=ALU.add), reads=[b_Ysb, b_sm[h]], writes=[b_yc])
                P.op("act", lambda e, hs=hs, vcol=vcol: e.activation(out=junk[:], in_=yc[:, hs], func=AF.Square, scale=0.125, accum_out=vcol), reads=[b_yc], writes=[b_junk, b_sm[2 + h]])
                P.op("dve", lambda e, vcol=vcol: e.tensor_scalar(out=vcol, in0=vcol, scalar1=64e-5, scalar2=None, op0=ALU.add), reads=[b_sm[2 + h]], writes=[b_sm[2 + h]])
                P.op("act", lambda e, vcol=vcol: e.activation(out=vcol, in_=vcol, func=AF.Sqrt), reads=[b_sm[2 + h]], writes=[b_sm[2 + h]])
                P.op("dve", lambda e, vcol=vcol: e.reciprocal(out=vcol, in_=vcol), reads=[b_sm[2 + h]], writes=[b_sm[2 + h]])
                P.op("dve", lambda e, hs=hs, vcol=vcol: e.scalar_tensor_tensor(out=yc[:, hs], in0=yc[:, hs], scalar=vcol, in1=lnwb[:, 0, hs], op0=ALU.mult, op1=ALU.mult), reads=[b_yc, b_sm[2 + h], b_lnwb], writes=[b_yc])
                P.op("dve", lambda e, hs=hs: e.tensor_tensor(out=yc[:, hs], in0=yc[:, hs], in1=lnwb[:, 1, hs], op=ALU.add), reads=[b_yc, b_lnwb], writes=[b_yc])
                P.op("dve", lambda e, hs=hs, h=h: e.scalar_tensor_tensor(out=yc[:, hs], in0=TK2[c % 2][0][:, h * 3 + 2, :], scalar=sm[:, 8 + 2 * h:9 + 2 * h], in1=yc[:, hs], op0=ALU.mult, op1=ALU.add), reads=[TK2[c % 2][1], b_sm[8], b_yc], writes=[b_yc])
            yield
            P.op("dve", lambda e, o_t=o_t: e.tensor_tensor(out=o_t[:], in0=yc[:], in1=pY[:, 128:256], op=ALU.mult), reads=[b_yc, b_pY], writes=[o_b])
            yield
            gci = sg_ * NCH + c
            P.dma("pool", lambda e, gci=gci, o_t=o_t: e.dma_start(out=y_d[gci], in_=o_t[:]), reads=[o_b], sembuf=o_b)
            yield
            yield

        for _ in pre(0):
            pass
        for c in range(NCH):
            gs = seq(c)
            gp = pre(c + 1) if c + 1 < NCH else iter(())
            s_done = False; p_done = (c + 1 >= NCH)
            while not (s_done and p_done):
                if not s_done:
                    try:
                        next(gs)
                    except StopIteration:
                        s_done = True
                for _ in range(2):
                    if p_done:
                        break
                    try:
                        next(gp)
                    except StopIteration:
                        p_done = True
    P.end()


TOK = 2048; NT = 16
def emit_merge_h(nc, P, PFX, x_out=None):
    P.begin(); B = P.buf
    def din(name, shape): return nc.dram_tensor(PFX + name, list(shape), F32, kind="ExternalInput").ap()
    x_in = din("x", [TOK, D]); g_mpre = din("g_mpre", [128, D]); g_mpost = din("g_mpost", [128, D])
    wgate = din("wgate", [8, 128, 3072]); wbr = din("wbr", [12, 128, D]); wo = din("wo", [8, 128, D])
    yT = din("yT", [NT, 128, 12 * 128]); idn_d = din("idn", [128, 128])
    ident_f = P.sb("ident_f", [128, 128]); ident = P.sb("ident", [128, 128], BF16)
    gpre_t = P.sb("gpre_t", [128, D]); gpost_t = P.sb("gpost_t", [128, D])
    Wg = P.sb("Wg", [128, 8, 3072], BF16); Wb = P.sb("Wb", [128, 12, D], BF16); Wo = P.sb("Wo", [128, 8, D], BF16)
    stg = [P.sb(f"stg{i}", [128, 3072]) for i in range(2)]
    xt = [P.sb(f"xt{i}", [128, D]) for i in range(2)]; ot = [P.sb(f"ot{i}", [128, D]) for i in range(2)]
    mg = P.sb("mg", [128, D]); tmp = P.sb("tmp", [128, 512]); junk = P.sb("junk", [128, D])
    hb = P.sb("hb", [128, D], BF16); mb = P.sb("mb", [128, D], BF16)
    hTt = P.sb("hTt", [128, 8, 128], BF16); mT = P.sb("mT", [128, 8, 128], BF16)
    ystg = [P.sb(f"ystg{i}", [128, 1536]) for i in range(2)]; ytb = [P.sb(f"ytb{i}", [128, 12, 128], BF16) for i in range(2)]
    sgt = [P.sb(f"sgt{i}", [128, 512]) for i in range(2)]
    st = P.sb("st", [128, 8])
    pA = [P.ps(f"pA{i}", [128, 512]) for i in range(2)]; pB = [P.ps(f"pB{i}", [128, 512]) for i in range(2)]
    pC = [P.ps(f"pC{i}", [128, 512]) for i in range(2)]; pT = P.ps("pT", [128, 1024], BF16)
    b_idf = B(); b_id = B(); b_gpre = B(); b_gpost = B(); b_Wg = B(); b_Wb = B(); b_Wo = B(); b_stg = [B(), B()]
    b_xt = [B(), B()]; b_ot = [B(), B()]; b_mg = B(); b_tmp = B(); b_junk = B(); b_hb = B(); b_mb = B(); b_hTt = B(); b_mT = B()
    b_ystg = [B(), B()]; b_ytb = [B(), B()]; b_sgt = [B(), B()]; b_st = [B() for _ in range(8)]
    b_pA = [B(), B()]; b_pB = [B(), B()]; b_pC = [B(), B()]; b_pT = B()
    P.dma("sp", lambda e: e.dma_start(out=ident_f[:], in_=idn_d), writes=[b_idf])
    P.op("dve", lambda e: e.tensor_copy(out=ident[:], in_=ident_f[:]), reads=[b_idf], writes=[b_id])
    P.dma("sp", lambda e: e.dma_start(out=gpre_t[:], in_=g_mpre), writes=[b_gpre])
    P.dma("sp", lambda e: e.dma_start(out=gpost_t[:], in_=g_mpost), writes=[b_gpost])
    n = 0
    for k in range(8):
        s = n % 2; n += 1
        P.dma("sp", lambda e, k=k, s=s: e.dma_start(out=stg[s][:, :], in_=wgate[k]), writes=[b_stg[s]])
        P.op("pool", lambda e, k=k, s=s: e.tensor_copy(out=Wg[:, k, :], in_=stg[s][:, :]), reads=[b_stg[s]], writes=[b_Wg])
    for k in range(12):
        s = n % 2; n += 1
        P.dma("sp", lambda e, k=k, s=s: e.dma_start(out=stg[s][:, 0:D], in_=wbr[k]), writes=[b_stg[s]])
        P.op("pool", lambda e, k=k, s=s: e.tensor_copy(out=Wb[:, k, :], in_=stg[s][:, 0:D]), reads=[b_stg[s]], writes=[b_Wb])
    for k in range(8):
        s = n % 2; n += 1
        P.dma("sp", lambda e, k=k, s=s: e.dma_start(out=stg[s][:, 0:D], in_=wo[k]), writes=[b_stg[s]])
        P.op("pool", lambda e, k=k, s=s: e.tensor_copy(out=Wo[:, k, :], in_=stg[s][:, 0:D]), reads=[b_stg[s]], writes=[b_Wo])

    def rstd_of(src_ap, src_bufs, col):
        ss = st[:, col:col + 1]; bss = b_st[col]
        P.op("act", lambda e: e.activation(out=junk[:], in_=src_ap, func=AF.Square, scale=float(D ** -0.5), accum_out=ss), reads=src_bufs, writes=[b_junk, bss])
        P.op("dve", lambda e: e.tensor_scalar(out=ss, in0=ss, scalar1=EPS, scalar2=None, op0=ALU.add), reads=[bss], writes=[bss])
        P.op("act", lambda e: e.activation(out=ss, in_=ss, func=AF.Sqrt), reads=[bss], writes=[bss])
        P.op("dve", lambda e: e.reciprocal(out=ss, in_=ss), reads=[bss], writes=[bss])
        return ss, bss

    qn = 0
    for ti in range(NT):
        par = ti % 2
        P.dma("sp", lambda e, ti=ti, par=par: e.dma_start(out=xt[par][:], in_=x_in[ti * 128:(ti + 1) * 128, :]), writes=[b_xt[par]])
        P.dma("sp", lambda e, ti=ti, par=par: e.dma_start(out=ystg[par][:], in_=yT[ti]), writes=[b_ystg[par]])
        P.op("pool", lambda e, par=par: e.tensor_copy(out=ytb[par][:].rearrange("p a b -> p (a b)"), in_=ystg[par][:]), reads=[b_ystg[par]], writes=[b_ytb[par]])
        rs, brs = rstd_of(xt[par][:], [b_xt[par]], 0)
        P.op("dve", lambda e, par=par, rs=rs: e.scalar_tensor_tensor(out=hb[:], in0=xt[par][:], scalar=rs, in1=gpre_t[:], op0=ALU.mult, op1=ALU.mult), reads=[b_xt[par], brs, b_gpre], writes=[b_hb])
        for k in range(8):
            P.op("pe", lambda e, k=k: e.transpose(pT[:, k * 128:(k + 1) * 128], hb[:, k * 128:(k + 1) * 128], ident[:]), reads=[b_hb, b_id], writes=[b_pT])
        P.op("act", lambda e: e.copy(out=hTt[:].rearrange("p k t -> p (k t)"), in_=pT[:]), reads=[b_pT], writes=[b_hTt])
        for half in range(2):
            for nb in range(3):
                q = qn % 2; qn += 1
                c0 = nb * 1024 + half * 512
                for k in range(8):
                    P.op("pe", lambda e, k=k, q=q, c0=c0: e.matmul(pA[q][:], lhsT=hTt[:, k, :], rhs=Wg[:, k, c0:c0 + 512], start=(k == 0), stop=(k == 7)), reads=[b_hTt, b_Wg], writes=[b_pA[q]])
                P.op("act", lambda e, q=q: e.activation(out=sgt[q][:], in_=pA[q][:], func=AF.Sigmoid), reads=[b_pA[q]], writes=[b_sgt[q]])
                for kc in range(4):
                    P.op("pe", lambda e, kc=kc, q=q, nb=nb, half=half, par=par: e.matmul(pB[q][:], lhsT=ytb[par][:, nb * 4 + kc, :], rhs=Wb[:, nb * 4 + kc, half * 512:(half + 1) * 512], start=(kc == 0), stop=(kc == 3)), reads=[b_ytb[par], b_Wb], writes=[b_pB[q]])
                if nb == 0:
                    P.op("dve", lambda e, q=q, half=half: e.tensor_tensor(out=mg[:, half * 512:(half + 1) * 512], in0=sgt[q][:], in1=pB[q][:], op=ALU.mult), reads=[b_sgt[q], b_pB[q]], writes=[b_mg])
                else:
                    P.op("dve", lambda e, q=q: e.tensor_tensor(out=tmp[:], in0=sgt[q][:], in1=pB[q][:], op=ALU.mult), reads=[b_sgt[q], b_pB[q]], writes=[b_tmp])
                    P.op("dve", lambda e, half=half: e.tensor_tensor(out=mg[:, half * 512:(half + 1) * 512], in0=mg[:, half * 512:(half + 1) * 512], in1=tmp[:], op=ALU.add), reads=[b_mg, b_tmp], writes=[b_mg])
        P.op("act", lambda e: e.copy(out=mb[:], in_=mg[:]), reads=[b_mg], writes=[b_mb])
        for k in range(8):
            P.op("pe", lambda e, k=k: e.transpose(pT[:, k * 128:(k + 1) * 128], mb[:, k * 128:(k + 1) * 128], ident[:]), reads=[b_mb, b_id], writes=[b_pT])
        P.op("act", lambda e: e.copy(out=mT[:].rearrange("p k t -> p (k t)"), in_=pT[:]), reads=[b_pT], writes=[b_mT])
        for half in range(2):
            for k in range(8):
                P.op("pe", lambda e, k=k, half=half: e.matmul(pC[half][:], lhsT=mT[:, k, :], rhs=Wo[:, k, half * 512:(half + 1) * 512], start=(k == 0), stop=(k == 7)), reads=[b_mT, b_Wo], writes=[b_pC[half]])
            P.op("act", lambda e, half=half, par=par: e.copy(out=ot[par][:, half * 512:(half + 1) * 512], in_=pC[half][:]), reads=[b_pC[half]], writes=[b_ot[par]])
        rs, brs = rstd_of(ot[par][:], [b_ot[par]], 1)
        P.op("dve", lambda e, par=par, rs=rs: e.scalar_tensor_tensor(out=ot[par][:], in0=ot[par][:], scalar=rs, in1=gpost_t[:], op0=ALU.mult, op1=ALU.mult), reads=[b_ot[par], brs, b_gpost], writes=[b_ot[par]])
        P.op("dve", lambda e, par=par: e.tensor_tensor(out=ot[par][:], in0=ot[par][:], in1=xt[par][:], op=ALU.add), reads=[b_ot[par], b_xt[par]], writes=[b_ot[par]])
        P.dma("pool", lambda e, ti=ti, par=par: e.dma_start(out=x_out[ti * 128:(ti + 1) * 128, :], in_=ot[par][:]), reads=[b_ot[par]], sembuf=b_ot[par])
    P.end()


import math as _math
_PROGS = {}
def _prog(key, fn):
    if key not in _PROGS:
        _PROGS[key] = fn()
    return _PROGS[key]

def _c(a):
    return np.ascontiguousarray(a, dtype=np.float32)

def _bc(v, p=128):
    return _c(np.broadcast_to(v[None, :], (p, v.shape[0])))

def _wl(w, nchunk):
    return _c(w.reshape(8, 128, nchunk, 128).transpose(2, 1, 0, 3))

def _pad1(a):
    return np.concatenate([np.zeros(a.shape[:-1] + (1,), np.float32), a], -1)

def _run(nc, maps):
    res = run_bass_kernel_spmd(nc, maps, core_ids=list(range(8)))
    return res.results

def _pfx(p, d):
    return {p + k: v for k, v in d.items()}

def build_mix(L):
    nc = bass.Bass("TRN2", target_bir_lowering=False)
    P = Prog(nc)
    emit_dsa_h(nc, P, "a_", L)
    emit_rwkv_h(nc, P, "b_", L)
    emit_diff_h(nc, P, "c_", L)
    P.finish()
    return nc

def build_mfa(with_a):
    nc = bass.Bass("TRN2", target_bir_lowering=False)
    P = Prog(nc)
    def din(name, shape): return nc.dram_tensor(name, list(shape), F32, kind="ExternalInput").ap()
    xb = nc.dram_tensor("xb_scr", [2048, D], F32)
    xc = nc.dram_tensor("xc_scr", [2048, D], F32)
    xo = nc.dram_tensor("xo", [2048, D], F32, kind="ExternalOutput").ap()
    emit_merge_h(nc, P, "m_", x_out=xb.ap())
    idn = din("idn", [128, 128])
    WF = {"g_pre": din("f2_gpre", [128, D]), "g_post": din("f2_gpost", [128, D]), "wg": din("f2_wg", [NFF, 128, 8, 128]),
          "wu": din("f2_wu", [NFF, 128, 8, 128]), "wd": din("f2_wd", [NFF, 128, D]), "idn": idn}
    if with_a:
        emit_tok(P, 2048, xb.ap(), xc.ap(), WF, zdst=None)
        zT = nc.dram_tensor("zT", [NWIN * 128, 2048], F32, kind="ExternalOutput").ap()
        WA = {"g_pre": din("f1_gpre", [128, D]), "g_post": din("f1_gpost", [128, D]), "wg": din("f1_wg", [NFF, 128, 8, 128]),
              "wu": din("f1_wu", [NFF, 128, 8, 128]), "wd": din("f1_wd", [NFF, 128, D]), "idn": idn,
              "g_mix": din("g_mix", [128, D]), "win": din("win", [NWIN, 128, 8, 128])}
        emit_tok(P, 2048, xc.ap(), xo, WA, zdst=zT)
    else:
        emit_tok(P, 2048, xb.ap(), xo, WF, zdst=None)
    P.finish()
    return nc

def kernel(**inp):
    inp = {k: np.asarray(v) for k, v in inp.items()}
    Bsz, L, Dm = 2, 8192, 1024
    NQ = L // 128
    x = _c(inp["x"].reshape(Bsz * L, Dm))
    idn = np.eye(128, dtype=np.float32)
    tri = np.triu(np.ones((128, 128), np.float32))
    negm = np.where(np.arange(128)[None, :] <= np.arange(128)[:, None], 0.0, -1e30).astype(np.float32)
    SEG = 512
    cmask = np.ones((64, 2 * SEG), np.float32); cmask[:, ::64] = 0
    sl = np.tril(np.ones((64, 64), np.float32), -1); su = sl.T; iu = np.triu(np.ones((64, 64), np.float32))
    mask5 = _c(np.concatenate([sl, su, iu, su, iu], 1))

    def a_weights(l):
        w_in = inp["w_in"][l]
        w_in_p = np.zeros((1024, 43 * 128), np.float32); w_in_p[:, :5448] = w_in[:, :5448]
        return {"g_pre": _bc(inp["ffn1_norm_pre"][l]), "g_post": _bc(inp["ffn1_norm_post"][l]),
                "wg": _wl(inp["ffn1_w_gate"][l], 22), "wu": _wl(inp["ffn1_w_up"][l], 22), "wd": _c(inp["ffn1_w_down"][l].reshape(22, 128, 1024)),
                "g_mix": _bc(inp["mix_norm_pre"][l]), "win": _wl(w_in_p, 43)}

    aw = a_weights(0)
    ncA = _prog("A", lambda: build_tok(False, True))
    res = _run(ncA, [dict(aw, idn=idn, x=x[c * 2048:(c + 1) * 2048]) for c in range(8)])
    x1 = np.concatenate([r["xo"] for r in res], 0)
    zT = np.concatenate([r["zT"] for r in res], 1)
    del res
    for l in range(2):
        w_in = inp["w_in"][l]
        li = 0.8 - 0.6 * _math.exp(-0.3 * l)
        lam = np.stack([inp["diff_lambda_q1"][l], inp["diff_lambda_k1"][l], inp["diff_lambda_q2"][l], inp["diff_lambda_k2"][l]])
        mu = inp["rwkv_mu"][l]
        maps = []
        for c in range(8):
            b, j = c // 4, c % 4
            zb = zT[:, b * L:(b + 1) * L]
            DO = 2120 + 1792
            zq = zb[DO:DO + 512]; zk = zb[DO + 512:DO + 1024]; zv = zb[DO + 1024:DO + 1536]
            qk = np.stack([zq[j * 128:j * 128 + 64], zq[j * 128 + 64:j * 128 + 128], zk[j * 128:j * 128 + 64], zk[j * 128 + 64:j * 128 + 128]])
            m_diff = {"qk": _c(qk), "v": _c(zv[j * 128:(j + 1) * 128].T.reshape(NQ, 128, 128).transpose(1, 0, 2)),
                      "lam": _c(np.broadcast_to(lam[None], (128, 4, 64))),
                      "cst": _c(np.broadcast_to(np.array([li, 1 - li], np.float32)[None], (128, 2))),
                      "gsub": _bc(inp["diff_subln"][l]), "tri": tri}
            q = zb[0:512][j * 128:(j + 1) * 128]; k = zb[512:1024][j * 128:(j + 1) * 128]; vv = zb[1024:1536][j * 128:(j + 1) * 128]
            qi = zb[1536:2048]; ki = zb[2048:2112]; wi = zb[2112:2120]
            m_dsa = {"qk": _c(np.stack([q, k])), "v": _c(vv.T.reshape(NQ, 128, 128).transpose(1, 0, 2)),
                     "qi": _c(qi.reshape(8, 64, NQ, 128).transpose(2, 1, 0, 3)), "ki": _c(ki),
                     "wi": _c(wi.T.reshape(NQ, 128, 8).transpose(1, 0, 2)), "negm": negm, "idn": idn}
            zrw = zb[2120:2120 + 1792]
            r_ = zrw[0:512].reshape(8, 64, L)[2 * j:2 * j + 2]; k_ = zrw[512:1024].reshape(8, 64, L)[2 * j:2 * j + 2]; v_ = zrw[1024:1536].reshape(8, 64, L)[2 * j:2 * j + 2]
            hp = lambda vec: np.ascontiguousarray(vec.reshape(8, 64)[2 * j:2 * j + 2].T)
            up = lambda w: w.reshape(64, 8, 64)[:, 2 * j:2 * j + 2, :]
            lnwb = np.stack([inp["rwkv_ln_w"][l][2 * j * 64:(2 * j + 2) * 64], inp["rwkv_ln_b"][l][2 * j * 64:(2 * j + 2) * 64]])
            m_rwkv = {"zr": _c(_pad1(np.stack([r_, k_, v_]).transpose(0, 2, 1, 3))),
                      "zl": _c(_pad1(np.stack([zrw[1536:1600], zrw[1600:1664]], 1))), "zg": _c(_pad1(zrw[1664:1792])),
                      "mu3": _c(np.stack([hp(mu[0:512]), hp(mu[512:1024]), hp(mu[1024:1536])], 1)),
                      "mul": _c(np.stack([mu[1536:1600], mu[1600:1664]], 1)), "mug": _c(mu[1664:1792][:, None]),
                      "pp": _c(np.stack([hp(inp["rwkv_w0"][l]), hp(inp["rwkv_a0"][l]), hp(inp["rwkv_k_k"][l]), hp(inp["rwkv_k_a"][l]), hp(inp["rwkv_r_k"][l])], 1)),
                      "wup": _c(up(inp["rwkv_w_up"][l])), "aup": _c(up(inp["rwkv_a_up"][l])),
                      "gup": _c(inp["rwkv_g_up"][l][:, 2 * j * 64:(2 * j + 2) * 64]),
                      "lnwb": _c(np.broadcast_to(lnwb[None], (64, 2, 128))), "cmask": cmask, "mask5": mask5, "idn": np.eye(64, dtype=np.float32)}
            m = {}
            m.update(_pfx("a_", m_dsa)); m.update(_pfx("b_", m_rwkv)); m.update(_pfx("c_", m_diff))
            maps.append(m)
        del zT
        res = _run(_prog("MIX", lambda: build_mix(L)), maps); del maps
        Y = np.zeros((3, Bsz * L, 512), np.float32)
        for c in range(8):
            b, j = c // 4, c % 4
            Y[0, b * L:(b + 1) * L, j * 128:(j + 1) * 128] = res[c]["a_y"].reshape(L, 128)
            Y[1, b * L:(b + 1) * L, j * 128:(j + 1) * 128] = res[c]["b_y"].reshape(L, 128)
            Y[2, b * L:(b + 1) * L, j * 128:(j + 1) * 128] = res[c]["c_y"].reshape(L, 128)
        del res
        cm = {"m_g_mpre": _bc(inp["mix_norm_pre"][l]), "m_g_mpost": _bc(inp["mix_norm_post"][l]),
              "m_wgate": _c(w_in[:, 5448:8520].reshape(8, 128, 3072)), "m_wbr": _c(inp["w_branch"][l].reshape(12, 128, 1024)),
              "m_wo": _c(inp["w_out"][l].reshape(8, 128, 1024)), "m_idn": idn, "idn": idn,
              "f2_gpre": _bc(inp["ffn2_norm_pre"][l]), "f2_gpost": _bc(inp["ffn2_norm_post"][l]),
              "f2_wg": _wl(inp["ffn2_w_gate"][l], 22), "f2_wu": _wl(inp["ffn2_w_up"][l], 22), "f2_wd": _c(inp["ffn2_w_down"][l].reshape(22, 128, 1024))}
        last = (l == 1)
        if not last:
            aw = a_weights(l + 1)
            cm.update({"f1_gpre": aw["g_pre"], "f1_gpost": aw["g_post"], "f1_wg": aw["wg"], "f1_wu": aw["wu"], "f1_wd": aw["wd"], "g_mix": aw["g_mix"], "win": aw["win"]})
        maps = []
        for c in range(8):
            yc_ = Y[:, c * 2048:(c + 1) * 2048, :].reshape(3, 16, 128, 4, 128)
            maps.append(dict(cm, m_x=x1[c * 2048:(c + 1) * 2048], m_yT=_c(yc_.transpose(1, 4, 0, 3, 2).reshape(16, 128, 12 * 128))))
        del Y
        res = _run(_prog("MF" if last else "MFA", (lambda: build_mfa(False)) if last else (lambda: build_mfa(True))), maps); del maps
        x1 = np.concatenate([r["xo"] for r in res], 0)
        if not last:
            zT = np.concatenate([r["zT"] for r in res], 1)
        del res
    return x1.reshape(Bsz, L, Dm).astype(np.float32)
```
